# Optimizing a Trainium2 kernel written in Bass

```python
import math
import jax, jax.numpy as jnp
from jax import lax
import numpy as np

D_MODEL = 1024
BATCH = 16
SEQ = 2048
DEPTH = 2

HG_HEADS = 4
HG_DK = 128
HG_DV = 128
HG_WIDTH = HG_HEADS * HG_DK
HG_CHUNK = 64
ATT_HEADS = 8
ATT_DH = 64
ATT_WIDTH = ATT_HEADS * ATT_DH
DILATED_PATTERNS = ((128, 1), (512, 4), (2048, 16))
ROPE_DIMS = ATT_DH // 4
ROPE_THETA = 500000.0
MIX_WIDTH = HG_WIDTH + ATT_WIDTH
IN_SPLITS = (HG_WIDTH, HG_WIDTH, HG_WIDTH, HG_WIDTH, HG_WIDTH, ATT_WIDTH, ATT_WIDTH, ATT_WIDTH)
IN_COLS = sum(IN_SPLITS)
N_GROUPS = 4
EXPERTS_PER_GROUP = 8
N_EXPERTS = N_GROUPS * EXPERTS_PER_GROUP
TOP_K = 2
D_EXPERT = D_MODEL // 2
MOE_BLOCK = 256
DEEPNORM_ALPHA = (2 * DEPTH) ** 0.25
DEEPNORM_BETA = (8 * DEPTH) ** -0.25
LN_EPS = 1e-5
RMS_EPS = 1e-6
NEG_BIG = -1e30

kernel_name = 'hymba_hgrn2_dilated_attn_hier_moe_deepnorm'


def _layernorm(x, g, b):
    xf = x.astype(jnp.float32)
    mu = xf.mean(-1, keepdims=True)
    var = jnp.square(xf - mu).mean(-1, keepdims=True)
    return ((xf - mu) * lax.rsqrt(var + LN_EPS) * g + b).astype(x.dtype)


def _heads(t, n):
    B, S, W = t.shape
    return t.reshape(B, S, n, W // n).transpose(0, 2, 1, 3)


def _forget_gate(a, lb):
    log_f = jnp.logaddexp(jnp.log(lb), jnp.log1p(-lb) + jax.nn.log_sigmoid(a))
    k = (1.0 - lb) * jax.nn.sigmoid(-a)
    return k, log_f


def _chunk_gla(q, k, v, log_f):
    B, H, S, dk = q.shape
    dv = v.shape[-1]
    n = S // HG_CHUNK

    def chunks(t):
        return jnp.moveaxis(t.reshape(B, H, n, HG_CHUNK, t.shape[-1]), 2, 0)

    tri = jnp.tril(jnp.ones((HG_CHUNK, HG_CHUNK), bool))[:, :, None]

    def step(state, inp):
        qc, kc, vc, gc = inp
        b = jnp.cumsum(gc, axis=2)
        rel = b[:, :, :, None, :] - b[:, :, None, :, :]
        decay = jnp.exp(jnp.where(tri, rel, -jnp.inf))
        scores = jnp.einsum('bhtc,bhsc,bhtsc->bhts', qc, kc, decay)
        o = (jnp.einsum('bhts,bhsv->bhtv', scores, vc)
             + jnp.einsum('bhtc,bhcv->bhtv', qc * jnp.exp(b), state))
        b_end = b[:, :, -1:, :]
        state = (jnp.exp(b_end[:, :, 0, :, None]) * state
                 + jnp.einsum('bhsc,bhsv->bhcv', kc * jnp.exp(b_end - b), vc))
        return state, o

    s0 = jnp.zeros((B, H, dk, dv), jnp.float32)
    _, o = lax.scan(step, s0, (chunks(q), chunks(k), chunks(v), chunks(log_f)))
    return jnp.moveaxis(o, 0, 2).reshape(B, H, S, dv)


def _hgrn2_group(hq, hf_f, hf_b, hi, hg, lb, norm_g):
    B, S, _ = hq.shape
    f32 = jnp.float32
    q = _heads(hq.astype(f32), HG_HEADS) * HG_DK ** -0.5
    v = _heads(hi.astype(f32), HG_HEADS)
    lb = lb.astype(f32).reshape(2, HG_HEADS, 1, HG_DK)
    k_f, g_f = _forget_gate(_heads(hf_f.astype(f32), HG_HEADS), lb[0])
    k_b, g_b = _forget_gate(_heads(hf_b.astype(f32), HG_HEADS), lb[1])
    flip = lambda t: jnp.flip(t, axis=2)
    o_fwd = _chunk_gla(q, k_f, v, g_f)
    o_bwd = flip(_chunk_gla(flip(q), flip(k_b), flip(v), flip(g_b)))
    o = (o_fwd + o_bwd).transpose(0, 2, 1, 3)
    o = o * lax.rsqrt(jnp.mean(jnp.square(o), -1, keepdims=True) + RMS_EPS)
    o = o * norm_g.astype(f32).reshape(HG_HEADS, HG_DV)
    return o.reshape(B, S, HG_WIDTH) * jax.nn.silu(hg.astype(f32))


def _rope(x, pos):
    half = ROPE_DIMS // 2
    inv_freq = ROPE_THETA ** (-jnp.arange(half, dtype=jnp.float32) / half)
    ang = pos[:, None] * inv_freq[None, :]
    cos = jnp.cos(ang)[None, :, None, :]
    sin = jnp.sin(ang)[None, :, None, :]
    x1, x2 = x[..., :half], x[..., half:ROPE_DIMS]
    return jnp.concatenate([x1 * cos - x2 * sin, x1 * sin + x2 * cos, x[..., ROPE_DIMS:]], axis=-1)


def _dilated_branch(q, k, v, dilation, radius):
    B, S, H, dh = q.shape
    L = S // dilation

    def fold(t):
        return t.reshape(B, L, dilation, H, dh).transpose(0, 2, 3, 1, 4)

    blk = radius
    nb = -(-L // blk)
    Lp = nb * blk
    qb = jnp.pad(fold(q), ((0, 0),) * 3 + ((0, Lp - L), (0, 0))).reshape(B, dilation, H, nb, blk, dh)
    pad_kv = ((0, 0),) * 3 + ((blk, Lp - L + blk), (0, 0))

    def neighbours(t):
        tb = jnp.pad(fold(t), pad_kv).reshape(B, dilation, H, nb + 2, blk, dh)
        return jnp.concatenate([tb[:, :, :, :-2], tb[:, :, :, 1:-1], tb[:, :, :, 2:]], axis=4)

    kb, vb = neighbours(k), neighbours(v)
    s = jnp.einsum('bdhnqe,bdhnke->bdhnqk', qb, kb)
    qpos = jnp.arange(nb)[:, None] * blk + jnp.arange(blk)[None, :]
    kpos = (jnp.arange(nb)[:, None] - 1) * blk + jnp.arange(3 * blk)[None, :]
    kp = kpos[:, None, :]
    mask = (jnp.abs(kp - qpos[:, :, None]) <= radius) & (kp >= 0) & (kp < L)
    s = jnp.where(mask, s, NEG_BIG)
    m = s.max(-1, keepdims=True)
    p = jnp.exp(s - m)
    den = p.sum(-1)
    o = jnp.einsum('bdhnqk,bdhnke->bdhnqe', p, vb) / den[..., None]
    lse = m[..., 0] + jnp.log(den)
    o = o.reshape(B, dilation, H, Lp, dh)[:, :, :, :L].transpose(0, 3, 1, 2, 4).reshape(B, S, H, dh)
    lse = lse.reshape(B, dilation, H, Lp)[..., :L].transpose(0, 3, 1, 2).reshape(B, S, H)
    return o, lse


def _dilated_attention_group(aq, ak, av):
    B, S, _ = aq.shape
    f32 = jnp.float32
    pos = jnp.arange(S, dtype=f32)
    shp = (B, S, ATT_HEADS, ATT_DH)
    q = _rope(aq.astype(f32).reshape(shp), pos) * ATT_DH ** -0.5
    k = _rope(ak.astype(f32).reshape(shp), pos)
    v = av.astype(f32).reshape(shp)
    outs, lses = [], []
    for window, dilation in DILATED_PATTERNS:
        o, lse = _dilated_branch(q, k, v, dilation, window // (2 * dilation))
        outs.append(o)
        lses.append(lse)
    w = jax.nn.softmax(jnp.stack(lses), axis=0)
    o = jnp.einsum('pbsh,pbshe->bshe', w, jnp.stack(outs))
    return o.reshape(B, S, ATT_WIDTH)


def _hybrid_mixer(h, w_in, lb, norm_g, w_out):
    proj = jnp.einsum('bsd,dc->bsc', h, w_in)
    cuts = [sum(IN_SPLITS[:i + 1]) for i in range(len(IN_SPLITS) - 1)]
    hq, hf_f, hf_b, hi, hg, aq, ak, av = jnp.split(proj, cuts, axis=-1)
    y_hg = _hgrn2_group(hq, hf_f, hf_b, hi, hg, lb, norm_g)
    y_att = _dilated_attention_group(aq, ak, av)
    y = jnp.concatenate([y_hg, y_att], axis=-1).astype(h.dtype)
    return jnp.einsum('bsc,cd->bsd', y, w_out)


def _hier_moe(h, w_r1, b_r1, w_r2, b_r2, w_gate, w_up, w_down):
    B, S, D = h.shape
    f32 = jnp.float32
    t = h.reshape(-1, D)
    T = t.shape[0]
    p1 = jax.nn.softmax((t @ w_r1 + b_r1).astype(f32), axis=-1)
    grp = jnp.argmax(p1, axis=-1)
    p_grp = jnp.max(p1, axis=-1)
    lg2 = (t @ w_r2 + b_r2).astype(f32).reshape(T, N_GROUPS, EXPERTS_PER_GROUP)
    lg2 = jnp.take_along_axis(lg2, grp[:, None, None], axis=1)[:, 0]
    w2, e2 = lax.top_k(jax.nn.softmax(lg2, axis=-1), TOP_K)
    w2 = w2 / w2.sum(-1, keepdims=True)
    expert = (grp[:, None] * EXPERTS_PER_GROUP + e2).reshape(-1)
    gate = (p_grp[:, None] * w2)
    A = T * TOP_K
    tok = jnp.arange(A, dtype=jnp.int32) // TOP_K
    order = jnp.argsort(expert)
    e_sorted = expert[order]
    counts = jnp.zeros((N_EXPERTS,), jnp.int32).at[expert].add(1)
    starts = jnp.cumsum(counts) - counts
    padded = (counts + MOE_BLOCK - 1) // MOE_BLOCK * MOE_BLOCK
    pends = jnp.cumsum(padded)
    pstarts = pends - padded
    dest_sorted = pstarts[e_sorted] + jnp.arange(A, dtype=jnp.int32) - starts[e_sorted]
    dest = jnp.zeros((A,), jnp.int32).at[order].set(dest_sorted)
    n_blocks = -(-A // MOE_BLOCK) + N_EXPERTS
    P = n_blocks * MOE_BLOCK
    slot_tok = jnp.full((P,), T, jnp.int32).at[dest].set(tok)
    block_exp = jnp.clip(jnp.searchsorted(pends, jnp.arange(n_blocks) * MOE_BLOCK, side='right'), 0, N_EXPERTS - 1)
    t_pad = jnp.concatenate([t, jnp.zeros((1, D), t.dtype)], axis=0)
    xs = t_pad[slot_tok].reshape(n_blocks, MOE_BLOCK, D)

    def expert_block(args):
        xb, e = args
        hid = jax.nn.silu(xb @ w_gate[e]) * (xb @ w_up[e])
        return hid @ w_down[e]

    ys = lax.map(expert_block, (xs, block_exp)).reshape(P, D)
    y = jnp.sum(ys[dest].reshape(T, TOP_K, D) * gate[..., None].astype(ys.dtype), axis=1)
    return y.reshape(B, S, D).astype(h.dtype)


def setup_inputs(seed: int = 0) -> dict:
    key = jax.random.key(seed)
    ks = jax.random.split(key, 16)
    f32 = jnp.float32

    def normal(k, shape, scale):
        return jax.random.normal(k, shape, f32) * scale

    beta = DEEPNORM_BETA
    x = normal(ks[0], (BATCH, SEQ, D_MODEL), 1.0)
    col_scale = jnp.concatenate([
        jnp.ones((3 * HG_WIDTH,), f32), jnp.full((HG_WIDTH,), beta, f32),
        jnp.ones((HG_WIDTH + 2 * ATT_WIDTH,), f32), jnp.full((ATT_WIDTH,), beta, f32)])
    w_in = normal(ks[1], (DEPTH, D_MODEL, IN_COLS), D_MODEL ** -0.5) * col_scale
    hg_lb_logits = normal(ks[2], (DEPTH, 2, HG_WIDTH), 0.5)
    hg_norm_g = 1.0 + normal(ks[3], (DEPTH, HG_WIDTH), 0.02)
    w_out = normal(ks[4], (DEPTH, MIX_WIDTH, D_MODEL), MIX_WIDTH ** -0.5 * beta)
    ln1_g = 1.0 + normal(ks[5], (DEPTH, D_MODEL), 0.02)
    ln1_b = normal(ks[6], (DEPTH, D_MODEL), 0.02)
    router_w1 = normal(ks[7], (DEPTH, D_MODEL, N_GROUPS), D_MODEL ** -0.5)
    router_b1 = normal(ks[8], (DEPTH, N_GROUPS), 0.01)
    router_w2 = normal(ks[9], (DEPTH, D_MODEL, N_EXPERTS), D_MODEL ** -0.5)
    router_b2 = normal(ks[10], (DEPTH, N_EXPERTS), 0.01)
    ex_w_gate = normal(ks[11], (DEPTH, N_EXPERTS, D_MODEL, D_EXPERT), D_MODEL ** -0.5)
    ex_w_up = normal(ks[12], (DEPTH, N_EXPERTS, D_MODEL, D_EXPERT), D_MODEL ** -0.5)
    ex_w_down = normal(ks[13], (DEPTH, N_EXPERTS, D_EXPERT, D_MODEL), D_EXPERT ** -0.5 * beta)
    ln2_g = 1.0 + normal(ks[14], (DEPTH, D_MODEL), 0.02)
    ln2_b = normal(ks[15], (DEPTH, D_MODEL), 0.02)
    return {'x': x, 'w_in': w_in, 'hg_lb_logits': hg_lb_logits, 'hg_norm_g': hg_norm_g,
            'w_out': w_out, 'ln1_g': ln1_g, 'ln1_b': ln1_b,
            'router_w1': router_w1, 'router_b1': router_b1, 'router_w2': router_w2, 'router_b2': router_b2,
            'ex_w_gate': ex_w_gate, 'ex_w_up': ex_w_up, 'ex_w_down': ex_w_down,
            'ln2_g': ln2_g, 'ln2_b': ln2_b}


def reference(x, w_in, hg_lb_logits, hg_norm_g, w_out, ln1_g, ln1_b,
              router_w1, router_b1, router_w2, router_b2,
              ex_w_gate, ex_w_up, ex_w_down, ln2_g, ln2_b):
    lb_all = jnp.cumsum(jax.nn.softmax(hg_lb_logits.astype(jnp.float32), axis=0), axis=0)
    lb_all = lb_all - lb_all[0:1]
    h = x
    for l in range(DEPTH):
        mix = _hybrid_mixer(h, w_in[l], lb_all[l], hg_norm_g[l], w_out[l])
        h = _layernorm(DEEPNORM_ALPHA * h + mix, ln1_g[l], ln1_b[l])
        ffn = _hier_moe(h, router_w1[l], router_b1[l], router_w2[l], router_b2[l],
                        ex_w_gate[l], ex_w_up[l], ex_w_down[l])
        h = _layernorm(DEEPNORM_ALPHA * h + ffn, ln2_g[l], ln2_b[l])
    return h
```

```python
import contextlib
import numpy as np
import ml_dtypes
import concourse.bass as bass
import concourse.mybir as mybir
from concourse.bass_utils import run_bass_kernel_spmd

F32 = mybir.dt.float32
BF16 = mybir.dt.bfloat16
I32 = mybir.dt.int32
AF = mybir.ActivationFunctionType
ALU = mybir.AluOpType
AX = mybir.AxisListType

NCORES = 8
S_ = 2048
D = 1024
TOK = 2 * S_
NTT = TOK // 128
CAP = 384
NE = 32
ALPHA = 4.0 ** 0.25
LN_EPS = 1e-5
RMS_EPS = 1e-6
OFFMIN = -1408
AMW = 1024 - OFFMIN + 512
BIG = 1.0e4


class _Op:
    __slots__ = ("eng", "fn", "deps", "odeps", "is_dma", "dkey", "sig", "val", "idx", "cost", "tag", "grp")


class Sched:
    ENGS = ("pe", "act", "dve", "pool", "sp")
    LAT = 0.25

    def __init__(self, nc):
        self.nc = nc
        self.ops = []
        self.last_writer = {}
        self.readers = {}
        self.dma_count = {}
        self.last_dma = {}
        self.fixed = []
        self.pool_dmas = []
        self.reorder_on = True
        self.seg_flags = []

    def op(self, eng, fn, reads=(), writes=(), dma=False, sem_key=None, force=False, cost=0.3):
        o = _Op()
        o.eng = eng
        o.fn = fn
        o.is_dma = dma
        o.idx = len(self.ops)
        o.sig = False
        o.val = None
        o.dkey = None
        o.cost = cost
        o.grp = None
        o.tag = "%s r=%s w=%s" % ("DMA" if dma else "", list(reads)[:3], list(writes)[:2])
        odeps = set()
        if dma:
            o.dkey = sem_key if sem_key is not None else writes[0]
            self.dma_count[o.dkey] = self.dma_count.get(o.dkey, 0) + 1
            o.val = 16 * self.dma_count[o.dkey]
            if o.dkey in self.last_dma:
                odeps.add(self.last_dma[o.dkey])
            self.last_dma[o.dkey] = o.idx
        deps = set()
        for k in reads:
            for j in self.last_writer.get(k, ()):
                deps.add(j)
        for k in writes:
            ws = self.last_writer.get(k, [])
            rs = self.readers.get(k, [])
            if (dma and not force and not rs and ws
                    and all(self.ops[j].is_dma for j in ws)):
                self.last_writer[k] = ws + [o.idx]
            else:
                for j in ws:
                    deps.add(j)
                for j in rs:
                    p = self.ops[j]
                    deps.add(j)
                self.last_writer[k] = [o.idx]
                self.readers[k] = []
        fdeps = []
        for j in deps:
            p = self.ops[j]
            if p.fn is None:
                continue
            if p.eng == "pe" and eng == "pe" and not p.is_dma and not dma:
                odeps.add(j)
                continue
            fdeps.append(j)
        o.deps = fdeps
        o.odeps = list(odeps)
        for j in fdeps:
            self.ops[j].sig = True
        for k in reads:
            if k not in writes:
                self.readers.setdefault(k, []).append(o.idx)
        self.ops.append(o)
        return o

    def fence(self):
        n = len(self.ops)
        self.fixed.append((n, n))
        self.seg_flags.append(self.reorder_on)

    def barrier(self, fn_tiny):
        keys = list(set(list(self.last_writer.keys()) + list(self.readers.keys())))
        keys = [k for k in keys if k != "__bar"]
        a = len(self.ops)
        self.op("sp", fn_tiny, reads=[], writes=keys + ["__bar"], dma=True,
                sem_key="__bar", force=True, cost=2.0)
        bw = self.last_writer["__bar"]
        self.last_writer = {"__bar": bw}
        self.readers = {}
        for e in ("pe", "act", "dve", "pool"):
            self.op(e, None, reads=["__bar"], cost=0.05)
        self.fixed.append((a, len(self.ops)))
        self.seg_flags.append(self.reorder_on)

    def _schedule_segment(self, a, b, order):
        import heapq
        ops = self.ops
        n = b - a
        if n == 0:
            return
        succ = [[] for _ in range(n)]
        ndep = [0] * n
        import os
        chain = set(os.environ.get("CHAIN_ENGS", "").split(","))
        lastop = {}
        for i in range(a, b):
            o = ops[i]
            ds = set(j for j in list(o.deps) + list(o.odeps) if j >= a)
            if o.eng in chain:
                if o.eng in lastop:
                    ds.add(lastop[o.eng])
                    if lastop[o.eng] not in o.deps and lastop[o.eng] not in o.odeps:
                        o.odeps.append(lastop[o.eng])
                lastop[o.eng] = i
            ndep[i - a] = len(ds)
            for j in ds:
                succ[j - a].append(i)
        bl = [0.0] * n
        for i in range(b - 1, a - 1, -1):
            m = 0.0
            for sidx in succ[i - a]:
                if bl[sidx - a] > m:
                    m = bl[sidx - a]
            bl[i - a] = m + ops[i].cost
        ready_t = [0.0] * n
        fin = [0.0] * n
        efree = {e: 0.0 for e in self.ENGS}
        avail = {e: [] for e in self.ENGS}
        for i in range(a, b):
            if ndep[i - a] == 0:
                avail[ops[i].eng].append(i)
        left = n
        open_multi = None
        while left:
            best = None
            for e in self.ENGS:
                av = avail[e]
                if e == "pe" and open_multi is not None:
                    av = [i for i in av if ops[i].grp is None or ops[i].grp[0] == open_multi]
                if not av:
                    continue
                mn = min(ready_t[i - a] for i in av)
                t = max(efree[e], mn)
                c = None
                for i in av:
                    if ready_t[i - a] <= t + 1e-9:
                        k = (-bl[i - a], i)
                        if c is None or k < c[0]:
                            c = (k, i)
                if best is None or t < best[0]:
                    best = (t, e, c[1])
            if best is None:
                assert open_multi is not None
                open_multi = None
                continue
            t, e, i = best
            avail[e].remove(i)
            o = ops[i]
            if e == "pe" and o.grp is not None:
                open_multi = None if o.grp[1] else o.grp[0]
            if o.is_dma:
                efree[e] = t + (1.0 if e == "pool" else 0.06)
            else:
                efree[e] = t + o.cost
            fin[i - a] = t + o.cost
            order[e].append(i)
            left -= 1
            for sidx in succ[i - a]:
                so = ops[sidx]
                if i in so.deps:
                    r = fin[i - a] + self.LAT
                else:
                    r = t
                if r > ready_t[sidx - a]:
                    ready_t[sidx - a] = r
                ndep[sidx - a] -= 1
                if ndep[sidx - a] == 0:
                    avail[so.eng].append(sidx)

    def _check(self, order):
        ops = self.ops
        sem = {}
        ptr = {e: 0 for e in self.ENGS}
        total = sum(len(v) for v in order.values())
        done = 0
        while done < total:
            prog = False
            for e in self.ENGS:
                while ptr[e] < len(order[e]):
                    o = ops[order[e][ptr[e]]]
                    ok = True
                    for j in o.deps:
                        p = ops[j]
                        k = ("d", p.dkey) if p.is_dma else ("e", p.eng)
                        if sem.get(k, 0) < p.val:
                            ok = False
                            break
                    if not ok:
                        break
                    if o.fn is not None:
                        if o.is_dma:
                            k = ("d", o.dkey)
                            sem[k] = sem.get(k, 0) + 16
                            assert sem[k] == o.val, ("dma order", o.dkey, sem[k], o.val)
                        elif o.sig:
                            k = ("e", o.eng)
                            sem[k] = sem.get(k, 0) + 1
                            assert sem[k] == o.val
                    ptr[e] += 1
                    done += 1
                    prog = True
            if not prog:
                msg = []
                for e in self.ENGS:
                    if ptr[e] < len(order[e]):
                        o = ops[order[e][ptr[e]]]
                        msg.append((e, o.idx, [(j, ops[j].eng, ops[j].val, ops[j].dkey) for j in o.deps]))
                raise RuntimeError("DEADLOCK in emitted order: %r" % (msg,))

    def emit(self, final_keys=(), reorder=True):
        nc = self.nc
        self.op("sp", None, reads=list(final_keys))
        ops = self.ops
        order = {e: [] for e in self.ENGS}
        pos = 0
        import os
        segsel = os.environ.get("REORDER_SEGS")
        segsel = None if segsel is None else set(int(v) for v in segsel.split(",") if v != "")
        for si, (fa, fb) in enumerate(self.fixed + [(len(ops), len(ops))]):
            flag = self.seg_flags[si] if si < len(self.seg_flags) else True
            if reorder and flag and (segsel is None or si in segsel):
                self._schedule_segment(pos, fa, order)
            else:
                for i in range(pos, fa):
                    order[ops[i].eng].append(i)
            for i in range(fa, fb):
                order[ops[i].eng].append(i)
            pos = fb
        assert sum(len(v) for v in order.values()) == len(ops)
        for e in self.ENGS:
            c = 0
            for i in order[e]:
                o = ops[i]
                if not o.is_dma and o.sig:
                    c += 1
                    o.val = c
        dkeys = list(self.dma_count.keys())
        self._check(order)
        import os
        if os.environ.get("DUMP_ORDER"):
            with open(os.environ["DUMP_ORDER"], "w") as f:
                for e in self.ENGS:
                    f.write("=== %s\n" % e)
                    for i in order[e]:
                        o = ops[i]
                        f.write("%6d %s deps=%s\n" % (i, o.tag, sorted(o.deps)))
        with contextlib.ExitStack() as es:
            esem = {e: es.enter_context(nc.semaphore("s_" + e)) for e in self.ENGS}
            dsem = {k: es.enter_context(nc.semaphore("d%d" % i)) for i, k in enumerate(dkeys)}
            block = es.enter_context(nc.Block())

            def run(engname, eng):
                waited = {}
                for i in order[engname]:
                    o = ops[i]
                    need = {}
                    for j in o.deps:
                        p = ops[j]
                        s = dsem[p.dkey] if p.is_dma else esem[p.eng]
                        v = p.val
                        key = id(s)
                        if v > need.get(key, (None, 0))[1]:
                            need[key] = (s, v)
                    for key, (s, v) in need.items():
                        if waited.get(key, 0) >= v:
                            continue
                        eng.wait_ge(s, v)
                        waited[key] = v
                    if o.fn is None:
                        continue
                    ins = o.fn(eng)
                    if o.is_dma:
                        ins.then_inc(dsem[o.dkey], 16)
                    elif o.sig:
                        ins.then_inc(esem[o.eng], 1)

            @block.sync
            def _(e):
                run("sp", e)

            @block.scalar
            def _(e):
                run("act", e)

            @block.vector
            def _(e):
                run("dve", e)

            @block.tensor
            def _(e):
                run("pe", e)

            @block.gpsimd
            def _(e):
                run("pool", e)


class Arena:
    def __init__(self, nc, base=16512, limit=229344):
        self.nc = nc
        self.cur = base
        self.limit = limit
        self.n = 0

    def alloc(self, name, shape, dt):
        esz = 2 if dt == BF16 else 4
        nbytes = int(np.prod(shape[1:])) * esz
        nbytes = (nbytes + 63) // 64 * 64
        assert self.cur + nbytes <= self.limit, ("SBUF OOM", name, self.cur, nbytes)
        self.n += 1
        t = self.nc.alloc_sbuf_tensor_at("%s_%d" % (name, self.n), list(shape), dt, offset=self.cur)
        self.cur += nbytes
        return t

    def mark(self):
        return self.cur

    def release(self, m):
        self.cur = m


def build(n_layers=2, dbg=None):
    nc = bass.Bass("TRN2", target_bir_lowering=False)

    def din(name, shape, dt=F32):
        return nc.dram_tensor(name, list(shape), dt, kind="ExternalInput").ap()

    x = din("x", [TOK, D])
    w_in = din("w_in", [2, D, 4096])
    lb_logits = din("hg_lb_logits", [2, 2, 512])
    hg_norm_g = din("hg_norm_g", [2, 512])
    w_out = din("w_out", [2, D, D])
    ln1_g = din("ln1_g", [2, D])
    ln1_b = din("ln1_b", [2, D])
    r_w1 = din("router_w1", [2, D, 4])
    r_b1 = din("router_b1", [2, 4])
    r_w2 = din("router_w2", [2, D, 32])
    r_b2 = din("router_b2", [2, 32])
    w_gate = din("ex_w_gate", [2, NE, D, 512])
    w_up = din("ex_w_up", [2, NE, D, 512])
    w_down = din("ex_w_down", [2, NE, 512, D])
    ln2_g = din("ln2_g", [2, D])
    ln2_b = din("ln2_b", [2, D])
    c_ident = din("c_ident", [128, 128], BF16)
    c_amask = din("c_amask", [128, AMW], BF16)
    c_hmask = din("c_hmask", [128, 2, 128], BF16)
    c_ustrict = din("c_ustrict", [128, 128], BF16)
    c_rstart = din("c_rstart", [128, 512])
    c_rope = din("c_rope", [16, 2, S_])
    c_ecap = din("c_ecap", [128, NE])
    out = nc.dram_tensor("out", [TOK, D], F32, kind="ExternalOutput").ap()
    hres = nc.dram_tensor("hres", [TOK, D], F32, kind="ExternalOutput" if dbg else "Internal").ap()
    xs = nc.dram_tensor("xs", [NE * CAP, D], BF16, kind="Internal").ap()
    ys = nc.dram_tensor("ys", [NE * CAP, D], F32, kind="Internal").ap()
    bar_d = nc.dram_tensor("bar_d", [1, 16], F32, kind="Internal").ap()
    dbg_yT = nc.dram_tensor("dbg_yT", [2, 128, 8, S_], BF16, kind="ExternalOutput").ap() if dbg else None

    S = Sched(nc)
    A = Arena(nc)
    import os
    RE_HG = os.environ.get("RE_HG") is not None
    RE_ATT = os.environ.get("RE_ATT") is not None

    def nfree(ap):
        sh = list(ap.shape)
        n = 1
        for v in sh[1:]:
            n *= int(v)
        return n

    def vcost(eng, o):
        n = nfree(o)
        if eng == "act":
            return 0.2 + n / 1400.0
        if eng == "dve":
            return 0.1 + n / 1000.0
        return 0.3 + n / 600.0

    GRP = {"n": 0, "cur": {}}

    def MM(o, lhsT, rhs, start, stop, r, w):
        op_ = S.op("pe", lambda e: e.matmul(o, lhsT, rhs, start=start, stop=stop), reads=r, writes=w,
                   cost=0.04 + max(nfree(o), 64) / 1800.0)
        bank = w[0]
        if start and stop:
            return
        if start:
            GRP["n"] += 1
            GRP["cur"][bank] = GRP["n"]
        op_.grp = (GRP["cur"][bank], bool(stop))

    def TR(o, i, r, w):
        S.op("pe", lambda e: e.transpose(o, i, ident[:]), reads=r, writes=w, cost=0.1)

    def ACT(o, i, func, r, w, scale=1.0, bias=0.0):
        S.op("act", lambda e: e.activation(o, i, func, bias=bias, scale=scale), reads=r, writes=w,
             cost=vcost("act", o))

    def TT(eng, o, a, b, op, r, w):
        S.op(eng, lambda e: e.tensor_tensor(o, a, b, op), reads=r, writes=w, cost=vcost(eng, o))

    def TS(eng, o, a, s1, s2, op0, op1, r, w):
        if s2 is None:
            S.op(eng, lambda e: e.tensor_scalar(o, a, s1, None, op0), reads=r, writes=w, cost=vcost(eng, o))
        else:
            S.op(eng, lambda e: e.tensor_scalar(o, a, s1, s2, op0, op1), reads=r, writes=w, cost=vcost(eng, o))

    def STT(eng, o, a, sc, b, op0, op1, r, w):
        S.op(eng, lambda e: e.scalar_tensor_tensor(o, a, sc, b, op0, op1), reads=r, writes=w, cost=vcost(eng, o))

    def CP(eng, o, i, r, w):
        if eng == "act":
            S.op("act", lambda e: e.copy(o, i), reads=r, writes=w, cost=vcost(eng, o))
        else:
            S.op(eng, lambda e: e.tensor_copy(o, i), reads=r, writes=w, cost=vcost(eng, o))

    def RECIP(o, i, r, w):
        S.op("dve", lambda e: e.reciprocal(o, i), reads=r, writes=w, cost=0.15 + nfree(o) / 200.0)

    def MEMSET(eng, o, val, w):
        S.op(eng, lambda e: e.memset(o, val), reads=[], writes=w, cost=vcost(eng, o))

    def DMA(q, o, i, r, w, sem_key=None, slow=False):
        sh = list(o.shape)
        nb = 1
        for v in sh:
            nb *= int(v)
        nb *= 2 if o.dtype == BF16 else 4
        cost = 2.0 + nb / 100e3
        if slow:
            S.op(q, lambda e: e.dma_start(out=o, in_=i, allow_slow_non_contiguous=True),
                 reads=r, writes=w, dma=True, sem_key=sem_key, cost=cost + 3.0)
        else:
            S.op(q, lambda e: e.dma_start(out=o, in_=i), reads=r, writes=w, dma=True, sem_key=sem_key, cost=cost)

    ident = A.alloc("ident", [128, 128], BF16)
    hmask = A.alloc("hmask", [128, 2, 128], BF16)
    ustrict = A.alloc("ustrict", [128, 128], BF16)
    ones_bf = A.alloc("ones_bf", [128, 128], BF16)
    ecap = A.alloc("ecap", [128, NE], F32)
    lbl = A.alloc("lbl", [128, 2, 2, 4], F32)
    lb_t = A.alloc("lb_t", [128, 2, 4], F32)
    oml_t = A.alloc("oml_t", [128, 2, 4], F32)
    lnoml_t = A.alloc("lnoml_t", [128, 2, 4], F32)
    ng_t = A.alloc("ng_t", [128, 2, 4], F32)
    gates = A.alloc("gates", [128, NTT, 2], F32)
    slots_i = A.alloc("slots_i", [128, NTT, 2], I32)
    bar_s = A.alloc("bar_s", [1, 16], F32)

    DMA("sp", ident[:], c_ident, [], ["ident"])
    DMA("sp", hmask[:], c_hmask, [], ["hmask"])
    DMA("sp", ustrict[:], c_ustrict, [], ["ustrict"])
    DMA("sp", ecap[:], c_ecap, [], ["ecap"])
    MEMSET("dve", ones_bf[:], 1.0, ["ones_bf"])
    MEMSET("dve", bar_s[:], 0.0, ["bar_s"])
    for l in range(2):
        for d in range(2):
            DMA("sp", lbl[:, l, d, :], lb_logits[l, d, :].rearrange("(h c) -> c h", c=128),
                [], ["lbl"], slow=True)
        DMA("sp", ng_t[:, l, :], hg_norm_g[l, :].rearrange("(h c) -> c h", c=128), [], ["ng_t"], slow=True)

    def barrier():
        S.barrier(lambda e: e.dma_start(out=bar_d, in_=bar_s[:]))

    zt = A.alloc("zt", [128, D], BF16)
    MEMSET("pool", zt[:], 0.0, ["zt"])
    for j in range(NE * CAP // 128):
        DMA("pool", xs[j * 128:(j + 1) * 128, :], zt[:], ["zt"], ["xs"])

    PSUM_STATE = {}

    def psum_banks():
        if not PSUM_STATE:
            PSUM_STATE["f"] = [nc.alloc_psum_tensor("pb%d" % i, [128, 512], F32) for i in range(6)]
            PSUM_STATE["t"] = [nc.alloc_psum_tensor("pbT%d" % i, [128, 1024], BF16) for i in range(2)]
        return PSUM_STATE["f"], PSUM_STATE["t"]

    pb, pbT = psum_banks()
    PB = ["pb%d" % i for i in range(6)]
    PT = ["pbT0", "pbT1"]

    base_mark = A.mark()

    def layer(l):
        src = x if l == 0 else hres
        last = (l == n_layers - 1)
        A.release(base_mark)
        if l == 0:
            MEMSET("dve", lb_t[:], 0.0, ["lb_t"])
        else:
            tmpd = A.alloc("tmpd", [128, 2, 4], F32)
            TT("dve", tmpd[:], lbl[:, 0, :, :], lbl[:, 1, :, :], ALU.subtract, ["lbl"], ["tmpd"])
            ACT(tmpd[:], tmpd[:], AF.Exp, ["tmpd"], ["tmpd"])
            TS("dve", tmpd[:], tmpd[:], 1.0, None, ALU.add, None, ["tmpd"], ["tmpd"])
            RECIP(lb_t[:], tmpd[:], ["tmpd"], ["lb_t"])
        TS("dve", oml_t[:], lb_t[:], -1.0, 1.0, ALU.mult, ALU.add, ["lb_t"], ["oml_t"])
        ACT(lnoml_t[:], oml_t[:], AF.Ln, ["oml_t"], ["lnoml_t"])
        cnt_bc = A.alloc("cnt_bc", [128, NE], F32)
        MEMSET("dve", cnt_bc[:], 0.0, ["cnt_bc"])
        lmark = A.mark()

        for s in range(2):
            A.release(lmark)
            t0 = s * S_
            hT = A.alloc("hT", [128, 8, S_], BF16)
            yT = A.alloc("yT", [128, 8, S_], BF16)
            rope = A.alloc("rope", [16, 2, S_], F32)
            amask = A.alloc("amask", [128, AMW], BF16)
            b1mark = A.mark()
            DMA("sp", rope[:], c_rope, [], ["rope"])
            DMA("sp", amask[:], c_amask, [], ["amask"])
            xt = [A.alloc("xt%d" % i, [128, D], F32) for i in range(2)]
            xb = [A.alloc("xb%d" % i, [128, D], BF16) for i in range(2)]
            for tt in range(16):
                b = tt % 2
                DMA("sp", xt[b][:], src[t0 + tt * 128: t0 + (tt + 1) * 128, :], [], ["xt%d" % b])
                CP("act", xb[b][:], xt[b][:], ["xt%d" % b], ["xb%d" % b])
                for c in range(8):
                    TR(pbT[b][:, c * 128:(c + 1) * 128], xb[b][:, c * 128:(c + 1) * 128],
                       ["xb%d" % b, "ident"], [PT[b]])
                CP("dve", hT[:, :, tt * 128:(tt + 1) * 128],
                   pbT[b][:].rearrange("p (c t) -> p c t", c=8), [PT[b]], [("hT", tt)])
            barrier()
            A.release(b1mark)

            S.reorder_on = False
            wh = [A.alloc("wh%d" % i, [128, 8, 640], BF16) for i in range(2)]
            T = [A.alloc("T%d" % i, [128, 512], F32) for i in range(7)]
            TK = ["T%d" % i for i in range(7)]
            q32 = A.alloc("q32", [128, 512], F32)
            Qb = [A.alloc("Qb%d" % d, [128, S_], BF16) for d in range(2)]
            Kinv = [A.alloc("Kinv%d" % d, [128, S_], BF16) for d in range(2)]
            Kd = [A.alloc("Kd%d" % d, [128, S_], BF16) for d in range(2)]
            KdT = [A.alloc("KdT%d" % d, [128, 16, 128], BF16) for d in range(2)]
            Vh = A.alloc("Vh", [128, 16, 128], BF16)
            sg = A.alloc("sg", [128, S_], BF16)
            oF = A.alloc("oF", [128, S_], F32)
            Dall = A.alloc("Dall", [128, 2, 32], F32)
            S32 = [A.alloc("S32_%d" % d, [128, 128], F32) for d in range(2)]
            Sbf = [A.alloc("Sbf_%d" % d, [128, 128], BF16) for d in range(2)]
            Am = [A.alloc("Am%d" % d, [128, 128], BF16) for d in range(2)]
            rstart = A.alloc("rstart", [128, 512], F32)
            DMA("sp", rstart[:], c_rstart, [], ["rstart"])

            for hh in range(4):
                S.fence()
                S.reorder_on = RE_HG
                w = wh[hh % 2]
                wk = "wh%d" % (hh % 2)
                for gi in range(5):
                    c0 = gi * 512 + hh * 128
                    DMA("pool", w[:, :, gi * 128:(gi + 1) * 128],
                        w_in[l, :, c0:c0 + 128].rearrange("(c p) n -> p c n", p=128), [], [wk])
                lbs = [lb_t[:, d, hh:hh + 1] for d in range(2)]
                omls = [oml_t[:, d, hh:hh + 1] for d in range(2)]
                lnomls = [lnoml_t[:, d, hh:hh + 1] for d in range(2)]
                for blk in range(4):
                    bs = slice(blk * 512, (blk + 1) * 512)
                    hkeys = [("hT", blk * 4 + j) for j in range(4)]
                    for gi, pbi in ((0, 0), (1, 1), (2, 2), (4, 3)):
                        for c in range(8):
                            MM(pb[pbi][:, :], w[:, c, gi * 128:(gi + 1) * 128], hT[:, c, bs],
                               c == 0, c == 7, [wk] + hkeys, [PB[pbi]])
                    for j in range(4):
                        ts_ = slice(blk * 512 + j * 128, blk * 512 + (j + 1) * 128)
                        for c in range(8):
                            MM(pb[4][:, j * 128:(j + 1) * 128], hT[:, c, ts_], w[:, c, 384:512],
                               c == 0, c == 7, [wk] + hkeys, [PB[4]])
                    CP("act", Vh[:, blk * 4:(blk + 1) * 4, :],
                       pb[4][:].rearrange("p (j v) -> p j v", j=4), [PB[4]], [("Vh", blk)])
                    ACT(q32[:], pb[0][:], AF.Identity, [PB[0]], ["q32"], scale=128.0 ** -0.5)
                    ACT(T[6][:], pb[3][:], AF.Exp, [PB[3]], [TK[6]], scale=-1.0)
                    ACT(T[6][:], T[6][:], AF.Ln, [TK[6]], [TK[6]], bias=1.0)
                    ACT(T[6][:], T[6][:], AF.Exp, [TK[6]], [TK[6]], scale=-1.0)
                    TT("dve", sg[:, bs], pb[3][:], T[6][:], ALU.mult, [PB[3], TK[6]], [("sg", blk)])
                    for d in range(2):
                        pa = pb[1 + d]
                        pak = PB[1 + d]
                        ACT(T[0][:], pa[:], AF.Exp, [pak], [TK[0]], scale=-1.0)
                        ACT(T[1][:], T[0][:], AF.Ln, [TK[0]], [TK[1]], bias=1.0)
                        ACT(T[5][:], T[0][:], AF.Ln, [TK[0], "lb_t"], [TK[5]], scale=lbs[d], bias=1.0)
                        TT("dve", T[5][:], T[5][:], T[1][:], ALU.subtract, [TK[5], TK[1]], [TK[5]])
                        STT("dve", T[0][:], pa[:], -1.0, T[1][:], ALU.mult, ALU.subtract,
                            [pak, TK[1]], [TK[0]])
                        S.op("dve", (lambda o_, a_, b_: (lambda e: e.tensor_tensor_scan(
                            o_, a_, b_, 0.0, ALU.mult, ALU.add)))(T[2][:], rstart[:], T[5][:]),
                            reads=["rstart", TK[5]], writes=[TK[2]], cost=1.2)
                        B3 = T[2][:].rearrange("p (n t) -> p n t", t=64)
                        if d == 0:
                            Bx, Bxk = T[2], TK[2]
                            tot = B3[:, :, 63:64]
                        else:
                            TT("pool", T[3][:], T[5][:], T[2][:], ALU.subtract, [TK[5], TK[2]], [TK[3]])
                            TT("pool", T[4][:].rearrange("p (n t) -> p n t", t=64),
                               T[3][:].rearrange("p (n t) -> p n t", t=64),
                               B3[:, :, 63:64].broadcast_to([128, 8, 64]), ALU.add,
                               [TK[3], TK[2]], [TK[4]])
                            Bx, Bxk = T[4], TK[4]
                            tot = T[4][:].rearrange("p (n t) -> p n t", t=64)[:, :, 0:1]
                        ACT(Dall[:, d, blk * 8:(blk + 1) * 8].rearrange("p (n o) -> p n o", o=1), tot,
                            AF.Exp, [Bxk], [("Dall", d, blk)])
                        ACT(T[5][:], Bx[:], AF.Exp, [Bxk], [TK[5]])
                        TT("dve", Qb[d][:, bs], q32[:], T[5][:], ALU.mult, ["q32", TK[5]], [("Qb", d, blk)])
                        TT("dve", T[3][:], T[0][:], Bx[:], ALU.subtract, [TK[0], Bxk], [TK[3]])
                        ACT(Kinv[d][:, bs], T[3][:], AF.Exp, [TK[3], "lnoml_t"], [("Kinv", d, blk)],
                            bias=lnomls[d])
                        TT("pool", T[3][:].rearrange("p (n t) -> p n t", t=64),
                           T[3][:].rearrange("p (n t) -> p n t", t=64),
                           tot.broadcast_to([128, 8, 64]), ALU.add, [TK[3], Bxk], [TK[3]])
                        ACT(Kd[d][:, bs], T[3][:], AF.Exp, [TK[3], "lnoml_t"], [("Kd", d, blk)],
                            bias=lnomls[d])
                for d in range(2):
                    for half in range(2):
                        for j in range(8):
                            tt = half * 8 + j
                            TR(pbT[d][:, j * 128:(j + 1) * 128], Kd[d][:, tt * 128:(tt + 1) * 128],
                               [("Kd", d, tt // 4), "ident"], [PT[d]])
                        CP("act" if d == 0 else "dve", KdT[d][:, half * 8:(half + 1) * 8, :],
                           pbT[d][:].rearrange("p (j c) -> p j c", j=8), [PT[d]], [("KdT", d, half)])
                for d in range(2):
                    MEMSET("pool", S32[d][:], 0.0, [("S32", d)])
                    MEMSET("pool", Sbf[d][:], 0.0, [("Sbf", d)])
                for tt in range(16):
                    for d in range(2):
                        blk = tt // 4
                        tsl = slice(tt * 128, (tt + 1) * 128)
                        pa_i = (2 * tt + d) % 2
                        MM(pb[pa_i][:, 0:128], Kinv[d][:, tsl], Qb[d][:, tsl], True, True,
                           [("Kinv", d, blk), ("Qb", d, blk)], [PB[pa_i]])
                        TT("dve", Kinv[d][:, tsl], pb[pa_i][:, 0:128], hmask[:, d, :], ALU.mult,
                           [PB[pa_i], "hmask"], [("Kinv", d, blk)])
                for i in range(16):
                    tts = [i, 15 - i]
                    for d in range(2):
                        tt = tts[d]
                        MM(pb[2 + d][:, 0:128], Vh[:, tt, :], Kinv[d][:, tt * 128:(tt + 1) * 128], True, False,
                           [("Vh", tt // 4), ("Kinv", d, tt // 4)], [PB[2 + d]])
                    for ci in range(2):
                        for d in range(2):
                            tt = tts[d]
                            blk = tt // 4
                            ch = ci if d == 0 else 1 - ci
                            n = tt * 2 + ch
                            csl = slice(tt * 128 + ch * 64, tt * 128 + (ch + 1) * 64)
                            prow = slice(ch * 64, (ch + 1) * 64)
                            psO, psOk = pb[2 + d], PB[2 + d]
                            psU, psUk = pb[4 + d], PB[4 + d]
                            MM(psO[:, ch * 64:(ch + 1) * 64], Sbf[d][:], Qb[d][:, csl], False, ci == 1,
                               [("Sbf", d), ("Qb", d, blk)], [psOk])
                            MM(psU[:, 0:128], KdT[d][prow, tt, :], Vh[prow, tt, :], True, True,
                               [("KdT", d, tt // 8), ("Vh", blk)], [psUk])
                            STT("dve", S32[d][:], S32[d][:], Dall[:, d, n:n + 1], psU[:, 0:128],
                                ALU.mult, ALU.add, [("S32", d), ("Dall", d, blk), psUk], [("S32", d)])
                            CP("act", Sbf[d][:], S32[d][:], [("S32", d)], [("Sbf", d)])
                    for d in range(2):
                        tt = tts[d]
                        tsl = slice(tt * 128, (tt + 1) * 128)
                        if (d == 0) == (tt <= 7):
                            CP("act", oF[:, tsl], pb[2 + d][:, 0:128], [PB[2 + d]], [("oF", tt)])
                        else:
                            TT("dve", oF[:, tsl], oF[:, tsl], pb[2 + d][:, 0:128], ALU.add,
                               [("oF", tt), PB[2 + d]], [("oF", tt)])
                for blk in range(4):
                    bs = slice(blk * 512, (blk + 1) * 512)
                    ok = [("oF", blk * 4 + j) for j in range(4)]
                    ACT(T[5][:], oF[:, bs], AF.Square, ok, [TK[5]])
                    hi = Qb[0][:, 0:512]
                    lo = Qb[0][:, 512:1024]
                    CP("dve", hi, T[5][:], [TK[5]], [("Qb", 0, 0)])
                    TT("dve", T[6][:], T[5][:], hi, ALU.subtract, [TK[5], ("Qb", 0, 0)], [TK[6]])
                    CP("dve", lo, T[6][:], [TK[6]], [("Qb", 0, 1)])
                    MM(pb[0][:, :], ones_bf[:], hi, True, False, ["ones_bf", ("Qb", 0, 0)], [PB[0]])
                    MM(pb[0][:, :], ones_bf[:], lo, False, True, ["ones_bf", ("Qb", 0, 1)], [PB[0]])
                    ACT(T[6][:], pb[0][:, :], AF.Ln, [PB[0]], [TK[6]], scale=1.0 / 128.0, bias=RMS_EPS)
                    ACT(T[6][:], T[6][:], AF.Exp, [TK[6]], [TK[6]], scale=-0.5)
                    STT("dve", T[5][:], oF[:, bs], ng_t[:, l, hh:hh + 1], T[6][:], ALU.mult, ALU.mult,
                        ok + ["ng_t", TK[6]], [TK[5]])
                    TT("dve", yT[:, hh, bs], T[5][:], sg[:, bs], ALU.mult, [TK[5], ("sg", blk)],
                       [("yT", hh, blk)])

            wa = [A.alloc("wa%d" % i, [128, 8, 224], BF16) for i in range(2)]
            qT = A.alloc("qT", [128, S_], BF16)
            kT = A.alloc("kT", [128, S_], BF16)
            Va = A.alloc("Va", [128, 16, 128], BF16)
            pt = [A.alloc("pt%d" % i, [128, 512], BF16) for i in range(2)]
            pm = [A.alloc("pm%d" % i, [128, 512], BF16) for i in range(2)]
            r1 = A.alloc("r1", [16, 512], F32)
            r2 = A.alloc("r2", [16, 512], F32)
            rc = A.alloc("rc", [64, 512], F32)
            MEMSET("pool", Va[:], 1.0, [("Va", b) for b in range(4)])
            MEMSET("pool", qT[:], 0.0, [("qT", b) for b in range(4)])
            MEMSET("pool", kT[:], 0.0, [("kT", b) for b in range(4)])
            for h in range(8):
                S.fence()
                S.reorder_on = RE_ATT
                w = wa[h % 2]
                wk = "wa%d" % (h % 2)
                for gi in range(3):
                    c0 = 2560 + gi * 512 + h * 64
                    DMA("pool", w[:, :, gi * 80:gi * 80 + 64],
                        w_in[l, :, c0:c0 + 64].rearrange("(c p) n -> p c n", p=128), [], [wk])
                for gi in range(2):
                    CP("pool", w[:, :, gi * 80 + 64:gi * 80 + 72], w[:, :, gi * 80 + 8:gi * 80 + 16], [wk], [wk])
                    CP("pool", w[:, :, gi * 80 + 72:gi * 80 + 80], w[:, :, gi * 80 + 0:gi * 80 + 8], [wk], [wk])
                for blk in range(4):
                    bs = slice(blk * 512, (blk + 1) * 512)
                    hkeys = [("hT", blk * 4 + j) for j in range(4)]
                    for gi in range(2):
                        for c in range(8):
                            MM(pb[gi][0:80, :], w[:, c, gi * 80:(gi + 1) * 80], hT[:, c, bs],
                               c == 0, c == 7, [wk] + hkeys, [PB[gi]])
                    for j in range(4):
                        ts_ = slice(blk * 512 + j * 128, blk * 512 + (j + 1) * 128)
                        for c in range(8):
                            MM(pb[2][:, j * 64:(j + 1) * 64], hT[:, c, ts_], w[:, c, 160:224],
                               c == 0, c == 7, [wk] + hkeys, [PB[2]])
                    CP("act", Va[:, blk * 4:(blk + 1) * 4, 0:64],
                       pb[2][:, 0:256].rearrange("p (j v) -> p j v", j=4), [PB[2]], [("Va", blk)])
                    for gi, dst, dk in ((0, qT, "qT"), (1, kT, "kT")):
                        CP("act", dst[0:64, bs], pb[gi][0:64, :], [PB[gi]], [(dk, blk)])
                        TT("dve", r1[:], pb[gi][0:16, :], rope[:, 0, bs], ALU.mult, [PB[gi], "rope"], ["r1"])
                        CP("act", r2[:], pb[gi][64:80, :], [PB[gi]], ["r2"])
                        TT("dve", r2[:], r2[:], rope[:, 1, bs], ALU.mult, ["r2", "rope"], ["r2"])
                        TT("dve", dst[0:16, bs], r1[:], r2[:], ALU.add, ["r1", "r2"], [(dk, blk)])
                pairs = []
                for qb in range(4):
                    q0 = qb * 512
                    kbs = [kb for kb in range(16)
                           if kb * 128 >= q0 - 1151 and kb * 128 <= q0 + 1535]
                    for ki, kb in enumerate(kbs):
                        pairs.append((qb, ki, kb, ki == len(kbs) - 1))

                def emit_S(i):
                    qb, ki, kb, lastk = pairs[i]
                    q0 = qb * 512
                    ms = q0 - kb * 128 - OFFMIN
                    assert 0 <= ms and ms + 512 <= AMW
                    b = i % 2
                    psS, psSk = pb[3 + b], PB[3 + b]
                    MM(psS[:, :], kT[:, kb * 128:(kb + 1) * 128], qT[:, q0:q0 + 512], True, True,
                       [("kT", kb // 4), ("qT", qb)], [psSk])
                    ACT(pt[b][:], psS[:, :], AF.Exp, [psSk], ["pt%d" % b], scale=0.125)
                    TT("dve", pm[b][:], pt[b][:], amask[:, ms:ms + 512], ALU.mult,
                       ["pt%d" % b, "amask"], ["pm%d" % b])

                def emit_PV(i):
                    qb, ki, kb, lastk = pairs[i]
                    q0 = qb * 512
                    b = i % 2
                    nb = 5 if qb % 2 == 0 else 2
                    psN, psNk = pb[nb], PB[nb]
                    MM(psN[:, :], Va[:, kb, :], pm[b][:], ki == 0, lastk,
                       [("Va", kb // 4), "pm%d" % b], [psNk])
                    if lastk:
                        ACT(rc[:], psN[64:128, :], AF.Ln, [psNk], ["rc"])
                        ACT(rc[:], rc[:], AF.Exp, ["rc"], ["rc"], scale=-1.0)
                        pr = slice((h % 2) * 64, (h % 2) * 64 + 64)
                        TT("dve", yT[pr, 4 + h // 2, q0:q0 + 512], psN[0:64, :], rc[:], ALU.mult,
                           [psNk, "rc"], [("yT", 4 + h // 2, qb)])

                emit_S(0)
                for i in range(len(pairs)):
                    if i + 1 < len(pairs):
                        emit_S(i + 1)
                    emit_PV(i)
            if dbg and l == n_layers - 1:
                DMA("sp", dbg_yT[s], yT[:], [("yT", c, q) for c in range(8) for q in range(4)], ["dbg_yT"])
            barrier()
            S.reorder_on = True
            A.release(b1mark)

            wo = A.alloc("wo", [128, 8, D], BF16)
            for c in range(8):
                DMA("pool", wo[:, c, :], w_out[l, c * 128:(c + 1) * 128, :], [], ["wo"])
            g1 = A.alloc("g1", [128, D], F32)
            b1 = A.alloc("b1", [128, D], F32)
            DMA("sp", g1[:], ln1_g[l:l + 1, :].broadcast_to([128, D]), [], ["g1"])
            DMA("sp", b1[:], ln1_b[l:l + 1, :].broadcast_to([128, D]), [], ["b1"])
            wr = A.alloc("wr", [128, 8, 36], F32)
            wr_hi = A.alloc("wr_hi", [128, 8, 36], BF16)
            wr_lo = A.alloc("wr_lo", [128, 8, 36], BF16)
            wr_t = A.alloc("wr_t", [128, 8, 36], F32)
            rb = A.alloc("rb", [128, 36], F32)
            DMA("sp", wr[:, :, 0:4], r_w1[l].rearrange("(c p) n -> p c n", p=128), [], ["wr"], slow=True)
            DMA("sp", wr[:, :, 4:36], r_w2[l].rearrange("(c p) n -> p c n", p=128), [], ["wr"], slow=True)
            DMA("sp", rb[:, 0:4], r_b1[l:l + 1, :].broadcast_to([128, 4]), [], ["rb"], slow=True)
            DMA("sp", rb[:, 4:36], r_b2[l:l + 1, :].broadcast_to([128, 32]), [], ["rb"], slow=True)
            CP("dve", wr_hi[:], wr[:], ["wr"], ["wr_hi"])
            TT("dve", wr_t[:], wr[:], wr_hi[:], ALU.subtract, ["wr", "wr_hi"], ["wr_t"])
            CP("dve", wr_lo[:], wr_t[:], ["wr_t"], ["wr_lo"])
            ht = [A.alloc("ht%d" % i, [128, D], F32) for i in range(2)]
            z = [A.alloc("z%d" % i, [128, D], F32) for i in range(2)]
            hb = [A.alloc("hb%d" % i, [128, D], BF16) for i in range(2)]
            hlo = [A.alloc("hlo%d" % i, [128, D], BF16) for i in range(2)]
            hTh = A.alloc("hTh", [128, 8, 128], BF16)
            hTl = A.alloc("hTl", [128, 8, 128], BF16)
            st = A.alloc("st", [128, 2, 6], F32)
            mv = A.alloc("mv", [128, 2], F32)
            rstd = A.alloc("rstd", [128, 1], F32)
            nmr = A.alloc("nmr", [128, 1], F32)
            L = A.alloc("L", [128, 36], F32)
            sm = A.alloc("sm", [128, 16], F32)
            e4 = A.alloc("e4", [128, 4], F32)
            oh1 = A.alloc("oh1", [128, 4], F32)
            L2 = A.alloc("L2", [128, 32], F32)
            L2b = A.alloc("L2b", [128, 32], F32)
            oha = A.alloc("oha", [128, 32], F32)
            ohb = A.alloc("ohb", [128, 32], F32)
            Mbf = A.alloc("Mbf", [128, 32], BF16)
            pos = A.alloc("pos", [128, 32], F32)
            tq = A.alloc("tq", [128, 32], F32)
            sl = A.alloc("sl", [128, 2], F32)
            for ti in range(16):
                tt = s * 16 + ti
                b = ti % 2
                tsl = slice(ti * 128, (ti + 1) * 128)
                rows = slice(t0 + ti * 128, t0 + (ti + 1) * 128)
                DMA("sp", ht[b][:], src[rows, :], ["hres_%d" % tt], ["ht%d" % b])
                for half in range(2):
                    for c in range(8):
                        MM(pb[half][:, :], yT[:, c, tsl], wo[:, c, half * 512:(half + 1) * 512],
                           c == 0, c == 7, ["wo"] + [("yT", c, ti // 4)], [PB[half]])
                    STT("dve", z[b][:, half * 512:(half + 1) * 512], ht[b][:, half * 512:(half + 1) * 512],
                        ALPHA, pb[half][:, :], ALU.mult, ALU.add, ["ht%d" % b, PB[half]], ["z%d" % b])
                ln_tail(z[b], "z%d" % b, st, mv, rstd, nmr, g1, "g1", b1, "b1")
                DMA("sp", hres[rows, :], z[b][:], ["z%d" % b], ["hres_%d" % tt], sem_key="st_z%d" % b)
                CP("act", hb[b][:], z[b][:], ["z%d" % b], ["hb%d" % b])
                TT("pool", ht[b][:], z[b][:], hb[b][:], ALU.subtract, ["z%d" % b, "hb%d" % b], ["ht%d" % b])
                CP("pool", hlo[b][:], ht[b][:], ["ht%d" % b], ["hlo%d" % b])
                for c in range(8):
                    TR(pbT[0][:, c * 128:(c + 1) * 128], hb[b][:, c * 128:(c + 1) * 128],
                       ["hb%d" % b, "ident"], [PT[0]])
                for c in range(8):
                    TR(pbT[1][:, c * 128:(c + 1) * 128], hlo[b][:, c * 128:(c + 1) * 128],
                       ["hlo%d" % b, "ident"], [PT[1]])
                CP("act", hTh[:], pbT[0][:].rearrange("p (c t) -> p c t", c=8), [PT[0]], ["hTh"])
                CP("dve", hTl[:], pbT[1][:].rearrange("p (c t) -> p c t", c=8), [PT[1]], ["hTl"])
                n = 0
                for (a_, ak, w_, wk_) in ((hTh, "hTh", wr_hi, "wr_hi"), (hTl, "hTl", wr_hi, "wr_hi"),
                                          (hTh, "hTh", wr_lo, "wr_lo")):
                    for c in range(8):
                        MM(pb[2][:, 0:36], a_[:, c, :], w_[:, c, :], n == 0, n == 23, [ak, wk_], [PB[2]])
                        n += 1
                route(tt, pb[2], PB[2], rb, L, sm, e4, oh1, L2, L2b, oha, ohb, Mbf, pos, tq, sl, cnt_bc)
                for k in range(2):
                    S.op("pool", (lambda idx_, src_: (lambda e: e.indirect_dma_start(
                        out=xs, out_offset=bass.IndirectOffsetOnAxis(ap=idx_, axis=0),
                        in_=src_, in_offset=None)))(slots_i[:, tt, k:k + 1], hb[b][:, :]),
                        reads=["hb%d" % b, ("slots", tt)], writes=["xs"], dma=True, sem_key="sc_hb%d" % b, cost=6.0)
            barrier()

        A.release(lmark)
        wg = [A.alloc("wg%d" % i, [128, 8, 512], BF16) for i in range(2)]
        wu = [A.alloc("wu%d" % i, [128, 8, 512], BF16) for i in range(2)]
        wd = [A.alloc("wd%d" % i, [128, 4, D], BF16) for i in range(2)]
        xr = [A.alloc("xr%d" % i, [128, 3, D], BF16) for i in range(2)]
        xT = [A.alloc("xT%d" % i, [128, 8, CAP], BF16) for i in range(2)]
        hid = A.alloc("hid", [128, 4, CAP], BF16)
        sil = [A.alloc("sil%d" % i, [128, CAP], F32) for i in range(2)]
        yo = [A.alloc("yo%d" % i, [128, D], F32) for i in range(2)]
        for e_ in range(NE):
            b = e_ % 2
            DMA("sp", xr[b][:], xs[e_ * CAP:(e_ + 1) * CAP, :].rearrange("(j p) d -> p j d", p=128),
                ["xs"], ["xr%d" % b])
            for c in range(8):
                DMA("pool", wg[b][:, c, :], w_gate[l, e_, c * 128:(c + 1) * 128, :], [], ["wg%d" % b])
                DMA("pool", wu[b][:, c, :], w_up[l, e_, c * 128:(c + 1) * 128, :], [], ["wu%d" % b])
            for f in range(4):
                DMA("pool", wd[b][:, f, :], w_down[l, e_, f * 128:(f + 1) * 128, :], [], ["wd%d" % b])
            for j in range(3):
                tb = j % 2
                for c in range(8):
                    TR(pbT[tb][:, c * 128:(c + 1) * 128], xr[b][:, j, c * 128:(c + 1) * 128],
                       ["xr%d" % b, "ident"], [PT[tb]])
                CP("act" if j % 2 == 0 else "dve", xT[b][:, :, j * 128:(j + 1) * 128],
                   pbT[tb][:].rearrange("p (c t) -> p c t", c=8), [PT[tb]], ["xT%d" % b])
            for f in range(4):
                fb = f % 2
                pg, pgk = pb[fb], PB[fb]
                pu, puk = pb[2 + fb], PB[2 + fb]
                for c in range(8):
                    MM(pg[:, 0:CAP], wg[b][:, c, f * 128:(f + 1) * 128], xT[b][:, c, :], c == 0, c == 7,
                       ["wg%d" % b, "xT%d" % b], [pgk])
                for c in range(8):
                    MM(pu[:, 0:CAP], wu[b][:, c, f * 128:(f + 1) * 128], xT[b][:, c, :], c == 0, c == 7,
                       ["wu%d" % b, "xT%d" % b], [puk])
                ACT(sil[fb][:], pg[:, 0:CAP], AF.Silu, [pgk], ["sil%d" % fb])
                TT("dve", hid[:, f, :], sil[fb][:], pu[:, 0:CAP], ALU.mult, ["sil%d" % fb, puk], [("hid", f)])
            for j in range(3):
                yb = j % 2
                for half in range(2):
                    py, pyk = pb[4 + half], PB[4 + half]
                    for f in range(4):
                        MM(py[:, :], hid[:, f, j * 128:(j + 1) * 128], wd[b][:, f, half * 512:(half + 1) * 512],
                           f == 0, f == 3, [("hid", f), "wd%d" % b], [pyk])
                    CP("act" if half == 0 else "dve", yo[yb][:, half * 512:(half + 1) * 512], py[:, :],
                       [pyk], ["yo%d" % yb])
                r0 = e_ * CAP + j * 128
                DMA("sp", ys[r0:r0 + 128, :], yo[yb][:], ["yo%d" % yb], ["ys"], sem_key="st_yo%d" % yb)
        barrier()

        A.release(lmark)
        g2 = A.alloc("g2", [128, D], F32)
        b2 = A.alloc("b2", [128, D], F32)
        DMA("sp", g2[:], ln2_g[l:l + 1, :].broadcast_to([128, D]), [], ["g2"])
        DMA("sp", b2[:], ln2_b[l:l + 1, :].broadcast_to([128, D]), [], ["b2"])
        ht = [A.alloc("ht%d" % i, [128, D], F32) for i in range(2)]
        yg = [A.alloc("yg%d" % i, [128, 2, D], F32) for i in range(2)]
        z = [A.alloc("z%d" % i, [128, D], F32) for i in range(2)]
        st = A.alloc("st", [128, 2, 6], F32)
        mv = A.alloc("mv", [128, 2], F32)
        rstd = A.alloc("rstd", [128, 1], F32)
        nmr = A.alloc("nmr", [128, 1], F32)
        dst = out if last else hres
        for tt in range(NTT):
            b = tt % 2
            rows = slice(tt * 128, (tt + 1) * 128)
            DMA("sp", ht[b][:], hres[rows, :], ["hres_%d" % tt], ["ht%d" % b])
            for k in range(2):
                S.op("pool", (lambda idx_, dst_: (lambda e: e.indirect_dma_start(
                    out=dst_, out_offset=None, in_=ys,
                    in_offset=bass.IndirectOffsetOnAxis(ap=idx_, axis=0))))(slots_i[:, tt, k:k + 1], yg[b][:, k, :]),
                    reads=["ys", ("slots", tt)], writes=["yg%d" % b], dma=True, cost=8.0)
            ACT(z[b][:], ht[b][:], AF.Identity, ["ht%d" % b], ["z%d" % b], scale=ALPHA)
            for k in range(2):
                STT("dve", z[b][:], yg[b][:, k, :], gates[:, tt, k:k + 1], z[b][:],
                    ALU.mult, ALU.add, ["yg%d" % b, ("gates", tt), "z%d" % b], ["z%d" % b])
            ln_tail(z[b], "z%d" % b, st, mv, rstd, nmr, g2, "g2", b2, "b2")
            DMA("sp", dst[rows, :], z[b][:], ["z%d" % b], ["out" if last else "hres_%d" % tt],
                sem_key="st_z%d" % b)
        barrier()

    def ln_tail(zt, zk, st, mv, rstd, nmr, g, gk, b_, bk):
        for half in range(2):
            S.op("dve", (lambda o_, i_: (lambda e: e.bn_stats(o_, i_)))(st[:, half, :], zt[:, half * 512:(half + 1) * 512]),
                 reads=[zk], writes=["st"], cost=0.7)
        S.op("dve", lambda e: e.bn_aggr(mv[:], st[:].rearrange("p a b -> p (a b)")), reads=["st"], writes=["mv"])
        ACT(rstd[:], mv[:, 1:2], AF.Ln, ["mv"], ["rstd"], bias=LN_EPS)
        ACT(rstd[:], rstd[:], AF.Exp, ["rstd"], ["rstd"], scale=-0.5)
        STT("dve", nmr[:], mv[:, 0:1], -1.0, rstd[:], ALU.mult, ALU.mult, ["mv", "rstd"], ["nmr"])
        ACT(zt[:], zt[:], AF.Identity, [zk, "rstd", "nmr"], [zk], scale=rstd[:, 0:1], bias=nmr[:, 0:1])
        TT("pool", zt[:], zt[:], g[:], ALU.mult, [zk, gk], [zk])
        TT("dve", zt[:], zt[:], b_[:], ALU.add, [zk, bk], [zk])

    def route(tt, pl, plk, rb, L, sm, e4, oh1, L2, L2b, oha, ohb, Mbf, pos, tq, sl, cnt_bc):
        TT("dve", L[:], pl[:, 0:36], rb[:], ALU.add, [plk, "rb"], ["L"])
        m1, nm1, s1, pg_, ma, mb, dd, ga = (sm[:, i:i + 1] for i in range(8))
        S.op("dve", lambda e: e.reduce_max(m1, L[:, 0:4], AX.X), reads=["L"], writes=["sm0"])
        TS("dve", oh1[:], L[:, 0:4], m1, None, ALU.is_equal, None, ["L", "sm0"], ["oh1"])
        TS("dve", nm1, m1, -1.0, None, ALU.mult, None, ["sm0"], ["sm1"])
        ACT(e4[:], L[:, 0:4], AF.Exp, ["L", "sm1"], ["e4"], bias=nm1)
        S.op("dve", lambda e: e.reduce_sum(s1, e4[:], AX.X), reads=["e4"], writes=["sm2"])
        RECIP(pg_, s1, ["sm2"], ["sm3"])
        TS("dve", e4[:], oh1[:], BIG, -BIG, ALU.mult, ALU.add, ["oh1", "e4"], ["e4"])
        TT("dve", L2[:].rearrange("p (g e) -> p g e", g=4), L[:, 4:36].rearrange("p (g e) -> p g e", g=4),
           e4[:].rearrange("p (g o) -> p g o", o=1).broadcast_to([128, 4, 8]), ALU.add, ["L", "e4"], ["L2"])
        S.op("dve", lambda e: e.reduce_max(ma, L2[:], AX.X), reads=["L2"], writes=["sm4"])
        TS("dve", oha[:], L2[:], ma, None, ALU.is_equal, None, ["L2", "sm4"], ["oha"])
        STT("dve", L2b[:], oha[:], -BIG, L2[:], ALU.mult, ALU.add, ["oha", "L2"], ["L2b"])
        S.op("dve", lambda e: e.reduce_max(mb, L2b[:], AX.X), reads=["L2b"], writes=["sm5"])
        TS("dve", ohb[:], L2b[:], mb, None, ALU.is_equal, None, ["L2b", "sm5"], ["ohb"])
        TT("dve", dd, mb, ma, ALU.subtract, ["sm4", "sm5"], ["sm6"])
        ACT(dd, dd, AF.Exp, ["sm6"], ["sm6"])
        TS("dve", dd, dd, 1.0, None, ALU.add, None, ["sm6"], ["sm6"])
        RECIP(ga, dd, ["sm6"], ["sm7"])
        TT("dve", gates[:, tt, 0:1], ga, pg_, ALU.mult, ["sm7", "sm3"], [("gates", tt)])
        TT("dve", gates[:, tt, 1:2], pg_, gates[:, tt, 0:1], ALU.subtract, ["sm3", ("gates", tt)], [("gates", tt)])
        TT("dve", Mbf[:], oha[:], ohb[:], ALU.add, ["oha", "ohb"], ["Mbf"])
        MM(pb[3][:, 0:32], ustrict[:], Mbf[:], True, True, ["ustrict", "Mbf"], [PB[3]])
        MM(pb[4][:, 0:32], ones_bf[:], Mbf[:], True, True, ["ones_bf", "Mbf"], [PB[4]])
        TT("dve", pos[:], pb[3][:, 0:32], cnt_bc[:], ALU.add, [PB[3], "cnt_bc"], ["pos"])
        TT("dve", cnt_bc[:], cnt_bc[:], pb[4][:, 0:32], ALU.add, ["cnt_bc", PB[4]], ["cnt_bc"])
        TT("dve", pos[:], pos[:], ecap[:], ALU.add, ["pos", "ecap"], ["pos"])
        TT("dve", tq[:], pos[:], oha[:], ALU.mult, ["pos", "oha"], ["tq"])
        S.op("dve", lambda e: e.reduce_sum(sl[:, 0:1], tq[:], AX.X), reads=["tq"], writes=["sl"])
        TT("dve", tq[:], pos[:], ohb[:], ALU.mult, ["pos", "ohb", "sl"], ["tq"])
        S.op("dve", lambda e: e.reduce_sum(sl[:, 1:2], tq[:], AX.X), reads=["tq"], writes=["sl"])
        TS("dve", sl[:], sl[:], 0.0, float(NE * CAP - 1), ALU.max, ALU.min, ["sl"], ["sl"])
        CP("dve", slots_i[:, tt, :], sl[:], ["sl"], [("slots", tt)])

    for l in range(n_layers):
        layer(l)
    import os
    S.emit(final_keys=["out"], reorder=os.environ.get("NOREORDER") is None)
    return nc


def _consts():
    bf = ml_dtypes.bfloat16
    ident = np.eye(128, dtype=np.float32).astype(bf)
    p = np.arange(128)[:, None]
    j = np.arange(AMW)[None, :]
    dl = j - p + OFFMIN
    ad = np.abs(dl)
    cnt = (ad <= 64).astype(np.float32) + ((dl % 4 == 0) & (ad <= 256)) + ((dl % 16 == 0) & (ad <= 1024))
    amask = cnt.astype(bf)
    s = np.arange(128)[:, None]
    t = np.arange(128)[None, :]
    same = (s // 64) == (t // 64)
    hm = np.stack([(same & (s <= t)), (same & (s >= t))], axis=1).astype(np.float32).astype(bf)
    ustrict = (s < t).astype(np.float32).astype(bf)
    rstart = np.ones((128, 512), np.float32)
    rstart[:, ::64] = 0.0
    half = 8
    inv_freq = (500000.0 ** (-np.arange(half, dtype=np.float32) / half)).astype(np.float32)
    pos = np.arange(S_, dtype=np.float32)
    ang = (pos[None, :] * inv_freq[:, None]).astype(np.float32)
    cos = np.cos(ang).astype(np.float32)
    sin = np.sin(ang).astype(np.float32)
    rope = np.zeros((16, 2, S_), np.float32)
    rope[0:8, 0] = cos
    rope[8:16, 0] = cos
    rope[0:8, 1] = -sin
    rope[8:16, 1] = sin
    ecap = np.tile((np.arange(NE, dtype=np.float32) * CAP)[None, :], (128, 1))
    return {"c_ident": ident, "c_amask": amask, "c_hmask": np.ascontiguousarray(hm), "c_ustrict": ustrict,
            "c_rstart": rstart, "c_rope": rope, "c_ecap": ecap}


_NC_CACHE = {}


def kernel(**inputs):
    if "nc" not in _NC_CACHE:
        _NC_CACHE["nc"] = build()
    nc = _NC_CACHE["nc"]
    consts = _consts()
    x = np.ascontiguousarray(inputs["x"], dtype=np.float32).reshape(NCORES, TOK, D)
    shared = {k: np.ascontiguousarray(v) for k, v in inputs.items() if k != "x"}
    in_maps = []
    for c in range(NCORES):
        m = {"x": x[c]}
        m.update(shared)
        m.update(consts)
        in_maps.append(m)
    res = run_bass_kernel_spmd(nc, in_maps, core_ids=list(range(NCORES)))
    o = np.stack([np.asarray(r["out"], dtype=np.float32) for r in res.results], axis=0)
    return o.reshape(16, S_, D)
```

```python
import contextlib
import numpy as np
import ml_dtypes
import concourse.bass as bass
import concourse.mybir as mybir
from concourse.bass_utils import run_bass_kernel_spmd

F32 = mybir.dt.float32
BF16 = mybir.dt.bfloat16
I32 = mybir.dt.int32
AF = mybir.ActivationFunctionType
ALU = mybir.AluOpType
AX = mybir.AxisListType

NCORES = 8
S_ = 2048
D = 1024
TOK = 2 * S_
NTT = TOK // 128
CAP = 384
NE = 32
ALPHA = 4.0 ** 0.25
LN_EPS = 1e-5
RMS_EPS = 1e-6
OFFMIN = -1408
AMW = 1024 - OFFMIN + 512
BIG = 1.0e4


class _Op:
    __slots__ = ("eng", "fn", "deps", "odeps", "is_dma", "dkey", "sig", "val", "idx", "cost", "tag", "grp")


class Sched:
    ENGS = ("pe", "act", "dve", "pool", "sp")
    LAT = 0.25

    def __init__(self, nc):
        self.nc = nc
        self.ops = []
        self.last_writer = {}
        self.readers = {}
        self.dma_count = {}
        self.last_dma = {}
        self.fixed = []
        self.pool_dmas = []
        self.reorder_on = True
        self.seg_flags = []

    def op(self, eng, fn, reads=(), writes=(), dma=False, sem_key=None, force=False, cost=0.3):
        o = _Op()
        o.eng = eng
        o.fn = fn
        o.is_dma = dma
        o.idx = len(self.ops)
        o.sig = False
        o.val = None
        o.dkey = None
        o.cost = cost
        o.grp = None
        o.tag = "%s r=%s w=%s" % ("DMA" if dma else "", list(reads)[:3], list(writes)[:2])
        odeps = set()
        if dma:
            o.dkey = sem_key if sem_key is not None else writes[0]
            self.dma_count[o.dkey] = self.dma_count.get(o.dkey, 0) + 1
            o.val = 16 * self.dma_count[o.dkey]
            if o.dkey in self.last_dma:
                odeps.add(self.last_dma[o.dkey])
            self.last_dma[o.dkey] = o.idx
        deps = set()
        for k in reads:
            for j in self.last_writer.get(k, ()):
                deps.add(j)
        for k in writes:
            ws = self.last_writer.get(k, [])
            rs = self.readers.get(k, [])
            if (dma and not force and not rs and ws
                    and all(self.ops[j].is_dma for j in ws)):
                self.last_writer[k] = ws + [o.idx]
            else:
                for j in ws:
                    deps.add(j)
                for j in rs:
                    p = self.ops[j]
                    deps.add(j)
                self.last_writer[k] = [o.idx]
                self.readers[k] = []
        fdeps = []
        for j in deps:
            p = self.ops[j]
            if p.fn is None:
                continue
            if p.eng == "pe" and eng == "pe" and not p.is_dma and not dma:
                odeps.add(j)
                continue
            fdeps.append(j)
        o.deps = fdeps
        o.odeps = list(odeps)
        for j in fdeps:
            self.ops[j].sig = True
        for k in reads:
            if k not in writes:
                self.readers.setdefault(k, []).append(o.idx)
        self.ops.append(o)
        return o

    def fence(self):
        n = len(self.ops)
        self.fixed.append((n, n))
        self.seg_flags.append(self.reorder_on)

    def barrier(self, fn_tiny):
        keys = list(set(list(self.last_writer.keys()) + list(self.readers.keys())))
        keys = [k for k in keys if k != "__bar"]
        a = len(self.ops)
        self.op("sp", fn_tiny, reads=[], writes=keys + ["__bar"], dma=True,
                sem_key="__bar", force=True, cost=2.0)
        bw = self.last_writer["__bar"]
        self.last_writer = {"__bar": bw}
        self.readers = {}
        for e in ("pe", "act", "dve", "pool"):
            self.op(e, None, reads=["__bar"], cost=0.05)
        self.fixed.append((a, len(self.ops)))
        self.seg_flags.append(self.reorder_on)

    def _schedule_segment(self, a, b, order):
        import heapq
        ops = self.ops
        n = b - a
        if n == 0:
            return
        succ = [[] for _ in range(n)]
        ndep = [0] * n
        import os
        chain = set(os.environ.get("CHAIN_ENGS", "").split(","))
        lastop = {}
        for i in range(a, b):
            o = ops[i]
            ds = set(j for j in list(o.deps) + list(o.odeps) if j >= a)
            if o.eng in chain:
                if o.eng in lastop:
                    ds.add(lastop[o.eng])
                    if lastop[o.eng] not in o.deps and lastop[o.eng] not in o.odeps:
                        o.odeps.append(lastop[o.eng])
                lastop[o.eng] = i
            ndep[i - a] = len(ds)
            for j in ds:
                succ[j - a].append(i)
        bl = [0.0] * n
        for i in range(b - 1, a - 1, -1):
            m = 0.0
            for sidx in succ[i - a]:
                if bl[sidx - a] > m:
                    m = bl[sidx - a]
            bl[i - a] = m + ops[i].cost
        ready_t = [0.0] * n
        fin = [0.0] * n
        efree = {e: 0.0 for e in self.ENGS}
        avail = {e: [] for e in self.ENGS}
        for i in range(a, b):
            if ndep[i - a] == 0:
                avail[ops[i].eng].append(i)
        left = n
        open_multi = None
        while left:
            best = None
            for e in self.ENGS:
                av = avail[e]
                if e == "pe" and open_multi is not None:
                    av = [i for i in av if ops[i].grp is None or ops[i].grp[0] == open_multi]
                if not av:
                    continue
                mn = min(ready_t[i - a] for i in av)
                t = max(efree[e], mn)
                c = None
                for i in av:
                    if ready_t[i - a] <= t + 1e-9:
                        k = (-bl[i - a], i)
                        if c is None or k < c[0]:
                            c = (k, i)
                if best is None or t < best[0]:
                    best = (t, e, c[1])
            if best is None:
                assert open_multi is not None
                open_multi = None
                continue
            t, e, i = best
            avail[e].remove(i)
            o = ops[i]
            if e == "pe" and o.grp is not None:
                open_multi = None if o.grp[1] else o.grp[0]
            if o.is_dma:
                efree[e] = t + (1.0 if e == "pool" else 0.06)
            else:
                efree[e] = t + o.cost
            fin[i - a] = t + o.cost
            order[e].append(i)
            left -= 1
            for sidx in succ[i - a]:
                so = ops[sidx]
                if i in so.deps:
                    r = fin[i - a] + self.LAT
                else:
                    r = t
                if r > ready_t[sidx - a]:
                    ready_t[sidx - a] = r
                ndep[sidx - a] -= 1
                if ndep[sidx - a] == 0:
                    avail[so.eng].append(sidx)

    def _check(self, order):
        ops = self.ops
        sem = {}
        ptr = {e: 0 for e in self.ENGS}
        total = sum(len(v) for v in order.values())
        done = 0
        while done < total:
            prog = False
            for e in self.ENGS:
                while ptr[e] < len(order[e]):
                    o = ops[order[e][ptr[e]]]
                    ok = True
                    for j in o.deps:
                        p = ops[j]
                        k = ("d", p.dkey) if p.is_dma else ("e", p.eng)
                        if sem.get(k, 0) < p.val:
                            ok = False
                            break
                    if not ok:
                        break
                    if o.fn is not None:
                        if o.is_dma:
                            k = ("d", o.dkey)
                            sem[k] = sem.get(k, 0) + 16
                            assert sem[k] == o.val, ("dma order", o.dkey, sem[k], o.val)
                        elif o.sig:
                            k = ("e", o.eng)
                            sem[k] = sem.get(k, 0) + 1
                            assert sem[k] == o.val
                    ptr[e] += 1
                    done += 1
                    prog = True
            if not prog:
                msg = []
                for e in self.ENGS:
                    if ptr[e] < len(order[e]):
                        o = ops[order[e][ptr[e]]]
                        msg.append((e, o.idx, [(j, ops[j].eng, ops[j].val, ops[j].dkey) for j in o.deps]))
                raise RuntimeError("DEADLOCK in emitted order: %r" % (msg,))

    def emit(self, final_keys=(), reorder=True):
        nc = self.nc
        self.op("sp", None, reads=list(final_keys))
        ops = self.ops
        order = {e: [] for e in self.ENGS}
        pos = 0
        import os
        segsel = os.environ.get("REORDER_SEGS")
        segsel = None if segsel is None else set(int(v) for v in segsel.split(",") if v != "")
        for si, (fa, fb) in enumerate(self.fixed + [(len(ops), len(ops))]):
            flag = self.seg_flags[si] if si < len(self.seg_flags) else True
            if reorder and flag and (segsel is None or si in segsel):
                self._schedule_segment(pos, fa, order)
            else:
                for i in range(pos, fa):
                    order[ops[i].eng].append(i)
            for i in range(fa, fb):
                order[ops[i].eng].append(i)
            pos = fb
        assert sum(len(v) for v in order.values()) == len(ops)
        for e in self.ENGS:
            c = 0
            for i in order[e]:
                o = ops[i]
                if not o.is_dma and o.sig:
                    c += 1
                    o.val = c
        dkeys = list(self.dma_count.keys())
        self._check(order)
        import os
        if os.environ.get("DUMP_ORDER"):
            with open(os.environ["DUMP_ORDER"], "w") as f:
                for e in self.ENGS:
                    f.write("=== %s\n" % e)
                    for i in order[e]:
                        o = ops[i]
                        f.write("%6d %s deps=%s\n" % (i, o.tag, sorted(o.deps)))
        with contextlib.ExitStack() as es:
            esem = {e: es.enter_context(nc.semaphore("s_" + e)) for e in self.ENGS}
            dsem = {k: es.enter_context(nc.semaphore("d%d" % i)) for i, k in enumerate(dkeys)}
            block = es.enter_context(nc.Block())

            def run(engname, eng):
                waited = {}
                for i in order[engname]:
                    o = ops[i]
                    need = {}
                    for j in o.deps:
                        p = ops[j]
                        s = dsem[p.dkey] if p.is_dma else esem[p.eng]
                        v = p.val
                        key = id(s)
                        if v > need.get(key, (None, 0))[1]:
                            need[key] = (s, v)
                    for key, (s, v) in need.items():
                        if waited.get(key, 0) >= v:
                            continue
                        eng.wait_ge(s, v)
                        waited[key] = v
                    if o.fn is None:
                        continue
                    ins = o.fn(eng)
                    if o.is_dma:
                        ins.then_inc(dsem[o.dkey], 16)
                    elif o.sig:
                        ins.then_inc(esem[o.eng], 1)

            @block.sync
            def _(e):
                run("sp", e)

            @block.scalar
            def _(e):
                run("act", e)

            @block.vector
            def _(e):
                run("dve", e)

            @block.tensor
            def _(e):
                run("pe", e)

            @block.gpsimd
            def _(e):
                run("pool", e)


class Arena:
    def __init__(self, nc, base=16512, limit=229344):
        self.nc = nc
        self.cur = base
        self.limit = limit
        self.n = 0

    def alloc(self, name, shape, dt):
        esz = 2 if dt == BF16 else 4
        nbytes = int(np.prod(shape[1:])) * esz
        nbytes = (nbytes + 63) // 64 * 64
        assert self.cur + nbytes <= self.limit, ("SBUF OOM", name, self.cur, nbytes)
        self.n += 1
        t = self.nc.alloc_sbuf_tensor_at("%s_%d" % (name, self.n), list(shape), dt, offset=self.cur)
        self.cur += nbytes
        return t

    def mark(self):
        return self.cur

    def release(self, m):
        self.cur = m


def build(n_layers=2, dbg=None):
    nc = bass.Bass("TRN2", target_bir_lowering=False)

    def din(name, shape, dt=F32):
        return nc.dram_tensor(name, list(shape), dt, kind="ExternalInput").ap()

    x = din("x", [TOK, D])
    w_in = din("w_in", [2, D, 4096])
    lb_logits = din("hg_lb_logits", [2, 2, 512])
    hg_norm_g = din("hg_norm_g", [2, 512])
    w_out = din("w_out", [2, D, D])
    ln1_g = din("ln1_g", [2, D])
    ln1_b = din("ln1_b", [2, D])
    r_w1 = din("router_w1", [2, D, 4])
    r_b1 = din("router_b1", [2, 4])
    r_w2 = din("router_w2", [2, D, 32])
    r_b2 = din("router_b2", [2, 32])
    w_gate = din("ex_w_gate", [2, NE, D, 512])
    w_up = din("ex_w_up", [2, NE, D, 512])
    w_down = din("ex_w_down", [2, NE, 512, D])
    ln2_g = din("ln2_g", [2, D])
    ln2_b = din("ln2_b", [2, D])
    c_ident = din("c_ident", [128, 128], BF16)
    c_amask = din("c_amask", [128, AMW], BF16)
    c_hmask = din("c_hmask", [128, 2, 128], BF16)
    c_ustrict = din("c_ustrict", [128, 128], BF16)
    c_rstart = din("c_rstart", [128, 512])
    c_rope = din("c_rope", [16, 2, S_])
    c_ecap = din("c_ecap", [128, NE])
    out = nc.dram_tensor("out", [TOK, D], F32, kind="ExternalOutput").ap()
    hres = nc.dram_tensor("hres", [TOK, D], F32, kind="ExternalOutput" if dbg else "Internal").ap()
    xs = nc.dram_tensor("xs", [NE * CAP, D], BF16, kind="Internal").ap()
    ys = nc.dram_tensor("ys", [NE * CAP, D], F32, kind="Internal").ap()
    bar_d = nc.dram_tensor("bar_d", [1, 16], F32, kind="Internal").ap()
    dbg_yT = nc.dram_tensor("dbg_yT", [2, 128, 8, S_], BF16, kind="ExternalOutput").ap() if dbg else None

    S = Sched(nc)
    A = Arena(nc)
    import os
    RE_HG = os.environ.get("RE_HG") is not None
    RE_ATT = os.environ.get("RE_ATT") is not None

    def nfree(ap):
        sh = list(ap.shape)
        n = 1
        for v in sh[1:]:
            n *= int(v)
        return n

    def vcost(eng, o):
        n = nfree(o)
        if eng == "act":
            return 0.2 + n / 1400.0
        if eng == "dve":
            return 0.1 + n / 1000.0
        return 0.3 + n / 600.0

    GRP = {"n": 0, "cur": {}}

    def MM(o, lhsT, rhs, start, stop, r, w):
        op_ = S.op("pe", lambda e: e.matmul(o, lhsT, rhs, start=start, stop=stop), reads=r, writes=w,
                   cost=0.04 + max(nfree(o), 64) / 1800.0)
        bank = w[0]
        if start and stop:
            return
        if start:
            GRP["n"] += 1
            GRP["cur"][bank] = GRP["n"]
        op_.grp = (GRP["cur"][bank], bool(stop))

    def TR(o, i, r, w):
        S.op("pe", lambda e: e.transpose(o, i, ident[:]), reads=r, writes=w, cost=0.1)

    def ACT(o, i, func, r, w, scale=1.0, bias=0.0):
        S.op("act", lambda e: e.activation(o, i, func, bias=bias, scale=scale), reads=r, writes=w,
             cost=vcost("act", o))

    def TT(eng, o, a, b, op, r, w):
        S.op(eng, lambda e: e.tensor_tensor(o, a, b, op), reads=r, writes=w, cost=vcost(eng, o))

    def TS(eng, o, a, s1, s2, op0, op1, r, w):
        if s2 is None:
            S.op(eng, lambda e: e.tensor_scalar(o, a, s1, None, op0), reads=r, writes=w, cost=vcost(eng, o))
        else:
            S.op(eng, lambda e: e.tensor_scalar(o, a, s1, s2, op0, op1), reads=r, writes=w, cost=vcost(eng, o))

    def STT(eng, o, a, sc, b, op0, op1, r, w):
        S.op(eng, lambda e: e.scalar_tensor_tensor(o, a, sc, b, op0, op1), reads=r, writes=w, cost=vcost(eng, o))

    def CP(eng, o, i, r, w):
        if eng == "act":
            S.op("act", lambda e: e.copy(o, i), reads=r, writes=w, cost=vcost(eng, o))
        else:
            S.op(eng, lambda e: e.tensor_copy(o, i), reads=r, writes=w, cost=vcost(eng, o))

    def RECIP(o, i, r, w):
        S.op("dve", lambda e: e.reciprocal(o, i), reads=r, writes=w, cost=0.15 + nfree(o) / 200.0)

    def MEMSET(eng, o, val, w):
        S.op(eng, lambda e: e.memset(o, val), reads=[], writes=w, cost=vcost(eng, o))

    def DMA(q, o, i, r, w, sem_key=None, slow=False):
        sh = list(o.shape)
        nb = 1
        for v in sh:
            nb *= int(v)
        nb *= 2 if o.dtype == BF16 else 4
        cost = 2.0 + nb / 100e3
        if slow:
            S.op(q, lambda e: e.dma_start(out=o, in_=i, allow_slow_non_contiguous=True),
                 reads=r, writes=w, dma=True, sem_key=sem_key, cost=cost + 3.0)
        else:
            S.op(q, lambda e: e.dma_start(out=o, in_=i), reads=r, writes=w, dma=True, sem_key=sem_key, cost=cost)

    ident = A.alloc("ident", [128, 128], BF16)
    hmask = A.alloc("hmask", [128, 2, 128], BF16)
    ustrict = A.alloc("ustrict", [128, 128], BF16)
    ones_bf = A.alloc("ones_bf", [128, 128], BF16)
    ecap = A.alloc("ecap", [128, NE], F32)
    lbl = A.alloc("lbl", [128, 2, 2, 4], F32)
    lb_t = A.alloc("lb_t", [128, 2, 4], F32)
    oml_t = A.alloc("oml_t", [128, 2, 4], F32)
    lnoml_t = A.alloc("lnoml_t", [128, 2, 4], F32)
    ng_t = A.alloc("ng_t", [128, 2, 4], F32)
    gates = A.alloc("gates", [128, NTT, 2], F32)
    slots_i = A.alloc("slots_i", [128, NTT, 2], I32)
    bar_s = A.alloc("bar_s", [1, 16], F32)

    DMA("sp", ident[:], c_ident, [], ["ident"])
    DMA("sp", hmask[:], c_hmask, [], ["hmask"])
    DMA("sp", ustrict[:], c_ustrict, [], ["ustrict"])
    DMA("sp", ecap[:], c_ecap, [], ["ecap"])
    MEMSET("dve", ones_bf[:], 1.0, ["ones_bf"])
    MEMSET("dve", bar_s[:], 0.0, ["bar_s"])
    for l in range(2):
        for d in range(2):
            DMA("sp", lbl[:, l, d, :], lb_logits[l, d, :].rearrange("(h c) -> c h", c=128),
                [], ["lbl"], slow=True)
        DMA("sp", ng_t[:, l, :], hg_norm_g[l, :].rearrange("(h c) -> c h", c=128), [], ["ng_t"], slow=True)

    def barrier():
        S.barrier(lambda e: e.dma_start(out=bar_d, in_=bar_s[:]))

    zt = A.alloc("zt", [128, D], BF16)
    MEMSET("pool", zt[:], 0.0, ["zt"])
    for j in range(NE * CAP // 128):
        DMA("pool", xs[j * 128:(j + 1) * 128, :], zt[:], ["zt"], ["xs"])

    PSUM_STATE = {}

    def psum_banks():
        if not PSUM_STATE:
            PSUM_STATE["f"] = [nc.alloc_psum_tensor("pb%d" % i, [128, 512], F32) for i in range(6)]
            PSUM_STATE["t"] = [nc.alloc_psum_tensor("pbT%d" % i, [128, 1024], BF16) for i in range(2)]
        return PSUM_STATE["f"], PSUM_STATE["t"]

    pb, pbT = psum_banks()
    PB = ["pb%d" % i for i in range(6)]
    PT = ["pbT0", "pbT1"]

    base_mark = A.mark()

    def layer(l):
        src = x if l == 0 else hres
        last = (l == n_layers - 1)
        A.release(base_mark)
        if l == 0:
            MEMSET("dve", lb_t[:], 0.0, ["lb_t"])
        else:
            tmpd = A.alloc("tmpd", [128, 2, 4], F32)
            TT("dve", tmpd[:], lbl[:, 0, :, :], lbl[:, 1, :, :], ALU.subtract, ["lbl"], ["tmpd"])
            ACT(tmpd[:], tmpd[:], AF.Exp, ["tmpd"], ["tmpd"])
            TS("dve", tmpd[:], tmpd[:], 1.0, None, ALU.add, None, ["tmpd"], ["tmpd"])
            RECIP(lb_t[:], tmpd[:], ["tmpd"], ["lb_t"])
        TS("dve", oml_t[:], lb_t[:], -1.0, 1.0, ALU.mult, ALU.add, ["lb_t"], ["oml_t"])
        ACT(lnoml_t[:], oml_t[:], AF.Ln, ["oml_t"], ["lnoml_t"])
        cnt_bc = A.alloc("cnt_bc", [128, NE], F32)
        MEMSET("dve", cnt_bc[:], 0.0, ["cnt_bc"])
        lmark = A.mark()

        for s in range(2):
            A.release(lmark)
            t0 = s * S_
            hT = A.alloc("hT", [128, 8, S_], BF16)
            yT = A.alloc("yT", [128, 8, S_], BF16)
            rope = A.alloc("rope", [16, 2, S_], F32)
            amask = A.alloc("amask", [128, AMW], BF16)
            b1mark = A.mark()
            DMA("sp", rope[:], c_rope, [], ["rope"])
            DMA("sp", amask[:], c_amask, [], ["amask"])
            xt = [A.alloc("xt%d" % i, [128, D], F32) for i in range(2)]
            xb = [A.alloc("xb%d" % i, [128, D], BF16) for i in range(2)]
            for tt in range(16):
                b = tt % 2
                DMA("sp", xt[b][:], src[t0 + tt * 128: t0 + (tt + 1) * 128, :], [], ["xt%d" % b])
                CP("act", xb[b][:], xt[b][:], ["xt%d" % b], ["xb%d" % b])
                for c in range(8):
                    TR(pbT[b][:, c * 128:(c + 1) * 128], xb[b][:, c * 128:(c + 1) * 128],
                       ["xb%d" % b, "ident"], [PT[b]])
                CP("dve", hT[:, :, tt * 128:(tt + 1) * 128],
                   pbT[b][:].rearrange("p (c t) -> p c t", c=8), [PT[b]], [("hT", tt)])
            barrier()
            A.release(b1mark)

            S.reorder_on = False
            wh = [A.alloc("wh%d" % i, [128, 8, 640], BF16) for i in range(2)]
            T = [A.alloc("T%d" % i, [128, 512], F32) for i in range(7)]
            TK = ["T%d" % i for i in range(7)]
            q32 = A.alloc("q32", [128, 512], F32)
            Qb = [A.alloc("Qb%d" % d, [128, S_], BF16) for d in range(2)]
            Kinv = [A.alloc("Kinv%d" % d, [128, S_], BF16) for d in range(2)]
            Kd = [A.alloc("Kd%d" % d, [128, S_], BF16) for d in range(2)]
            KdT = [A.alloc("KdT%d" % d, [128, 16, 128], BF16) for d in range(2)]
            Vh = A.alloc("Vh", [128, 16, 128], BF16)
            sg = A.alloc("sg", [128, S_], BF16)
            oF = A.alloc("oF", [128, S_], F32)
            Dall = A.alloc("Dall", [128, 2, 32], F32)
            S32 = [A.alloc("S32_%d" % d, [128, 128], F32) for d in range(2)]
            Sbf = [A.alloc("Sbf_%d" % d, [128, 128], BF16) for d in range(2)]
            T7 = A.alloc("T7", [128, 512], F32)
            ALIAS = {"oFa%d" % k: [("oF", 4 * k + j) for j in range(4)] for k in range(4)}

            def expand_keys(keys):
                out_ = []
                for k in keys:
                    out_ += ALIAS.get(k, [k])
                return out_

            TSET = [T[0:6], [oF[:, k * 512:(k + 1) * 512] for k in range(4)] + [T[4], T7]]
            TKSET = [TK[0:6], ["oFa0", "oFa1", "oFa2", "oFa3", TK[4], "T7"]]
            rstart = A.alloc("rstart", [128, 512], F32)
            DMA("sp", rstart[:], c_rstart, [], ["rstart"])

            for hh in range(4):
                S.fence()
                S.reorder_on = RE_HG
                w = wh[hh % 2]
                wk = "wh%d" % (hh % 2)
                for gi in range(5):
                    c0 = gi * 512 + hh * 128
                    DMA("pool", w[:, :, gi * 128:(gi + 1) * 128],
                        w_in[l, :, c0:c0 + 128].rearrange("(c p) n -> p c n", p=128), [], [wk])
                lbs = [lb_t[:, d, hh:hh + 1] for d in range(2)]
                omls = [oml_t[:, d, hh:hh + 1] for d in range(2)]
                lnomls = [lnoml_t[:, d, hh:hh + 1] for d in range(2)]
                for blk in range(4):
                    bs = slice(blk * 512, (blk + 1) * 512)
                    hkeys = [("hT", blk * 4 + j) for j in range(4)]
                    for gi, pbi in ((0, 0), (1, 1), (2, 2), (4, 3)):
                        for c in range(8):
                            MM(pb[pbi][:, :], w[:, c, gi * 128:(gi + 1) * 128], hT[:, c, bs],
                               c == 0, c == 7, [wk] + hkeys, [PB[pbi]])
                    for j in range(4):
                        ts_ = slice(blk * 512 + j * 128, blk * 512 + (j + 1) * 128)
                        for c in range(8):
                            MM(pb[4][:, j * 128:(j + 1) * 128], hT[:, c, ts_], w[:, c, 384:512],
                               c == 0, c == 7, [wk] + hkeys, [PB[4]])
                    CP("act", Vh[:, blk * 4:(blk + 1) * 4, :],
                       pb[4][:].rearrange("p (j v) -> p j v", j=4), [PB[4]], [("Vh", blk)])
                    ACT(q32[:], pb[0][:], AF.Identity, [PB[0]], ["q32"], scale=128.0 ** -0.5)
                    ACT(T[6][:], pb[3][:], AF.Exp, [PB[3]], [TK[6]], scale=-1.0)
                    ACT(T[6][:], T[6][:], AF.Ln, [TK[6]], [TK[6]], bias=1.0)
                    ACT(T[6][:], T[6][:], AF.Exp, [TK[6]], [TK[6]], scale=-1.0)
                    TT("dve", sg[:, bs], pb[3][:], T[6][:], ALU.mult, [PB[3], TK[6]], [("sg", blk)])
                    def gate_dir(d, T, TK):
                        pa = pb[1 + d]
                        pak = PB[1 + d]
                        ACT(T[0][:], pa[:], AF.Exp, [pak], [TK[0]], scale=-1.0)
                        ACT(T[1][:], T[0][:], AF.Ln, [TK[0]], [TK[1]], bias=1.0)
                        ACT(T[5][:], T[0][:], AF.Ln, [TK[0], "lb_t"], [TK[5]], scale=lbs[d], bias=1.0)
                        TT("pool", T[5][:], T[5][:], T[1][:], ALU.subtract, [TK[5], TK[1]], [TK[5]])
                        STT("dve", T[0][:], pa[:], -1.0, T[1][:], ALU.mult, ALU.subtract,
                            [pak, TK[1]], [TK[0]])
                        S.op("dve", (lambda o_, a_, b_: (lambda e: e.tensor_tensor_scan(
                            o_, a_, b_, 0.0, ALU.mult, ALU.add)))(T[2][:], rstart[:], T[5][:]),
                            reads=["rstart", TK[5]], writes=[TK[2]], cost=1.2)
                        B3 = T[2][:].rearrange("p (n t) -> p n t", t=64)
                        if d == 0:
                            Bx, Bxk = T[2], TK[2]
                            tot = B3[:, :, 63:64]
                        else:
                            TT("pool", T[3][:], T[5][:], T[2][:], ALU.subtract, [TK[5], TK[2]], [TK[3]])
                            TT("pool", T[4][:].rearrange("p (n t) -> p n t", t=64),
                               T[3][:].rearrange("p (n t) -> p n t", t=64),
                               B3[:, :, 63:64].broadcast_to([128, 8, 64]), ALU.add,
                               [TK[3], TK[2]], [TK[4]])
                            Bx, Bxk = T[4], TK[4]
                            tot = T[4][:].rearrange("p (n t) -> p n t", t=64)[:, :, 0:1]
                        ACT(Dall[:, d, blk * 8:(blk + 1) * 8].rearrange("p (n o) -> p n o", o=1), tot,
                            AF.Exp, [Bxk], [("Dall", d, blk)])
                        ACT(T[5][:], Bx[:], AF.Exp, [Bxk], [TK[5]])
                        TT("dve", Qb[d][:, bs], q32[:], T[5][:], ALU.mult, ["q32", TK[5]], [("Qb", d, blk)])
                        TT("pool", T[3][:], T[0][:], Bx[:], ALU.subtract, [TK[0], Bxk], [TK[3]])
                        ACT(Kinv[d][:, bs], T[3][:], AF.Exp, [TK[3], "lnoml_t"], [("Kinv", d, blk)],
                            bias=lnomls[d])
                        TT("pool", T[3][:].rearrange("p (n t) -> p n t", t=64),
                           T[3][:].rearrange("p (n t) -> p n t", t=64),
                           tot.broadcast_to([128, 8, 64]), ALU.add, [TK[3], Bxk], [TK[3]])
                        ACT(Kd[d][:, bs], T[3][:], AF.Exp, [TK[3], "lnoml_t"], [("Kd", d, blk)],
                            bias=lnomls[d])
                    recs = []
                    for d in range(2):
                        rec = []
                        S.op = (lambda rec_: (lambda *a, **k: rec_.append((a, k))))(rec)
                        gate_dir(d, TSET[d], TKSET[d])
                        del S.op
                        recs.append(rec)
                    for i_ in range(max(len(recs[0]), len(recs[1]))):
                        for rec in recs:
                            if i_ < len(rec):
                                a_, k_ = rec[i_]
                                k_ = dict(k_)
                                k_["reads"] = expand_keys(k_.get("reads", ()))
                                k_["writes"] = expand_keys(k_.get("writes", ()))
                                S.op(*a_, **k_)
                for d in range(2):
                    for half in range(2):
                        for j in range(8):
                            tt = half * 8 + j
                            TR(pbT[d][:, j * 128:(j + 1) * 128], Kd[d][:, tt * 128:(tt + 1) * 128],
                               [("Kd", d, tt // 4), "ident"], [PT[d]])
                        CP("act" if d == 0 else "dve", KdT[d][:, half * 8:(half + 1) * 8, :],
                           pbT[d][:].rearrange("p (j c) -> p j c", j=8), [PT[d]], [("KdT", d, half)])
                for d in range(2):
                    MEMSET("pool", S32[d][:], 0.0, [("S32", d)])
                    MEMSET("pool", Sbf[d][:], 0.0, [("Sbf", d)])
                for tt in range(16):
                    for d in range(2):
                        blk = tt // 4
                        tsl = slice(tt * 128, (tt + 1) * 128)
                        pa_i = (2 * tt + d) % 2
                        MM(pb[pa_i][:, 0:128], Kinv[d][:, tsl], Qb[d][:, tsl], True, True,
                           [("Kinv", d, blk), ("Qb", d, blk)], [PB[pa_i]])
                        TT("dve", Kinv[d][:, tsl], pb[pa_i][:, 0:128], hmask[:, d, :], ALU.mult,
                           [PB[pa_i], "hmask"], [("Kinv", d, blk)])
                for i in range(16):
                    tts = [i, 15 - i]
                    for d in range(2):
                        tt = tts[d]
                        MM(pb[2 + d][:, 0:128], Vh[:, tt, :], Kinv[d][:, tt * 128:(tt + 1) * 128], True, False,
                           [("Vh", tt // 4), ("Kinv", d, tt // 4)], [PB[2 + d]])
                    for ci in range(2):
                        for d in range(2):
                            tt = tts[d]
                            blk = tt // 4
                            ch = ci if d == 0 else 1 - ci
                            n = tt * 2 + ch
                            csl = slice(tt * 128 + ch * 64, tt * 128 + (ch + 1) * 64)
                            prow = slice(ch * 64, (ch + 1) * 64)
                            psO, psOk = pb[2 + d], PB[2 + d]
                            psU, psUk = pb[4 + d], PB[4 + d]
                            MM(psO[:, ch * 64:(ch + 1) * 64], Sbf[d][:], Qb[d][:, csl], False, ci == 1,
                               [("Sbf", d), ("Qb", d, blk)], [psOk])
                            MM(psU[:, 0:128], KdT[d][prow, tt, :], Vh[prow, tt, :], True, True,
                               [("KdT", d, tt // 8), ("Vh", blk)], [psUk])
                            STT("dve", S32[d][:], S32[d][:], Dall[:, d, n:n + 1], psU[:, 0:128],
                                ALU.mult, ALU.add, [("S32", d), ("Dall", d, blk), psUk], [("S32", d)])
                            CP("act", Sbf[d][:], S32[d][:], [("S32", d)], [("Sbf", d)])
                    for d in range(2):
                        tt = tts[d]
                        tsl = slice(tt * 128, (tt + 1) * 128)
                        if (d == 0) == (tt <= 7):
                            CP("act", oF[:, tsl], pb[2 + d][:, 0:128], [PB[2 + d]], [("oF", tt)])
                        else:
                            TT("dve", oF[:, tsl], oF[:, tsl], pb[2 + d][:, 0:128], ALU.add,
                               [("oF", tt), PB[2 + d]], [("oF", tt)])
                for blk in range(4):
                    bs = slice(blk * 512, (blk + 1) * 512)
                    ok = [("oF", blk * 4 + j) for j in range(4)]
                    ACT(T[5][:], oF[:, bs], AF.Square, ok, [TK[5]])
                    hi = Qb[0][:, 0:512]
                    lo = Qb[0][:, 512:1024]
                    CP("dve", hi, T[5][:], [TK[5]], [("Qb", 0, 0)])
                    TT("dve", T[6][:], T[5][:], hi, ALU.subtract, [TK[5], ("Qb", 0, 0)], [TK[6]])
                    CP("dve", lo, T[6][:], [TK[6]], [("Qb", 0, 1)])
                    MM(pb[0][:, :], ones_bf[:], hi, True, False, ["ones_bf", ("Qb", 0, 0)], [PB[0]])
                    MM(pb[0][:, :], ones_bf[:], lo, False, True, ["ones_bf", ("Qb", 0, 1)], [PB[0]])
                    ACT(T[6][:], pb[0][:, :], AF.Ln, [PB[0]], [TK[6]], scale=1.0 / 128.0, bias=RMS_EPS)
                    ACT(T[6][:], T[6][:], AF.Exp, [TK[6]], [TK[6]], scale=-0.5)
                    STT("dve", T[5][:], oF[:, bs], ng_t[:, l, hh:hh + 1], T[6][:], ALU.mult, ALU.mult,
                        ok + ["ng_t", TK[6]], [TK[5]])
                    TT("dve", yT[:, hh, bs], T[5][:], sg[:, bs], ALU.mult, [TK[5], ("sg", blk)],
                       [("yT", hh, blk)])

            wa = [A.alloc("wa%d" % i, [128, 8, 224], BF16) for i in range(2)]
            qT = A.alloc("qT", [128, S_], BF16)
            kT = A.alloc("kT", [128, S_], BF16)
            Va = A.alloc("Va", [128, 16, 128], BF16)
            pt = [A.alloc("pt%d" % i, [128, 512], BF16) for i in range(2)]
            pm = [A.alloc("pm%d" % i, [128, 512], BF16) for i in range(2)]
            r2 = A.alloc("r2", [16, 512], F32)
            rc = A.alloc("rc", [64, 512], F32)
            r1 = rc
            MEMSET("pool", Va[:], 1.0, [("Va", b) for b in range(4)])
            MEMSET("pool", qT[:], 0.0, [("qT", b) for b in range(4)])
            MEMSET("pool", kT[:], 0.0, [("kT", b) for b in range(4)])
            for h in range(8):
                S.fence()
                S.reorder_on = RE_ATT
                w = wa[h % 2]
                wk = "wa%d" % (h % 2)
                for gi in range(3):
                    c0 = 2560 + gi * 512 + h * 64
                    DMA("pool", w[:, :, gi * 80:gi * 80 + 64],
                        w_in[l, :, c0:c0 + 64].rearrange("(c p) n -> p c n", p=128), [], [wk])
                for gi in range(2):
                    CP("pool", w[:, :, gi * 80 + 64:gi * 80 + 72], w[:, :, gi * 80 + 8:gi * 80 + 16], [wk], [wk])
                    CP("pool", w[:, :, gi * 80 + 72:gi * 80 + 80], w[:, :, gi * 80 + 0:gi * 80 + 8], [wk], [wk])
                for blk in range(4):
                    bs = slice(blk * 512, (blk + 1) * 512)
                    hkeys = [("hT", blk * 4 + j) for j in range(4)]
                    for gi in range(2):
                        for c in range(8):
                            MM(pb[gi][0:80, :], w[:, c, gi * 80:(gi + 1) * 80], hT[:, c, bs],
                               c == 0, c == 7, [wk] + hkeys, [PB[gi]])
                    for j in range(4):
                        ts_ = slice(blk * 512 + j * 128, blk * 512 + (j + 1) * 128)
                        for c in range(8):
                            MM(pb[2][:, j * 64:(j + 1) * 64], hT[:, c, ts_], w[:, c, 160:224],
                               c == 0, c == 7, [wk] + hkeys, [PB[2]])
                    CP("act", Va[:, blk * 4:(blk + 1) * 4, 0:64],
                       pb[2][:, 0:256].rearrange("p (j v) -> p j v", j=4), [PB[2]], [("Va", blk)])
                    for gi, dst, dk in ((0, qT, "qT"), (1, kT, "kT")):
                        CP("act", dst[0:64, bs], pb[gi][0:64, :], [PB[gi]], [(dk, blk)])
                        TT("dve", r1[0:16, :], pb[gi][0:16, :], rope[:, 0, bs], ALU.mult, [PB[gi], "rope"], ["rc"])
                        CP("act", r2[:], pb[gi][64:80, :], [PB[gi]], ["r2"])
                        TT("dve", r2[:], r2[:], rope[:, 1, bs], ALU.mult, ["r2", "rope"], ["r2"])
                        TT("dve", dst[0:16, bs], r1[0:16, :], r2[:], ALU.add, ["rc", "r2"], [(dk, blk)])
                pairs = []
                for qb in range(4):
                    q0 = qb * 512
                    kbs = [kb for kb in range(16)
                           if kb * 128 >= q0 - 1151 and kb * 128 <= q0 + 1535]
                    for ki, kb in enumerate(kbs):
                        pairs.append((qb, ki, kb, ki == len(kbs) - 1))

                def emit_S(i):
                    qb, ki, kb, lastk = pairs[i]
                    q0 = qb * 512
                    ms = q0 - kb * 128 - OFFMIN
                    assert 0 <= ms and ms + 512 <= AMW
                    b = i % 2
                    psS, psSk = pb[3 + b], PB[3 + b]
                    MM(psS[:, :], kT[:, kb * 128:(kb + 1) * 128], qT[:, q0:q0 + 512], True, True,
                       [("kT", kb // 4), ("qT", qb)], [psSk])
                    ACT(pt[b][:], psS[:, :], AF.Exp, [psSk], ["pt%d" % b], scale=0.125)
                    TT("dve", pm[b][:], pt[b][:], amask[:, ms:ms + 512], ALU.mult,
                       ["pt%d" % b, "amask"], ["pm%d" % b])

                def emit_PV(i):
                    qb, ki, kb, lastk = pairs[i]
                    q0 = qb * 512
                    b = i % 2
                    nb = 5 if qb % 2 == 0 else 2
                    psN, psNk = pb[nb], PB[nb]
                    MM(psN[:, :], Va[:, kb, :], pm[b][:], ki == 0, lastk,
                       [("Va", kb // 4), "pm%d" % b], [psNk])
                    if lastk:
                        ACT(rc[:], psN[64:128, :], AF.Ln, [psNk], ["rc"])
                        ACT(rc[:], rc[:], AF.Exp, ["rc"], ["rc"], scale=-1.0)
                        pr = slice((h % 2) * 64, (h % 2) * 64 + 64)
                        TT("dve", yT[pr, 4 + h // 2, q0:q0 + 512], psN[0:64, :], rc[:], ALU.mult,
                           [psNk, "rc"], [("yT", 4 + h // 2, qb)])

                emit_S(0)
                for i in range(len(pairs)):
                    if i + 1 < len(pairs):
                        emit_S(i + 1)
                    emit_PV(i)
            if dbg and l == n_layers - 1:
                DMA("sp", dbg_yT[s], yT[:], [("yT", c, q) for c in range(8) for q in range(4)], ["dbg_yT"])
            barrier()
            S.reorder_on = True
            A.release(b1mark)

            wo = A.alloc("wo", [128, 8, D], BF16)
            for c in range(8):
                DMA("pool", wo[:, c, :], w_out[l, c * 128:(c + 1) * 128, :], [], ["wo"])
            g1 = A.alloc("g1", [128, D], F32)
            b1 = A.alloc("b1", [128, D], F32)
            DMA("sp", g1[:], ln1_g[l:l + 1, :].broadcast_to([128, D]), [], ["g1"])
            DMA("sp", b1[:], ln1_b[l:l + 1, :].broadcast_to([128, D]), [], ["b1"])
            wr = A.alloc("wr", [128, 8, 36], F32)
            wr_hi = A.alloc("wr_hi", [128, 8, 36], BF16)
            wr_lo = A.alloc("wr_lo", [128, 8, 36], BF16)
            wr_t = A.alloc("wr_t", [128, 8, 36], F32)
            rb = A.alloc("rb", [128, 36], F32)
            DMA("sp", wr[:, :, 0:4], r_w1[l].rearrange("(c p) n -> p c n", p=128), [], ["wr"], slow=True)
            DMA("sp", wr[:, :, 4:36], r_w2[l].rearrange("(c p) n -> p c n", p=128), [], ["wr"], slow=True)
            DMA("sp", rb[:, 0:4], r_b1[l:l + 1, :].broadcast_to([128, 4]), [], ["rb"], slow=True)
            DMA("sp", rb[:, 4:36], r_b2[l:l + 1, :].broadcast_to([128, 32]), [], ["rb"], slow=True)
            CP("dve", wr_hi[:], wr[:], ["wr"], ["wr_hi"])
            TT("dve", wr_t[:], wr[:], wr_hi[:], ALU.subtract, ["wr", "wr_hi"], ["wr_t"])
            CP("dve", wr_lo[:], wr_t[:], ["wr_t"], ["wr_lo"])
            ht = [A.alloc("ht%d" % i, [128, D], F32) for i in range(2)]
            z = [A.alloc("z%d" % i, [128, D], F32) for i in range(2)]
            hb = [A.alloc("hb%d" % i, [128, D], BF16) for i in range(2)]
            hlo = [A.alloc("hlo%d" % i, [128, D], BF16) for i in range(2)]
            hTh = A.alloc("hTh", [128, 8, 128], BF16)
            hTl = A.alloc("hTl", [128, 8, 128], BF16)
            st = A.alloc("st", [128, 2, 6], F32)
            mv = A.alloc("mv", [128, 2], F32)
            rstd = A.alloc("rstd", [128, 1], F32)
            nmr = A.alloc("nmr", [128, 1], F32)
            L = A.alloc("L", [128, 36], F32)
            sm = A.alloc("sm", [128, 16], F32)
            e4 = A.alloc("e4", [128, 4], F32)
            oh1 = A.alloc("oh1", [128, 4], F32)
            L2 = A.alloc("L2", [128, 32], F32)
            L2b = A.alloc("L2b", [128, 32], F32)
            oha = A.alloc("oha", [128, 32], F32)
            ohb = A.alloc("ohb", [128, 32], F32)
            Mbf = A.alloc("Mbf", [128, 32], BF16)
            pos = A.alloc("pos", [128, 32], F32)
            tq = A.alloc("tq", [128, 32], F32)
            sl = A.alloc("sl", [128, 2], F32)
            for ti in range(16):
                tt = s * 16 + ti
                b = ti % 2
                tsl = slice(ti * 128, (ti + 1) * 128)
                rows = slice(t0 + ti * 128, t0 + (ti + 1) * 128)
                DMA("sp", ht[b][:], src[rows, :], ["hres_%d" % tt], ["ht%d" % b])
                for half in range(2):
                    for c in range(8):
                        MM(pb[half][:, :], yT[:, c, tsl], wo[:, c, half * 512:(half + 1) * 512],
                           c == 0, c == 7, ["wo"] + [("yT", c, ti // 4)], [PB[half]])
                    STT("dve", z[b][:, half * 512:(half + 1) * 512], ht[b][:, half * 512:(half + 1) * 512],
                        ALPHA, pb[half][:, :], ALU.mult, ALU.add, ["ht%d" % b, PB[half]], ["z%d" % b])
                ln_tail(z[b], "z%d" % b, st, mv, rstd, nmr, g1, "g1", b1, "b1")
                DMA("sp", hres[rows, :], z[b][:], ["z%d" % b], ["hres_%d" % tt], sem_key="st_z%d" % b)
                CP("act", hb[b][:], z[b][:], ["z%d" % b], ["hb%d" % b])
                TT("pool", ht[b][:], z[b][:], hb[b][:], ALU.subtract, ["z%d" % b, "hb%d" % b], ["ht%d" % b])
                CP("pool", hlo[b][:], ht[b][:], ["ht%d" % b], ["hlo%d" % b])
                for c in range(8):
                    TR(pbT[0][:, c * 128:(c + 1) * 128], hb[b][:, c * 128:(c + 1) * 128],
                       ["hb%d" % b, "ident"], [PT[0]])
                for c in range(8):
                    TR(pbT[1][:, c * 128:(c + 1) * 128], hlo[b][:, c * 128:(c + 1) * 128],
                       ["hlo%d" % b, "ident"], [PT[1]])
                CP("act", hTh[:], pbT[0][:].rearrange("p (c t) -> p c t", c=8), [PT[0]], ["hTh"])
                CP("dve", hTl[:], pbT[1][:].rearrange("p (c t) -> p c t", c=8), [PT[1]], ["hTl"])
                n = 0
                for (a_, ak, w_, wk_) in ((hTh, "hTh", wr_hi, "wr_hi"), (hTl, "hTl", wr_hi, "wr_hi"),
                                          (hTh, "hTh", wr_lo, "wr_lo")):
                    for c in range(8):
                        MM(pb[2][:, 0:36], a_[:, c, :], w_[:, c, :], n == 0, n == 23, [ak, wk_], [PB[2]])
                        n += 1
                route(tt, pb[2], PB[2], rb, L, sm, e4, oh1, L2, L2b, oha, ohb, Mbf, pos, tq, sl, cnt_bc)
                for k in range(2):
                    S.op("pool", (lambda idx_, src_: (lambda e: e.indirect_dma_start(
                        out=xs, out_offset=bass.IndirectOffsetOnAxis(ap=idx_, axis=0),
                        in_=src_, in_offset=None)))(slots_i[:, tt, k:k + 1], hb[b][:, :]),
                        reads=["hb%d" % b, ("slots", tt)], writes=["xs"], dma=True, sem_key="sc_hb%d" % b, cost=6.0)
            barrier()

        A.release(lmark)
        wg = [A.alloc("wg%d" % i, [128, 8, 512], BF16) for i in range(2)]
        wu = [A.alloc("wu%d" % i, [128, 8, 512], BF16) for i in range(2)]
        wd = [A.alloc("wd%d" % i, [128, 4, D], BF16) for i in range(2)]
        xr = [A.alloc("xr%d" % i, [128, 3, D], BF16) for i in range(2)]
        xT = [A.alloc("xT%d" % i, [128, 8, CAP], BF16) for i in range(2)]
        hid = A.alloc("hid", [128, 4, CAP], BF16)
        sil = [A.alloc("sil%d" % i, [128, CAP], F32) for i in range(2)]
        yo = [A.alloc("yo%d" % i, [128, D], F32) for i in range(2)]
        for e_ in range(NE):
            b = e_ % 2
            DMA("sp", xr[b][:], xs[e_ * CAP:(e_ + 1) * CAP, :].rearrange("(j p) d -> p j d", p=128),
                ["xs"], ["xr%d" % b])
            for c in range(8):
                DMA("pool", wg[b][:, c, :], w_gate[l, e_, c * 128:(c + 1) * 128, :], [], ["wg%d" % b])
                DMA("pool", wu[b][:, c, :], w_up[l, e_, c * 128:(c + 1) * 128, :], [], ["wu%d" % b])
            for f in range(4):
                DMA("pool", wd[b][:, f, :], w_down[l, e_, f * 128:(f + 1) * 128, :], [], ["wd%d" % b])
            for j in range(3):
                tb = j % 2
                for c in range(8):
                    TR(pbT[tb][:, c * 128:(c + 1) * 128], xr[b][:, j, c * 128:(c + 1) * 128],
                       ["xr%d" % b, "ident"], [PT[tb]])
                CP("act" if j % 2 == 0 else "dve", xT[b][:, :, j * 128:(j + 1) * 128],
                   pbT[tb][:].rearrange("p (c t) -> p c t", c=8), [PT[tb]], ["xT%d" % b])
            for f in range(4):
                fb = f % 2
                pg, pgk = pb[fb], PB[fb]
                pu, puk = pb[2 + fb], PB[2 + fb]
                for c in range(8):
                    MM(pg[:, 0:CAP], wg[b][:, c, f * 128:(f + 1) * 128], xT[b][:, c, :], c == 0, c == 7,
                       ["wg%d" % b, "xT%d" % b], [pgk])
                for c in range(8):
                    MM(pu[:, 0:CAP], wu[b][:, c, f * 128:(f + 1) * 128], xT[b][:, c, :], c == 0, c == 7,
                       ["wu%d" % b, "xT%d" % b], [puk])
                ACT(sil[fb][:], pg[:, 0:CAP], AF.Silu, [pgk], ["sil%d" % fb])
                TT("dve", hid[:, f, :], sil[fb][:], pu[:, 0:CAP], ALU.mult, ["sil%d" % fb, puk], [("hid", f)])
            for j in range(3):
                yb = j % 2
                for half in range(2):
                    py, pyk = pb[4 + half], PB[4 + half]
                    for f in range(4):
                        MM(py[:, :], hid[:, f, j * 128:(j + 1) * 128], wd[b][:, f, half * 512:(half + 1) * 512],
                           f == 0, f == 3, [("hid", f), "wd%d" % b], [pyk])
                    CP("act" if half == 0 else "dve", yo[yb][:, half * 512:(half + 1) * 512], py[:, :],
                       [pyk], ["yo%d" % yb])
                r0 = e_ * CAP + j * 128
                DMA("sp", ys[r0:r0 + 128, :], yo[yb][:], ["yo%d" % yb], ["ys"], sem_key="st_yo%d" % yb)
        barrier()

        A.release(lmark)
        g2 = A.alloc("g2", [128, D], F32)
        b2 = A.alloc("b2", [128, D], F32)
        DMA("sp", g2[:], ln2_g[l:l + 1, :].broadcast_to([128, D]), [], ["g2"])
        DMA("sp", b2[:], ln2_b[l:l + 1, :].broadcast_to([128, D]), [], ["b2"])
        ht = [A.alloc("ht%d" % i, [128, D], F32) for i in range(2)]
        yg = [A.alloc("yg%d" % i, [128, 2, D], F32) for i in range(2)]
        z = [A.alloc("z%d" % i, [128, D], F32) for i in range(2)]
        st = A.alloc("st", [128, 2, 6], F32)
        mv = A.alloc("mv", [128, 2], F32)
        rstd = A.alloc("rstd", [128, 1], F32)
        nmr = A.alloc("nmr", [128, 1], F32)
        dst = out if last else hres
        for tt in range(NTT):
            b = tt % 2
            rows = slice(tt * 128, (tt + 1) * 128)
            DMA("sp", ht[b][:], hres[rows, :], ["hres_%d" % tt], ["ht%d" % b])
            for k in range(2):
                S.op("pool", (lambda idx_, dst_: (lambda e: e.indirect_dma_start(
                    out=dst_, out_offset=None, in_=ys,
                    in_offset=bass.IndirectOffsetOnAxis(ap=idx_, axis=0))))(slots_i[:, tt, k:k + 1], yg[b][:, k, :]),
                    reads=["ys", ("slots", tt)], writes=["yg%d" % b], dma=True, cost=8.0)
            ACT(z[b][:], ht[b][:], AF.Identity, ["ht%d" % b], ["z%d" % b], scale=ALPHA)
            for k in range(2):
                STT("dve", z[b][:], yg[b][:, k, :], gates[:, tt, k:k + 1], z[b][:],
                    ALU.mult, ALU.add, ["yg%d" % b, ("gates", tt), "z%d" % b], ["z%d" % b])
            ln_tail(z[b], "z%d" % b, st, mv, rstd, nmr, g2, "g2", b2, "b2")
            DMA("sp", dst[rows, :], z[b][:], ["z%d" % b], ["out" if last else "hres_%d" % tt],
                sem_key="st_z%d" % b)
        barrier()

    def ln_tail(zt, zk, st, mv, rstd, nmr, g, gk, b_, bk):
        for half in range(2):
            S.op("dve", (lambda o_, i_: (lambda e: e.bn_stats(o_, i_)))(st[:, half, :], zt[:, half * 512:(half + 1) * 512]),
                 reads=[zk], writes=["st"], cost=0.7)
        S.op("dve", lambda e: e.bn_aggr(mv[:], st[:].rearrange("p a b -> p (a b)")), reads=["st"], writes=["mv"])
        ACT(rstd[:], mv[:, 1:2], AF.Ln, ["mv"], ["rstd"], bias=LN_EPS)
        ACT(rstd[:], rstd[:], AF.Exp, ["rstd"], ["rstd"], scale=-0.5)
        STT("dve", nmr[:], mv[:, 0:1], -1.0, rstd[:], ALU.mult, ALU.mult, ["mv", "rstd"], ["nmr"])
        ACT(zt[:], zt[:], AF.Identity, [zk, "rstd", "nmr"], [zk], scale=rstd[:, 0:1], bias=nmr[:, 0:1])
        TT("pool", zt[:], zt[:], g[:], ALU.mult, [zk, gk], [zk])
        TT("dve", zt[:], zt[:], b_[:], ALU.add, [zk, bk], [zk])

    def route(tt, pl, plk, rb, L, sm, e4, oh1, L2, L2b, oha, ohb, Mbf, pos, tq, sl, cnt_bc):
        TT("dve", L[:], pl[:, 0:36], rb[:], ALU.add, [plk, "rb"], ["L"])
        m1, nm1, s1, pg_, ma, mb, dd, ga = (sm[:, i:i + 1] for i in range(8))
        S.op("dve", lambda e: e.reduce_max(m1, L[:, 0:4], AX.X), reads=["L"], writes=["sm0"])
        TS("dve", oh1[:], L[:, 0:4], m1, None, ALU.is_equal, None, ["L", "sm0"], ["oh1"])
        TS("dve", nm1, m1, -1.0, None, ALU.mult, None, ["sm0"], ["sm1"])
        ACT(e4[:], L[:, 0:4], AF.Exp, ["L", "sm1"], ["e4"], bias=nm1)
        S.op("dve", lambda e: e.reduce_sum(s1, e4[:], AX.X), reads=["e4"], writes=["sm2"])
        RECIP(pg_, s1, ["sm2"], ["sm3"])
        TS("dve", e4[:], oh1[:], BIG, -BIG, ALU.mult, ALU.add, ["oh1", "e4"], ["e4"])
        TT("dve", L2[:].rearrange("p (g e) -> p g e", g=4), L[:, 4:36].rearrange("p (g e) -> p g e", g=4),
           e4[:].rearrange("p (g o) -> p g o", o=1).broadcast_to([128, 4, 8]), ALU.add, ["L", "e4"], ["L2"])
        S.op("dve", lambda e: e.reduce_max(ma, L2[:], AX.X), reads=["L2"], writes=["sm4"])
        TS("dve", oha[:], L2[:], ma, None, ALU.is_equal, None, ["L2", "sm4"], ["oha"])
        STT("dve", L2b[:], oha[:], -BIG, L2[:], ALU.mult, ALU.add, ["oha", "L2"], ["L2b"])
        S.op("dve", lambda e: e.reduce_max(mb, L2b[:], AX.X), reads=["L2b"], writes=["sm5"])
        TS("dve", ohb[:], L2b[:], mb, None, ALU.is_equal, None, ["L2b", "sm5"], ["ohb"])
        TT("dve", dd, mb, ma, ALU.subtract, ["sm4", "sm5"], ["sm6"])
        ACT(dd, dd, AF.Exp, ["sm6"], ["sm6"])
        TS("dve", dd, dd, 1.0, None, ALU.add, None, ["sm6"], ["sm6"])
        RECIP(ga, dd, ["sm6"], ["sm7"])
        TT("dve", gates[:, tt, 0:1], ga, pg_, ALU.mult, ["sm7", "sm3"], [("gates", tt)])
        TT("dve", gates[:, tt, 1:2], pg_, gates[:, tt, 0:1], ALU.subtract, ["sm3", ("gates", tt)], [("gates", tt)])
        TT("dve", Mbf[:], oha[:], ohb[:], ALU.add, ["oha", "ohb"], ["Mbf"])
        MM(pb[3][:, 0:32], ustrict[:], Mbf[:], True, True, ["ustrict", "Mbf"], [PB[3]])
        MM(pb[4][:, 0:32], ones_bf[:], Mbf[:], True, True, ["ones_bf", "Mbf"], [PB[4]])
        TT("dve", pos[:], pb[3][:, 0:32], cnt_bc[:], ALU.add, [PB[3], "cnt_bc"], ["pos"])
        TT("dve", cnt_bc[:], cnt_bc[:], pb[4][:, 0:32], ALU.add, ["cnt_bc", PB[4]], ["cnt_bc"])
        TT("dve", pos[:], pos[:], ecap[:], ALU.add, ["pos", "ecap"], ["pos"])
        TT("dve", tq[:], pos[:], oha[:], ALU.mult, ["pos", "oha"], ["tq"])
        S.op("dve", lambda e: e.reduce_sum(sl[:, 0:1], tq[:], AX.X), reads=["tq"], writes=["sl"])
        TT("dve", tq[:], pos[:], ohb[:], ALU.mult, ["pos", "ohb", "sl"], ["tq"])
        S.op("dve", lambda e: e.reduce_sum(sl[:, 1:2], tq[:], AX.X), reads=["tq"], writes=["sl"])
        TS("dve", sl[:], sl[:], 0.0, float(NE * CAP - 1), ALU.max, ALU.min, ["sl"], ["sl"])
        CP("dve", slots_i[:, tt, :], sl[:], ["sl"], [("slots", tt)])

    for l in range(n_layers):
        layer(l)
    import os
    S.emit(final_keys=["out"], reorder=os.environ.get("NOREORDER") is None)
    return nc


def _consts():
    bf = ml_dtypes.bfloat16
    ident = np.eye(128, dtype=np.float32).astype(bf)
    p = np.arange(128)[:, None]
    j = np.arange(AMW)[None, :]
    dl = j - p + OFFMIN
    ad = np.abs(dl)
    cnt = (ad <= 64).astype(np.float32) + ((dl % 4 == 0) & (ad <= 256)) + ((dl % 16 == 0) & (ad <= 1024))
    amask = cnt.astype(bf)
    s = np.arange(128)[:, None]
    t = np.arange(128)[None, :]
    same = (s // 64) == (t // 64)
    hm = np.stack([(same & (s <= t)), (same & (s >= t))], axis=1).astype(np.float32).astype(bf)
    ustrict = (s < t).astype(np.float32).astype(bf)
    rstart = np.ones((128, 512), np.float32)
    rstart[:, ::64] = 0.0
    half = 8
    inv_freq = (500000.0 ** (-np.arange(half, dtype=np.float32) / half)).astype(np.float32)
    pos = np.arange(S_, dtype=np.float32)
    ang = (pos[None, :] * inv_freq[:, None]).astype(np.float32)
    cos = np.cos(ang).astype(np.float32)
    sin = np.sin(ang).astype(np.float32)
    rope = np.zeros((16, 2, S_), np.float32)
    rope[0:8, 0] = cos
    rope[8:16, 0] = cos
    rope[0:8, 1] = -sin
    rope[8:16, 1] = sin
    ecap = np.tile((np.arange(NE, dtype=np.float32) * CAP)[None, :], (128, 1))
    return {"c_ident": ident, "c_amask": amask, "c_hmask": np.ascontiguousarray(hm), "c_ustrict": ustrict,
            "c_rstart": rstart, "c_rope": rope, "c_ecap": ecap}


_NC_CACHE = {}


def kernel(**inputs):
    if "nc" not in _NC_CACHE:
        _NC_CACHE["nc"] = build()
    nc = _NC_CACHE["nc"]
    consts = _consts()
    x = np.ascontiguousarray(inputs["x"], dtype=np.float32).reshape(NCORES, TOK, D)
    shared = {k: np.ascontiguousarray(v) for k, v in inputs.items() if k != "x"}
    in_maps = []
    for c in range(NCORES):
        m = {"x": x[c]}
        m.update(shared)
        m.update(consts)
        in_maps.append(m)
    res = run_bass_kernel_spmd(nc, in_maps, core_ids=list(range(NCORES)))
    o = np.stack([np.asarray(r["out"], dtype=np.float32) for r in res.results], axis=0)
    return o.reshape(16, S_, D)
```

```python
import contextlib
import numpy as np
import ml_dtypes
import concourse.bass as bass
import concourse.mybir as mybir
from concourse.bass_utils import run_bass_kernel_spmd

F32 = mybir.dt.float32
BF16 = mybir.dt.bfloat16
I32 = mybir.dt.int32
AF = mybir.ActivationFunctionType
ALU = mybir.AluOpType
AX = mybir.AxisListType

NCORES = 8
S_ = 2048
D = 1024
TOK = 2 * S_
NTT = TOK // 128
CAP = 384
NE = 32
ALPHA = 4.0 ** 0.25
LN_EPS = 1e-5
RMS_EPS = 1e-6
OFFMIN = -1408
AMW = 1024 - OFFMIN + 512
BIG = 1.0e4


class _Op:
    __slots__ = ("eng", "fn", "deps", "odeps", "is_dma", "dkey", "sig", "val", "idx", "cost", "tag", "grp")


class Sched:
    ENGS = ("pe", "act", "dve", "pool", "sp")
    LAT = 0.25

    def __init__(self, nc):
        self.nc = nc
        self.ops = []
        self.last_writer = {}
        self.readers = {}
        self.dma_count = {}
        self.last_dma = {}
        self.fixed = []
        self.pool_dmas = []
        self.reorder_on = True
        self.seg_flags = []

    def op(self, eng, fn, reads=(), writes=(), dma=False, sem_key=None, force=False, cost=0.3):
        o = _Op()
        o.eng = eng
        o.fn = fn
        o.is_dma = dma
        o.idx = len(self.ops)
        o.sig = False
        o.val = None
        o.dkey = None
        o.cost = cost
        o.grp = None
        o.tag = "%s r=%s w=%s" % ("DMA" if dma else "", list(reads)[:3], list(writes)[:2])
        odeps = set()
        if dma:
            o.dkey = sem_key if sem_key is not None else writes[0]
            self.dma_count[o.dkey] = self.dma_count.get(o.dkey, 0) + 1
            o.val = 16 * self.dma_count[o.dkey]
            if o.dkey in self.last_dma:
                odeps.add(self.last_dma[o.dkey])
            self.last_dma[o.dkey] = o.idx
        deps = set()
        for k in reads:
            for j in self.last_writer.get(k, ()):
                deps.add(j)
        for k in writes:
            ws = self.last_writer.get(k, [])
            rs = self.readers.get(k, [])
            if (dma and not force and not rs and ws
                    and all(self.ops[j].is_dma for j in ws)):
                self.last_writer[k] = ws + [o.idx]
            else:
                for j in ws:
                    deps.add(j)
                for j in rs:
                    p = self.ops[j]
                    deps.add(j)
                self.last_writer[k] = [o.idx]
                self.readers[k] = []
        fdeps = []
        for j in deps:
            p = self.ops[j]
            if p.fn is None:
                continue
            if p.eng == "pe" and eng == "pe" and not p.is_dma and not dma:
                odeps.add(j)
                continue
            fdeps.append(j)
        o.deps = fdeps
        o.odeps = list(odeps)
        for j in fdeps:
            self.ops[j].sig = True
        for k in reads:
            if k not in writes:
                self.readers.setdefault(k, []).append(o.idx)
        self.ops.append(o)
        return o

    def fence(self):
        n = len(self.ops)
        self.fixed.append((n, n))
        self.seg_flags.append(self.reorder_on)

    def barrier(self, fn_tiny):
        keys = list(set(list(self.last_writer.keys()) + list(self.readers.keys())))
        keys = [k for k in keys if k != "__bar"]
        a = len(self.ops)
        self.op("sp", fn_tiny, reads=[], writes=keys + ["__bar"], dma=True,
                sem_key="__bar", force=True, cost=2.0)
        bw = self.last_writer["__bar"]
        self.last_writer = {"__bar": bw}
        self.readers = {}
        for e in ("pe", "act", "dve", "pool"):
            self.op(e, None, reads=["__bar"], cost=0.05)
        self.fixed.append((a, len(self.ops)))
        self.seg_flags.append(self.reorder_on)

    def _schedule_segment(self, a, b, order):
        import heapq
        ops = self.ops
        n = b - a
        if n == 0:
            return
        succ = [[] for _ in range(n)]
        ndep = [0] * n
        import os
        chain = set(os.environ.get("CHAIN_ENGS", "").split(","))
        lastop = {}
        for i in range(a, b):
            o = ops[i]
            ds = set(j for j in list(o.deps) + list(o.odeps) if j >= a)
            if o.eng in chain:
                if o.eng in lastop:
                    ds.add(lastop[o.eng])
                    if lastop[o.eng] not in o.deps and lastop[o.eng] not in o.odeps:
                        o.odeps.append(lastop[o.eng])
                lastop[o.eng] = i
            ndep[i - a] = len(ds)
            for j in ds:
                succ[j - a].append(i)
        bl = [0.0] * n
        for i in range(b - 1, a - 1, -1):
            m = 0.0
            for sidx in succ[i - a]:
                if bl[sidx - a] > m:
                    m = bl[sidx - a]
            bl[i - a] = m + ops[i].cost
        ready_t = [0.0] * n
        fin = [0.0] * n
        efree = {e: 0.0 for e in self.ENGS}
        avail = {e: [] for e in self.ENGS}
        for i in range(a, b):
            if ndep[i - a] == 0:
                avail[ops[i].eng].append(i)
        left = n
        open_multi = None
        while left:
            best = None
            for e in self.ENGS:
                av = avail[e]
                if e == "pe" and open_multi is not None:
                    av = [i for i in av if ops[i].grp is None or ops[i].grp[0] == open_multi]
                if not av:
                    continue
                mn = min(ready_t[i - a] for i in av)
                t = max(efree[e], mn)
                c = None
                for i in av:
                    if ready_t[i - a] <= t + 1e-9:
                        k = (-bl[i - a], i)
                        if c is None or k < c[0]:
                            c = (k, i)
                if best is None or t < best[0]:
                    best = (t, e, c[1])
            if best is None:
                assert open_multi is not None
                open_multi = None
                continue
            t, e, i = best
            avail[e].remove(i)
            o = ops[i]
            if e == "pe" and o.grp is not None:
                open_multi = None if o.grp[1] else o.grp[0]
            if o.is_dma:
                efree[e] = t + (1.0 if e == "pool" else 0.06)
            else:
                efree[e] = t + o.cost
            fin[i - a] = t + o.cost
            order[e].append(i)
            left -= 1
            for sidx in succ[i - a]:
                so = ops[sidx]
                if i in so.deps:
                    r = fin[i - a] + self.LAT
                else:
                    r = t
                if r > ready_t[sidx - a]:
                    ready_t[sidx - a] = r
                ndep[sidx - a] -= 1
                if ndep[sidx - a] == 0:
                    avail[so.eng].append(sidx)

    def _check(self, order):
        ops = self.ops
        sem = {}
        ptr = {e: 0 for e in self.ENGS}
        total = sum(len(v) for v in order.values())
        done = 0
        while done < total:
            prog = False
            for e in self.ENGS:
                while ptr[e] < len(order[e]):
                    o = ops[order[e][ptr[e]]]
                    ok = True
                    for j in o.deps:
                        p = ops[j]
                        k = ("d", p.dkey) if p.is_dma else ("e", p.eng)
                        if sem.get(k, 0) < p.val:
                            ok = False
                            break
                    if not ok:
                        break
                    if o.fn is not None:
                        if o.is_dma:
                            k = ("d", o.dkey)
                            sem[k] = sem.get(k, 0) + 16
                            assert sem[k] == o.val, ("dma order", o.dkey, sem[k], o.val)
                        elif o.sig:
                            k = ("e", o.eng)
                            sem[k] = sem.get(k, 0) + 1
                            assert sem[k] == o.val
                    ptr[e] += 1
                    done += 1
                    prog = True
            if not prog:
                msg = []
                for e in self.ENGS:
                    if ptr[e] < len(order[e]):
                        o = ops[order[e][ptr[e]]]
                        msg.append((e, o.idx, [(j, ops[j].eng, ops[j].val, ops[j].dkey) for j in o.deps]))
                raise RuntimeError("DEADLOCK in emitted order: %r" % (msg,))

    def emit(self, final_keys=(), reorder=True):
        nc = self.nc
        self.op("sp", None, reads=list(final_keys))
        ops = self.ops
        order = {e: [] for e in self.ENGS}
        pos = 0
        import os
        segsel = os.environ.get("REORDER_SEGS")
        segsel = None if segsel is None else set(int(v) for v in segsel.split(",") if v != "")
        for si, (fa, fb) in enumerate(self.fixed + [(len(ops), len(ops))]):
            flag = self.seg_flags[si] if si < len(self.seg_flags) else True
            if reorder and flag and (segsel is None or si in segsel):
                self._schedule_segment(pos, fa, order)
            else:
                for i in range(pos, fa):
                    order[ops[i].eng].append(i)
            for i in range(fa, fb):
                order[ops[i].eng].append(i)
            pos = fb
        assert sum(len(v) for v in order.values()) == len(ops)
        for e in self.ENGS:
            c = 0
            for i in order[e]:
                o = ops[i]
                if not o.is_dma and o.sig:
                    c += 1
                    o.val = c
        dkeys = list(self.dma_count.keys())
        self._check(order)
        import os
        if os.environ.get("DUMP_ORDER"):
            with open(os.environ["DUMP_ORDER"], "w") as f:
                for e in self.ENGS:
                    f.write("=== %s\n" % e)
                    for i in order[e]:
                        o = ops[i]
                        f.write("%6d %s deps=%s\n" % (i, o.tag, sorted(o.deps)))
        with contextlib.ExitStack() as es:
            esem = {e: es.enter_context(nc.semaphore("s_" + e)) for e in self.ENGS}
            dsem = {k: es.enter_context(nc.semaphore("d%d" % i)) for i, k in enumerate(dkeys)}
            block = es.enter_context(nc.Block())

            def run(engname, eng):
                waited = {}
                for i in order[engname]:
                    o = ops[i]
                    need = {}
                    for j in o.deps:
                        p = ops[j]
                        s = dsem[p.dkey] if p.is_dma else esem[p.eng]
                        v = p.val
                        key = id(s)
                        if v > need.get(key, (None, 0))[1]:
                            need[key] = (s, v)
                    for key, (s, v) in need.items():
                        if waited.get(key, 0) >= v:
                            continue
                        eng.wait_ge(s, v)
                        waited[key] = v
                    if o.fn is None:
                        continue
                    ins = o.fn(eng)
                    if o.is_dma:
                        ins.then_inc(dsem[o.dkey], 16)
                    elif o.sig:
                        ins.then_inc(esem[o.eng], 1)

            @block.sync
            def _(e):
                run("sp", e)

            @block.scalar
            def _(e):
                run("act", e)

            @block.vector
            def _(e):
                run("dve", e)

            @block.tensor
            def _(e):
                run("pe", e)

            @block.gpsimd
            def _(e):
                run("pool", e)


class Arena:
    def __init__(self, nc, base=16512, limit=229344):
        self.nc = nc
        self.cur = base
        self.limit = limit
        self.n = 0

    def alloc(self, name, shape, dt):
        esz = 2 if dt == BF16 else 4
        nbytes = int(np.prod(shape[1:])) * esz
        nbytes = (nbytes + 63) // 64 * 64
        assert self.cur + nbytes <= self.limit, ("SBUF OOM", name, self.cur, nbytes)
        self.n += 1
        t = self.nc.alloc_sbuf_tensor_at("%s_%d" % (name, self.n), list(shape), dt, offset=self.cur)
        self.cur += nbytes
        return t

    def mark(self):
        return self.cur

    def release(self, m):
        self.cur = m


def build(n_layers=2, dbg=None):
    nc = bass.Bass("TRN2", target_bir_lowering=False)

    def din(name, shape, dt=F32):
        return nc.dram_tensor(name, list(shape), dt, kind="ExternalInput").ap()

    x = din("x", [TOK, D])
    w_in = din("w_in", [2, D, 4096])
    lb_logits = din("hg_lb_logits", [2, 2, 512])
    hg_norm_g = din("hg_norm_g", [2, 512])
    w_out = din("w_out", [2, D, D])
    ln1_g = din("ln1_g", [2, D])
    ln1_b = din("ln1_b", [2, D])
    r_w1 = din("router_w1", [2, D, 4])
    r_b1 = din("router_b1", [2, 4])
    r_w2 = din("router_w2", [2, D, 32])
    r_b2 = din("router_b2", [2, 32])
    w_gate = din("ex_w_gate", [2, NE, D, 512])
    w_up = din("ex_w_up", [2, NE, D, 512])
    w_down = din("ex_w_down", [2, NE, 512, D])
    ln2_g = din("ln2_g", [2, D])
    ln2_b = din("ln2_b", [2, D])
    c_ident = din("c_ident", [128, 128], BF16)
    c_amask = din("c_amask", [128, AMW], BF16)
    c_hmask = din("c_hmask", [128, 2, 128], BF16)
    c_ustrict = din("c_ustrict", [128, 128], BF16)
    c_rstart = din("c_rstart", [128, 512])
    c_rope = din("c_rope", [16, 2, S_])
    c_ecap = din("c_ecap", [128, NE])
    out = nc.dram_tensor("out", [TOK, D], F32, kind="ExternalOutput").ap()
    hres = nc.dram_tensor("hres", [TOK, D], F32, kind="ExternalOutput" if dbg else "Internal").ap()
    xs = nc.dram_tensor("xs", [NE * CAP, D], BF16, kind="Internal").ap()
    ys = nc.dram_tensor("ys", [NE * CAP, D], F32, kind="Internal").ap()
    bar_d = nc.dram_tensor("bar_d", [1, 16], F32, kind="Internal").ap()
    dbg_yT = nc.dram_tensor("dbg_yT", [2, 128, 8, S_], BF16, kind="ExternalOutput").ap() if dbg else None

    S = Sched(nc)
    A = Arena(nc)
    import os
    RE_HG = os.environ.get("RE_HG") is not None
    RE_ATT = os.environ.get("RE_ATT") is not None

    def nfree(ap):
        sh = list(ap.shape)
        n = 1
        for v in sh[1:]:
            n *= int(v)
        return n

    def vcost(eng, o):
        n = nfree(o)
        if eng == "act":
            return 0.2 + n / 1400.0
        if eng == "dve":
            return 0.1 + n / 1000.0
        return 0.3 + n / 600.0

    GRP = {"n": 0, "cur": {}}

    def MM(o, lhsT, rhs, start, stop, r, w):
        op_ = S.op("pe", lambda e: e.matmul(o, lhsT, rhs, start=start, stop=stop), reads=r, writes=w,
                   cost=0.04 + max(nfree(o), 64) / 1800.0)
        bank = w[0]
        if start and stop:
            return
        if start:
            GRP["n"] += 1
            GRP["cur"][bank] = GRP["n"]
        op_.grp = (GRP["cur"][bank], bool(stop))

    def TR(o, i, r, w):
        S.op("pe", lambda e: e.transpose(o, i, ident[:]), reads=r, writes=w, cost=0.1)

    def ACT(o, i, func, r, w, scale=1.0, bias=0.0):
        S.op("act", lambda e: e.activation(o, i, func, bias=bias, scale=scale), reads=r, writes=w,
             cost=vcost("act", o))

    def TT(eng, o, a, b, op, r, w):
        S.op(eng, lambda e: e.tensor_tensor(o, a, b, op), reads=r, writes=w, cost=vcost(eng, o))

    def TS(eng, o, a, s1, s2, op0, op1, r, w):
        if s2 is None:
            S.op(eng, lambda e: e.tensor_scalar(o, a, s1, None, op0), reads=r, writes=w, cost=vcost(eng, o))
        else:
            S.op(eng, lambda e: e.tensor_scalar(o, a, s1, s2, op0, op1), reads=r, writes=w, cost=vcost(eng, o))

    def STT(eng, o, a, sc, b, op0, op1, r, w):
        S.op(eng, lambda e: e.scalar_tensor_tensor(o, a, sc, b, op0, op1), reads=r, writes=w, cost=vcost(eng, o))

    def CP(eng, o, i, r, w):
        if eng == "act":
            S.op("act", lambda e: e.copy(o, i), reads=r, writes=w, cost=vcost(eng, o))
        else:
            S.op(eng, lambda e: e.tensor_copy(o, i), reads=r, writes=w, cost=vcost(eng, o))

    def RECIP(o, i, r, w):
        S.op("dve", lambda e: e.reciprocal(o, i), reads=r, writes=w, cost=0.15 + nfree(o) / 200.0)

    def MEMSET(eng, o, val, w):
        S.op(eng, lambda e: e.memset(o, val), reads=[], writes=w, cost=vcost(eng, o))

    def DMA(q, o, i, r, w, sem_key=None, slow=False):
        sh = list(o.shape)
        nb = 1
        for v in sh:
            nb *= int(v)
        nb *= 2 if o.dtype == BF16 else 4
        cost = 2.0 + nb / 100e3
        if slow:
            S.op(q, lambda e: e.dma_start(out=o, in_=i, allow_slow_non_contiguous=True),
                 reads=r, writes=w, dma=True, sem_key=sem_key, cost=cost + 3.0)
        else:
            S.op(q, lambda e: e.dma_start(out=o, in_=i), reads=r, writes=w, dma=True, sem_key=sem_key, cost=cost)

    ident = A.alloc("ident", [128, 128], BF16)
    hmask = A.alloc("hmask", [128, 2, 128], BF16)
    ustrict = A.alloc("ustrict", [128, 128], BF16)
    ones_bf = A.alloc("ones_bf", [128, 128], BF16)
    ecap = A.alloc("ecap", [128, NE], F32)
    lbl = A.alloc("lbl", [128, 2, 2, 4], F32)
    lb_t = A.alloc("lb_t", [128, 2, 4], F32)
    oml_t = A.alloc("oml_t", [128, 2, 4], F32)
    lnoml_t = A.alloc("lnoml_t", [128, 2, 4], F32)
    ng_t = A.alloc("ng_t", [128, 2, 4], F32)
    gates = A.alloc("gates", [128, NTT, 2], F32)
    slots_i = A.alloc("slots_i", [128, NTT, 2], I32)
    bar_s = A.alloc("bar_s", [1, 16], F32)

    DMA("sp", ident[:], c_ident, [], ["ident"])
    DMA("sp", hmask[:], c_hmask, [], ["hmask"])
    DMA("sp", ustrict[:], c_ustrict, [], ["ustrict"])
    DMA("sp", ecap[:], c_ecap, [], ["ecap"])
    MEMSET("dve", ones_bf[:], 1.0, ["ones_bf"])
    MEMSET("dve", bar_s[:], 0.0, ["bar_s"])
    for l in range(2):
        for d in range(2):
            DMA("sp", lbl[:, l, d, :], lb_logits[l, d, :].rearrange("(h c) -> c h", c=128),
                [], ["lbl"], slow=True)
        DMA("sp", ng_t[:, l, :], hg_norm_g[l, :].rearrange("(h c) -> c h", c=128), [], ["ng_t"], slow=True)

    def barrier():
        S.barrier(lambda e: e.dma_start(out=bar_d, in_=bar_s[:]))

    zt = A.alloc("zt", [128, D], BF16)
    MEMSET("pool", zt[:], 0.0, ["zt"])
    for j in range(NE * CAP // 128):
        DMA("pool", xs[j * 128:(j + 1) * 128, :], zt[:], ["zt"], ["xs"])

    PSUM_STATE = {}

    def psum_banks():
        if not PSUM_STATE:
            PSUM_STATE["f"] = [nc.alloc_psum_tensor("pb%d" % i, [128, 512], F32) for i in range(6)]
            PSUM_STATE["t"] = [nc.alloc_psum_tensor("pbT%d" % i, [128, 1024], BF16) for i in range(2)]
        return PSUM_STATE["f"], PSUM_STATE["t"]

    pb, pbT = psum_banks()
    PB = ["pb%d" % i for i in range(6)]
    PT = ["pbT0", "pbT1"]

    base_mark = A.mark()

    def layer(l):
        src = x if l == 0 else hres
        last = (l == n_layers - 1)
        A.release(base_mark)
        if l == 0:
            MEMSET("dve", lb_t[:], 0.0, ["lb_t"])
        else:
            tmpd = A.alloc("tmpd", [128, 2, 4], F32)
            TT("dve", tmpd[:], lbl[:, 0, :, :], lbl[:, 1, :, :], ALU.subtract, ["lbl"], ["tmpd"])
            ACT(tmpd[:], tmpd[:], AF.Exp, ["tmpd"], ["tmpd"])
            TS("dve", tmpd[:], tmpd[:], 1.0, None, ALU.add, None, ["tmpd"], ["tmpd"])
            RECIP(lb_t[:], tmpd[:], ["tmpd"], ["lb_t"])
        TS("dve", oml_t[:], lb_t[:], -1.0, 1.0, ALU.mult, ALU.add, ["lb_t"], ["oml_t"])
        ACT(lnoml_t[:], oml_t[:], AF.Ln, ["oml_t"], ["lnoml_t"])
        cnt_bc = A.alloc("cnt_bc", [128, NE], F32)
        MEMSET("dve", cnt_bc[:], 0.0, ["cnt_bc"])
        lmark = A.mark()

        for s in range(2):
            A.release(lmark)
            t0 = s * S_
            hT = A.alloc("hT", [128, 8, S_], BF16)
            yT = A.alloc("yT", [128, 8, S_], BF16)
            rope = A.alloc("rope", [16, 2, S_], F32)
            amask = A.alloc("amask", [128, AMW], BF16)
            b1mark = A.mark()
            DMA("sp", rope[:], c_rope, [], ["rope"])
            DMA("sp", amask[:], c_amask, [], ["amask"])
            xt = [A.alloc("xt%d" % i, [128, D], F32) for i in range(2)]
            xb = [A.alloc("xb%d" % i, [128, D], BF16) for i in range(2)]
            for tt in range(16):
                b = tt % 2
                DMA("sp", xt[b][:], src[t0 + tt * 128: t0 + (tt + 1) * 128, :], [], ["xt%d" % b])
                CP("act", xb[b][:], xt[b][:], ["xt%d" % b], ["xb%d" % b])
                for c in range(8):
                    TR(pbT[b][:, c * 128:(c + 1) * 128], xb[b][:, c * 128:(c + 1) * 128],
                       ["xb%d" % b, "ident"], [PT[b]])
                CP("dve", hT[:, :, tt * 128:(tt + 1) * 128],
                   pbT[b][:].rearrange("p (c t) -> p c t", c=8), [PT[b]], [("hT", tt)])
            barrier()
            A.release(b1mark)

            S.reorder_on = False
            wh = [A.alloc("wh%d" % i, [128, 8, 640], BF16) for i in range(2)]
            T = [A.alloc("T%d" % i, [128, 512], F32) for i in range(7)]
            TK = ["T%d" % i for i in range(7)]
            q32 = A.alloc("q32", [128, 512], F32)
            Qb = [A.alloc("Qb%d" % d, [128, S_], BF16) for d in range(2)]
            Kinv = [A.alloc("Kinv%d" % d, [128, S_], BF16) for d in range(2)]
            Kd = [A.alloc("Kd%d" % d, [128, S_], BF16) for d in range(2)]
            KdT = [A.alloc("KdT%d" % d, [128, 16, 128], BF16) for d in range(2)]
            Vh = A.alloc("Vh", [128, 16, 128], BF16)
            sg = A.alloc("sg", [128, S_], BF16)
            oF = A.alloc("oF", [128, S_], F32)
            Dall = A.alloc("Dall", [128, 2, 32], F32)
            S32 = [A.alloc("S32_%d" % d, [128, 128], F32) for d in range(2)]
            Sbf = [A.alloc("Sbf_%d" % d, [128, 128], BF16) for d in range(2)]
            T7 = A.alloc("T7", [128, 512], F32)
            ALIAS = {"oFa%d" % k: [("oF", 4 * k + j) for j in range(4)] for k in range(4)}

            def expand_keys(keys):
                out_ = []
                for k in keys:
                    out_ += ALIAS.get(k, [k])
                return out_

            TSET = [T[0:6], [oF[:, k * 512:(k + 1) * 512] for k in range(4)] + [T[4], T7]]
            TKSET = [TK[0:6], ["oFa0", "oFa1", "oFa2", "oFa3", TK[4], "T7"]]
            rstart = A.alloc("rstart", [128, 512], F32)
            DMA("sp", rstart[:], c_rstart, [], ["rstart"])

            for hh in range(4):
                S.fence()
                S.reorder_on = RE_HG
                w = wh[hh % 2]
                wk = "wh%d" % (hh % 2)
                for gi in range(5):
                    c0 = gi * 512 + hh * 128
                    DMA("pool", w[:, :, gi * 128:(gi + 1) * 128],
                        w_in[l, :, c0:c0 + 128].rearrange("(c p) n -> p c n", p=128), [], [wk])
                lbs = [lb_t[:, d, hh:hh + 1] for d in range(2)]
                omls = [oml_t[:, d, hh:hh + 1] for d in range(2)]
                lnomls = [lnoml_t[:, d, hh:hh + 1] for d in range(2)]
                for blk in range(4):
                    bs = slice(blk * 512, (blk + 1) * 512)
                    hkeys = [("hT", blk * 4 + j) for j in range(4)]
                    for gi, pbi in ((0, 0), (1, 1), (2, 2), (4, 3)):
                        for c in range(8):
                            MM(pb[pbi][:, :], w[:, c, gi * 128:(gi + 1) * 128], hT[:, c, bs],
                               c == 0, c == 7, [wk] + hkeys, [PB[pbi]])
                    for j in range(4):
                        ts_ = slice(blk * 512 + j * 128, blk * 512 + (j + 1) * 128)
                        for c in range(8):
                            MM(pb[4][:, j * 128:(j + 1) * 128], hT[:, c, ts_], w[:, c, 384:512],
                               c == 0, c == 7, [wk] + hkeys, [PB[4]])
                    CP("act", Vh[:, blk * 4:(blk + 1) * 4, :],
                       pb[4][:].rearrange("p (j v) -> p j v", j=4), [PB[4]], [("Vh", blk)])
                    ACT(q32[:], pb[0][:], AF.Identity, [PB[0]], ["q32"], scale=128.0 ** -0.5)
                    ACT(T[6][:], pb[3][:], AF.Exp, [PB[3]], [TK[6]], scale=-1.0)
                    ACT(T[6][:], T[6][:], AF.Ln, [TK[6]], [TK[6]], bias=1.0)
                    ACT(T[6][:], T[6][:], AF.Exp, [TK[6]], [TK[6]], scale=-1.0)
                    TT("dve", sg[:, bs], pb[3][:], T[6][:], ALU.mult, [PB[3], TK[6]], [("sg", blk)])
                    def gate_dir(d, T, TK):
                        pa = pb[1 + d]
                        pak = PB[1 + d]
                        ACT(T[0][:], pa[:], AF.Exp, [pak], [TK[0]], scale=-1.0)
                        ACT(T[1][:], T[0][:], AF.Ln, [TK[0]], [TK[1]], bias=1.0)
                        ACT(T[5][:], T[0][:], AF.Ln, [TK[0], "lb_t"], [TK[5]], scale=lbs[d], bias=1.0)
                        TT("pool", T[5][:], T[5][:], T[1][:], ALU.subtract, [TK[5], TK[1]], [TK[5]])
                        STT("dve", T[0][:], pa[:], -1.0, T[1][:], ALU.mult, ALU.subtract,
                            [pak, TK[1]], [TK[0]])
                        S.op("dve", (lambda o_, a_, b_: (lambda e: e.tensor_tensor_scan(
                            o_, a_, b_, 0.0, ALU.mult, ALU.add)))(T[2][:], rstart[:], T[5][:]),
                            reads=["rstart", TK[5]], writes=[TK[2]], cost=1.2)
                        B3 = T[2][:].rearrange("p (n t) -> p n t", t=64)
                        if d == 0:
                            Bx, Bxk = T[2], TK[2]
                            tot = B3[:, :, 63:64]
                        else:
                            TT("pool", T[3][:], T[5][:], T[2][:], ALU.subtract, [TK[5], TK[2]], [TK[3]])
                            TT("pool", T[4][:].rearrange("p (n t) -> p n t", t=64),
                               T[3][:].rearrange("p (n t) -> p n t", t=64),
                               B3[:, :, 63:64].broadcast_to([128, 8, 64]), ALU.add,
                               [TK[3], TK[2]], [TK[4]])
                            Bx, Bxk = T[4], TK[4]
                            tot = T[4][:].rearrange("p (n t) -> p n t", t=64)[:, :, 0:1]
                        ACT(Dall[:, d, blk * 8:(blk + 1) * 8].rearrange("p (n o) -> p n o", o=1), tot,
                            AF.Exp, [Bxk], [("Dall", d, blk)])
                        ACT(T[5][:], Bx[:], AF.Exp, [Bxk], [TK[5]])
                        TT("dve", Qb[d][:, bs], q32[:], T[5][:], ALU.mult, ["q32", TK[5]], [("Qb", d, blk)])
                        TT("pool", T[3][:], T[0][:], Bx[:], ALU.subtract, [TK[0], Bxk], [TK[3]])
                        ACT(Kinv[d][:, bs], T[3][:], AF.Exp, [TK[3], "lnoml_t"], [("Kinv", d, blk)],
                            bias=lnomls[d])
                        TT("pool", T[3][:].rearrange("p (n t) -> p n t", t=64),
                           T[3][:].rearrange("p (n t) -> p n t", t=64),
                           tot.broadcast_to([128, 8, 64]), ALU.add, [TK[3], Bxk], [TK[3]])
                        ACT(Kd[d][:, bs], T[3][:], AF.Exp, [TK[3], "lnoml_t"], [("Kd", d, blk)],
                            bias=lnomls[d])
                    recs = []
                    for d in range(2):
                        rec = []
                        S.op = (lambda rec_: (lambda *a, **k: rec_.append((a, k))))(rec)
                        gate_dir(d, TSET[d], TKSET[d])
                        del S.op
                        recs.append(rec)
                    for i_ in range(max(len(recs[0]), len(recs[1]))):
                        for rec in recs:
                            if i_ < len(rec):
                                a_, k_ = rec[i_]
                                k_ = dict(k_)
                                k_["reads"] = expand_keys(k_.get("reads", ()))
                                k_["writes"] = expand_keys(k_.get("writes", ()))
                                S.op(*a_, **k_)
                for d in range(2):
                    for half in range(2):
                        for j in range(8):
                            tt = half * 8 + j
                            TR(pbT[d][:, j * 128:(j + 1) * 128], Kd[d][:, tt * 128:(tt + 1) * 128],
                               [("Kd", d, tt // 4), "ident"], [PT[d]])
                        CP("act" if d == 0 else "dve", KdT[d][:, half * 8:(half + 1) * 8, :],
                           pbT[d][:].rearrange("p (j c) -> p j c", j=8), [PT[d]], [("KdT", d, half)])
                for d in range(2):
                    MEMSET("pool", S32[d][:], 0.0, [("S32", d)])
                    MEMSET("pool", Sbf[d][:], 0.0, [("Sbf", d)])
                for tt in range(16):
                    for d in range(2):
                        blk = tt // 4
                        tsl = slice(tt * 128, (tt + 1) * 128)
                        pa_i = (2 * tt + d) % 2
                        MM(pb[pa_i][:, 0:128], Kinv[d][:, tsl], Qb[d][:, tsl], True, True,
                           [("Kinv", d, blk), ("Qb", d, blk)], [PB[pa_i]])
                        TT("dve", Kinv[d][:, tsl], pb[pa_i][:, 0:128], hmask[:, d, :], ALU.mult,
                           [PB[pa_i], "hmask"], [("Kinv", d, blk)])
                for i in range(16):
                    tts = [i, 15 - i]
                    for d in range(2):
                        tt = tts[d]
                        MM(pb[2 + d][:, 0:128], Vh[:, tt, :], Kinv[d][:, tt * 128:(tt + 1) * 128], True, False,
                           [("Vh", tt // 4), ("Kinv", d, tt // 4)], [PB[2 + d]])
                    for ci in range(2):
                        for d in range(2):
                            tt = tts[d]
                            blk = tt // 4
                            ch = ci if d == 0 else 1 - ci
                            n = tt * 2 + ch
                            csl = slice(tt * 128 + ch * 64, tt * 128 + (ch + 1) * 64)
                            prow = slice(ch * 64, (ch + 1) * 64)
                            psO, psOk = pb[2 + d], PB[2 + d]
                            psU, psUk = pb[4 + d], PB[4 + d]
                            MM(psO[:, ch * 64:(ch + 1) * 64], Sbf[d][:], Qb[d][:, csl], False, ci == 1,
                               [("Sbf", d), ("Qb", d, blk)], [psOk])
                            MM(psU[:, 0:128], KdT[d][prow, tt, :], Vh[prow, tt, :], True, True,
                               [("KdT", d, tt // 8), ("Vh", blk)], [psUk])
                            STT("dve", S32[d][:], S32[d][:], Dall[:, d, n:n + 1], psU[:, 0:128],
                                ALU.mult, ALU.add, [("S32", d), ("Dall", d, blk), psUk], [("S32", d)])
                            CP("act", Sbf[d][:], S32[d][:], [("S32", d)], [("Sbf", d)])
                    for d in range(2):
                        tt = tts[d]
                        tsl = slice(tt * 128, (tt + 1) * 128)
                        if (d == 0) == (tt <= 7):
                            CP("act", oF[:, tsl], pb[2 + d][:, 0:128], [PB[2 + d]], [("oF", tt)])
                        else:
                            TT("dve", oF[:, tsl], oF[:, tsl], pb[2 + d][:, 0:128], ALU.add,
                               [("oF", tt), PB[2 + d]], [("oF", tt)])
                for blk in range(4):
                    bs = slice(blk * 512, (blk + 1) * 512)
                    ok = [("oF", blk * 4 + j) for j in range(4)]
                    ACT(T[5][:], oF[:, bs], AF.Square, ok, [TK[5]])
                    hi = Qb[0][:, 0:512]
                    lo = Qb[0][:, 512:1024]
                    CP("dve", hi, T[5][:], [TK[5]], [("Qb", 0, 0)])
                    TT("dve", T[6][:], T[5][:], hi, ALU.subtract, [TK[5], ("Qb", 0, 0)], [TK[6]])
                    CP("dve", lo, T[6][:], [TK[6]], [("Qb", 0, 1)])
                    MM(pb[0][:, :], ones_bf[:], hi, True, False, ["ones_bf", ("Qb", 0, 0)], [PB[0]])
                    MM(pb[0][:, :], ones_bf[:], lo, False, True, ["ones_bf", ("Qb", 0, 1)], [PB[0]])
                    ACT(T[6][:], pb[0][:, :], AF.Ln, [PB[0]], [TK[6]], scale=1.0 / 128.0, bias=RMS_EPS)
                    ACT(T[6][:], T[6][:], AF.Exp, [TK[6]], [TK[6]], scale=-0.5)
                    STT("dve", T[5][:], oF[:, bs], ng_t[:, l, hh:hh + 1], T[6][:], ALU.mult, ALU.mult,
                        ok + ["ng_t", TK[6]], [TK[5]])
                    TT("dve", yT[:, hh, bs], T[5][:], sg[:, bs], ALU.mult, [TK[5], ("sg", blk)],
                       [("yT", hh, blk)])

            wa = [A.alloc("wa%d" % i, [128, 8, 224], BF16) for i in range(2)]
            qT = A.alloc("qT", [128, S_], BF16)
            kT = A.alloc("kT", [128, S_], BF16)
            Va = A.alloc("Va", [128, 16, 128], BF16)
            pt = [A.alloc("pt%d" % i, [128, 512], BF16) for i in range(3)]
            pm = [A.alloc("pm%d" % i, [128, 512], BF16) for i in range(3)]
            rc = A.alloc("rc", [64, 512], F32)
            r1 = rc
            MEMSET("pool", Va[:], 1.0, [("Va", b) for b in range(4)])
            MEMSET("pool", qT[:], 0.0, [("qT", b) for b in range(4)])
            MEMSET("pool", kT[:], 0.0, [("kT", b) for b in range(4)])
            for h in range(8):
                S.fence()
                S.reorder_on = RE_ATT
                w = wa[h % 2]
                wk = "wa%d" % (h % 2)
                for gi in range(3):
                    c0 = 2560 + gi * 512 + h * 64
                    DMA("pool", w[:, :, gi * 80:gi * 80 + 64],
                        w_in[l, :, c0:c0 + 64].rearrange("(c p) n -> p c n", p=128), [], [wk])
                for gi in range(2):
                    CP("pool", w[:, :, gi * 80 + 64:gi * 80 + 72], w[:, :, gi * 80 + 8:gi * 80 + 16], [wk], [wk])
                    CP("pool", w[:, :, gi * 80 + 72:gi * 80 + 80], w[:, :, gi * 80 + 0:gi * 80 + 8], [wk], [wk])
                for blk in range(4):
                    bs = slice(blk * 512, (blk + 1) * 512)
                    hkeys = [("hT", blk * 4 + j) for j in range(4)]
                    for gi in range(2):
                        for c in range(8):
                            MM(pb[gi][0:80, :], w[:, c, gi * 80:(gi + 1) * 80], hT[:, c, bs],
                               c == 0, c == 7, [wk] + hkeys, [PB[gi]])
                    for j in range(4):
                        ts_ = slice(blk * 512 + j * 128, blk * 512 + (j + 1) * 128)
                        for c in range(8):
                            MM(pb[2][:, j * 64:(j + 1) * 64], hT[:, c, ts_], w[:, c, 160:224],
                               c == 0, c == 7, [wk] + hkeys, [PB[2]])
                    CP("act", Va[:, blk * 4:(blk + 1) * 4, 0:64],
                       pb[2][:, 0:256].rearrange("p (j v) -> p j v", j=4), [PB[2]], [("Va", blk)])
                    for gi, dst, dk in ((0, qT, "qT"), (1, kT, "kT")):
                        CP("act", dst[0:64, bs], pb[gi][0:64, :], [PB[gi]], [(dk, blk)])
                        TT("dve", r1[0:16, :], pb[gi][0:16, :], rope[:, 0, bs], ALU.mult, [PB[gi], "rope"], ["rc"])
                        CP("act", T7[0:16, :], pb[gi][64:80, :], [PB[gi]], ["T7"])
                        TT("dve", T7[0:16, :], T7[0:16, :], rope[:, 1, bs], ALU.mult, ["T7", "rope"], ["T7"])
                        TT("dve", dst[0:16, bs], r1[0:16, :], T7[0:16, :], ALU.add, ["rc", "T7"], [(dk, blk)])
                pairs = []
                for qb in range(4):
                    q0 = qb * 512
                    kbs = [kb for kb in range(16)
                           if kb * 128 >= q0 - 1151 and kb * 128 <= q0 + 1535]
                    for ki, kb in enumerate(kbs):
                        pairs.append((qb, ki, kb, ki == len(kbs) - 1))

                def emit_S(i):
                    qb, ki, kb, lastk = pairs[i]
                    q0 = qb * 512
                    ms = q0 - kb * 128 - OFFMIN
                    assert 0 <= ms and ms + 512 <= AMW
                    b = i % 3
                    sb_ = (3, 4, 0)[b]
                    psS, psSk = pb[sb_], PB[sb_]
                    MM(psS[:, :], kT[:, kb * 128:(kb + 1) * 128], qT[:, q0:q0 + 512], True, True,
                       [("kT", kb // 4), ("qT", qb)], [psSk])
                    ACT(pt[b][:], psS[:, :], AF.Exp, [psSk], ["pt%d" % b], scale=0.125)
                    TT("dve", pm[b][:], pt[b][:], amask[:, ms:ms + 512], ALU.mult,
                       ["pt%d" % b, "amask"], ["pm%d" % b])

                def emit_PV(i):
                    qb, ki, kb, lastk = pairs[i]
                    q0 = qb * 512
                    b = i % 3
                    nb = 5 if qb % 2 == 0 else 2
                    psN, psNk = pb[nb], PB[nb]
                    MM(psN[:, :], Va[:, kb, :], pm[b][:], ki == 0, lastk,
                       [("Va", kb // 4), "pm%d" % b], [psNk])
                    if lastk:
                        ACT(rc[:], psN[64:128, :], AF.Ln, [psNk], ["rc"])
                        ACT(rc[:], rc[:], AF.Exp, ["rc"], ["rc"], scale=-1.0)
                        pr = slice((h % 2) * 64, (h % 2) * 64 + 64)
                        TT("dve", yT[pr, 4 + h // 2, q0:q0 + 512], psN[0:64, :], rc[:], ALU.mult,
                           [psNk, "rc"], [("yT", 4 + h // 2, qb)])

                emit_S(0)
                emit_S(1)
                for i in range(len(pairs)):
                    if i + 2 < len(pairs):
                        emit_S(i + 2)
                    emit_PV(i)
            if dbg and l == n_layers - 1:
                DMA("sp", dbg_yT[s], yT[:], [("yT", c, q) for c in range(8) for q in range(4)], ["dbg_yT"])
            barrier()
            S.reorder_on = True
            A.release(b1mark)

            wo = A.alloc("wo", [128, 8, D], BF16)
            for c in range(8):
                DMA("pool", wo[:, c, :], w_out[l, c * 128:(c + 1) * 128, :], [], ["wo"])
            g1 = A.alloc("g1", [128, D], F32)
            b1 = A.alloc("b1", [128, D], F32)
            DMA("sp", g1[:], ln1_g[l:l + 1, :].broadcast_to([128, D]), [], ["g1"])
            DMA("sp", b1[:], ln1_b[l:l + 1, :].broadcast_to([128, D]), [], ["b1"])
            wr = A.alloc("wr", [128, 8, 36], F32)
            wr_hi = A.alloc("wr_hi", [128, 8, 36], BF16)
            wr_lo = A.alloc("wr_lo", [128, 8, 36], BF16)
            wr_t = A.alloc("wr_t", [128, 8, 36], F32)
            rb = A.alloc("rb", [128, 36], F32)
            DMA("sp", wr[:, :, 0:4], r_w1[l].rearrange("(c p) n -> p c n", p=128), [], ["wr"], slow=True)
            DMA("sp", wr[:, :, 4:36], r_w2[l].rearrange("(c p) n -> p c n", p=128), [], ["wr"], slow=True)
            DMA("sp", rb[:, 0:4], r_b1[l:l + 1, :].broadcast_to([128, 4]), [], ["rb"], slow=True)
            DMA("sp", rb[:, 4:36], r_b2[l:l + 1, :].broadcast_to([128, 32]), [], ["rb"], slow=True)
            CP("dve", wr_hi[:], wr[:], ["wr"], ["wr_hi"])
            TT("dve", wr_t[:], wr[:], wr_hi[:], ALU.subtract, ["wr", "wr_hi"], ["wr_t"])
            CP("dve", wr_lo[:], wr_t[:], ["wr_t"], ["wr_lo"])
            ht = [A.alloc("ht%d" % i, [128, D], F32) for i in range(2)]
            z = [A.alloc("z%d" % i, [128, D], F32) for i in range(2)]
            hb = [A.alloc("hb%d" % i, [128, D], BF16) for i in range(2)]
            hlo = [A.alloc("hlo%d" % i, [128, D], BF16) for i in range(2)]
            hTh = A.alloc("hTh", [128, 8, 128], BF16)
            hTl = A.alloc("hTl", [128, 8, 128], BF16)
            st = A.alloc("st", [128, 2, 6], F32)
            mv = A.alloc("mv", [128, 2], F32)
            rstd = A.alloc("rstd", [128, 1], F32)
            nmr = A.alloc("nmr", [128, 1], F32)
            L = A.alloc("L", [128, 36], F32)
            sm = A.alloc("sm", [128, 16], F32)
            e4 = A.alloc("e4", [128, 4], F32)
            oh1 = A.alloc("oh1", [128, 4], F32)
            L2 = A.alloc("L2", [128, 32], F32)
            L2b = A.alloc("L2b", [128, 32], F32)
            oha = A.alloc("oha", [128, 32], F32)
            ohb = A.alloc("ohb", [128, 32], F32)
            Mbf = A.alloc("Mbf", [128, 32], BF16)
            pos = A.alloc("pos", [128, 32], F32)
            tq = A.alloc("tq", [128, 32], F32)
            sl = A.alloc("sl", [128, 2], F32)
            for ti in range(16):
                tt = s * 16 + ti
                b = ti % 2
                tsl = slice(ti * 128, (ti + 1) * 128)
                rows = slice(t0 + ti * 128, t0 + (ti + 1) * 128)
                DMA("sp", ht[b][:], src[rows, :], ["hres_%d" % tt], ["ht%d" % b])
                for half in range(2):
                    for c in range(8):
                        MM(pb[half][:, :], yT[:, c, tsl], wo[:, c, half * 512:(half + 1) * 512],
                           c == 0, c == 7, ["wo"] + [("yT", c, ti // 4)], [PB[half]])
                    STT("dve", z[b][:, half * 512:(half + 1) * 512], ht[b][:, half * 512:(half + 1) * 512],
                        ALPHA, pb[half][:, :], ALU.mult, ALU.add, ["ht%d" % b, PB[half]], ["z%d" % b])
                ln_tail(z[b], "z%d" % b, st, mv, rstd, nmr, g1, "g1", b1, "b1")
                DMA("sp", hres[rows, :], z[b][:], ["z%d" % b], ["hres_%d" % tt], sem_key="st_z%d" % b)
                CP("act", hb[b][:], z[b][:], ["z%d" % b], ["hb%d" % b])
                TT("pool", ht[b][:], z[b][:], hb[b][:], ALU.subtract, ["z%d" % b, "hb%d" % b], ["ht%d" % b])
                CP("pool", hlo[b][:], ht[b][:], ["ht%d" % b], ["hlo%d" % b])
                for c in range(8):
                    TR(pbT[0][:, c * 128:(c + 1) * 128], hb[b][:, c * 128:(c + 1) * 128],
                       ["hb%d" % b, "ident"], [PT[0]])
                for c in range(8):
                    TR(pbT[1][:, c * 128:(c + 1) * 128], hlo[b][:, c * 128:(c + 1) * 128],
                       ["hlo%d" % b, "ident"], [PT[1]])
                CP("act", hTh[:], pbT[0][:].rearrange("p (c t) -> p c t", c=8), [PT[0]], ["hTh"])
                CP("dve", hTl[:], pbT[1][:].rearrange("p (c t) -> p c t", c=8), [PT[1]], ["hTl"])
                n = 0
                for (a_, ak, w_, wk_) in ((hTh, "hTh", wr_hi, "wr_hi"), (hTl, "hTl", wr_hi, "wr_hi"),
                                          (hTh, "hTh", wr_lo, "wr_lo")):
                    for c in range(8):
                        MM(pb[2][:, 0:36], a_[:, c, :], w_[:, c, :], n == 0, n == 23, [ak, wk_], [PB[2]])
                        n += 1
                route(tt, pb[2], PB[2], rb, L, sm, e4, oh1, L2, L2b, oha, ohb, Mbf, pos, tq, sl, cnt_bc)
                for k in range(2):
                    S.op("pool", (lambda idx_, src_: (lambda e: e.indirect_dma_start(
                        out=xs, out_offset=bass.IndirectOffsetOnAxis(ap=idx_, axis=0),
                        in_=src_, in_offset=None)))(slots_i[:, tt, k:k + 1], hb[b][:, :]),
                        reads=["hb%d" % b, ("slots", tt)], writes=["xs"], dma=True, sem_key="sc_hb%d" % b, cost=6.0)
            barrier()

        A.release(lmark)
        wg = [A.alloc("wg%d" % i, [128, 8, 512], BF16) for i in range(2)]
        wu = [A.alloc("wu%d" % i, [128, 8, 512], BF16) for i in range(2)]
        wd = [A.alloc("wd%d" % i, [128, 4, D], BF16) for i in range(2)]
        xr = [A.alloc("xr%d" % i, [128, 3, D], BF16) for i in range(2)]
        xT = [A.alloc("xT%d" % i, [128, 8, CAP], BF16) for i in range(2)]
        hid = A.alloc("hid", [128, 4, CAP], BF16)
        sil = [A.alloc("sil%d" % i, [128, CAP], F32) for i in range(2)]
        yo = [A.alloc("yo%d" % i, [128, D], F32) for i in range(2)]
        for e_ in range(NE):
            b = e_ % 2
            DMA("sp", xr[b][:], xs[e_ * CAP:(e_ + 1) * CAP, :].rearrange("(j p) d -> p j d", p=128),
                ["xs"], ["xr%d" % b])
            for c in range(8):
                DMA("pool", wg[b][:, c, :], w_gate[l, e_, c * 128:(c + 1) * 128, :], [], ["wg%d" % b])
                DMA("pool", wu[b][:, c, :], w_up[l, e_, c * 128:(c + 1) * 128, :], [], ["wu%d" % b])
            for f in range(4):
                DMA("pool", wd[b][:, f, :], w_down[l, e_, f * 128:(f + 1) * 128, :], [], ["wd%d" % b])
            for j in range(3):
                tb = j % 2
                for c in range(8):
                    TR(pbT[tb][:, c * 128:(c + 1) * 128], xr[b][:, j, c * 128:(c + 1) * 128],
                       ["xr%d" % b, "ident"], [PT[tb]])
                CP("act" if j % 2 == 0 else "dve", xT[b][:, :, j * 128:(j + 1) * 128],
                   pbT[tb][:].rearrange("p (c t) -> p c t", c=8), [PT[tb]], ["xT%d" % b])
            for f in range(4):
                fb = f % 2
                pg, pgk = pb[fb], PB[fb]
                pu, puk = pb[2 + fb], PB[2 + fb]
                for c in range(8):
                    MM(pg[:, 0:CAP], wg[b][:, c, f * 128:(f + 1) * 128], xT[b][:, c, :], c == 0, c == 7,
                       ["wg%d" % b, "xT%d" % b], [pgk])
                for c in range(8):
                    MM(pu[:, 0:CAP], wu[b][:, c, f * 128:(f + 1) * 128], xT[b][:, c, :], c == 0, c == 7,
                       ["wu%d" % b, "xT%d" % b], [puk])
                ACT(sil[fb][:], pg[:, 0:CAP], AF.Silu, [pgk], ["sil%d" % fb])
                TT("dve", hid[:, f, :], sil[fb][:], pu[:, 0:CAP], ALU.mult, ["sil%d" % fb, puk], [("hid", f)])
            for j in range(3):
                yb = j % 2
                for half in range(2):
                    py, pyk = pb[4 + half], PB[4 + half]
                    for f in range(4):
                        MM(py[:, :], hid[:, f, j * 128:(j + 1) * 128], wd[b][:, f, half * 512:(half + 1) * 512],
                           f == 0, f == 3, [("hid", f), "wd%d" % b], [pyk])
                    CP("act" if half == 0 else "dve", yo[yb][:, half * 512:(half + 1) * 512], py[:, :],
                       [pyk], ["yo%d" % yb])
                r0 = e_ * CAP + j * 128
                DMA("sp", ys[r0:r0 + 128, :], yo[yb][:], ["yo%d" % yb], ["ys"], sem_key="st_yo%d" % yb)
        barrier()

        A.release(lmark)
        g2 = A.alloc("g2", [128, D], F32)
        b2 = A.alloc("b2", [128, D], F32)
        DMA("sp", g2[:], ln2_g[l:l + 1, :].broadcast_to([128, D]), [], ["g2"])
        DMA("sp", b2[:], ln2_b[l:l + 1, :].broadcast_to([128, D]), [], ["b2"])
        ht = [A.alloc("ht%d" % i, [128, D], F32) for i in range(2)]
        yg = [A.alloc("yg%d" % i, [128, 2, D], F32) for i in range(2)]
        z = [A.alloc("z%d" % i, [128, D], F32) for i in range(2)]
        st = A.alloc("st", [128, 2, 6], F32)
        mv = A.alloc("mv", [128, 2], F32)
        rstd = A.alloc("rstd", [128, 1], F32)
        nmr = A.alloc("nmr", [128, 1], F32)
        dst = out if last else hres
        for tt in range(NTT):
            b = tt % 2
            rows = slice(tt * 128, (tt + 1) * 128)
            DMA("sp", ht[b][:], hres[rows, :], ["hres_%d" % tt], ["ht%d" % b])
            for k in range(2):
                S.op("pool", (lambda idx_, dst_: (lambda e: e.indirect_dma_start(
                    out=dst_, out_offset=None, in_=ys,
                    in_offset=bass.IndirectOffsetOnAxis(ap=idx_, axis=0))))(slots_i[:, tt, k:k + 1], yg[b][:, k, :]),
                    reads=["ys", ("slots", tt)], writes=["yg%d" % b], dma=True, cost=8.0)
            ACT(z[b][:], ht[b][:], AF.Identity, ["ht%d" % b], ["z%d" % b], scale=ALPHA)
            for k in range(2):
                STT("dve", z[b][:], yg[b][:, k, :], gates[:, tt, k:k + 1], z[b][:],
                    ALU.mult, ALU.add, ["yg%d" % b, ("gates", tt), "z%d" % b], ["z%d" % b])
            ln_tail(z[b], "z%d" % b, st, mv, rstd, nmr, g2, "g2", b2, "b2")
            DMA("sp", dst[rows, :], z[b][:], ["z%d" % b], ["out" if last else "hres_%d" % tt],
                sem_key="st_z%d" % b)
        barrier()

    def ln_tail(zt, zk, st, mv, rstd, nmr, g, gk, b_, bk):
        for half in range(2):
            S.op("dve", (lambda o_, i_: (lambda e: e.bn_stats(o_, i_)))(st[:, half, :], zt[:, half * 512:(half + 1) * 512]),
                 reads=[zk], writes=["st"], cost=0.7)
        S.op("dve", lambda e: e.bn_aggr(mv[:], st[:].rearrange("p a b -> p (a b)")), reads=["st"], writes=["mv"])
        ACT(rstd[:], mv[:, 1:2], AF.Ln, ["mv"], ["rstd"], bias=LN_EPS)
        ACT(rstd[:], rstd[:], AF.Exp, ["rstd"], ["rstd"], scale=-0.5)
        STT("dve", nmr[:], mv[:, 0:1], -1.0, rstd[:], ALU.mult, ALU.mult, ["mv", "rstd"], ["nmr"])
        ACT(zt[:], zt[:], AF.Identity, [zk, "rstd", "nmr"], [zk], scale=rstd[:, 0:1], bias=nmr[:, 0:1])
        TT("pool", zt[:], zt[:], g[:], ALU.mult, [zk, gk], [zk])
        TT("dve", zt[:], zt[:], b_[:], ALU.add, [zk, bk], [zk])

    def route(tt, pl, plk, rb, L, sm, e4, oh1, L2, L2b, oha, ohb, Mbf, pos, tq, sl, cnt_bc):
        TT("dve", L[:], pl[:, 0:36], rb[:], ALU.add, [plk, "rb"], ["L"])
        m1, nm1, s1, pg_, ma, mb, dd, ga = (sm[:, i:i + 1] for i in range(8))
        S.op("dve", lambda e: e.reduce_max(m1, L[:, 0:4], AX.X), reads=["L"], writes=["sm0"])
        TS("dve", oh1[:], L[:, 0:4], m1, None, ALU.is_equal, None, ["L", "sm0"], ["oh1"])
        TS("dve", nm1, m1, -1.0, None, ALU.mult, None, ["sm0"], ["sm1"])
        ACT(e4[:], L[:, 0:4], AF.Exp, ["L", "sm1"], ["e4"], bias=nm1)
        S.op("dve", lambda e: e.reduce_sum(s1, e4[:], AX.X), reads=["e4"], writes=["sm2"])
        RECIP(pg_, s1, ["sm2"], ["sm3"])
        TS("dve", e4[:], oh1[:], BIG, -BIG, ALU.mult, ALU.add, ["oh1", "e4"], ["e4"])
        TT("dve", L2[:].rearrange("p (g e) -> p g e", g=4), L[:, 4:36].rearrange("p (g e) -> p g e", g=4),
           e4[:].rearrange("p (g o) -> p g o", o=1).broadcast_to([128, 4, 8]), ALU.add, ["L", "e4"], ["L2"])
        S.op("dve", lambda e: e.reduce_max(ma, L2[:], AX.X), reads=["L2"], writes=["sm4"])
        TS("dve", oha[:], L2[:], ma, None, ALU.is_equal, None, ["L2", "sm4"], ["oha"])
        STT("dve", L2b[:], oha[:], -BIG, L2[:], ALU.mult, ALU.add, ["oha", "L2"], ["L2b"])
        S.op("dve", lambda e: e.reduce_max(mb, L2b[:], AX.X), reads=["L2b"], writes=["sm5"])
        TS("dve", ohb[:], L2b[:], mb, None, ALU.is_equal, None, ["L2b", "sm5"], ["ohb"])
        TT("dve", dd, mb, ma, ALU.subtract, ["sm4", "sm5"], ["sm6"])
        ACT(dd, dd, AF.Exp, ["sm6"], ["sm6"])
        TS("dve", dd, dd, 1.0, None, ALU.add, None, ["sm6"], ["sm6"])
        RECIP(ga, dd, ["sm6"], ["sm7"])
        TT("dve", gates[:, tt, 0:1], ga, pg_, ALU.mult, ["sm7", "sm3"], [("gates", tt)])
        TT("dve", gates[:, tt, 1:2], pg_, gates[:, tt, 0:1], ALU.subtract, ["sm3", ("gates", tt)], [("gates", tt)])
        TT("dve", Mbf[:], oha[:], ohb[:], ALU.add, ["oha", "ohb"], ["Mbf"])
        MM(pb[3][:, 0:32], ustrict[:], Mbf[:], True, True, ["ustrict", "Mbf"], [PB[3]])
        MM(pb[4][:, 0:32], ones_bf[:], Mbf[:], True, True, ["ones_bf", "Mbf"], [PB[4]])
        TT("dve", pos[:], pb[3][:, 0:32], cnt_bc[:], ALU.add, [PB[3], "cnt_bc"], ["pos"])
        TT("dve", cnt_bc[:], cnt_bc[:], pb[4][:, 0:32], ALU.add, ["cnt_bc", PB[4]], ["cnt_bc"])
        TT("dve", pos[:], pos[:], ecap[:], ALU.add, ["pos", "ecap"], ["pos"])
        TT("dve", tq[:], pos[:], oha[:], ALU.mult, ["pos", "oha"], ["tq"])
        S.op("dve", lambda e: e.reduce_sum(sl[:, 0:1], tq[:], AX.X), reads=["tq"], writes=["sl"])
        TT("dve", tq[:], pos[:], ohb[:], ALU.mult, ["pos", "ohb", "sl"], ["tq"])
        S.op("dve", lambda e: e.reduce_sum(sl[:, 1:2], tq[:], AX.X), reads=["tq"], writes=["sl"])
        TS("dve", sl[:], sl[:], 0.0, float(NE * CAP - 1), ALU.max, ALU.min, ["sl"], ["sl"])
        CP("dve", slots_i[:, tt, :], sl[:], ["sl"], [("slots", tt)])

    for l in range(n_layers):
        layer(l)
    import os
    S.emit(final_keys=["out"], reorder=os.environ.get("NOREORDER") is None)
    return nc


def _consts():
    bf = ml_dtypes.bfloat16
    ident = np.eye(128, dtype=np.float32).astype(bf)
    p = np.arange(128)[:, None]
    j = np.arange(AMW)[None, :]
    dl = j - p + OFFMIN
    ad = np.abs(dl)
    cnt = (ad <= 64).astype(np.float32) + ((dl % 4 == 0) & (ad <= 256)) + ((dl % 16 == 0) & (ad <= 1024))
    amask = cnt.astype(bf)
    s = np.arange(128)[:, None]
    t = np.arange(128)[None, :]
    same = (s // 64) == (t // 64)
    hm = np.stack([(same & (s <= t)), (same & (s >= t))], axis=1).astype(np.float32).astype(bf)
    ustrict = (s < t).astype(np.float32).astype(bf)
    rstart = np.ones((128, 512), np.float32)
    rstart[:, ::64] = 0.0
    half = 8
    inv_freq = (500000.0 ** (-np.arange(half, dtype=np.float32) / half)).astype(np.float32)
    pos = np.arange(S_, dtype=np.float32)
    ang = (pos[None, :] * inv_freq[:, None]).astype(np.float32)
    cos = np.cos(ang).astype(np.float32)
    sin = np.sin(ang).astype(np.float32)
    rope = np.zeros((16, 2, S_), np.float32)
    rope[0:8, 0] = cos
    rope[8:16, 0] = cos
    rope[0:8, 1] = -sin
    rope[8:16, 1] = sin
    ecap = np.tile((np.arange(NE, dtype=np.float32) * CAP)[None, :], (128, 1))
    return {"c_ident": ident, "c_amask": amask, "c_hmask": np.ascontiguousarray(hm), "c_ustrict": ustrict,
            "c_rstart": rstart, "c_rope": rope, "c_ecap": ecap}


_NC_CACHE = {}


def kernel(**inputs):
    if "nc" not in _NC_CACHE:
        _NC_CACHE["nc"] = build()
    nc = _NC_CACHE["nc"]
    consts = _consts()
    x = np.ascontiguousarray(inputs["x"], dtype=np.float32).reshape(NCORES, TOK, D)
    shared = {k: np.ascontiguousarray(v) for k, v in inputs.items() if k != "x"}
    in_maps = []
    for c in range(NCORES):
        m = {"x": x[c]}
        m.update(shared)
        m.update(consts)
        in_maps.append(m)
    res = run_bass_kernel_spmd(nc, in_maps, core_ids=list(range(NCORES)))
    o = np.stack([np.asarray(r["out"], dtype=np.float32) for r in res.results], axis=0)
    return o.reshape(16, S_, D)
```

```python
import contextlib
import numpy as np
import ml_dtypes
import concourse.bass as bass
import concourse.mybir as mybir
from concourse.bass_utils import run_bass_kernel_spmd

F32 = mybir.dt.float32
BF16 = mybir.dt.bfloat16
I32 = mybir.dt.int32
AF = mybir.ActivationFunctionType
ALU = mybir.AluOpType
AX = mybir.AxisListType

NCORES = 8
S_ = 2048
D = 1024
TOK = 2 * S_
NTT = TOK // 128
CAP = 384
NE = 32
ALPHA = 4.0 ** 0.25
LN_EPS = 1e-5
RMS_EPS = 1e-6
OFFMIN = -1408
AMW = 1024 - OFFMIN + 512
BIG = 1.0e4


class _Op:
    __slots__ = ("eng", "fn", "deps", "odeps", "is_dma", "dkey", "sig", "val", "idx", "cost", "tag", "grp")


class Sched:
    ENGS = ("pe", "act", "dve", "pool", "sp")
    LAT = 0.25

    def __init__(self, nc):
        self.nc = nc
        self.ops = []
        self.last_writer = {}
        self.readers = {}
        self.dma_count = {}
        self.last_dma = {}
        self.fixed = []
        self.pool_dmas = []
        self.reorder_on = True
        self.seg_flags = []

    def op(self, eng, fn, reads=(), writes=(), dma=False, sem_key=None, force=False, cost=0.3):
        o = _Op()
        o.eng = eng
        o.fn = fn
        o.is_dma = dma
        o.idx = len(self.ops)
        o.sig = False
        o.val = None
        o.dkey = None
        o.cost = cost
        o.grp = None
        o.tag = "%s r=%s w=%s" % ("DMA" if dma else "", list(reads)[:3], list(writes)[:2])
        odeps = set()
        if dma:
            o.dkey = sem_key if sem_key is not None else writes[0]
            self.dma_count[o.dkey] = self.dma_count.get(o.dkey, 0) + 1
            o.val = 16 * self.dma_count[o.dkey]
            if o.dkey in self.last_dma:
                odeps.add(self.last_dma[o.dkey])
            self.last_dma[o.dkey] = o.idx
        deps = set()
        for k in reads:
            for j in self.last_writer.get(k, ()):
                deps.add(j)
        for k in writes:
            ws = self.last_writer.get(k, [])
            rs = self.readers.get(k, [])
            if (dma and not force and not rs and ws
                    and all(self.ops[j].is_dma for j in ws)):
                self.last_writer[k] = ws + [o.idx]
            else:
                for j in ws:
                    deps.add(j)
                for j in rs:
                    p = self.ops[j]
                    deps.add(j)
                self.last_writer[k] = [o.idx]
                self.readers[k] = []
        fdeps = []
        for j in deps:
            p = self.ops[j]
            if p.fn is None:
                continue
            if p.eng == "pe" and eng == "pe" and not p.is_dma and not dma:
                odeps.add(j)
                continue
            fdeps.append(j)
        o.deps = fdeps
        o.odeps = list(odeps)
        for j in fdeps:
            self.ops[j].sig = True
        for k in reads:
            if k not in writes:
                self.readers.setdefault(k, []).append(o.idx)
        self.ops.append(o)
        return o

    def fence(self):
        n = len(self.ops)
        self.fixed.append((n, n))
        self.seg_flags.append(self.reorder_on)

    def barrier(self, fn_tiny):
        keys = list(set(list(self.last_writer.keys()) + list(self.readers.keys())))
        keys = [k for k in keys if k != "__bar"]
        a = len(self.ops)
        self.op("sp", fn_tiny, reads=[], writes=keys + ["__bar"], dma=True,
                sem_key="__bar", force=True, cost=2.0)
        bw = self.last_writer["__bar"]
        self.last_writer = {"__bar": bw}
        self.readers = {}
        for e in ("pe", "act", "dve", "pool"):
            self.op(e, None, reads=["__bar"], cost=0.05)
        self.fixed.append((a, len(self.ops)))
        self.seg_flags.append(self.reorder_on)

    def _schedule_segment(self, a, b, order):
        import heapq
        ops = self.ops
        n = b - a
        if n == 0:
            return
        succ = [[] for _ in range(n)]
        ndep = [0] * n
        import os
        chain = set(os.environ.get("CHAIN_ENGS", "").split(","))
        lastop = {}
        for i in range(a, b):
            o = ops[i]
            ds = set(j for j in list(o.deps) + list(o.odeps) if j >= a)
            if o.eng in chain:
                if o.eng in lastop:
                    ds.add(lastop[o.eng])
                    if lastop[o.eng] not in o.deps and lastop[o.eng] not in o.odeps:
                        o.odeps.append(lastop[o.eng])
                lastop[o.eng] = i
            ndep[i - a] = len(ds)
            for j in ds:
                succ[j - a].append(i)
        bl = [0.0] * n
        for i in range(b - 1, a - 1, -1):
            m = 0.0
            for sidx in succ[i - a]:
                if bl[sidx - a] > m:
                    m = bl[sidx - a]
            bl[i - a] = m + ops[i].cost
        ready_t = [0.0] * n
        fin = [0.0] * n
        efree = {e: 0.0 for e in self.ENGS}
        avail = {e: [] for e in self.ENGS}
        for i in range(a, b):
            if ndep[i - a] == 0:
                avail[ops[i].eng].append(i)
        left = n
        open_multi = None
        while left:
            best = None
            for e in self.ENGS:
                av = avail[e]
                if e == "pe" and open_multi is not None:
                    av = [i for i in av if ops[i].grp is None or ops[i].grp[0] == open_multi]
                if not av:
                    continue
                mn = min(ready_t[i - a] for i in av)
                t = max(efree[e], mn)
                c = None
                for i in av:
                    if ready_t[i - a] <= t + 1e-9:
                        k = (-bl[i - a], i)
                        if c is None or k < c[0]:
                            c = (k, i)
                if best is None or t < best[0]:
                    best = (t, e, c[1])
            if best is None:
                assert open_multi is not None
                open_multi = None
                continue
            t, e, i = best
            avail[e].remove(i)
            o = ops[i]
            if e == "pe" and o.grp is not None:
                open_multi = None if o.grp[1] else o.grp[0]
            if o.is_dma:
                efree[e] = t + (1.0 if e == "pool" else 0.06)
            else:
                efree[e] = t + o.cost
            fin[i - a] = t + o.cost
            order[e].append(i)
            left -= 1
            for sidx in succ[i - a]:
                so = ops[sidx]
                if i in so.deps:
                    r = fin[i - a] + self.LAT
                else:
                    r = t
                if r > ready_t[sidx - a]:
                    ready_t[sidx - a] = r
                ndep[sidx - a] -= 1
                if ndep[sidx - a] == 0:
                    avail[so.eng].append(sidx)

    def _check(self, order):
        ops = self.ops
        sem = {}
        ptr = {e: 0 for e in self.ENGS}
        total = sum(len(v) for v in order.values())
        done = 0
        while done < total:
            prog = False
            for e in self.ENGS:
                while ptr[e] < len(order[e]):
                    o = ops[order[e][ptr[e]]]
                    ok = True
                    for j in o.deps:
                        p = ops[j]
                        k = ("d", p.dkey) if p.is_dma else ("e", p.eng)
                        if sem.get(k, 0) < p.val:
                            ok = False
                            break
                    if not ok:
                        break
                    if o.fn is not None:
                        if o.is_dma:
                            k = ("d", o.dkey)
                            sem[k] = sem.get(k, 0) + 16
                            assert sem[k] == o.val, ("dma order", o.dkey, sem[k], o.val)
                        elif o.sig:
                            k = ("e", o.eng)
                            sem[k] = sem.get(k, 0) + 1
                            assert sem[k] == o.val
                    ptr[e] += 1
                    done += 1
                    prog = True
            if not prog:
                msg = []
                for e in self.ENGS:
                    if ptr[e] < len(order[e]):
                        o = ops[order[e][ptr[e]]]
                        msg.append((e, o.idx, [(j, ops[j].eng, ops[j].val, ops[j].dkey) for j in o.deps]))
                raise RuntimeError("DEADLOCK in emitted order: %r" % (msg,))

    def emit(self, final_keys=(), reorder=True):
        nc = self.nc
        self.op("sp", None, reads=list(final_keys))
        ops = self.ops
        order = {e: [] for e in self.ENGS}
        pos = 0
        import os
        segsel = os.environ.get("REORDER_SEGS")
        segsel = None if segsel is None else set(int(v) for v in segsel.split(",") if v != "")
        for si, (fa, fb) in enumerate(self.fixed + [(len(ops), len(ops))]):
            flag = self.seg_flags[si] if si < len(self.seg_flags) else True
            if reorder and flag and (segsel is None or si in segsel):
                self._schedule_segment(pos, fa, order)
            else:
                for i in range(pos, fa):
                    order[ops[i].eng].append(i)
            for i in range(fa, fb):
                order[ops[i].eng].append(i)
            pos = fb
        assert sum(len(v) for v in order.values()) == len(ops)
        for e in self.ENGS:
            c = 0
            for i in order[e]:
                o = ops[i]
                if not o.is_dma and o.sig:
                    c += 1
                    o.val = c
        dkeys = list(self.dma_count.keys())
        self._check(order)
        import os
        if os.environ.get("DUMP_ORDER"):
            with open(os.environ["DUMP_ORDER"], "w") as f:
                for e in self.ENGS:
                    f.write("=== %s\n" % e)
                    for i in order[e]:
                        o = ops[i]
                        f.write("%6d %s deps=%s\n" % (i, o.tag, sorted(o.deps)))
        with contextlib.ExitStack() as es:
            esem = {e: es.enter_context(nc.semaphore("s_" + e)) for e in self.ENGS}
            dsem = {k: es.enter_context(nc.semaphore("d%d" % i)) for i, k in enumerate(dkeys)}
            block = es.enter_context(nc.Block())

            def run(engname, eng):
                waited = {}
                for i in order[engname]:
                    o = ops[i]
                    need = {}
                    for j in o.deps:
                        p = ops[j]
                        s = dsem[p.dkey] if p.is_dma else esem[p.eng]
                        v = p.val
                        key = id(s)
                        if v > need.get(key, (None, 0))[1]:
                            need[key] = (s, v)
                    for key, (s, v) in need.items():
                        if waited.get(key, 0) >= v:
                            continue
                        eng.wait_ge(s, v)
                        waited[key] = v
                    if o.fn is None:
                        continue
                    ins = o.fn(eng)
                    if o.is_dma:
                        ins.then_inc(dsem[o.dkey], 16)
                    elif o.sig:
                        ins.then_inc(esem[o.eng], 1)

            @block.sync
            def _(e):
                run("sp", e)

            @block.scalar
            def _(e):
                run("act", e)

            @block.vector
            def _(e):
                run("dve", e)

            @block.tensor
            def _(e):
                run("pe", e)

            @block.gpsimd
            def _(e):
                run("pool", e)


class Arena:
    def __init__(self, nc, base=16512, limit=229344):
        self.nc = nc
        self.cur = base
        self.limit = limit
        self.n = 0

    def alloc(self, name, shape, dt):
        esz = 2 if dt == BF16 else 4
        nbytes = int(np.prod(shape[1:])) * esz
        nbytes = (nbytes + 63) // 64 * 64
        assert self.cur + nbytes <= self.limit, ("SBUF OOM", name, self.cur, nbytes)
        self.n += 1
        t = self.nc.alloc_sbuf_tensor_at("%s_%d" % (name, self.n), list(shape), dt, offset=self.cur)
        self.cur += nbytes
        return t

    def mark(self):
        return self.cur

    def release(self, m):
        self.cur = m


def build(n_layers=2, dbg=None):
    nc = bass.Bass("TRN2", target_bir_lowering=False)

    def din(name, shape, dt=F32):
        return nc.dram_tensor(name, list(shape), dt, kind="ExternalInput").ap()

    x = din("x", [TOK, D])
    w_in = din("w_in", [2, D, 4096])
    lb_logits = din("hg_lb_logits", [2, 2, 512])
    hg_norm_g = din("hg_norm_g", [2, 512])
    w_out = din("w_out", [2, D, D])
    ln1_g = din("ln1_g", [2, D])
    ln1_b = din("ln1_b", [2, D])
    r_w1 = din("router_w1", [2, D, 4])
    r_b1 = din("router_b1", [2, 4])
    r_w2 = din("router_w2", [2, D, 32])
    r_b2 = din("router_b2", [2, 32])
    w_gate = din("ex_w_gate", [2, NE, D, 512])
    w_up = din("ex_w_up", [2, NE, D, 512])
    w_down = din("ex_w_down", [2, NE, 512, D])
    ln2_g = din("ln2_g", [2, D])
    ln2_b = din("ln2_b", [2, D])
    c_ident = din("c_ident", [128, 128], BF16)
    c_amask = din("c_amask", [128, AMW], BF16)
    c_hmask = din("c_hmask", [128, 2, 128], BF16)
    c_ustrict = din("c_ustrict", [128, 128], BF16)
    c_rstart = din("c_rstart", [128, 512])
    c_rope = din("c_rope", [16, 2, S_])
    c_ecap = din("c_ecap", [128, NE])
    out = nc.dram_tensor("out", [TOK, D], F32, kind="ExternalOutput").ap()
    hres = nc.dram_tensor("hres", [TOK, D], F32, kind="ExternalOutput" if dbg else "Internal").ap()
    xs = nc.dram_tensor("xs", [NE * CAP, D], BF16, kind="Internal").ap()
    ys = nc.dram_tensor("ys", [NE * CAP, D], F32, kind="Internal").ap()
    bar_d = nc.dram_tensor("bar_d", [1, 16], F32, kind="Internal").ap()
    dbg_yT = nc.dram_tensor("dbg_yT", [2, 128, 8, S_], BF16, kind="ExternalOutput").ap() if dbg else None

    S = Sched(nc)
    A = Arena(nc)
    import os
    RE_HG = os.environ.get("RE_HG") is not None
    RE_ATT = os.environ.get("RE_ATT") is not None

    def nfree(ap):
        sh = list(ap.shape)
        n = 1
        for v in sh[1:]:
            n *= int(v)
        return n

    def vcost(eng, o):
        n = nfree(o)
        if eng == "act":
            return 0.2 + n / 1400.0
        if eng == "dve":
            return 0.1 + n / 1000.0
        return 0.3 + n / 600.0

    GRP = {"n": 0, "cur": {}}

    def MM(o, lhsT, rhs, start, stop, r, w):
        op_ = S.op("pe", lambda e: e.matmul(o, lhsT, rhs, start=start, stop=stop), reads=r, writes=w,
                   cost=0.04 + max(nfree(o), 64) / 1800.0)
        bank = w[0]
        if start and stop:
            return
        if start:
            GRP["n"] += 1
            GRP["cur"][bank] = GRP["n"]
        op_.grp = (GRP["cur"][bank], bool(stop))

    def TR(o, i, r, w):
        S.op("pe", lambda e: e.transpose(o, i, ident[:]), reads=r, writes=w, cost=0.1)

    def ACT(o, i, func, r, w, scale=1.0, bias=0.0):
        S.op("act", lambda e: e.activation(o, i, func, bias=bias, scale=scale), reads=r, writes=w,
             cost=vcost("act", o))

    def TT(eng, o, a, b, op, r, w):
        S.op(eng, lambda e: e.tensor_tensor(o, a, b, op), reads=r, writes=w, cost=vcost(eng, o))

    def TS(eng, o, a, s1, s2, op0, op1, r, w):
        if s2 is None:
            S.op(eng, lambda e: e.tensor_scalar(o, a, s1, None, op0), reads=r, writes=w, cost=vcost(eng, o))
        else:
            S.op(eng, lambda e: e.tensor_scalar(o, a, s1, s2, op0, op1), reads=r, writes=w, cost=vcost(eng, o))

    def STT(eng, o, a, sc, b, op0, op1, r, w):
        S.op(eng, lambda e: e.scalar_tensor_tensor(o, a, sc, b, op0, op1), reads=r, writes=w, cost=vcost(eng, o))

    def CP(eng, o, i, r, w):
        if eng == "act":
            S.op("act", lambda e: e.copy(o, i), reads=r, writes=w, cost=vcost(eng, o))
        else:
            S.op(eng, lambda e: e.tensor_copy(o, i), reads=r, writes=w, cost=vcost(eng, o))

    def RECIP(o, i, r, w):
        S.op("dve", lambda e: e.reciprocal(o, i), reads=r, writes=w, cost=0.15 + nfree(o) / 200.0)

    def MEMSET(eng, o, val, w):
        S.op(eng, lambda e: e.memset(o, val), reads=[], writes=w, cost=vcost(eng, o))

    def DMA(q, o, i, r, w, sem_key=None, slow=False):
        sh = list(o.shape)
        nb = 1
        for v in sh:
            nb *= int(v)
        nb *= 2 if o.dtype == BF16 else 4
        cost = 2.0 + nb / 100e3
        if slow:
            S.op(q, lambda e: e.dma_start(out=o, in_=i, allow_slow_non_contiguous=True),
                 reads=r, writes=w, dma=True, sem_key=sem_key, cost=cost + 3.0)
        else:
            S.op(q, lambda e: e.dma_start(out=o, in_=i), reads=r, writes=w, dma=True, sem_key=sem_key, cost=cost)

    ident = A.alloc("ident", [128, 128], BF16)
    hmask = A.alloc("hmask", [128, 2, 128], BF16)
    ustrict = A.alloc("ustrict", [128, 128], BF16)
    ones_bf = A.alloc("ones_bf", [128, 128], BF16)
    ecap = A.alloc("ecap", [128, NE], F32)
    lbl = A.alloc("lbl", [128, 2, 2, 4], F32)
    lb_t = A.alloc("lb_t", [128, 2, 4], F32)
    oml_t = A.alloc("oml_t", [128, 2, 4], F32)
    lnoml_t = A.alloc("lnoml_t", [128, 2, 4], F32)
    ng_t = A.alloc("ng_t", [128, 2, 4], F32)
    gates = A.alloc("gates", [128, NTT, 2], F32)
    slots_i = A.alloc("slots_i", [128, NTT, 2], I32)
    bar_s = A.alloc("bar_s", [1, 16], F32)

    DMA("sp", ident[:], c_ident, [], ["ident"])
    DMA("sp", hmask[:], c_hmask, [], ["hmask"])
    DMA("sp", ustrict[:], c_ustrict, [], ["ustrict"])
    DMA("sp", ecap[:], c_ecap, [], ["ecap"])
    MEMSET("dve", ones_bf[:], 1.0, ["ones_bf"])
    MEMSET("dve", bar_s[:], 0.0, ["bar_s"])
    for l in range(2):
        for d in range(2):
            DMA("sp", lbl[:, l, d, :], lb_logits[l, d, :].rearrange("(h c) -> c h", c=128),
                [], ["lbl"], slow=True)
        DMA("sp", ng_t[:, l, :], hg_norm_g[l, :].rearrange("(h c) -> c h", c=128), [], ["ng_t"], slow=True)

    def barrier():
        S.barrier(lambda e: e.dma_start(out=bar_d, in_=bar_s[:]))

    zt = A.alloc("zt", [128, D], BF16)
    MEMSET("pool", zt[:], 0.0, ["zt"])
    for j in range(NE * CAP // 128):
        DMA("pool", xs[j * 128:(j + 1) * 128, :], zt[:], ["zt"], ["xs"])

    PSUM_STATE = {}

    def psum_banks():
        if not PSUM_STATE:
            PSUM_STATE["f"] = [nc.alloc_psum_tensor("pb%d" % i, [128, 512], F32) for i in range(6)]
            PSUM_STATE["t"] = [nc.alloc_psum_tensor("pbT%d" % i, [128, 1024], BF16) for i in range(2)]
        return PSUM_STATE["f"], PSUM_STATE["t"]

    pb, pbT = psum_banks()
    PB = ["pb%d" % i for i in range(6)]
    PT = ["pbT0", "pbT1"]

    base_mark = A.mark()

    def layer(l):
        src = x if l == 0 else hres
        last = (l == n_layers - 1)
        A.release(base_mark)
        if l == 0:
            MEMSET("dve", lb_t[:], 0.0, ["lb_t"])
        else:
            tmpd = A.alloc("tmpd", [128, 2, 4], F32)
            TT("dve", tmpd[:], lbl[:, 0, :, :], lbl[:, 1, :, :], ALU.subtract, ["lbl"], ["tmpd"])
            ACT(tmpd[:], tmpd[:], AF.Exp, ["tmpd"], ["tmpd"])
            TS("dve", tmpd[:], tmpd[:], 1.0, None, ALU.add, None, ["tmpd"], ["tmpd"])
            RECIP(lb_t[:], tmpd[:], ["tmpd"], ["lb_t"])
        TS("dve", oml_t[:], lb_t[:], -1.0, 1.0, ALU.mult, ALU.add, ["lb_t"], ["oml_t"])
        ACT(lnoml_t[:], oml_t[:], AF.Ln, ["oml_t"], ["lnoml_t"])
        cnt_bc = A.alloc("cnt_bc", [128, NE], F32)
        MEMSET("dve", cnt_bc[:], 0.0, ["cnt_bc"])
        lmark = A.mark()

        for s in range(2):
            A.release(lmark)
            t0 = s * S_
            hT = A.alloc("hT", [128, 8, S_], BF16)
            yT = A.alloc("yT", [128, 8, S_], BF16)
            rope = A.alloc("rope", [16, 2, S_], F32)
            amask = A.alloc("amask", [128, AMW], BF16)
            b1mark = A.mark()
            DMA("sp", rope[:], c_rope, [], ["rope"])
            DMA("sp", amask[:], c_amask, [], ["amask"])
            xt = [A.alloc("xt%d" % i, [128, D], F32) for i in range(2)]
            xb = [A.alloc("xb%d" % i, [128, D], BF16) for i in range(2)]
            for tt in range(16):
                b = tt % 2
                DMA("sp", xt[b][:], src[t0 + tt * 128: t0 + (tt + 1) * 128, :], [], ["xt%d" % b])
                CP("act", xb[b][:], xt[b][:], ["xt%d" % b], ["xb%d" % b])
                for c in range(8):
                    TR(pbT[b][:, c * 128:(c + 1) * 128], xb[b][:, c * 128:(c + 1) * 128],
                       ["xb%d" % b, "ident"], [PT[b]])
                CP("dve", hT[:, :, tt * 128:(tt + 1) * 128],
                   pbT[b][:].rearrange("p (c t) -> p c t", c=8), [PT[b]], [("hT", tt)])
            barrier()
            A.release(b1mark)

            S.reorder_on = False
            wh = [A.alloc("wh%d" % i, [128, 8, 640], BF16) for i in range(2)]
            T = [A.alloc("T%d" % i, [128, 512], F32) for i in range(7)]
            TK = ["T%d" % i for i in range(7)]
            q32 = A.alloc("q32", [128, 512], F32)
            Qb = [A.alloc("Qb%d" % d, [128, S_], BF16) for d in range(2)]
            Kinv = [A.alloc("Kinv%d" % d, [128, S_], BF16) for d in range(2)]
            Kd = [A.alloc("Kd%d" % d, [128, S_], BF16) for d in range(2)]
            KdT = [A.alloc("KdT%d" % d, [128, 16, 128], BF16) for d in range(2)]
            Vh = A.alloc("Vh", [128, 16, 128], BF16)
            sg = A.alloc("sg", [128, S_], BF16)
            oF = A.alloc("oF", [128, S_], F32)
            Dall = A.alloc("Dall", [128, 2, 32], F32)
            S32 = [A.alloc("S32_%d" % d, [128, 128], F32) for d in range(2)]
            Sbf = [A.alloc("Sbf_%d" % d, [128, 128], BF16) for d in range(2)]
            T7 = A.alloc("T7", [128, 512], F32)
            ALIAS = {"oFa%d" % k: [("oF", 4 * k + j) for j in range(4)] for k in range(4)}

            def expand_keys(keys):
                out_ = []
                for k in keys:
                    out_ += ALIAS.get(k, [k])
                return out_

            TSET = [T[0:6], [oF[:, k * 512:(k + 1) * 512] for k in range(4)] + [T[4], T7]]
            TKSET = [TK[0:6], ["oFa0", "oFa1", "oFa2", "oFa3", TK[4], "T7"]]
            rstart = A.alloc("rstart", [128, 512], F32)
            DMA("sp", rstart[:], c_rstart, [], ["rstart"])

            for hh in range(4):
                S.fence()
                S.reorder_on = RE_HG
                w = wh[hh % 2]
                wk = "wh%d" % (hh % 2)
                for gi in range(5):
                    c0 = gi * 512 + hh * 128
                    DMA("pool", w[:, :, gi * 128:(gi + 1) * 128],
                        w_in[l, :, c0:c0 + 128].rearrange("(c p) n -> p c n", p=128), [], [wk])
                lbs = [lb_t[:, d, hh:hh + 1] for d in range(2)]
                omls = [oml_t[:, d, hh:hh + 1] for d in range(2)]
                lnomls = [lnoml_t[:, d, hh:hh + 1] for d in range(2)]
                for blk in range(4):
                    bs = slice(blk * 512, (blk + 1) * 512)
                    hkeys = [("hT", blk * 4 + j) for j in range(4)]
                    for gi, pbi in ((0, 0), (1, 1), (2, 2), (4, 3)):
                        for c in range(8):
                            MM(pb[pbi][:, :], w[:, c, gi * 128:(gi + 1) * 128], hT[:, c, bs],
                               c == 0, c == 7, [wk] + hkeys, [PB[pbi]])
                    for j in range(4):
                        ts_ = slice(blk * 512 + j * 128, blk * 512 + (j + 1) * 128)
                        for c in range(8):
                            MM(pb[4][:, j * 128:(j + 1) * 128], hT[:, c, ts_], w[:, c, 384:512],
                               c == 0, c == 7, [wk] + hkeys, [PB[4]])
                    CP("act", Vh[:, blk * 4:(blk + 1) * 4, :],
                       pb[4][:].rearrange("p (j v) -> p j v", j=4), [PB[4]], [("Vh", blk)])
                    ACT(q32[:], pb[0][:], AF.Identity, [PB[0]], ["q32"], scale=128.0 ** -0.5)
                    ACT(T[6][:], pb[3][:], AF.Exp, [PB[3]], [TK[6]], scale=-1.0)
                    ACT(T[6][:], T[6][:], AF.Ln, [TK[6]], [TK[6]], bias=1.0)
                    ACT(T[6][:], T[6][:], AF.Exp, [TK[6]], [TK[6]], scale=-1.0)
                    TT("dve", sg[:, bs], pb[3][:], T[6][:], ALU.mult, [PB[3], TK[6]], [("sg", blk)])
                    def gate_dir(d, T, TK):
                        pa = pb[1 + d]
                        pak = PB[1 + d]
                        ACT(T[0][:], pa[:], AF.Exp, [pak], [TK[0]], scale=-1.0)
                        ACT(T[1][:], T[0][:], AF.Ln, [TK[0]], [TK[1]], bias=1.0)
                        ACT(T[5][:], T[0][:], AF.Ln, [TK[0], "lb_t"], [TK[5]], scale=lbs[d], bias=1.0)
                        TT("pool", T[5][:], T[5][:], T[1][:], ALU.subtract, [TK[5], TK[1]], [TK[5]])
                        STT("dve", T[0][:], pa[:], -1.0, T[1][:], ALU.mult, ALU.subtract,
                            [pak, TK[1]], [TK[0]])
                        S.op("dve", (lambda o_, a_, b_: (lambda e: e.tensor_tensor_scan(
                            o_, a_, b_, 0.0, ALU.mult, ALU.add)))(T[2][:], rstart[:], T[5][:]),
                            reads=["rstart", TK[5]], writes=[TK[2]], cost=1.2)
                        B3 = T[2][:].rearrange("p (n t) -> p n t", t=64)
                        if d == 0:
                            Bx, Bxk = T[2], TK[2]
                            tot = B3[:, :, 63:64]
                        else:
                            TT("pool", T[3][:], T[5][:], T[2][:], ALU.subtract, [TK[5], TK[2]], [TK[3]])
                            TT("pool", T[4][:].rearrange("p (n t) -> p n t", t=64),
                               T[3][:].rearrange("p (n t) -> p n t", t=64),
                               B3[:, :, 63:64].broadcast_to([128, 8, 64]), ALU.add,
                               [TK[3], TK[2]], [TK[4]])
                            Bx, Bxk = T[4], TK[4]
                            tot = T[4][:].rearrange("p (n t) -> p n t", t=64)[:, :, 0:1]
                        ACT(Dall[:, d, blk * 8:(blk + 1) * 8].rearrange("p (n o) -> p n o", o=1), tot,
                            AF.Exp, [Bxk], [("Dall", d, blk)])
                        ACT(T[5][:], Bx[:], AF.Exp, [Bxk], [TK[5]])
                        TT("dve", Qb[d][:, bs], q32[:], T[5][:], ALU.mult, ["q32", TK[5]], [("Qb", d, blk)])
                        TT("pool", T[3][:], T[0][:], Bx[:], ALU.subtract, [TK[0], Bxk], [TK[3]])
                        ACT(Kinv[d][:, bs], T[3][:], AF.Exp, [TK[3], "lnoml_t"], [("Kinv", d, blk)],
                            bias=lnomls[d])
                        TT("pool", T[3][:].rearrange("p (n t) -> p n t", t=64),
                           T[3][:].rearrange("p (n t) -> p n t", t=64),
                           tot.broadcast_to([128, 8, 64]), ALU.add, [TK[3], Bxk], [TK[3]])
                        ACT(Kd[d][:, bs], T[3][:], AF.Exp, [TK[3], "lnoml_t"], [("Kd", d, blk)],
                            bias=lnomls[d])
                    recs = []
                    for d in range(2):
                        rec = []
                        S.op = (lambda rec_: (lambda *a, **k: rec_.append((a, k))))(rec)
                        gate_dir(d, TSET[d], TKSET[d])
                        del S.op
                        recs.append(rec)
                    for i_ in range(max(len(recs[0]), len(recs[1]))):
                        for rec in recs:
                            if i_ < len(rec):
                                a_, k_ = rec[i_]
                                k_ = dict(k_)
                                k_["reads"] = expand_keys(k_.get("reads", ()))
                                k_["writes"] = expand_keys(k_.get("writes", ()))
                                S.op(*a_, **k_)
                for d in range(2):
                    for half in range(2):
                        for j in range(8):
                            tt = half * 8 + j
                            TR(pbT[d][:, j * 128:(j + 1) * 128], Kd[d][:, tt * 128:(tt + 1) * 128],
                               [("Kd", d, tt // 4), "ident"], [PT[d]])
                        CP("act" if d == 0 else "dve", KdT[d][:, half * 8:(half + 1) * 8, :],
                           pbT[d][:].rearrange("p (j c) -> p j c", j=8), [PT[d]], [("KdT", d, half)])
                for d in range(2):
                    MEMSET("pool", S32[d][:], 0.0, [("S32", d)])
                    MEMSET("pool", Sbf[d][:], 0.0, [("Sbf", d)])
                for tt in range(16):
                    for d in range(2):
                        blk = tt // 4
                        tsl = slice(tt * 128, (tt + 1) * 128)
                        pa_i = (2 * tt + d) % 2
                        MM(pb[pa_i][:, 0:128], Kinv[d][:, tsl], Qb[d][:, tsl], True, True,
                           [("Kinv", d, blk), ("Qb", d, blk)], [PB[pa_i]])
                        TT("dve", Kinv[d][:, tsl], pb[pa_i][:, 0:128], hmask[:, d, :], ALU.mult,
                           [PB[pa_i], "hmask"], [("Kinv", d, blk)])
                for i in range(16):
                    tts = [i, 15 - i]
                    for d in range(2):
                        tt = tts[d]
                        MM(pb[2 + d][:, 0:128], Vh[:, tt, :], Kinv[d][:, tt * 128:(tt + 1) * 128], True, False,
                           [("Vh", tt // 4), ("Kinv", d, tt // 4)], [PB[2 + d]])
                    for ci in range(2):
                        for d in range(2):
                            tt = tts[d]
                            blk = tt // 4
                            ch = ci if d == 0 else 1 - ci
                            n = tt * 2 + ch
                            csl = slice(tt * 128 + ch * 64, tt * 128 + (ch + 1) * 64)
                            prow = slice(ch * 64, (ch + 1) * 64)
                            psO, psOk = pb[2 + d], PB[2 + d]
                            psU, psUk = pb[4 + d], PB[4 + d]
                            MM(psO[:, ch * 64:(ch + 1) * 64], Sbf[d][:], Qb[d][:, csl], False, ci == 1,
                               [("Sbf", d), ("Qb", d, blk)], [psOk])
                            MM(psU[:, 0:128], KdT[d][prow, tt, :], Vh[prow, tt, :], True, True,
                               [("KdT", d, tt // 8), ("Vh", blk)], [psUk])
                            STT("dve", S32[d][:], S32[d][:], Dall[:, d, n:n + 1], psU[:, 0:128],
                                ALU.mult, ALU.add, [("S32", d), ("Dall", d, blk), psUk], [("S32", d)])
                            CP("act", Sbf[d][:], S32[d][:], [("S32", d)], [("Sbf", d)])
                    for d in range(2):
                        tt = tts[d]
                        tsl = slice(tt * 128, (tt + 1) * 128)
                        if (d == 0) == (tt <= 7):
                            CP("act", oF[:, tsl], pb[2 + d][:, 0:128], [PB[2 + d]], [("oF", tt)])
                        else:
                            TT("dve", oF[:, tsl], oF[:, tsl], pb[2 + d][:, 0:128], ALU.add,
                               [("oF", tt), PB[2 + d]], [("oF", tt)])
                for blk in range(4):
                    bs = slice(blk * 512, (blk + 1) * 512)
                    ok = [("oF", blk * 4 + j) for j in range(4)]
                    ACT(T[5][:], oF[:, bs], AF.Square, ok, [TK[5]])
                    hi = Qb[0][:, 0:512]
                    lo = Qb[0][:, 512:1024]
                    CP("dve", hi, T[5][:], [TK[5]], [("Qb", 0, 0)])
                    TT("dve", T[6][:], T[5][:], hi, ALU.subtract, [TK[5], ("Qb", 0, 0)], [TK[6]])
                    CP("dve", lo, T[6][:], [TK[6]], [("Qb", 0, 1)])
                    MM(pb[0][:, :], ones_bf[:], hi, True, False, ["ones_bf", ("Qb", 0, 0)], [PB[0]])
                    MM(pb[0][:, :], ones_bf[:], lo, False, True, ["ones_bf", ("Qb", 0, 1)], [PB[0]])
                    ACT(T[6][:], pb[0][:, :], AF.Ln, [PB[0]], [TK[6]], scale=1.0 / 128.0, bias=RMS_EPS)
                    ACT(T[6][:], T[6][:], AF.Exp, [TK[6]], [TK[6]], scale=-0.5)
                    STT("dve", T[5][:], oF[:, bs], ng_t[:, l, hh:hh + 1], T[6][:], ALU.mult, ALU.mult,
                        ok + ["ng_t", TK[6]], [TK[5]])
                    TT("dve", yT[:, hh, bs], T[5][:], sg[:, bs], ALU.mult, [TK[5], ("sg", blk)],
                       [("yT", hh, blk)])

            wa = [A.alloc("wa%d" % i, [128, 8, 224], BF16) for i in range(2)]
            qT = A.alloc("qT", [128, S_], BF16)
            kT = A.alloc("kT", [128, S_], BF16)
            Va = A.alloc("Va", [128, 16, 128], BF16)
            pt = [A.alloc("pt%d" % i, [128, 512], BF16) for i in range(3)] + [T[6].bitcast(BF16)[:, 0:512]]
            PTK = ["pt0", "pt1", "pt2", TK[6]]
            pm = [A.alloc("pm%d" % i, [128, 512], BF16) for i in range(3)] + [q32.bitcast(BF16)[:, 0:512]]
            PMK = ["pm0", "pm1", "pm2", "q32"]
            rc = A.alloc("rc", [64, 512], F32)
            r1 = rc
            MEMSET("pool", Va[:], 1.0, [("Va", b) for b in range(4)])
            MEMSET("pool", qT[:], 0.0, [("qT", b) for b in range(4)])
            MEMSET("pool", kT[:], 0.0, [("kT", b) for b in range(4)])
            for h in range(8):
                S.fence()
                S.reorder_on = RE_ATT
                w = wa[h % 2]
                wk = "wa%d" % (h % 2)
                for gi in range(3):
                    c0 = 2560 + gi * 512 + h * 64
                    DMA("pool", w[:, :, gi * 80:gi * 80 + 64],
                        w_in[l, :, c0:c0 + 64].rearrange("(c p) n -> p c n", p=128), [], [wk])
                for gi in range(2):
                    CP("pool", w[:, :, gi * 80 + 64:gi * 80 + 72], w[:, :, gi * 80 + 8:gi * 80 + 16], [wk], [wk])
                    CP("pool", w[:, :, gi * 80 + 72:gi * 80 + 80], w[:, :, gi * 80 + 0:gi * 80 + 8], [wk], [wk])
                for blk in range(4):
                    bs = slice(blk * 512, (blk + 1) * 512)
                    hkeys = [("hT", blk * 4 + j) for j in range(4)]
                    for gi in range(2):
                        for c in range(8):
                            MM(pb[gi][0:80, :], w[:, c, gi * 80:(gi + 1) * 80], hT[:, c, bs],
                               c == 0, c == 7, [wk] + hkeys, [PB[gi]])
                    for j in range(4):
                        ts_ = slice(blk * 512 + j * 128, blk * 512 + (j + 1) * 128)
                        for c in range(8):
                            MM(pb[2][:, j * 64:(j + 1) * 64], hT[:, c, ts_], w[:, c, 160:224],
                               c == 0, c == 7, [wk] + hkeys, [PB[2]])
                    CP("act", Va[:, blk * 4:(blk + 1) * 4, 0:64],
                       pb[2][:, 0:256].rearrange("p (j v) -> p j v", j=4), [PB[2]], [("Va", blk)])
                    for gi, dst, dk in ((0, qT, "qT"), (1, kT, "kT")):
                        CP("act", dst[0:64, bs], pb[gi][0:64, :], [PB[gi]], [(dk, blk)])
                        TT("dve", r1[0:16, :], pb[gi][0:16, :], rope[:, 0, bs], ALU.mult, [PB[gi], "rope"], ["rc"])
                        CP("act", T7[0:16, :], pb[gi][64:80, :], [PB[gi]], ["T7"])
                        TT("dve", T7[0:16, :], T7[0:16, :], rope[:, 1, bs], ALU.mult, ["T7", "rope"], ["T7"])
                        TT("dve", dst[0:16, bs], r1[0:16, :], T7[0:16, :], ALU.add, ["rc", "T7"], [(dk, blk)])
                pairs = []
                for qb in range(4):
                    q0 = qb * 512
                    kbs = [kb for kb in range(16)
                           if kb * 128 >= q0 - 1151 and kb * 128 <= q0 + 1535]
                    for ki, kb in enumerate(kbs):
                        pairs.append((qb, ki, kb, ki == len(kbs) - 1))

                def emit_S(i):
                    qb, ki, kb, lastk = pairs[i]
                    q0 = qb * 512
                    ms = q0 - kb * 128 - OFFMIN
                    assert 0 <= ms and ms + 512 <= AMW
                    b = i % 4
                    sb_ = (3, 4, 0, 1)[b]
                    psS, psSk = pb[sb_], PB[sb_]
                    MM(psS[:, :], kT[:, kb * 128:(kb + 1) * 128], qT[:, q0:q0 + 512], True, True,
                       [("kT", kb // 4), ("qT", qb)], [psSk])
                    ACT(pt[b][:], psS[:, :], AF.Exp, [psSk], [PTK[b]], scale=0.125)
                    TT("dve", pm[b][:], pt[b][:], amask[:, ms:ms + 512], ALU.mult,
                       [PTK[b], "amask"], [PMK[b]])

                def emit_PV(i):
                    qb, ki, kb, lastk = pairs[i]
                    q0 = qb * 512
                    b = i % 4
                    nb = 5 if qb % 2 == 0 else 2
                    psN, psNk = pb[nb], PB[nb]
                    MM(psN[:, :], Va[:, kb, :], pm[b][:], ki == 0, lastk,
                       [("Va", kb // 4), PMK[b]], [psNk])
                    if lastk:
                        ACT(rc[:], psN[64:128, :], AF.Ln, [psNk], ["rc"])
                        ACT(rc[:], rc[:], AF.Exp, ["rc"], ["rc"], scale=-1.0)
                        pr = slice((h % 2) * 64, (h % 2) * 64 + 64)
                        TT("dve", yT[pr, 4 + h // 2, q0:q0 + 512], psN[0:64, :], rc[:], ALU.mult,
                           [psNk, "rc"], [("yT", 4 + h // 2, qb)])

                emit_S(0)
                emit_S(1)
                emit_S(2)
                for i in range(len(pairs)):
                    if i + 3 < len(pairs):
                        emit_S(i + 3)
                    emit_PV(i)
            if dbg and l == n_layers - 1:
                DMA("sp", dbg_yT[s], yT[:], [("yT", c, q) for c in range(8) for q in range(4)], ["dbg_yT"])
            barrier()
            S.reorder_on = True
            A.release(b1mark)

            wo = A.alloc("wo", [128, 8, D], BF16)
            for c in range(8):
                DMA("pool", wo[:, c, :], w_out[l, c * 128:(c + 1) * 128, :], [], ["wo"])
            g1 = A.alloc("g1", [128, D], F32)
            b1 = A.alloc("b1", [128, D], F32)
            DMA("sp", g1[:], ln1_g[l:l + 1, :].broadcast_to([128, D]), [], ["g1"])
            DMA("sp", b1[:], ln1_b[l:l + 1, :].broadcast_to([128, D]), [], ["b1"])
            wr = A.alloc("wr", [128, 8, 36], F32)
            wr_hi = A.alloc("wr_hi", [128, 8, 36], BF16)
            wr_lo = A.alloc("wr_lo", [128, 8, 36], BF16)
            wr_t = A.alloc("wr_t", [128, 8, 36], F32)
            rb = A.alloc("rb", [128, 36], F32)
            DMA("sp", wr[:, :, 0:4], r_w1[l].rearrange("(c p) n -> p c n", p=128), [], ["wr"], slow=True)
            DMA("sp", wr[:, :, 4:36], r_w2[l].rearrange("(c p) n -> p c n", p=128), [], ["wr"], slow=True)
            DMA("sp", rb[:, 0:4], r_b1[l:l + 1, :].broadcast_to([128, 4]), [], ["rb"], slow=True)
            DMA("sp", rb[:, 4:36], r_b2[l:l + 1, :].broadcast_to([128, 32]), [], ["rb"], slow=True)
            CP("dve", wr_hi[:], wr[:], ["wr"], ["wr_hi"])
            TT("dve", wr_t[:], wr[:], wr_hi[:], ALU.subtract, ["wr", "wr_hi"], ["wr_t"])
            CP("dve", wr_lo[:], wr_t[:], ["wr_t"], ["wr_lo"])
            ht = [A.alloc("ht%d" % i, [128, D], F32) for i in range(2)]
            z = [A.alloc("z%d" % i, [128, D], F32) for i in range(2)]
            hb = [A.alloc("hb%d" % i, [128, D], BF16) for i in range(2)]
            hlo = [A.alloc("hlo%d" % i, [128, D], BF16) for i in range(2)]
            hTh = A.alloc("hTh", [128, 8, 128], BF16)
            hTl = A.alloc("hTl", [128, 8, 128], BF16)
            st = A.alloc("st", [128, 2, 6], F32)
            mv = A.alloc("mv", [128, 2], F32)
            rstd = A.alloc("rstd", [128, 1], F32)
            nmr = A.alloc("nmr", [128, 1], F32)
            L = A.alloc("L", [128, 36], F32)
            sm = A.alloc("sm", [128, 16], F32)
            e4 = A.alloc("e4", [128, 4], F32)
            oh1 = A.alloc("oh1", [128, 4], F32)
            L2 = A.alloc("L2", [128, 32], F32)
            L2b = A.alloc("L2b", [128, 32], F32)
            oha = A.alloc("oha", [128, 32], F32)
            ohb = A.alloc("ohb", [128, 32], F32)
            Mbf = A.alloc("Mbf", [128, 32], BF16)
            pos = A.alloc("pos", [128, 32], F32)
            tq = A.alloc("tq", [128, 32], F32)
            sl = A.alloc("sl", [128, 2], F32)
            for ti in range(16):
                tt = s * 16 + ti
                b = ti % 2
                tsl = slice(ti * 128, (ti + 1) * 128)
                rows = slice(t0 + ti * 128, t0 + (ti + 1) * 128)
                DMA("sp", ht[b][:], src[rows, :], ["hres_%d" % tt], ["ht%d" % b])
                for half in range(2):
                    for c in range(8):
                        MM(pb[half][:, :], yT[:, c, tsl], wo[:, c, half * 512:(half + 1) * 512],
                           c == 0, c == 7, ["wo"] + [("yT", c, ti // 4)], [PB[half]])
                    STT("dve", z[b][:, half * 512:(half + 1) * 512], ht[b][:, half * 512:(half + 1) * 512],
                        ALPHA, pb[half][:, :], ALU.mult, ALU.add, ["ht%d" % b, PB[half]], ["z%d" % b])
                ln_tail(z[b], "z%d" % b, st, mv, rstd, nmr, g1, "g1", b1, "b1")
                DMA("sp", hres[rows, :], z[b][:], ["z%d" % b], ["hres_%d" % tt], sem_key="st_z%d" % b)
                CP("act", hb[b][:], z[b][:], ["z%d" % b], ["hb%d" % b])
                TT("pool", ht[b][:], z[b][:], hb[b][:], ALU.subtract, ["z%d" % b, "hb%d" % b], ["ht%d" % b])
                CP("pool", hlo[b][:], ht[b][:], ["ht%d" % b], ["hlo%d" % b])
                for c in range(8):
                    TR(pbT[0][:, c * 128:(c + 1) * 128], hb[b][:, c * 128:(c + 1) * 128],
                       ["hb%d" % b, "ident"], [PT[0]])
                for c in range(8):
                    TR(pbT[1][:, c * 128:(c + 1) * 128], hlo[b][:, c * 128:(c + 1) * 128],
                       ["hlo%d" % b, "ident"], [PT[1]])
                CP("act", hTh[:], pbT[0][:].rearrange("p (c t) -> p c t", c=8), [PT[0]], ["hTh"])
                CP("dve", hTl[:], pbT[1][:].rearrange("p (c t) -> p c t", c=8), [PT[1]], ["hTl"])
                n = 0
                for (a_, ak, w_, wk_) in ((hTh, "hTh", wr_hi, "wr_hi"), (hTl, "hTl", wr_hi, "wr_hi"),
                                          (hTh, "hTh", wr_lo, "wr_lo")):
                    for c in range(8):
                        MM(pb[2][:, 0:36], a_[:, c, :], w_[:, c, :], n == 0, n == 23, [ak, wk_], [PB[2]])
                        n += 1
                route(tt, pb[2], PB[2], rb, L, sm, e4, oh1, L2, L2b, oha, ohb, Mbf, pos, tq, sl, cnt_bc)
                for k in range(2):
                    S.op("pool", (lambda idx_, src_: (lambda e: e.indirect_dma_start(
                        out=xs, out_offset=bass.IndirectOffsetOnAxis(ap=idx_, axis=0),
                        in_=src_, in_offset=None)))(slots_i[:, tt, k:k + 1], hb[b][:, :]),
                        reads=["hb%d" % b, ("slots", tt)], writes=["xs"], dma=True, sem_key="sc_hb%d" % b, cost=6.0)
            barrier()

        A.release(lmark)
        wg = [A.alloc("wg%d" % i, [128, 8, 512], BF16) for i in range(2)]
        wu = [A.alloc("wu%d" % i, [128, 8, 512], BF16) for i in range(2)]
        wd = [A.alloc("wd%d" % i, [128, 4, D], BF16) for i in range(2)]
        xr = [A.alloc("xr%d" % i, [128, 3, D], BF16) for i in range(2)]
        xT = [A.alloc("xT%d" % i, [128, 8, CAP], BF16) for i in range(2)]
        hid = A.alloc("hid", [128, 4, CAP], BF16)
        sil = [A.alloc("sil%d" % i, [128, CAP], F32) for i in range(2)]
        yo = [A.alloc("yo%d" % i, [128, D], F32) for i in range(2)]
        for e_ in range(NE):
            b = e_ % 2
            DMA("sp", xr[b][:], xs[e_ * CAP:(e_ + 1) * CAP, :].rearrange("(j p) d -> p j d", p=128),
                ["xs"], ["xr%d" % b])
            for c in range(8):
                DMA("pool", wg[b][:, c, :], w_gate[l, e_, c * 128:(c + 1) * 128, :], [], ["wg%d" % b])
                DMA("pool", wu[b][:, c, :], w_up[l, e_, c * 128:(c + 1) * 128, :], [], ["wu%d" % b])
            for f in range(4):
                DMA("pool", wd[b][:, f, :], w_down[l, e_, f * 128:(f + 1) * 128, :], [], ["wd%d" % b])
            for j in range(3):
                tb = j % 2
                for c in range(8):
                    TR(pbT[tb][:, c * 128:(c + 1) * 128], xr[b][:, j, c * 128:(c + 1) * 128],
                       ["xr%d" % b, "ident"], [PT[tb]])
                CP("act" if j % 2 == 0 else "dve", xT[b][:, :, j * 128:(j + 1) * 128],
                   pbT[tb][:].rearrange("p (c t) -> p c t", c=8), [PT[tb]], ["xT%d" % b])
            for f in range(4):
                fb = f % 2
                pg, pgk = pb[fb], PB[fb]
                pu, puk = pb[2 + fb], PB[2 + fb]
                for c in range(8):
                    MM(pg[:, 0:CAP], wg[b][:, c, f * 128:(f + 1) * 128], xT[b][:, c, :], c == 0, c == 7,
                       ["wg%d" % b, "xT%d" % b], [pgk])
                for c in range(8):
                    MM(pu[:, 0:CAP], wu[b][:, c, f * 128:(f + 1) * 128], xT[b][:, c, :], c == 0, c == 7,
                       ["wu%d" % b, "xT%d" % b], [puk])
                ACT(sil[fb][:], pg[:, 0:CAP], AF.Silu, [pgk], ["sil%d" % fb])
                TT("dve", hid[:, f, :], sil[fb][:], pu[:, 0:CAP], ALU.mult, ["sil%d" % fb, puk], [("hid", f)])
            for j in range(3):
                yb = j % 2
                for half in range(2):
                    py, pyk = pb[4 + half], PB[4 + half]
                    for f in range(4):
                        MM(py[:, :], hid[:, f, j * 128:(j + 1) * 128], wd[b][:, f, half * 512:(half + 1) * 512],
                           f == 0, f == 3, [("hid", f), "wd%d" % b], [pyk])
                    CP("act" if half == 0 else "dve", yo[yb][:, half * 512:(half + 1) * 512], py[:, :],
                       [pyk], ["yo%d" % yb])
                r0 = e_ * CAP + j * 128
                DMA("sp", ys[r0:r0 + 128, :], yo[yb][:], ["yo%d" % yb], ["ys"], sem_key="st_yo%d" % yb)
        barrier()

        A.release(lmark)
        g2 = A.alloc("g2", [128, D], F32)
        b2 = A.alloc("b2", [128, D], F32)
        DMA("sp", g2[:], ln2_g[l:l + 1, :].broadcast_to([128, D]), [], ["g2"])
        DMA("sp", b2[:], ln2_b[l:l + 1, :].broadcast_to([128, D]), [], ["b2"])
        ht = [A.alloc("ht%d" % i, [128, D], F32) for i in range(2)]
        yg = [A.alloc("yg%d" % i, [128, 2, D], F32) for i in range(2)]
        z = [A.alloc("z%d" % i, [128, D], F32) for i in range(2)]
        st = A.alloc("st", [128, 2, 6], F32)
        mv = A.alloc("mv", [128, 2], F32)
        rstd = A.alloc("rstd", [128, 1], F32)
        nmr = A.alloc("nmr", [128, 1], F32)
        dst = out if last else hres
        for tt in range(NTT):
            b = tt % 2
            rows = slice(tt * 128, (tt + 1) * 128)
            DMA("sp", ht[b][:], hres[rows, :], ["hres_%d" % tt], ["ht%d" % b])
            for k in range(2):
                S.op("pool", (lambda idx_, dst_: (lambda e: e.indirect_dma_start(
                    out=dst_, out_offset=None, in_=ys,
                    in_offset=bass.IndirectOffsetOnAxis(ap=idx_, axis=0))))(slots_i[:, tt, k:k + 1], yg[b][:, k, :]),
                    reads=["ys", ("slots", tt)], writes=["yg%d" % b], dma=True, cost=8.0)
            ACT(z[b][:], ht[b][:], AF.Identity, ["ht%d" % b], ["z%d" % b], scale=ALPHA)
            for k in range(2):
                STT("dve", z[b][:], yg[b][:, k, :], gates[:, tt, k:k + 1], z[b][:],
                    ALU.mult, ALU.add, ["yg%d" % b, ("gates", tt), "z%d" % b], ["z%d" % b])
            ln_tail(z[b], "z%d" % b, st, mv, rstd, nmr, g2, "g2", b2, "b2")
            DMA("sp", dst[rows, :], z[b][:], ["z%d" % b], ["out" if last else "hres_%d" % tt],
                sem_key="st_z%d" % b)
        barrier()

    def ln_tail(zt, zk, st, mv, rstd, nmr, g, gk, b_, bk):
        for half in range(2):
            S.op("dve", (lambda o_, i_: (lambda e: e.bn_stats(o_, i_)))(st[:, half, :], zt[:, half * 512:(half + 1) * 512]),
                 reads=[zk], writes=["st"], cost=0.7)
        S.op("dve", lambda e: e.bn_aggr(mv[:], st[:].rearrange("p a b -> p (a b)")), reads=["st"], writes=["mv"])
        ACT(rstd[:], mv[:, 1:2], AF.Ln, ["mv"], ["rstd"], bias=LN_EPS)
        ACT(rstd[:], rstd[:], AF.Exp, ["rstd"], ["rstd"], scale=-0.5)
        STT("dve", nmr[:], mv[:, 0:1], -1.0, rstd[:], ALU.mult, ALU.mult, ["mv", "rstd"], ["nmr"])
        ACT(zt[:], zt[:], AF.Identity, [zk, "rstd", "nmr"], [zk], scale=rstd[:, 0:1], bias=nmr[:, 0:1])
        TT("pool", zt[:], zt[:], g[:], ALU.mult, [zk, gk], [zk])
        TT("dve", zt[:], zt[:], b_[:], ALU.add, [zk, bk], [zk])

    def route(tt, pl, plk, rb, L, sm, e4, oh1, L2, L2b, oha, ohb, Mbf, pos, tq, sl, cnt_bc):
        TT("dve", L[:], pl[:, 0:36], rb[:], ALU.add, [plk, "rb"], ["L"])
        m1, nm1, s1, pg_, ma, mb, dd, ga = (sm[:, i:i + 1] for i in range(8))
        S.op("dve", lambda e: e.reduce_max(m1, L[:, 0:4], AX.X), reads=["L"], writes=["sm0"])
        TS("dve", oh1[:], L[:, 0:4], m1, None, ALU.is_equal, None, ["L", "sm0"], ["oh1"])
        TS("dve", nm1, m1, -1.0, None, ALU.mult, None, ["sm0"], ["sm1"])
        ACT(e4[:], L[:, 0:4], AF.Exp, ["L", "sm1"], ["e4"], bias=nm1)
        S.op("dve", lambda e: e.reduce_sum(s1, e4[:], AX.X), reads=["e4"], writes=["sm2"])
        RECIP(pg_, s1, ["sm2"], ["sm3"])
        TS("dve", e4[:], oh1[:], BIG, -BIG, ALU.mult, ALU.add, ["oh1", "e4"], ["e4"])
        TT("dve", L2[:].rearrange("p (g e) -> p g e", g=4), L[:, 4:36].rearrange("p (g e) -> p g e", g=4),
           e4[:].rearrange("p (g o) -> p g o", o=1).broadcast_to([128, 4, 8]), ALU.add, ["L", "e4"], ["L2"])
        S.op("dve", lambda e: e.reduce_max(ma, L2[:], AX.X), reads=["L2"], writes=["sm4"])
        TS("dve", oha[:], L2[:], ma, None, ALU.is_equal, None, ["L2", "sm4"], ["oha"])
        STT("dve", L2b[:], oha[:], -BIG, L2[:], ALU.mult, ALU.add, ["oha", "L2"], ["L2b"])
        S.op("dve", lambda e: e.reduce_max(mb, L2b[:], AX.X), reads=["L2b"], writes=["sm5"])
        TS("dve", ohb[:], L2b[:], mb, None, ALU.is_equal, None, ["L2b", "sm5"], ["ohb"])
        TT("dve", dd, mb, ma, ALU.subtract, ["sm4", "sm5"], ["sm6"])
        ACT(dd, dd, AF.Exp, ["sm6"], ["sm6"])
        TS("dve", dd, dd, 1.0, None, ALU.add, None, ["sm6"], ["sm6"])
        RECIP(ga, dd, ["sm6"], ["sm7"])
        TT("dve", gates[:, tt, 0:1], ga, pg_, ALU.mult, ["sm7", "sm3"], [("gates", tt)])
        TT("dve", gates[:, tt, 1:2], pg_, gates[:, tt, 0:1], ALU.subtract, ["sm3", ("gates", tt)], [("gates", tt)])
        TT("dve", Mbf[:], oha[:], ohb[:], ALU.add, ["oha", "ohb"], ["Mbf"])
        MM(pb[3][:, 0:32], ustrict[:], Mbf[:], True, True, ["ustrict", "Mbf"], [PB[3]])
        MM(pb[4][:, 0:32], ones_bf[:], Mbf[:], True, True, ["ones_bf", "Mbf"], [PB[4]])
        TT("dve", pos[:], pb[3][:, 0:32], cnt_bc[:], ALU.add, [PB[3], "cnt_bc"], ["pos"])
        TT("dve", cnt_bc[:], cnt_bc[:], pb[4][:, 0:32], ALU.add, ["cnt_bc", PB[4]], ["cnt_bc"])
        TT("dve", pos[:], pos[:], ecap[:], ALU.add, ["pos", "ecap"], ["pos"])
        TT("dve", tq[:], pos[:], oha[:], ALU.mult, ["pos", "oha"], ["tq"])
        S.op("dve", lambda e: e.reduce_sum(sl[:, 0:1], tq[:], AX.X), reads=["tq"], writes=["sl"])
        TT("dve", tq[:], pos[:], ohb[:], ALU.mult, ["pos", "ohb", "sl"], ["tq"])
        S.op("dve", lambda e: e.reduce_sum(sl[:, 1:2], tq[:], AX.X), reads=["tq"], writes=["sl"])
        TS("dve", sl[:], sl[:], 0.0, float(NE * CAP - 1), ALU.max, ALU.min, ["sl"], ["sl"])
        CP("dve", slots_i[:, tt, :], sl[:], ["sl"], [("slots", tt)])

    for l in range(n_layers):
        layer(l)
    import os
    S.emit(final_keys=["out"], reorder=os.environ.get("NOREORDER") is None)
    return nc


def _consts():
    bf = ml_dtypes.bfloat16
    ident = np.eye(128, dtype=np.float32).astype(bf)
    p = np.arange(128)[:, None]
    j = np.arange(AMW)[None, :]
    dl = j - p + OFFMIN
    ad = np.abs(dl)
    cnt = (ad <= 64).astype(np.float32) + ((dl % 4 == 0) & (ad <= 256)) + ((dl % 16 == 0) & (ad <= 1024))
    amask = cnt.astype(bf)
    s = np.arange(128)[:, None]
    t = np.arange(128)[None, :]
    same = (s // 64) == (t // 64)
    hm = np.stack([(same & (s <= t)), (same & (s >= t))], axis=1).astype(np.float32).astype(bf)
    ustrict = (s < t).astype(np.float32).astype(bf)
    rstart = np.ones((128, 512), np.float32)
    rstart[:, ::64] = 0.0
    half = 8
    inv_freq = (500000.0 ** (-np.arange(half, dtype=np.float32) / half)).astype(np.float32)
    pos = np.arange(S_, dtype=np.float32)
    ang = (pos[None, :] * inv_freq[:, None]).astype(np.float32)
    cos = np.cos(ang).astype(np.float32)
    sin = np.sin(ang).astype(np.float32)
    rope = np.zeros((16, 2, S_), np.float32)
    rope[0:8, 0] = cos
    rope[8:16, 0] = cos
    rope[0:8, 1] = -sin
    rope[8:16, 1] = sin
    ecap = np.tile((np.arange(NE, dtype=np.float32) * CAP)[None, :], (128, 1))
    return {"c_ident": ident, "c_amask": amask, "c_hmask": np.ascontiguousarray(hm), "c_ustrict": ustrict,
            "c_rstart": rstart, "c_rope": rope, "c_ecap": ecap}


_NC_CACHE = {}


def kernel(**inputs):
    if "nc" not in _NC_CACHE:
        _NC_CACHE["nc"] = build()
    nc = _NC_CACHE["nc"]
    consts = _consts()
    x = np.ascontiguousarray(inputs["x"], dtype=np.float32).reshape(NCORES, TOK, D)
    shared = {k: np.ascontiguousarray(v) for k, v in inputs.items() if k != "x"}
    in_maps = []
    for c in range(NCORES):
        m = {"x": x[c]}
        m.update(shared)
        m.update(consts)
        in_maps.append(m)
    res = run_bass_kernel_spmd(nc, in_maps, core_ids=list(range(NCORES)))
    o = np.stack([np.asarray(r["out"], dtype=np.float32) for r in res.results], axis=0)
    return o.reshape(16, S_, D)
```

```python
import contextlib
import numpy as np
import ml_dtypes
import concourse.bass as bass
import concourse.mybir as mybir
from concourse.bass_utils import run_bass_kernel_spmd

F32 = mybir.dt.float32
BF16 = mybir.dt.bfloat16
I32 = mybir.dt.int32
AF = mybir.ActivationFunctionType
ALU = mybir.AluOpType
AX = mybir.AxisListType

NCORES = 8
S_ = 2048
D = 1024
TOK = 2 * S_
NTT = TOK // 128
CAP = 384
NE = 32
ALPHA = 4.0 ** 0.25
LN_EPS = 1e-5
RMS_EPS = 1e-6
OFFMIN = -1408
AMW = 1024 - OFFMIN + 512
BIG = 1.0e4


class _Op:
    __slots__ = ("eng", "fn", "deps", "odeps", "is_dma", "dkey", "sig", "val", "idx", "cost", "tag", "grp")


class Sched:
    ENGS = ("pe", "act", "dve", "pool", "sp")
    LAT = 0.25

    def __init__(self, nc):
        self.nc = nc
        self.ops = []
        self.last_writer = {}
        self.readers = {}
        self.dma_count = {}
        self.last_dma = {}
        self.fixed = []
        self.pool_dmas = []
        self.reorder_on = True
        self.seg_flags = []

    def op(self, eng, fn, reads=(), writes=(), dma=False, sem_key=None, force=False, cost=0.3):
        o = _Op()
        o.eng = eng
        o.fn = fn
        o.is_dma = dma
        o.idx = len(self.ops)
        o.sig = False
        o.val = None
        o.dkey = None
        o.cost = cost
        o.grp = None
        o.tag = "%s r=%s w=%s" % ("DMA" if dma else "", list(reads)[:3], list(writes)[:2])
        odeps = set()
        if dma:
            o.dkey = sem_key if sem_key is not None else writes[0]
            self.dma_count[o.dkey] = self.dma_count.get(o.dkey, 0) + 1
            o.val = 16 * self.dma_count[o.dkey]
            if o.dkey in self.last_dma:
                odeps.add(self.last_dma[o.dkey])
            self.last_dma[o.dkey] = o.idx
        deps = set()
        for k in reads:
            for j in self.last_writer.get(k, ()):
                deps.add(j)
        for k in writes:
            ws = self.last_writer.get(k, [])
            rs = self.readers.get(k, [])
            if (dma and not force and not rs and ws
                    and all(self.ops[j].is_dma for j in ws)):
                self.last_writer[k] = ws + [o.idx]
            else:
                for j in ws:
                    deps.add(j)
                for j in rs:
                    p = self.ops[j]
                    deps.add(j)
                self.last_writer[k] = [o.idx]
                self.readers[k] = []
        fdeps = []
        for j in deps:
            p = self.ops[j]
            if p.fn is None:
                continue
            if p.eng == "pe" and eng == "pe" and not p.is_dma and not dma:
                odeps.add(j)
                continue
            fdeps.append(j)
        o.deps = fdeps
        o.odeps = list(odeps)
        for j in fdeps:
            self.ops[j].sig = True
        for k in reads:
            if k not in writes:
                self.readers.setdefault(k, []).append(o.idx)
        self.ops.append(o)
        return o

    def fence(self):
        n = len(self.ops)
        self.fixed.append((n, n))
        self.seg_flags.append(self.reorder_on)

    def barrier(self, fn_tiny):
        keys = list(set(list(self.last_writer.keys()) + list(self.readers.keys())))
        keys = [k for k in keys if k != "__bar"]
        a = len(self.ops)
        self.op("sp", fn_tiny, reads=[], writes=keys + ["__bar"], dma=True,
                sem_key="__bar", force=True, cost=2.0)
        bw = self.last_writer["__bar"]
        self.last_writer = {"__bar": bw}
        self.readers = {}
        for e in ("pe", "act", "dve", "pool"):
            self.op(e, None, reads=["__bar"], cost=0.05)
        self.fixed.append((a, len(self.ops)))
        self.seg_flags.append(self.reorder_on)

    def _schedule_segment(self, a, b, order):
        import heapq
        ops = self.ops
        n = b - a
        if n == 0:
            return
        succ = [[] for _ in range(n)]
        ndep = [0] * n
        import os
        chain = set(os.environ.get("CHAIN_ENGS", "").split(","))
        lastop = {}
        for i in range(a, b):
            o = ops[i]
            ds = set(j for j in list(o.deps) + list(o.odeps) if j >= a)
            if o.eng in chain:
                if o.eng in lastop:
                    ds.add(lastop[o.eng])
                    if lastop[o.eng] not in o.deps and lastop[o.eng] not in o.odeps:
                        o.odeps.append(lastop[o.eng])
                lastop[o.eng] = i
            ndep[i - a] = len(ds)
            for j in ds:
                succ[j - a].append(i)
        bl = [0.0] * n
        for i in range(b - 1, a - 1, -1):
            m = 0.0
            for sidx in succ[i - a]:
                if bl[sidx - a] > m:
                    m = bl[sidx - a]
            bl[i - a] = m + ops[i].cost
        ready_t = [0.0] * n
        fin = [0.0] * n
        efree = {e: 0.0 for e in self.ENGS}
        avail = {e: [] for e in self.ENGS}
        for i in range(a, b):
            if ndep[i - a] == 0:
                avail[ops[i].eng].append(i)
        left = n
        open_multi = None
        while left:
            best = None
            for e in self.ENGS:
                av = avail[e]
                if e == "pe" and open_multi is not None:
                    av = [i for i in av if ops[i].grp is None or ops[i].grp[0] == open_multi]
                if not av:
                    continue
                mn = min(ready_t[i - a] for i in av)
                t = max(efree[e], mn)
                c = None
                for i in av:
                    if ready_t[i - a] <= t + 1e-9:
                        k = (-bl[i - a], i)
                        if c is None or k < c[0]:
                            c = (k, i)
                if best is None or t < best[0]:
                    best = (t, e, c[1])
            if best is None:
                assert open_multi is not None
                open_multi = None
                continue
            t, e, i = best
            avail[e].remove(i)
            o = ops[i]
            if e == "pe" and o.grp is not None:
                open_multi = None if o.grp[1] else o.grp[0]
            if o.is_dma:
                efree[e] = t + (1.0 if e == "pool" else 0.06)
            else:
                efree[e] = t + o.cost
            fin[i - a] = t + o.cost
            order[e].append(i)
            left -= 1
            for sidx in succ[i - a]:
                so = ops[sidx]
                if i in so.deps:
                    r = fin[i - a] + self.LAT
                else:
                    r = t
                if r > ready_t[sidx - a]:
                    ready_t[sidx - a] = r
                ndep[sidx - a] -= 1
                if ndep[sidx - a] == 0:
                    avail[so.eng].append(sidx)

    def _check(self, order):
        ops = self.ops
        sem = {}
        ptr = {e: 0 for e in self.ENGS}
        total = sum(len(v) for v in order.values())
        done = 0
        while done < total:
            prog = False
            for e in self.ENGS:
                while ptr[e] < len(order[e]):
                    o = ops[order[e][ptr[e]]]
                    ok = True
                    for j in o.deps:
                        p = ops[j]
                        k = ("d", p.dkey) if p.is_dma else ("e", p.eng)
                        if sem.get(k, 0) < p.val:
                            ok = False
                            break
                    if not ok:
                        break
                    if o.fn is not None:
                        if o.is_dma:
                            k = ("d", o.dkey)
                            sem[k] = sem.get(k, 0) + 16
                            assert sem[k] == o.val, ("dma order", o.dkey, sem[k], o.val)
                        elif o.sig:
                            k = ("e", o.eng)
                            sem[k] = sem.get(k, 0) + 1
                            assert sem[k] == o.val
                    ptr[e] += 1
                    done += 1
                    prog = True
            if not prog:
                msg = []
                for e in self.ENGS:
                    if ptr[e] < len(order[e]):
                        o = ops[order[e][ptr[e]]]
                        msg.append((e, o.idx, [(j, ops[j].eng, ops[j].val, ops[j].dkey) for j in o.deps]))
                raise RuntimeError("DEADLOCK in emitted order: %r" % (msg,))

    def emit(self, final_keys=(), reorder=True):
        nc = self.nc
        self.op("sp", None, reads=list(final_keys))
        ops = self.ops
        order = {e: [] for e in self.ENGS}
        pos = 0
        import os
        segsel = os.environ.get("REORDER_SEGS")
        segsel = None if segsel is None else set(int(v) for v in segsel.split(",") if v != "")
        for si, (fa, fb) in enumerate(self.fixed + [(len(ops), len(ops))]):
            flag = self.seg_flags[si] if si < len(self.seg_flags) else True
            if reorder and flag and (segsel is None or si in segsel):
                self._schedule_segment(pos, fa, order)
            else:
                for i in range(pos, fa):
                    order[ops[i].eng].append(i)
            for i in range(fa, fb):
                order[ops[i].eng].append(i)
            pos = fb
        assert sum(len(v) for v in order.values()) == len(ops)
        for e in self.ENGS:
            c = 0
            for i in order[e]:
                o = ops[i]
                if not o.is_dma and o.sig:
                    c += 1
                    o.val = c
        dkeys = list(self.dma_count.keys())
        self._check(order)
        import os
        if os.environ.get("DUMP_ORDER"):
            with open(os.environ["DUMP_ORDER"], "w") as f:
                for e in self.ENGS:
                    f.write("=== %s\n" % e)
                    for i in order[e]:
                        o = ops[i]
                        f.write("%6d %s deps=%s\n" % (i, o.tag, sorted(o.deps)))
        with contextlib.ExitStack() as es:
            esem = {e: es.enter_context(nc.semaphore("s_" + e)) for e in self.ENGS}
            dsem = {k: es.enter_context(nc.semaphore("d%d" % i)) for i, k in enumerate(dkeys)}
            block = es.enter_context(nc.Block())

            def run(engname, eng):
                waited = {}
                for i in order[engname]:
                    o = ops[i]
                    need = {}
                    for j in o.deps:
                        p = ops[j]
                        s = dsem[p.dkey] if p.is_dma else esem[p.eng]
                        v = p.val
                        key = id(s)
                        if v > need.get(key, (None, 0))[1]:
                            need[key] = (s, v)
                    for key, (s, v) in need.items():
                        if waited.get(key, 0) >= v:
                            continue
                        eng.wait_ge(s, v)
                        waited[key] = v
                    if o.fn is None:
                        continue
                    ins = o.fn(eng)
                    if o.is_dma:
                        ins.then_inc(dsem[o.dkey], 16)
                    elif o.sig:
                        ins.then_inc(esem[o.eng], 1)

            @block.sync
            def _(e):
                run("sp", e)

            @block.scalar
            def _(e):
                run("act", e)

            @block.vector
            def _(e):
                run("dve", e)

            @block.tensor
            def _(e):
                run("pe", e)

            @block.gpsimd
            def _(e):
                run("pool", e)


class Arena:
    def __init__(self, nc, base=16512, limit=229344):
        self.nc = nc
        self.cur = base
        self.limit = limit
        self.n = 0

    def alloc(self, name, shape, dt):
        esz = 2 if dt == BF16 else 4
        nbytes = int(np.prod(shape[1:])) * esz
        nbytes = (nbytes + 63) // 64 * 64
        assert self.cur + nbytes <= self.limit, ("SBUF OOM", name, self.cur, nbytes)
        self.n += 1
        t = self.nc.alloc_sbuf_tensor_at("%s_%d" % (name, self.n), list(shape), dt, offset=self.cur)
        self.cur += nbytes
        return t

    def mark(self):
        return self.cur

    def release(self, m):
        self.cur = m


def build(n_layers=2, dbg=None):
    nc = bass.Bass("TRN2", target_bir_lowering=False)

    def din(name, shape, dt=F32):
        return nc.dram_tensor(name, list(shape), dt, kind="ExternalInput").ap()

    x = din("x", [TOK, D])
    w_in = din("w_in", [2, D, 4096])
    lb_logits = din("hg_lb_logits", [2, 2, 512])
    hg_norm_g = din("hg_norm_g", [2, 512])
    w_out = din("w_out", [2, D, D])
    ln1_g = din("ln1_g", [2, D])
    ln1_b = din("ln1_b", [2, D])
    r_w1 = din("router_w1", [2, D, 4])
    r_b1 = din("router_b1", [2, 4])
    r_w2 = din("router_w2", [2, D, 32])
    r_b2 = din("router_b2", [2, 32])
    w_gate = din("ex_w_gate", [2, NE, D, 512])
    w_up = din("ex_w_up", [2, NE, D, 512])
    w_down = din("ex_w_down", [2, NE, 512, D])
    ln2_g = din("ln2_g", [2, D])
    ln2_b = din("ln2_b", [2, D])
    c_ident = din("c_ident", [128, 128], BF16)
    c_amask = din("c_amask", [128, AMW], BF16)
    c_hmask = din("c_hmask", [128, 2, 128], BF16)
    c_ustrict = din("c_ustrict", [128, 128], BF16)
    c_rstart = din("c_rstart", [128, 512])
    c_rope = din("c_rope", [16, 2, S_])
    c_ecap = din("c_ecap", [128, NE])
    out = nc.dram_tensor("out", [TOK, D], F32, kind="ExternalOutput").ap()
    hres = nc.dram_tensor("hres", [TOK, D], F32, kind="ExternalOutput" if dbg else "Internal").ap()
    xs = nc.dram_tensor("xs", [NE * CAP, D], BF16, kind="Internal").ap()
    ys = nc.dram_tensor("ys", [NE * CAP, D], F32, kind="Internal").ap()
    bar_d = nc.dram_tensor("bar_d", [1, 16], F32, kind="Internal").ap()
    dbg_yT = nc.dram_tensor("dbg_yT", [2, 128, 8, S_], BF16, kind="ExternalOutput").ap() if dbg else None

    S = Sched(nc)
    A = Arena(nc)
    import os
    RE_HG = os.environ.get("RE_HG") is not None
    RE_ATT = os.environ.get("RE_ATT") is not None

    def nfree(ap):
        sh = list(ap.shape)
        n = 1
        for v in sh[1:]:
            n *= int(v)
        return n

    def vcost(eng, o):
        n = nfree(o)
        if eng == "act":
            return 0.2 + n / 1400.0
        if eng == "dve":
            return 0.1 + n / 1000.0
        return 0.3 + n / 600.0

    GRP = {"n": 0, "cur": {}}

    def MM(o, lhsT, rhs, start, stop, r, w):
        op_ = S.op("pe", lambda e: e.matmul(o, lhsT, rhs, start=start, stop=stop), reads=r, writes=w,
                   cost=0.04 + max(nfree(o), 64) / 1800.0)
        bank = w[0]
        if start and stop:
            return
        if start:
            GRP["n"] += 1
            GRP["cur"][bank] = GRP["n"]
        op_.grp = (GRP["cur"][bank], bool(stop))

    def TR(o, i, r, w):
        S.op("pe", lambda e: e.transpose(o, i, ident[:]), reads=r, writes=w, cost=0.1)

    def ACT(o, i, func, r, w, scale=1.0, bias=0.0):
        S.op("act", lambda e: e.activation(o, i, func, bias=bias, scale=scale), reads=r, writes=w,
             cost=vcost("act", o))

    def TT(eng, o, a, b, op, r, w):
        S.op(eng, lambda e: e.tensor_tensor(o, a, b, op), reads=r, writes=w, cost=vcost(eng, o))

    def TS(eng, o, a, s1, s2, op0, op1, r, w):
        if s2 is None:
            S.op(eng, lambda e: e.tensor_scalar(o, a, s1, None, op0), reads=r, writes=w, cost=vcost(eng, o))
        else:
            S.op(eng, lambda e: e.tensor_scalar(o, a, s1, s2, op0, op1), reads=r, writes=w, cost=vcost(eng, o))

    def STT(eng, o, a, sc, b, op0, op1, r, w):
        S.op(eng, lambda e: e.scalar_tensor_tensor(o, a, sc, b, op0, op1), reads=r, writes=w, cost=vcost(eng, o))

    def CP(eng, o, i, r, w):
        if eng == "act":
            S.op("act", lambda e: e.copy(o, i), reads=r, writes=w, cost=vcost(eng, o))
        else:
            S.op(eng, lambda e: e.tensor_copy(o, i), reads=r, writes=w, cost=vcost(eng, o))

    def RECIP(o, i, r, w):
        S.op("dve", lambda e: e.reciprocal(o, i), reads=r, writes=w, cost=0.15 + nfree(o) / 200.0)

    def MEMSET(eng, o, val, w):
        S.op(eng, lambda e: e.memset(o, val), reads=[], writes=w, cost=vcost(eng, o))

    def DMA(q, o, i, r, w, sem_key=None, slow=False):
        sh = list(o.shape)
        nb = 1
        for v in sh:
            nb *= int(v)
        nb *= 2 if o.dtype == BF16 else 4
        cost = 2.0 + nb / 100e3
        if slow:
            S.op(q, lambda e: e.dma_start(out=o, in_=i, allow_slow_non_contiguous=True),
                 reads=r, writes=w, dma=True, sem_key=sem_key, cost=cost + 3.0)
        else:
            S.op(q, lambda e: e.dma_start(out=o, in_=i), reads=r, writes=w, dma=True, sem_key=sem_key, cost=cost)

    ident = A.alloc("ident", [128, 128], BF16)
    hmask = A.alloc("hmask", [128, 2, 128], BF16)
    ustrict = A.alloc("ustrict", [128, 128], BF16)
    ones_bf = A.alloc("ones_bf", [128, 128], BF16)
    ecap = A.alloc("ecap", [128, NE], F32)
    lbl = A.alloc("lbl", [128, 2, 2, 4], F32)
    lb_t = A.alloc("lb_t", [128, 2, 4], F32)
    oml_t = A.alloc("oml_t", [128, 2, 4], F32)
    lnoml_t = A.alloc("lnoml_t", [128, 2, 4], F32)
    ng_t = A.alloc("ng_t", [128, 2, 4], F32)
    gates = A.alloc("gates", [128, NTT, 2], F32)
    slots_i = A.alloc("slots_i", [128, NTT, 2], I32)
    bar_s = A.alloc("bar_s", [1, 16], F32)

    DMA("sp", ident[:], c_ident, [], ["ident"])
    DMA("sp", hmask[:], c_hmask, [], ["hmask"])
    DMA("sp", ustrict[:], c_ustrict, [], ["ustrict"])
    DMA("sp", ecap[:], c_ecap, [], ["ecap"])
    MEMSET("dve", ones_bf[:], 1.0, ["ones_bf"])
    MEMSET("dve", bar_s[:], 0.0, ["bar_s"])
    for l in range(2):
        for d in range(2):
            DMA("sp", lbl[:, l, d, :], lb_logits[l, d, :].rearrange("(h c) -> c h", c=128),
                [], ["lbl"], slow=True)
        DMA("sp", ng_t[:, l, :], hg_norm_g[l, :].rearrange("(h c) -> c h", c=128), [], ["ng_t"], slow=True)

    def barrier():
        S.barrier(lambda e: e.dma_start(out=bar_d, in_=bar_s[:]))

    zt = A.alloc("zt", [128, D], BF16)
    MEMSET("pool", zt[:], 0.0, ["zt"])
    for j in range(NE * CAP // 128):
        DMA("pool", xs[j * 128:(j + 1) * 128, :], zt[:], ["zt"], ["xs"])

    PSUM_STATE = {}

    def psum_banks():
        if not PSUM_STATE:
            PSUM_STATE["f"] = [nc.alloc_psum_tensor("pb%d" % i, [128, 512], F32) for i in range(6)]
            PSUM_STATE["t"] = [nc.alloc_psum_tensor("pbT%d" % i, [128, 1024], BF16) for i in range(2)]
        return PSUM_STATE["f"], PSUM_STATE["t"]

    pb, pbT = psum_banks()
    PB = ["pb%d" % i for i in range(6)]
    PT = ["pbT0", "pbT1"]

    base_mark = A.mark()

    def layer(l):
        src = x if l == 0 else hres
        last = (l == n_layers - 1)
        A.release(base_mark)
        if l == 0:
            MEMSET("dve", lb_t[:], 0.0, ["lb_t"])
        else:
            tmpd = A.alloc("tmpd", [128, 2, 4], F32)
            TT("dve", tmpd[:], lbl[:, 0, :, :], lbl[:, 1, :, :], ALU.subtract, ["lbl"], ["tmpd"])
            ACT(tmpd[:], tmpd[:], AF.Exp, ["tmpd"], ["tmpd"])
            TS("dve", tmpd[:], tmpd[:], 1.0, None, ALU.add, None, ["tmpd"], ["tmpd"])
            RECIP(lb_t[:], tmpd[:], ["tmpd"], ["lb_t"])
        TS("dve", oml_t[:], lb_t[:], -1.0, 1.0, ALU.mult, ALU.add, ["lb_t"], ["oml_t"])
        ACT(lnoml_t[:], oml_t[:], AF.Ln, ["oml_t"], ["lnoml_t"])
        cnt_bc = A.alloc("cnt_bc", [128, NE], F32)
        MEMSET("dve", cnt_bc[:], 0.0, ["cnt_bc"])
        lmark = A.mark()

        for s in range(2):
            A.release(lmark)
            t0 = s * S_
            hT = A.alloc("hT", [128, 8, S_], BF16)
            yT = A.alloc("yT", [128, 8, S_], BF16)
            rope = A.alloc("rope", [16, 2, S_], F32)
            amask = A.alloc("amask", [128, AMW], BF16)
            b1mark = A.mark()
            DMA("sp", rope[:], c_rope, [], ["rope"])
            DMA("sp", amask[:], c_amask, [], ["amask"])
            xt = [A.alloc("xt%d" % i, [128, D], F32) for i in range(2)]
            xb = [A.alloc("xb%d" % i, [128, D], BF16) for i in range(2)]
            for tt in range(16):
                b = tt % 2
                DMA("sp", xt[b][:], src[t0 + tt * 128: t0 + (tt + 1) * 128, :], [], ["xt%d" % b])
                CP("act", xb[b][:], xt[b][:], ["xt%d" % b], ["xb%d" % b])
                for c in range(8):
                    TR(pbT[b][:, c * 128:(c + 1) * 128], xb[b][:, c * 128:(c + 1) * 128],
                       ["xb%d" % b, "ident"], [PT[b]])
                CP("dve", hT[:, :, tt * 128:(tt + 1) * 128],
                   pbT[b][:].rearrange("p (c t) -> p c t", c=8), [PT[b]], [("hT", tt)])
            barrier()
            A.release(b1mark)

            S.reorder_on = False
            wh = [A.alloc("wh%d" % i, [128, 8, 640], BF16) for i in range(2)]
            T = [A.alloc("T%d" % i, [128, 512], F32) for i in range(7)]
            TK = ["T%d" % i for i in range(7)]
            q32 = A.alloc("q32", [128, 512], F32)
            Qb = [A.alloc("Qb%d" % d, [128, S_], BF16) for d in range(2)]
            Kinv = [A.alloc("Kinv%d" % d, [128, S_], BF16) for d in range(2)]
            Kd = [A.alloc("Kd%d" % d, [128, S_], BF16) for d in range(2)]
            KdT = [A.alloc("KdT%d" % d, [128, 16, 128], BF16) for d in range(2)]
            Vh = A.alloc("Vh", [128, 16, 128], BF16)
            sg = A.alloc("sg", [128, S_], BF16)
            oF = A.alloc("oF", [128, S_], F32)
            Dall = A.alloc("Dall", [128, 2, 32], F32)
            S32 = [A.alloc("S32_%d" % d, [128, 128], F32) for d in range(2)]
            Sbf = [A.alloc("Sbf_%d" % d, [128, 128], BF16) for d in range(2)]
            T7 = A.alloc("T7", [128, 512], F32)
            ALIAS = {"oFa%d" % k: [("oF", 4 * k + j) for j in range(4)] for k in range(4)}

            def expand_keys(keys):
                out_ = []
                for k in keys:
                    out_ += ALIAS.get(k, [k])
                return out_

            TSET = [T[0:6], [oF[:, k * 512:(k + 1) * 512] for k in range(4)] + [T[4], T7]]
            TKSET = [TK[0:6], ["oFa0", "oFa1", "oFa2", "oFa3", TK[4], "T7"]]
            rstart = A.alloc("rstart", [128, 512], F32)
            DMA("sp", rstart[:], c_rstart, [], ["rstart"])

            for hh in range(4):
                S.fence()
                S.reorder_on = RE_HG
                w = wh[hh % 2]
                wk = "wh%d" % (hh % 2)
                for gi in range(5):
                    c0 = gi * 512 + hh * 128
                    DMA("pool", w[:, :, gi * 128:(gi + 1) * 128],
                        w_in[l, :, c0:c0 + 128].rearrange("(c p) n -> p c n", p=128), [], [wk])
                lbs = [lb_t[:, d, hh:hh + 1] for d in range(2)]
                omls = [oml_t[:, d, hh:hh + 1] for d in range(2)]
                lnomls = [lnoml_t[:, d, hh:hh + 1] for d in range(2)]
                for blk in range(4):
                    bs = slice(blk * 512, (blk + 1) * 512)
                    hkeys = [("hT", blk * 4 + j) for j in range(4)]
                    for gi, pbi in ((0, 0), (1, 1), (2, 2), (4, 3)):
                        for c in range(8):
                            MM(pb[pbi][:, :], w[:, c, gi * 128:(gi + 1) * 128], hT[:, c, bs],
                               c == 0, c == 7, [wk] + hkeys, [PB[pbi]])
                    for j in range(4):
                        ts_ = slice(blk * 512 + j * 128, blk * 512 + (j + 1) * 128)
                        for c in range(8):
                            MM(pb[4][:, j * 128:(j + 1) * 128], hT[:, c, ts_], w[:, c, 384:512],
                               c == 0, c == 7, [wk] + hkeys, [PB[4]])
                    CP("act", Vh[:, blk * 4:(blk + 1) * 4, :],
                       pb[4][:].rearrange("p (j v) -> p j v", j=4), [PB[4]], [("Vh", blk)])
                    ACT(q32[:], pb[0][:], AF.Identity, [PB[0]], ["q32"], scale=128.0 ** -0.5)
                    ACT(T[6][:], pb[3][:], AF.Exp, [PB[3]], [TK[6]], scale=-1.0)
                    ACT(T[6][:], T[6][:], AF.Ln, [TK[6]], [TK[6]], bias=1.0)
                    ACT(T[6][:], T[6][:], AF.Exp, [TK[6]], [TK[6]], scale=-1.0)
                    TT("dve", sg[:, bs], pb[3][:], T[6][:], ALU.mult, [PB[3], TK[6]], [("sg", blk)])
                    def gate_dir(d, T, TK):
                        pa = pb[1 + d]
                        pak = PB[1 + d]
                        ACT(T[0][:], pa[:], AF.Exp, [pak], [TK[0]], scale=-1.0)
                        ACT(T[1][:], T[0][:], AF.Ln, [TK[0]], [TK[1]], bias=1.0)
                        ACT(T[5][:], T[0][:], AF.Ln, [TK[0], "lb_t"], [TK[5]], scale=lbs[d], bias=1.0)
                        TT("pool", T[5][:], T[5][:], T[1][:], ALU.subtract, [TK[5], TK[1]], [TK[5]])
                        STT("dve", T[0][:], pa[:], -1.0, T[1][:], ALU.mult, ALU.subtract,
                            [pak, TK[1]], [TK[0]])
                        S.op("dve", (lambda o_, a_, b_: (lambda e: e.tensor_tensor_scan(
                            o_, a_, b_, 0.0, ALU.mult, ALU.add)))(T[2][:], rstart[:], T[5][:]),
                            reads=["rstart", TK[5]], writes=[TK[2]], cost=1.2)
                        B3 = T[2][:].rearrange("p (n t) -> p n t", t=64)
                        if d == 0:
                            Bx, Bxk = T[2], TK[2]
                            tot = B3[:, :, 63:64]
                        else:
                            TT("pool", T[3][:], T[5][:], T[2][:], ALU.subtract, [TK[5], TK[2]], [TK[3]])
                            TT("pool", T[4][:].rearrange("p (n t) -> p n t", t=64),
                               T[3][:].rearrange("p (n t) -> p n t", t=64),
                               B3[:, :, 63:64].broadcast_to([128, 8, 64]), ALU.add,
                               [TK[3], TK[2]], [TK[4]])
                            Bx, Bxk = T[4], TK[4]
                            tot = T[4][:].rearrange("p (n t) -> p n t", t=64)[:, :, 0:1]
                        ACT(Dall[:, d, blk * 8:(blk + 1) * 8].rearrange("p (n o) -> p n o", o=1), tot,
                            AF.Exp, [Bxk], [("Dall", d, blk)])
                        ACT(T[5][:], Bx[:], AF.Exp, [Bxk], [TK[5]])
                        TT("dve", Qb[d][:, bs], q32[:], T[5][:], ALU.mult, ["q32", TK[5]], [("Qb", d, blk)])
                        TT("pool", T[3][:], T[0][:], Bx[:], ALU.subtract, [TK[0], Bxk], [TK[3]])
                        ACT(Kinv[d][:, bs], T[3][:], AF.Exp, [TK[3], "lnoml_t"], [("Kinv", d, blk)],
                            bias=lnomls[d])
                        TT("pool", T[3][:].rearrange("p (n t) -> p n t", t=64),
                           T[3][:].rearrange("p (n t) -> p n t", t=64),
                           tot.broadcast_to([128, 8, 64]), ALU.add, [TK[3], Bxk], [TK[3]])
                        ACT(Kd[d][:, bs], T[3][:], AF.Exp, [TK[3], "lnoml_t"], [("Kd", d, blk)],
                            bias=lnomls[d])
                    recs = []
                    for d in range(2):
                        rec = []
                        S.op = (lambda rec_: (lambda *a, **k: rec_.append((a, k))))(rec)
                        gate_dir(d, TSET[d], TKSET[d])
                        del S.op
                        recs.append(rec)
                    for i_ in range(max(len(recs[0]), len(recs[1]))):
                        for rec in recs:
                            if i_ < len(rec):
                                a_, k_ = rec[i_]
                                k_ = dict(k_)
                                k_["reads"] = expand_keys(k_.get("reads", ()))
                                k_["writes"] = expand_keys(k_.get("writes", ()))
                                S.op(*a_, **k_)
                for d in range(2):
                    for half in range(2):
                        for j in range(8):
                            tt = half * 8 + j
                            TR(pbT[d][:, j * 128:(j + 1) * 128], Kd[d][:, tt * 128:(tt + 1) * 128],
                               [("Kd", d, tt // 4), "ident"], [PT[d]])
                        CP("act" if d == 0 else "dve", KdT[d][:, half * 8:(half + 1) * 8, :],
                           pbT[d][:].rearrange("p (j c) -> p j c", j=8), [PT[d]], [("KdT", d, half)])
                for d in range(2):
                    MEMSET("pool", S32[d][:], 0.0, [("S32", d)])
                    MEMSET("pool", Sbf[d][:], 0.0, [("Sbf", d)])
                for tt in range(16):
                    for d in range(2):
                        blk = tt // 4
                        tsl = slice(tt * 128, (tt + 1) * 128)
                        pa_i = (2 * tt + d) % 2
                        MM(pb[pa_i][:, 0:128], Kinv[d][:, tsl], Qb[d][:, tsl], True, True,
                           [("Kinv", d, blk), ("Qb", d, blk)], [PB[pa_i]])
                        TT("dve", Kinv[d][:, tsl], pb[pa_i][:, 0:128], hmask[:, d, :], ALU.mult,
                           [PB[pa_i], "hmask"], [("Kinv", d, blk)])
                for i in range(16):
                    tts = [i, 15 - i]
                    for d in range(2):
                        tt = tts[d]
                        MM(pb[2 + d][:, 0:128], Vh[:, tt, :], Kinv[d][:, tt * 128:(tt + 1) * 128], True, False,
                           [("Vh", tt // 4), ("Kinv", d, tt // 4)], [PB[2 + d]])
                    for ci in range(2):
                        for d in range(2):
                            tt = tts[d]
                            blk = tt // 4
                            ch = ci if d == 0 else 1 - ci
                            n = tt * 2 + ch
                            csl = slice(tt * 128 + ch * 64, tt * 128 + (ch + 1) * 64)
                            prow = slice(ch * 64, (ch + 1) * 64)
                            psO, psOk = pb[2 + d], PB[2 + d]
                            psU, psUk = pb[4 + d], PB[4 + d]
                            MM(psO[:, ch * 64:(ch + 1) * 64], Sbf[d][:], Qb[d][:, csl], False, ci == 1,
                               [("Sbf", d), ("Qb", d, blk)], [psOk])
                            MM(psU[:, 0:128], KdT[d][prow, tt, :], Vh[prow, tt, :], True, True,
                               [("KdT", d, tt // 8), ("Vh", blk)], [psUk])
                            STT("dve", S32[d][:], S32[d][:], Dall[:, d, n:n + 1], psU[:, 0:128],
                                ALU.mult, ALU.add, [("S32", d), ("Dall", d, blk), psUk], [("S32", d)])
                            CP("act", Sbf[d][:], S32[d][:], [("S32", d)], [("Sbf", d)])
                    for d in range(2):
                        tt = tts[d]
                        tsl = slice(tt * 128, (tt + 1) * 128)
                        if (d == 0) == (tt <= 7):
                            CP("act", oF[:, tsl], pb[2 + d][:, 0:128], [PB[2 + d]], [("oF", tt)])
                        else:
                            TT("dve", oF[:, tsl], oF[:, tsl], pb[2 + d][:, 0:128], ALU.add,
                               [("oF", tt), PB[2 + d]], [("oF", tt)])
                for blk in range(4):
                    bs = slice(blk * 512, (blk + 1) * 512)
                    ok = [("oF", blk * 4 + j) for j in range(4)]
                    ACT(T[5][:], oF[:, bs], AF.Square, ok, [TK[5]])
                    hi = Qb[0][:, 0:512]
                    lo = Qb[0][:, 512:1024]
                    CP("dve", hi, T[5][:], [TK[5]], [("Qb", 0, 0)])
                    TT("dve", T[6][:], T[5][:], hi, ALU.subtract, [TK[5], ("Qb", 0, 0)], [TK[6]])
                    CP("dve", lo, T[6][:], [TK[6]], [("Qb", 0, 1)])
                    MM(pb[0][:, :], ones_bf[:], hi, True, False, ["ones_bf", ("Qb", 0, 0)], [PB[0]])
                    MM(pb[0][:, :], ones_bf[:], lo, False, True, ["ones_bf", ("Qb", 0, 1)], [PB[0]])
                    ACT(T[6][:], pb[0][:, :], AF.Ln, [PB[0]], [TK[6]], scale=1.0 / 128.0, bias=RMS_EPS)
                    ACT(T[6][:], T[6][:], AF.Exp, [TK[6]], [TK[6]], scale=-0.5)
                    STT("dve", T[5][:], oF[:, bs], ng_t[:, l, hh:hh + 1], T[6][:], ALU.mult, ALU.mult,
                        ok + ["ng_t", TK[6]], [TK[5]])
                    TT("dve", yT[:, hh, bs], T[5][:], sg[:, bs], ALU.mult, [TK[5], ("sg", blk)],
                       [("yT", hh, blk)])

            wa = [A.alloc("wa%d" % i, [128, 8, 224], BF16) for i in range(2)]
            qT = A.alloc("qT", [128, S_], BF16)
            kT = A.alloc("kT", [128, S_], BF16)
            Va = A.alloc("Va", [128, 16, 128], BF16)
            pt = [A.alloc("pt%d" % i, [128, 512], BF16) for i in range(3)] + [T[6].bitcast(BF16)[:, 0:512]]
            PTK = ["pt0", "pt1", "pt2", TK[6]]
            pm = [A.alloc("pm%d" % i, [128, 512], BF16) for i in range(3)] + [q32.bitcast(BF16)[:, 0:512]]
            PMK = ["pm0", "pm1", "pm2", "q32"]
            rc = A.alloc("rc", [64, 512], F32)
            r1 = rc
            MEMSET("pool", Va[:], 1.0, [("Va", b) for b in range(4)])
            MEMSET("pool", qT[:], 0.0, [("qT", b) for b in range(4)])
            MEMSET("pool", kT[:], 0.0, [("kT", b) for b in range(4)])
            for h in range(8):
                S.fence()
                S.reorder_on = RE_ATT
                w = wa[h % 2]
                wk = "wa%d" % (h % 2)
                for gi in range(3):
                    c0 = 2560 + gi * 512 + h * 64
                    DMA("pool", w[:, :, gi * 80:gi * 80 + 64],
                        w_in[l, :, c0:c0 + 64].rearrange("(c p) n -> p c n", p=128), [], [wk])
                for gi in range(2):
                    CP("pool", w[:, :, gi * 80 + 64:gi * 80 + 72], w[:, :, gi * 80 + 8:gi * 80 + 16], [wk], [wk])
                    CP("pool", w[:, :, gi * 80 + 72:gi * 80 + 80], w[:, :, gi * 80 + 0:gi * 80 + 8], [wk], [wk])
                for blk in range(4):
                    bs = slice(blk * 512, (blk + 1) * 512)
                    hkeys = [("hT", blk * 4 + j) for j in range(4)]
                    for gi in range(2):
                        for c in range(8):
                            MM(pb[gi][0:80, :], w[:, c, gi * 80:(gi + 1) * 80], hT[:, c, bs],
                               c == 0, c == 7, [wk] + hkeys, [PB[gi]])
                    for j in range(4):
                        ts_ = slice(blk * 512 + j * 128, blk * 512 + (j + 1) * 128)
                        for c in range(8):
                            MM(pb[2][:, j * 64:(j + 1) * 64], hT[:, c, ts_], w[:, c, 160:224],
                               c == 0, c == 7, [wk] + hkeys, [PB[2]])
                    CP("act", Va[:, blk * 4:(blk + 1) * 4, 0:64],
                       pb[2][:, 0:256].rearrange("p (j v) -> p j v", j=4), [PB[2]], [("Va", blk)])
                    for gi, dst, dk in ((0, qT, "qT"), (1, kT, "kT")):
                        CP("act", dst[0:64, bs], pb[gi][0:64, :], [PB[gi]], [(dk, blk)])
                        ra, rak = T[2 * gi], TK[2 * gi]
                        rb, rbk = T[2 * gi + 1], TK[2 * gi + 1]
                        TT("dve", ra[0:16, :], pb[gi][0:16, :], rope[:, 0, bs], ALU.mult, [PB[gi], "rope"], [rak])
                        CP("act", rb[0:16, :], pb[gi][64:80, :], [PB[gi]], [rbk])
                        TT("dve", rb[0:16, :], rb[0:16, :], rope[:, 1, bs], ALU.mult, [rbk, "rope"], [rbk])
                        TT("dve", dst[0:16, bs], ra[0:16, :], rb[0:16, :], ALU.add, [rak, rbk], [(dk, blk)])
                pairs = []
                for qb in range(4):
                    q0 = qb * 512
                    kbs = [kb for kb in range(16)
                           if kb * 128 >= q0 - 1151 and kb * 128 <= q0 + 1535]
                    for ki, kb in enumerate(kbs):
                        pairs.append((qb, ki, kb, ki == len(kbs) - 1))

                def emit_S(i):
                    qb, ki, kb, lastk = pairs[i]
                    q0 = qb * 512
                    ms = q0 - kb * 128 - OFFMIN
                    assert 0 <= ms and ms + 512 <= AMW
                    b = i % 4
                    sb_ = (3, 4, 0, 1)[b]
                    psS, psSk = pb[sb_], PB[sb_]
                    MM(psS[:, :], kT[:, kb * 128:(kb + 1) * 128], qT[:, q0:q0 + 512], True, True,
                       [("kT", kb // 4), ("qT", qb)], [psSk])
                    ACT(pt[b][:], psS[:, :], AF.Exp, [psSk], [PTK[b]], scale=0.125)
                    TT("dve", pm[b][:], pt[b][:], amask[:, ms:ms + 512], ALU.mult,
                       [PTK[b], "amask"], [PMK[b]])

                def emit_PV(i):
                    qb, ki, kb, lastk = pairs[i]
                    q0 = qb * 512
                    b = i % 4
                    nb = 5 if qb % 2 == 0 else 2
                    psN, psNk = pb[nb], PB[nb]
                    MM(psN[:, :], Va[:, kb, :], pm[b][:], ki == 0, lastk,
                       [("Va", kb // 4), PMK[b]], [psNk])
                    if lastk:
                        ACT(rc[:], psN[64:128, :], AF.Ln, [psNk], ["rc"])
                        ACT(rc[:], rc[:], AF.Exp, ["rc"], ["rc"], scale=-1.0)
                        pr = slice((h % 2) * 64, (h % 2) * 64 + 64)
                        TT("dve", yT[pr, 4 + h // 2, q0:q0 + 512], psN[0:64, :], rc[:], ALU.mult,
                           [psNk, "rc"], [("yT", 4 + h // 2, qb)])

                emit_S(0)
                emit_S(1)
                emit_S(2)
                for i in range(len(pairs)):
                    if i + 3 < len(pairs):
                        emit_S(i + 3)
                    emit_PV(i)
            if dbg and l == n_layers - 1:
                DMA("sp", dbg_yT[s], yT[:], [("yT", c, q) for c in range(8) for q in range(4)], ["dbg_yT"])
            barrier()
            S.reorder_on = True
            A.release(b1mark)

            wo = A.alloc("wo", [128, 8, D], BF16)
            for c in range(8):
                DMA("pool", wo[:, c, :], w_out[l, c * 128:(c + 1) * 128, :], [], ["wo"])
            g1 = A.alloc("g1", [128, D], F32)
            b1 = A.alloc("b1", [128, D], F32)
            DMA("sp", g1[:], ln1_g[l:l + 1, :].broadcast_to([128, D]), [], ["g1"])
            DMA("sp", b1[:], ln1_b[l:l + 1, :].broadcast_to([128, D]), [], ["b1"])
            wr = A.alloc("wr", [128, 8, 36], F32)
            wr_hi = A.alloc("wr_hi", [128, 8, 36], BF16)
            wr_lo = A.alloc("wr_lo", [128, 8, 36], BF16)
            wr_t = A.alloc("wr_t", [128, 8, 36], F32)
            rb = A.alloc("rb", [128, 36], F32)
            DMA("sp", wr[:, :, 0:4], r_w1[l].rearrange("(c p) n -> p c n", p=128), [], ["wr"], slow=True)
            DMA("sp", wr[:, :, 4:36], r_w2[l].rearrange("(c p) n -> p c n", p=128), [], ["wr"], slow=True)
            DMA("sp", rb[:, 0:4], r_b1[l:l + 1, :].broadcast_to([128, 4]), [], ["rb"], slow=True)
            DMA("sp", rb[:, 4:36], r_b2[l:l + 1, :].broadcast_to([128, 32]), [], ["rb"], slow=True)
            CP("dve", wr_hi[:], wr[:], ["wr"], ["wr_hi"])
            TT("dve", wr_t[:], wr[:], wr_hi[:], ALU.subtract, ["wr", "wr_hi"], ["wr_t"])
            CP("dve", wr_lo[:], wr_t[:], ["wr_t"], ["wr_lo"])
            ht = [A.alloc("ht%d" % i, [128, D], F32) for i in range(2)]
            z = [A.alloc("z%d" % i, [128, D], F32) for i in range(2)]
            hb = [A.alloc("hb%d" % i, [128, D], BF16) for i in range(2)]
            hlo = [A.alloc("hlo%d" % i, [128, D], BF16) for i in range(2)]
            hTh = A.alloc("hTh", [128, 8, 128], BF16)
            hTl = A.alloc("hTl", [128, 8, 128], BF16)
            st = A.alloc("st", [128, 2, 6], F32)
            mv = A.alloc("mv", [128, 2], F32)
            rstd = A.alloc("rstd", [128, 1], F32)
            nmr = A.alloc("nmr", [128, 1], F32)
            L = A.alloc("L", [128, 36], F32)
            sm = A.alloc("sm", [128, 16], F32)
            e4 = A.alloc("e4", [128, 4], F32)
            oh1 = A.alloc("oh1", [128, 4], F32)
            L2 = A.alloc("L2", [128, 32], F32)
            L2b = A.alloc("L2b", [128, 32], F32)
            oha = A.alloc("oha", [128, 32], F32)
            ohb = A.alloc("ohb", [128, 32], F32)
            Mbf = A.alloc("Mbf", [128, 32], BF16)
            pos = A.alloc("pos", [128, 32], F32)
            tq = A.alloc("tq", [128, 32], F32)
            sl = A.alloc("sl", [128, 2], F32)
            for ti in range(16):
                tt = s * 16 + ti
                b = ti % 2
                tsl = slice(ti * 128, (ti + 1) * 128)
                rows = slice(t0 + ti * 128, t0 + (ti + 1) * 128)
                DMA("sp", ht[b][:], src[rows, :], ["hres_%d" % tt], ["ht%d" % b])
                for half in range(2):
                    for c in range(8):
                        MM(pb[half][:, :], yT[:, c, tsl], wo[:, c, half * 512:(half + 1) * 512],
                           c == 0, c == 7, ["wo"] + [("yT", c, ti // 4)], [PB[half]])
                    STT("dve", z[b][:, half * 512:(half + 1) * 512], ht[b][:, half * 512:(half + 1) * 512],
                        ALPHA, pb[half][:, :], ALU.mult, ALU.add, ["ht%d" % b, PB[half]], ["z%d" % b])
                ln_tail(z[b], "z%d" % b, st, mv, rstd, nmr, g1, "g1", b1, "b1")
                DMA("sp", hres[rows, :], z[b][:], ["z%d" % b], ["hres_%d" % tt], sem_key="st_z%d" % b)
                CP("act", hb[b][:], z[b][:], ["z%d" % b], ["hb%d" % b])
                TT("pool", ht[b][:], z[b][:], hb[b][:], ALU.subtract, ["z%d" % b, "hb%d" % b], ["ht%d" % b])
                CP("pool", hlo[b][:], ht[b][:], ["ht%d" % b], ["hlo%d" % b])
                for c in range(8):
                    TR(pbT[0][:, c * 128:(c + 1) * 128], hb[b][:, c * 128:(c + 1) * 128],
                       ["hb%d" % b, "ident"], [PT[0]])
                for c in range(8):
                    TR(pbT[1][:, c * 128:(c + 1) * 128], hlo[b][:, c * 128:(c + 1) * 128],
                       ["hlo%d" % b, "ident"], [PT[1]])
                CP("act", hTh[:], pbT[0][:].rearrange("p (c t) -> p c t", c=8), [PT[0]], ["hTh"])
                CP("dve", hTl[:], pbT[1][:].rearrange("p (c t) -> p c t", c=8), [PT[1]], ["hTl"])
                n = 0
                for (a_, ak, w_, wk_) in ((hTh, "hTh", wr_hi, "wr_hi"), (hTl, "hTl", wr_hi, "wr_hi"),
                                          (hTh, "hTh", wr_lo, "wr_lo")):
                    for c in range(8):
                        MM(pb[2][:, 0:36], a_[:, c, :], w_[:, c, :], n == 0, n == 23, [ak, wk_], [PB[2]])
                        n += 1
                route(tt, pb[2], PB[2], rb, L, sm, e4, oh1, L2, L2b, oha, ohb, Mbf, pos, tq, sl, cnt_bc)
                for k in range(2):
                    S.op("pool", (lambda idx_, src_: (lambda e: e.indirect_dma_start(
                        out=xs, out_offset=bass.IndirectOffsetOnAxis(ap=idx_, axis=0),
                        in_=src_, in_offset=None)))(slots_i[:, tt, k:k + 1], hb[b][:, :]),
                        reads=["hb%d" % b, ("slots", tt)], writes=["xs"], dma=True, sem_key="sc_hb%d" % b, cost=6.0)
            barrier()

        A.release(lmark)
        wg = [A.alloc("wg%d" % i, [128, 8, 512], BF16) for i in range(2)]
        wu = [A.alloc("wu%d" % i, [128, 8, 512], BF16) for i in range(2)]
        wd = [A.alloc("wd%d" % i, [128, 4, D], BF16) for i in range(2)]
        xr = [A.alloc("xr%d" % i, [128, 3, D], BF16) for i in range(2)]
        xT = [A.alloc("xT%d" % i, [128, 8, CAP], BF16) for i in range(2)]
        hid = A.alloc("hid", [128, 4, CAP], BF16)
        sil = [A.alloc("sil%d" % i, [128, CAP], F32) for i in range(2)]
        yo = [A.alloc("yo%d" % i, [128, D], F32) for i in range(2)]
        for e_ in range(NE):
            b = e_ % 2
            DMA("sp", xr[b][:], xs[e_ * CAP:(e_ + 1) * CAP, :].rearrange("(j p) d -> p j d", p=128),
                ["xs"], ["xr%d" % b])
            for c in range(8):
                DMA("pool", wg[b][:, c, :], w_gate[l, e_, c * 128:(c + 1) * 128, :], [], ["wg%d" % b])
                DMA("pool", wu[b][:, c, :], w_up[l, e_, c * 128:(c + 1) * 128, :], [], ["wu%d" % b])
            for f in range(4):
                DMA("pool", wd[b][:, f, :], w_down[l, e_, f * 128:(f + 1) * 128, :], [], ["wd%d" % b])
            for j in range(3):
                tb = j % 2
                for c in range(8):
                    TR(pbT[tb][:, c * 128:(c + 1) * 128], xr[b][:, j, c * 128:(c + 1) * 128],
                       ["xr%d" % b, "ident"], [PT[tb]])
                CP("act" if j % 2 == 0 else "dve", xT[b][:, :, j * 128:(j + 1) * 128],
                   pbT[tb][:].rearrange("p (c t) -> p c t", c=8), [PT[tb]], ["xT%d" % b])
            for f in range(4):
                fb = f % 2
                pg, pgk = pb[fb], PB[fb]
                pu, puk = pb[2 + fb], PB[2 + fb]
                for c in range(8):
                    MM(pg[:, 0:CAP], wg[b][:, c, f * 128:(f + 1) * 128], xT[b][:, c, :], c == 0, c == 7,
                       ["wg%d" % b, "xT%d" % b], [pgk])
                for c in range(8):
                    MM(pu[:, 0:CAP], wu[b][:, c, f * 128:(f + 1) * 128], xT[b][:, c, :], c == 0, c == 7,
                       ["wu%d" % b, "xT%d" % b], [puk])
                ACT(sil[fb][:], pg[:, 0:CAP], AF.Silu, [pgk], ["sil%d" % fb])
                TT("dve", hid[:, f, :], sil[fb][:], pu[:, 0:CAP], ALU.mult, ["sil%d" % fb, puk], [("hid", f)])
            for j in range(3):
                yb = j % 2
                for half in range(2):
                    py, pyk = pb[4 + half], PB[4 + half]
                    for f in range(4):
                        MM(py[:, :], hid[:, f, j * 128:(j + 1) * 128], wd[b][:, f, half * 512:(half + 1) * 512],
                           f == 0, f == 3, [("hid", f), "wd%d" % b], [pyk])
                    CP("act" if half == 0 else "dve", yo[yb][:, half * 512:(half + 1) * 512], py[:, :],
                       [pyk], ["yo%d" % yb])
                r0 = e_ * CAP + j * 128
                DMA("sp", ys[r0:r0 + 128, :], yo[yb][:], ["yo%d" % yb], ["ys"], sem_key="st_yo%d" % yb)
        barrier()

        A.release(lmark)
        g2 = A.alloc("g2", [128, D], F32)
        b2 = A.alloc("b2", [128, D], F32)
        DMA("sp", g2[:], ln2_g[l:l + 1, :].broadcast_to([128, D]), [], ["g2"])
        DMA("sp", b2[:], ln2_b[l:l + 1, :].broadcast_to([128, D]), [], ["b2"])
        ht = [A.alloc("ht%d" % i, [128, D], F32) for i in range(2)]
        yg = [A.alloc("yg%d" % i, [128, 2, D], F32) for i in range(2)]
        z = [A.alloc("z%d" % i, [128, D], F32) for i in range(2)]
        st = A.alloc("st", [128, 2, 6], F32)
        mv = A.alloc("mv", [128, 2], F32)
        rstd = A.alloc("rstd", [128, 1], F32)
        nmr = A.alloc("nmr", [128, 1], F32)
        dst = out if last else hres
        for tt in range(NTT):
            b = tt % 2
            rows = slice(tt * 128, (tt + 1) * 128)
            DMA("sp", ht[b][:], hres[rows, :], ["hres_%d" % tt], ["ht%d" % b])
            for k in range(2):
                S.op("pool", (lambda idx_, dst_: (lambda e: e.indirect_dma_start(
                    out=dst_, out_offset=None, in_=ys,
                    in_offset=bass.IndirectOffsetOnAxis(ap=idx_, axis=0))))(slots_i[:, tt, k:k + 1], yg[b][:, k, :]),
                    reads=["ys", ("slots", tt)], writes=["yg%d" % b], dma=True, cost=8.0)
            ACT(z[b][:], ht[b][:], AF.Identity, ["ht%d" % b], ["z%d" % b], scale=ALPHA)
            for k in range(2):
                STT("dve", z[b][:], yg[b][:, k, :], gates[:, tt, k:k + 1], z[b][:],
                    ALU.mult, ALU.add, ["yg%d" % b, ("gates", tt), "z%d" % b], ["z%d" % b])
            ln_tail(z[b], "z%d" % b, st, mv, rstd, nmr, g2, "g2", b2, "b2")
            DMA("sp", dst[rows, :], z[b][:], ["z%d" % b], ["out" if last else "hres_%d" % tt],
                sem_key="st_z%d" % b)
        barrier()

    def ln_tail(zt, zk, st, mv, rstd, nmr, g, gk, b_, bk):
        for half in range(2):
            S.op("dve", (lambda o_, i_: (lambda e: e.bn_stats(o_, i_)))(st[:, half, :], zt[:, half * 512:(half + 1) * 512]),
                 reads=[zk], writes=["st"], cost=0.7)
        S.op("dve", lambda e: e.bn_aggr(mv[:], st[:].rearrange("p a b -> p (a b)")), reads=["st"], writes=["mv"])
        ACT(rstd[:], mv[:, 1:2], AF.Ln, ["mv"], ["rstd"], bias=LN_EPS)
        ACT(rstd[:], rstd[:], AF.Exp, ["rstd"], ["rstd"], scale=-0.5)
        STT("dve", nmr[:], mv[:, 0:1], -1.0, rstd[:], ALU.mult, ALU.mult, ["mv", "rstd"], ["nmr"])
        ACT(zt[:], zt[:], AF.Identity, [zk, "rstd", "nmr"], [zk], scale=rstd[:, 0:1], bias=nmr[:, 0:1])
        TT("pool", zt[:], zt[:], g[:], ALU.mult, [zk, gk], [zk])
        TT("dve", zt[:], zt[:], b_[:], ALU.add, [zk, bk], [zk])

    def route(tt, pl, plk, rb, L, sm, e4, oh1, L2, L2b, oha, ohb, Mbf, pos, tq, sl, cnt_bc):
        TT("dve", L[:], pl[:, 0:36], rb[:], ALU.add, [plk, "rb"], ["L"])
        m1, nm1, s1, pg_, ma, mb, dd, ga = (sm[:, i:i + 1] for i in range(8))
        S.op("dve", lambda e: e.reduce_max(m1, L[:, 0:4], AX.X), reads=["L"], writes=["sm0"])
        TS("dve", oh1[:], L[:, 0:4], m1, None, ALU.is_equal, None, ["L", "sm0"], ["oh1"])
        TS("dve", nm1, m1, -1.0, None, ALU.mult, None, ["sm0"], ["sm1"])
        ACT(e4[:], L[:, 0:4], AF.Exp, ["L", "sm1"], ["e4"], bias=nm1)
        S.op("dve", lambda e: e.reduce_sum(s1, e4[:], AX.X), reads=["e4"], writes=["sm2"])
        RECIP(pg_, s1, ["sm2"], ["sm3"])
        TS("dve", e4[:], oh1[:], BIG, -BIG, ALU.mult, ALU.add, ["oh1", "e4"], ["e4"])
        TT("dve", L2[:].rearrange("p (g e) -> p g e", g=4), L[:, 4:36].rearrange("p (g e) -> p g e", g=4),
           e4[:].rearrange("p (g o) -> p g o", o=1).broadcast_to([128, 4, 8]), ALU.add, ["L", "e4"], ["L2"])
        S.op("dve", lambda e: e.reduce_max(ma, L2[:], AX.X), reads=["L2"], writes=["sm4"])
        TS("dve", oha[:], L2[:], ma, None, ALU.is_equal, None, ["L2", "sm4"], ["oha"])
        STT("dve", L2b[:], oha[:], -BIG, L2[:], ALU.mult, ALU.add, ["oha", "L2"], ["L2b"])
        S.op("dve", lambda e: e.reduce_max(mb, L2b[:], AX.X), reads=["L2b"], writes=["sm5"])
        TS("dve", ohb[:], L2b[:], mb, None, ALU.is_equal, None, ["L2b", "sm5"], ["ohb"])
        TT("dve", dd, mb, ma, ALU.subtract, ["sm4", "sm5"], ["sm6"])
        ACT(dd, dd, AF.Exp, ["sm6"], ["sm6"])
        TS("dve", dd, dd, 1.0, None, ALU.add, None, ["sm6"], ["sm6"])
        RECIP(ga, dd, ["sm6"], ["sm7"])
        TT("dve", gates[:, tt, 0:1], ga, pg_, ALU.mult, ["sm7", "sm3"], [("gates", tt)])
        TT("dve", gates[:, tt, 1:2], pg_, gates[:, tt, 0:1], ALU.subtract, ["sm3", ("gates", tt)], [("gates", tt)])
        TT("dve", Mbf[:], oha[:], ohb[:], ALU.add, ["oha", "ohb"], ["Mbf"])
        MM(pb[3][:, 0:32], ustrict[:], Mbf[:], True, True, ["ustrict", "Mbf"], [PB[3]])
        MM(pb[4][:, 0:32], ones_bf[:], Mbf[:], True, True, ["ones_bf", "Mbf"], [PB[4]])
        TT("dve", pos[:], pb[3][:, 0:32], cnt_bc[:], ALU.add, [PB[3], "cnt_bc"], ["pos"])
        TT("dve", cnt_bc[:], cnt_bc[:], pb[4][:, 0:32], ALU.add, ["cnt_bc", PB[4]], ["cnt_bc"])
        TT("dve", pos[:], pos[:], ecap[:], ALU.add, ["pos", "ecap"], ["pos"])
        TT("dve", tq[:], pos[:], oha[:], ALU.mult, ["pos", "oha"], ["tq"])
        S.op("dve", lambda e: e.reduce_sum(sl[:, 0:1], tq[:], AX.X), reads=["tq"], writes=["sl"])
        TT("dve", tq[:], pos[:], ohb[:], ALU.mult, ["pos", "ohb", "sl"], ["tq"])
        S.op("dve", lambda e: e.reduce_sum(sl[:, 1:2], tq[:], AX.X), reads=["tq"], writes=["sl"])
        TS("dve", sl[:], sl[:], 0.0, float(NE * CAP - 1), ALU.max, ALU.min, ["sl"], ["sl"])
        CP("dve", slots_i[:, tt, :], sl[:], ["sl"], [("slots", tt)])

    for l in range(n_layers):
        layer(l)
    import os
    S.emit(final_keys=["out"], reorder=os.environ.get("NOREORDER") is None)
    return nc


def _consts():
    bf = ml_dtypes.bfloat16
    ident = np.eye(128, dtype=np.float32).astype(bf)
    p = np.arange(128)[:, None]
    j = np.arange(AMW)[None, :]
    dl = j - p + OFFMIN
    ad = np.abs(dl)
    cnt = (ad <= 64).astype(np.float32) + ((dl % 4 == 0) & (ad <= 256)) + ((dl % 16 == 0) & (ad <= 1024))
    amask = cnt.astype(bf)
    s = np.arange(128)[:, None]
    t = np.arange(128)[None, :]
    same = (s // 64) == (t // 64)
    hm = np.stack([(same & (s <= t)), (same & (s >= t))], axis=1).astype(np.float32).astype(bf)
    ustrict = (s < t).astype(np.float32).astype(bf)
    rstart = np.ones((128, 512), np.float32)
    rstart[:, ::64] = 0.0
    half = 8
    inv_freq = (500000.0 ** (-np.arange(half, dtype=np.float32) / half)).astype(np.float32)
    pos = np.arange(S_, dtype=np.float32)
    ang = (pos[None, :] * inv_freq[:, None]).astype(np.float32)
    cos = np.cos(ang).astype(np.float32)
    sin = np.sin(ang).astype(np.float32)
    rope = np.zeros((16, 2, S_), np.float32)
    rope[0:8, 0] = cos
    rope[8:16, 0] = cos
    rope[0:8, 1] = -sin
    rope[8:16, 1] = sin
    ecap = np.tile((np.arange(NE, dtype=np.float32) * CAP)[None, :], (128, 1))
    return {"c_ident": ident, "c_amask": amask, "c_hmask": np.ascontiguousarray(hm), "c_ustrict": ustrict,
            "c_rstart": rstart, "c_rope": rope, "c_ecap": ecap}


_NC_CACHE = {}


def kernel(**inputs):
    if "nc" not in _NC_CACHE:
        _NC_CACHE["nc"] = build()
    nc = _NC_CACHE["nc"]
    consts = _consts()
    x = np.ascontiguousarray(inputs["x"], dtype=np.float32).reshape(NCORES, TOK, D)
    shared = {k: np.ascontiguousarray(v) for k, v in inputs.items() if k != "x"}
    in_maps = []
    for c in range(NCORES):
        m = {"x": x[c]}
        m.update(shared)
        m.update(consts)
        in_maps.append(m)
    res = run_bass_kernel_spmd(nc, in_maps, core_ids=list(range(NCORES)))
    o = np.stack([np.asarray(r["out"], dtype=np.float32) for r in res.results], axis=0)
    return o.reshape(16, S_, D)
```

```python
import contextlib
import numpy as np
import ml_dtypes
import concourse.bass as bass
import concourse.mybir as mybir
from concourse.bass_utils import run_bass_kernel_spmd

F32 = mybir.dt.float32
BF16 = mybir.dt.bfloat16
I32 = mybir.dt.int32
AF = mybir.ActivationFunctionType
ALU = mybir.AluOpType
AX = mybir.AxisListType

NCORES = 8
S_ = 2048
D = 1024
TOK = 2 * S_
NTT = TOK // 128
CAP = 384
NE = 32
ALPHA = 4.0 ** 0.25
LN_EPS = 1e-5
RMS_EPS = 1e-6
OFFMIN = -1408
AMW = 1024 - OFFMIN + 512
BIG = 1.0e4


class _Op:
    __slots__ = ("eng", "fn", "deps", "odeps", "is_dma", "dkey", "sig", "val", "idx", "cost", "tag", "grp")


class Sched:
    ENGS = ("pe", "act", "dve", "pool", "sp")
    LAT = 0.25

    def __init__(self, nc):
        self.nc = nc
        self.ops = []
        self.last_writer = {}
        self.readers = {}
        self.dma_count = {}
        self.last_dma = {}
        self.fixed = []
        self.pool_dmas = []
        self.reorder_on = True
        self.seg_flags = []

    def op(self, eng, fn, reads=(), writes=(), dma=False, sem_key=None, force=False, cost=0.3):
        o = _Op()
        o.eng = eng
        o.fn = fn
        o.is_dma = dma
        o.idx = len(self.ops)
        o.sig = False
        o.val = None
        o.dkey = None
        o.cost = cost
        o.grp = None
        o.tag = "%s r=%s w=%s" % ("DMA" if dma else "", list(reads)[:3], list(writes)[:2])
        odeps = set()
        if dma:
            o.dkey = sem_key if sem_key is not None else writes[0]
            self.dma_count[o.dkey] = self.dma_count.get(o.dkey, 0) + 1
            o.val = 16 * self.dma_count[o.dkey]
            if o.dkey in self.last_dma:
                odeps.add(self.last_dma[o.dkey])
            self.last_dma[o.dkey] = o.idx
        deps = set()
        for k in reads:
            for j in self.last_writer.get(k, ()):
                deps.add(j)
        for k in writes:
            ws = self.last_writer.get(k, [])
            rs = self.readers.get(k, [])
            if (dma and not force and not rs and ws
                    and all(self.ops[j].is_dma for j in ws)):
                self.last_writer[k] = ws + [o.idx]
            else:
                for j in ws:
                    deps.add(j)
                for j in rs:
                    p = self.ops[j]
                    deps.add(j)
                self.last_writer[k] = [o.idx]
                self.readers[k] = []
        fdeps = []
        for j in deps:
            p = self.ops[j]
            if p.fn is None:
                continue
            if p.eng == "pe" and eng == "pe" and not p.is_dma and not dma:
                odeps.add(j)
                continue
            fdeps.append(j)
        o.deps = fdeps
        o.odeps = list(odeps)
        for j in fdeps:
            self.ops[j].sig = True
        for k in reads:
            if k not in writes:
                self.readers.setdefault(k, []).append(o.idx)
        self.ops.append(o)
        return o

    def fence(self):
        n = len(self.ops)
        self.fixed.append((n, n))
        self.seg_flags.append(self.reorder_on)

    def barrier(self, fn_tiny):
        keys = list(set(list(self.last_writer.keys()) + list(self.readers.keys())))
        keys = [k for k in keys if k != "__bar"]
        a = len(self.ops)
        self.op("sp", fn_tiny, reads=[], writes=keys + ["__bar"], dma=True,
                sem_key="__bar", force=True, cost=2.0)
        bw = self.last_writer["__bar"]
        self.last_writer = {"__bar": bw}
        self.readers = {}
        for e in ("pe", "act", "dve", "pool"):
            self.op(e, None, reads=["__bar"], cost=0.05)
        self.fixed.append((a, len(self.ops)))
        self.seg_flags.append(self.reorder_on)

    def _schedule_segment(self, a, b, order):
        import heapq
        ops = self.ops
        n = b - a
        if n == 0:
            return
        succ = [[] for _ in range(n)]
        ndep = [0] * n
        import os
        chain = set(os.environ.get("CHAIN_ENGS", "").split(","))
        lastop = {}
        for i in range(a, b):
            o = ops[i]
            ds = set(j for j in list(o.deps) + list(o.odeps) if j >= a)
            if o.eng in chain:
                if o.eng in lastop:
                    ds.add(lastop[o.eng])
                    if lastop[o.eng] not in o.deps and lastop[o.eng] not in o.odeps:
                        o.odeps.append(lastop[o.eng])
                lastop[o.eng] = i
            ndep[i - a] = len(ds)
            for j in ds:
                succ[j - a].append(i)
        bl = [0.0] * n
        for i in range(b - 1, a - 1, -1):
            m = 0.0
            for sidx in succ[i - a]:
                if bl[sidx - a] > m:
                    m = bl[sidx - a]
            bl[i - a] = m + ops[i].cost
        ready_t = [0.0] * n
        fin = [0.0] * n
        efree = {e: 0.0 for e in self.ENGS}
        avail = {e: [] for e in self.ENGS}
        for i in range(a, b):
            if ndep[i - a] == 0:
                avail[ops[i].eng].append(i)
        left = n
        open_multi = None
        while left:
            best = None
            for e in self.ENGS:
                av = avail[e]
                if e == "pe" and open_multi is not None:
                    av = [i for i in av if ops[i].grp is None or ops[i].grp[0] == open_multi]
                if not av:
                    continue
                mn = min(ready_t[i - a] for i in av)
                t = max(efree[e], mn)
                c = None
                for i in av:
                    if ready_t[i - a] <= t + 1e-9:
                        k = (-bl[i - a], i)
                        if c is None or k < c[0]:
                            c = (k, i)
                if best is None or t < best[0]:
                    best = (t, e, c[1])
            if best is None:
                assert open_multi is not None
                open_multi = None
                continue
            t, e, i = best
            avail[e].remove(i)
            o = ops[i]
            if e == "pe" and o.grp is not None:
                open_multi = None if o.grp[1] else o.grp[0]
            if o.is_dma:
                efree[e] = t + (1.0 if e == "pool" else 0.06)
            else:
                efree[e] = t + o.cost
            fin[i - a] = t + o.cost
            order[e].append(i)
            left -= 1
            for sidx in succ[i - a]:
                so = ops[sidx]
                if i in so.deps:
                    r = fin[i - a] + self.LAT
                else:
                    r = t
                if r > ready_t[sidx - a]:
                    ready_t[sidx - a] = r
                ndep[sidx - a] -= 1
                if ndep[sidx - a] == 0:
                    avail[so.eng].append(sidx)

    def _check(self, order):
        ops = self.ops
        sem = {}
        ptr = {e: 0 for e in self.ENGS}
        total = sum(len(v) for v in order.values())
        done = 0
        while done < total:
            prog = False
            for e in self.ENGS:
                while ptr[e] < len(order[e]):
                    o = ops[order[e][ptr[e]]]
                    ok = True
                    for j in o.deps:
                        p = ops[j]
                        k = ("d", p.dkey) if p.is_dma else ("e", p.eng)
                        if sem.get(k, 0) < p.val:
                            ok = False
                            break
                    if not ok:
                        break
                    if o.fn is not None:
                        if o.is_dma:
                            k = ("d", o.dkey)
                            sem[k] = sem.get(k, 0) + 16
                            assert sem[k] == o.val, ("dma order", o.dkey, sem[k], o.val)
                        elif o.sig:
                            k = ("e", o.eng)
                            sem[k] = sem.get(k, 0) + 1
                            assert sem[k] == o.val
                    ptr[e] += 1
                    done += 1
                    prog = True
            if not prog:
                msg = []
                for e in self.ENGS:
                    if ptr[e] < len(order[e]):
                        o = ops[order[e][ptr[e]]]
                        msg.append((e, o.idx, [(j, ops[j].eng, ops[j].val, ops[j].dkey) for j in o.deps]))
                raise RuntimeError("DEADLOCK in emitted order: %r" % (msg,))

    def emit(self, final_keys=(), reorder=True):
        nc = self.nc
        self.op("sp", None, reads=list(final_keys))
        ops = self.ops
        order = {e: [] for e in self.ENGS}
        pos = 0
        import os
        segsel = os.environ.get("REORDER_SEGS")
        segsel = None if segsel is None else set(int(v) for v in segsel.split(",") if v != "")
        for si, (fa, fb) in enumerate(self.fixed + [(len(ops), len(ops))]):
            flag = self.seg_flags[si] if si < len(self.seg_flags) else True
            if reorder and flag and (segsel is None or si in segsel):
                self._schedule_segment(pos, fa, order)
            else:
                for i in range(pos, fa):
                    order[ops[i].eng].append(i)
            for i in range(fa, fb):
                order[ops[i].eng].append(i)
            pos = fb
        assert sum(len(v) for v in order.values()) == len(ops)
        for e in self.ENGS:
            c = 0
            for i in order[e]:
                o = ops[i]
                if not o.is_dma and o.sig:
                    c += 1
                    o.val = c
        dkeys = list(self.dma_count.keys())
        self._check(order)
        import os
        if os.environ.get("DUMP_ORDER"):
            with open(os.environ["DUMP_ORDER"], "w") as f:
                for e in self.ENGS:
                    f.write("=== %s\n" % e)
                    for i in order[e]:
                        o = ops[i]
                        f.write("%6d %s deps=%s\n" % (i, o.tag, sorted(o.deps)))
        with contextlib.ExitStack() as es:
            esem = {e: es.enter_context(nc.semaphore("s_" + e)) for e in self.ENGS}
            dsem = {k: es.enter_context(nc.semaphore("d%d" % i)) for i, k in enumerate(dkeys)}
            block = es.enter_context(nc.Block())

            def run(engname, eng):
                waited = {}
                for i in order[engname]:
                    o = ops[i]
                    need = {}
                    for j in o.deps:
                        p = ops[j]
                        s = dsem[p.dkey] if p.is_dma else esem[p.eng]
                        v = p.val
                        key = id(s)
                        if v > need.get(key, (None, 0))[1]:
                            need[key] = (s, v)
                    for key, (s, v) in need.items():
                        if waited.get(key, 0) >= v:
                            continue
                        eng.wait_ge(s, v)
                        waited[key] = v
                    if o.fn is None:
                        continue
                    ins = o.fn(eng)
                    if o.is_dma:
                        ins.then_inc(dsem[o.dkey], 16)
                    elif o.sig:
                        ins.then_inc(esem[o.eng], 1)

            @block.sync
            def _(e):
                run("sp", e)

            @block.scalar
            def _(e):
                run("act", e)

            @block.vector
            def _(e):
                run("dve", e)

            @block.tensor
            def _(e):
                run("pe", e)

            @block.gpsimd
            def _(e):
                run("pool", e)


class Arena:
    def __init__(self, nc, base=16512, limit=229344):
        self.nc = nc
        self.cur = base
        self.limit = limit
        self.n = 0

    def alloc(self, name, shape, dt):
        esz = 2 if dt == BF16 else 4
        nbytes = int(np.prod(shape[1:])) * esz
        nbytes = (nbytes + 63) // 64 * 64
        assert self.cur + nbytes <= self.limit, ("SBUF OOM", name, self.cur, nbytes)
        self.n += 1
        t = self.nc.alloc_sbuf_tensor_at("%s_%d" % (name, self.n), list(shape), dt, offset=self.cur)
        self.cur += nbytes
        return t

    def mark(self):
        return self.cur

    def release(self, m):
        self.cur = m


def build(n_layers=2, dbg=None):
    nc = bass.Bass("TRN2", target_bir_lowering=False)

    def din(name, shape, dt=F32):
        return nc.dram_tensor(name, list(shape), dt, kind="ExternalInput").ap()

    x = din("x", [TOK, D])
    w_in = din("w_in", [2, D, 4096])
    lb_logits = din("hg_lb_logits", [2, 2, 512])
    hg_norm_g = din("hg_norm_g", [2, 512])
    w_out = din("w_out", [2, D, D])
    ln1_g = din("ln1_g", [2, D])
    ln1_b = din("ln1_b", [2, D])
    r_w1 = din("router_w1", [2, D, 4])
    r_b1 = din("router_b1", [2, 4])
    r_w2 = din("router_w2", [2, D, 32])
    r_b2 = din("router_b2", [2, 32])
    w_gate = din("ex_w_gate", [2, NE, D, 512])
    w_up = din("ex_w_up", [2, NE, D, 512])
    w_down = din("ex_w_down", [2, NE, 512, D])
    ln2_g = din("ln2_g", [2, D])
    ln2_b = din("ln2_b", [2, D])
    c_ident = din("c_ident", [128, 128], BF16)
    c_amask = din("c_amask", [128, AMW], BF16)
    c_hmask = din("c_hmask", [128, 2, 128], BF16)
    c_ustrict = din("c_ustrict", [128, 128], BF16)
    c_rstart = din("c_rstart", [128, 512])
    c_rope = din("c_rope", [16, 2, S_])
    c_ecap = din("c_ecap", [128, NE])
    out = nc.dram_tensor("out", [TOK, D], F32, kind="ExternalOutput").ap()
    hres = nc.dram_tensor("hres", [TOK, D], F32, kind="ExternalOutput" if dbg else "Internal").ap()
    xs = nc.dram_tensor("xs", [NE * CAP, D], BF16, kind="Internal").ap()
    ys = nc.dram_tensor("ys", [NE * CAP, D], F32, kind="Internal").ap()
    bar_d = nc.dram_tensor("bar_d", [1, 16], F32, kind="Internal").ap()
    dbg_yT = nc.dram_tensor("dbg_yT", [2, 128, 8, S_], BF16, kind="ExternalOutput").ap() if dbg else None

    S = Sched(nc)
    A = Arena(nc)
    import os
    RE_HG = os.environ.get("RE_HG") is not None
    RE_ATT = os.environ.get("RE_ATT") is not None

    def nfree(ap):
        sh = list(ap.shape)
        n = 1
        for v in sh[1:]:
            n *= int(v)
        return n

    def vcost(eng, o):
        n = nfree(o)
        if eng == "act":
            return 0.2 + n / 1400.0
        if eng == "dve":
            return 0.1 + n / 1000.0
        return 0.3 + n / 600.0

    GRP = {"n": 0, "cur": {}}

    def MM(o, lhsT, rhs, start, stop, r, w):
        op_ = S.op("pe", lambda e: e.matmul(o, lhsT, rhs, start=start, stop=stop), reads=r, writes=w,
                   cost=0.04 + max(nfree(o), 64) / 1800.0)
        bank = w[0]
        if start and stop:
            return
        if start:
            GRP["n"] += 1
            GRP["cur"][bank] = GRP["n"]
        op_.grp = (GRP["cur"][bank], bool(stop))

    def TR(o, i, r, w):
        S.op("pe", lambda e: e.transpose(o, i, ident[:]), reads=r, writes=w, cost=0.1)

    def ACT(o, i, func, r, w, scale=1.0, bias=0.0):
        S.op("act", lambda e: e.activation(o, i, func, bias=bias, scale=scale), reads=r, writes=w,
             cost=vcost("act", o))

    def TT(eng, o, a, b, op, r, w):
        S.op(eng, lambda e: e.tensor_tensor(o, a, b, op), reads=r, writes=w, cost=vcost(eng, o))

    def TS(eng, o, a, s1, s2, op0, op1, r, w):
        if s2 is None:
            S.op(eng, lambda e: e.tensor_scalar(o, a, s1, None, op0), reads=r, writes=w, cost=vcost(eng, o))
        else:
            S.op(eng, lambda e: e.tensor_scalar(o, a, s1, s2, op0, op1), reads=r, writes=w, cost=vcost(eng, o))

    def STT(eng, o, a, sc, b, op0, op1, r, w):
        S.op(eng, lambda e: e.scalar_tensor_tensor(o, a, sc, b, op0, op1), reads=r, writes=w, cost=vcost(eng, o))

    def CP(eng, o, i, r, w):
        if eng == "act":
            S.op("act", lambda e: e.copy(o, i), reads=r, writes=w, cost=vcost(eng, o))
        else:
            S.op(eng, lambda e: e.tensor_copy(o, i), reads=r, writes=w, cost=vcost(eng, o))

    def RECIP(o, i, r, w):
        S.op("dve", lambda e: e.reciprocal(o, i), reads=r, writes=w, cost=0.15 + nfree(o) / 200.0)

    def MEMSET(eng, o, val, w):
        S.op(eng, lambda e: e.memset(o, val), reads=[], writes=w, cost=vcost(eng, o))

    def DMA(q, o, i, r, w, sem_key=None, slow=False):
        sh = list(o.shape)
        nb = 1
        for v in sh:
            nb *= int(v)
        nb *= 2 if o.dtype == BF16 else 4
        cost = 2.0 + nb / 100e3
        if slow:
            S.op(q, lambda e: e.dma_start(out=o, in_=i, allow_slow_non_contiguous=True),
                 reads=r, writes=w, dma=True, sem_key=sem_key, cost=cost + 3.0)
        else:
            S.op(q, lambda e: e.dma_start(out=o, in_=i), reads=r, writes=w, dma=True, sem_key=sem_key, cost=cost)

    ident = A.alloc("ident", [128, 128], BF16)
    hmask = A.alloc("hmask", [128, 2, 128], BF16)
    ustrict = A.alloc("ustrict", [128, 128], BF16)
    ones_bf = A.alloc("ones_bf", [128, 128], BF16)
    ecap = A.alloc("ecap", [128, NE], F32)
    lbl = A.alloc("lbl", [128, 2, 2, 4], F32)
    lb_t = A.alloc("lb_t", [128, 2, 4], F32)
    oml_t = A.alloc("oml_t", [128, 2, 4], F32)
    lnoml_t = A.alloc("lnoml_t", [128, 2, 4], F32)
    ng_t = A.alloc("ng_t", [128, 2, 4], F32)
    gates = A.alloc("gates", [128, NTT, 2], F32)
    slots_i = A.alloc("slots_i", [128, NTT, 2], I32)
    bar_s = A.alloc("bar_s", [1, 16], F32)

    DMA("sp", ident[:], c_ident, [], ["ident"])
    DMA("sp", hmask[:], c_hmask, [], ["hmask"])
    DMA("sp", ustrict[:], c_ustrict, [], ["ustrict"])
    DMA("sp", ecap[:], c_ecap, [], ["ecap"])
    MEMSET("dve", ones_bf[:], 1.0, ["ones_bf"])
    MEMSET("dve", bar_s[:], 0.0, ["bar_s"])
    for l in range(2):
        for d in range(2):
            DMA("sp", lbl[:, l, d, :], lb_logits[l, d, :].rearrange("(h c) -> c h", c=128),
                [], ["lbl"], slow=True)
        DMA("sp", ng_t[:, l, :], hg_norm_g[l, :].rearrange("(h c) -> c h", c=128), [], ["ng_t"], slow=True)

    def barrier():
        S.barrier(lambda e: e.dma_start(out=bar_d, in_=bar_s[:]))

    zt = A.alloc("zt", [128, D], BF16)
    MEMSET("pool", zt[:], 0.0, ["zt"])
    for j in range(NE * CAP // 128):
        DMA("pool", xs[j * 128:(j + 1) * 128, :], zt[:], ["zt"], ["xs"])

    PSUM_STATE = {}

    def psum_banks():
        if not PSUM_STATE:
            PSUM_STATE["f"] = [nc.alloc_psum_tensor("pb%d" % i, [128, 512], F32) for i in range(6)]
            PSUM_STATE["t"] = [nc.alloc_psum_tensor("pbT%d" % i, [128, 1024], BF16) for i in range(2)]
        return PSUM_STATE["f"], PSUM_STATE["t"]

    pb, pbT = psum_banks()
    PB = ["pb%d" % i for i in range(6)]
    PT = ["pbT0", "pbT1"]

    base_mark = A.mark()

    def layer(l):
        src = x if l == 0 else hres
        last = (l == n_layers - 1)
        A.release(base_mark)
        if l == 0:
            MEMSET("dve", lb_t[:], 0.0, ["lb_t"])
        else:
            tmpd = A.alloc("tmpd", [128, 2, 4], F32)
            TT("dve", tmpd[:], lbl[:, 0, :, :], lbl[:, 1, :, :], ALU.subtract, ["lbl"], ["tmpd"])
            ACT(tmpd[:], tmpd[:], AF.Exp, ["tmpd"], ["tmpd"])
            TS("dve", tmpd[:], tmpd[:], 1.0, None, ALU.add, None, ["tmpd"], ["tmpd"])
            RECIP(lb_t[:], tmpd[:], ["tmpd"], ["lb_t"])
        TS("dve", oml_t[:], lb_t[:], -1.0, 1.0, ALU.mult, ALU.add, ["lb_t"], ["oml_t"])
        ACT(lnoml_t[:], oml_t[:], AF.Ln, ["oml_t"], ["lnoml_t"])
        cnt_bc = A.alloc("cnt_bc", [128, NE], F32)
        MEMSET("dve", cnt_bc[:], 0.0, ["cnt_bc"])
        lmark = A.mark()

        for s in range(2):
            A.release(lmark)
            t0 = s * S_
            hT = A.alloc("hT", [128, 8, S_], BF16)
            yT = A.alloc("yT", [128, 8, S_], BF16)
            rope = A.alloc("rope", [16, 2, S_], F32)
            amask = A.alloc("amask", [128, AMW], BF16)
            b1mark = A.mark()
            DMA("sp", rope[:], c_rope, [], ["rope"])
            DMA("sp", amask[:], c_amask, [], ["amask"])
            xt = [A.alloc("xt%d" % i, [128, D], F32) for i in range(2)]
            xb = [A.alloc("xb%d" % i, [128, D], BF16) for i in range(2)]
            for tt in range(16):
                b = tt % 2
                DMA("sp", xt[b][:], src[t0 + tt * 128: t0 + (tt + 1) * 128, :], [], ["xt%d" % b])
                CP("act", xb[b][:], xt[b][:], ["xt%d" % b], ["xb%d" % b])
                for c in range(8):
                    TR(pbT[b][:, c * 128:(c + 1) * 128], xb[b][:, c * 128:(c + 1) * 128],
                       ["xb%d" % b, "ident"], [PT[b]])
                CP("dve", hT[:, :, tt * 128:(tt + 1) * 128],
                   pbT[b][:].rearrange("p (c t) -> p c t", c=8), [PT[b]], [("hT", tt)])
            barrier()
            A.release(b1mark)

            S.reorder_on = False
            wh = [A.alloc("wh%d" % i, [128, 8, 640], BF16) for i in range(2)]
            T = [A.alloc("T%d" % i, [128, 512], F32) for i in range(7)]
            TK = ["T%d" % i for i in range(7)]
            q32 = A.alloc("q32", [128, 512], F32)
            Qb = [A.alloc("Qb%d" % d, [128, S_], BF16) for d in range(2)]
            Kinv = [A.alloc("Kinv%d" % d, [128, S_], BF16) for d in range(2)]
            Kd = [A.alloc("Kd%d" % d, [128, S_], BF16) for d in range(2)]
            KdT = [A.alloc("KdT%d" % d, [128, 16, 128], BF16) for d in range(2)]
            Vh = A.alloc("Vh", [128, 16, 128], BF16)
            sg = A.alloc("sg", [128, S_], BF16)
            oF = A.alloc("oF", [128, S_], F32)
            Dall = A.alloc("Dall", [128, 2, 32], F32)
            S32 = [A.alloc("S32_%d" % d, [128, 128], F32) for d in range(2)]
            Sbf = [A.alloc("Sbf_%d" % d, [128, 128], BF16) for d in range(2)]
            T7 = A.alloc("T7", [128, 512], F32)
            ALIAS = {"oFa%d" % k: [("oF", 4 * k + j) for j in range(4)] for k in range(4)}

            def expand_keys(keys):
                out_ = []
                for k in keys:
                    out_ += ALIAS.get(k, [k])
                return out_

            TSET = [T[0:6], [oF[:, k * 512:(k + 1) * 512] for k in range(4)] + [T[4], T7]]
            TKSET = [TK[0:6], ["oFa0", "oFa1", "oFa2", "oFa3", TK[4], "T7"]]
            rstart = A.alloc("rstart", [128, 512], F32)
            DMA("sp", rstart[:], c_rstart, [], ["rstart"])

            for hh in range(4):
                S.fence()
                S.reorder_on = RE_HG
                w = wh[hh % 2]
                wk = "wh%d" % (hh % 2)
                for gi in range(5):
                    c0 = gi * 512 + hh * 128
                    DMA("pool", w[:, :, gi * 128:(gi + 1) * 128],
                        w_in[l, :, c0:c0 + 128].rearrange("(c p) n -> p c n", p=128), [], [wk])
                lbs = [lb_t[:, d, hh:hh + 1] for d in range(2)]
                omls = [oml_t[:, d, hh:hh + 1] for d in range(2)]
                lnomls = [lnoml_t[:, d, hh:hh + 1] for d in range(2)]
                for blk in range(4):
                    bs = slice(blk * 512, (blk + 1) * 512)
                    hkeys = [("hT", blk * 4 + j) for j in range(4)]
                    for gi, pbi in ((0, 0), (1, 1), (2, 2), (4, 3)):
                        for c in range(8):
                            MM(pb[pbi][:, :], w[:, c, gi * 128:(gi + 1) * 128], hT[:, c, bs],
                               c == 0, c == 7, [wk] + hkeys, [PB[pbi]])
                    for j in range(4):
                        ts_ = slice(blk * 512 + j * 128, blk * 512 + (j + 1) * 128)
                        for c in range(8):
                            MM(pb[4][:, j * 128:(j + 1) * 128], hT[:, c, ts_], w[:, c, 384:512],
                               c == 0, c == 7, [wk] + hkeys, [PB[4]])
                    CP("act", Vh[:, blk * 4:(blk + 1) * 4, :],
                       pb[4][:].rearrange("p (j v) -> p j v", j=4), [PB[4]], [("Vh", blk)])
                    ACT(q32[:], pb[0][:], AF.Identity, [PB[0]], ["q32"], scale=128.0 ** -0.5)
                    ACT(T[6][:], pb[3][:], AF.Exp, [PB[3]], [TK[6]], scale=-1.0)
                    ACT(T[6][:], T[6][:], AF.Ln, [TK[6]], [TK[6]], bias=1.0)
                    ACT(T[6][:], T[6][:], AF.Exp, [TK[6]], [TK[6]], scale=-1.0)
                    TT("dve", sg[:, bs], pb[3][:], T[6][:], ALU.mult, [PB[3], TK[6]], [("sg", blk)])
                    def gate_dir(d, T, TK):
                        pa = pb[1 + d]
                        pak = PB[1 + d]
                        ACT(T[0][:], pa[:], AF.Exp, [pak], [TK[0]], scale=-1.0)
                        ACT(T[1][:], T[0][:], AF.Ln, [TK[0]], [TK[1]], bias=1.0)
                        ACT(T[5][:], T[0][:], AF.Ln, [TK[0], "lb_t"], [TK[5]], scale=lbs[d], bias=1.0)
                        TT("pool", T[5][:], T[5][:], T[1][:], ALU.subtract, [TK[5], TK[1]], [TK[5]])
                        STT("dve", T[0][:], pa[:], -1.0, T[1][:], ALU.mult, ALU.subtract,
                            [pak, TK[1]], [TK[0]])
                        S.op("dve", (lambda o_, a_, b_: (lambda e: e.tensor_tensor_scan(
                            o_, a_, b_, 0.0, ALU.mult, ALU.add)))(T[2][:], rstart[:], T[5][:]),
                            reads=["rstart", TK[5]], writes=[TK[2]], cost=1.2)
                        B3 = T[2][:].rearrange("p (n t) -> p n t", t=64)
                        if d == 0:
                            Bx, Bxk = T[2], TK[2]
                            tot = B3[:, :, 63:64]
                        else:
                            TT("pool", T[3][:], T[5][:], T[2][:], ALU.subtract, [TK[5], TK[2]], [TK[3]])
                            TT("pool", T[4][:].rearrange("p (n t) -> p n t", t=64),
                               T[3][:].rearrange("p (n t) -> p n t", t=64),
                               B3[:, :, 63:64].broadcast_to([128, 8, 64]), ALU.add,
                               [TK[3], TK[2]], [TK[4]])
                            Bx, Bxk = T[4], TK[4]
                            tot = T[4][:].rearrange("p (n t) -> p n t", t=64)[:, :, 0:1]
                        ACT(Dall[:, d, blk * 8:(blk + 1) * 8].rearrange("p (n o) -> p n o", o=1), tot,
                            AF.Exp, [Bxk], [("Dall", d, blk)])
                        ACT(T[5][:], Bx[:], AF.Exp, [Bxk], [TK[5]])
                        TT("dve", Qb[d][:, bs], q32[:], T[5][:], ALU.mult, ["q32", TK[5]], [("Qb", d, blk)])
                        TT("pool", T[3][:], T[0][:], Bx[:], ALU.subtract, [TK[0], Bxk], [TK[3]])
                        ACT(Kinv[d][:, bs], T[3][:], AF.Exp, [TK[3], "lnoml_t"], [("Kinv", d, blk)],
                            bias=lnomls[d])
                        TT("pool", T[3][:].rearrange("p (n t) -> p n t", t=64),
                           T[3][:].rearrange("p (n t) -> p n t", t=64),
                           tot.broadcast_to([128, 8, 64]), ALU.add, [TK[3], Bxk], [TK[3]])
                        ACT(Kd[d][:, bs], T[3][:], AF.Exp, [TK[3], "lnoml_t"], [("Kd", d, blk)],
                            bias=lnomls[d])
                    recs = []
                    for d in range(2):
                        rec = []
                        S.op = (lambda rec_: (lambda *a, **k: rec_.append((a, k))))(rec)
                        gate_dir(d, TSET[d], TKSET[d])
                        del S.op
                        recs.append(rec)
                    for i_ in range(max(len(recs[0]), len(recs[1]))):
                        for rec in recs:
                            if i_ < len(rec):
                                a_, k_ = rec[i_]
                                k_ = dict(k_)
                                k_["reads"] = expand_keys(k_.get("reads", ()))
                                k_["writes"] = expand_keys(k_.get("writes", ()))
                                S.op(*a_, **k_)
                for d in range(2):
                    for half in range(2):
                        for j in range(8):
                            tt = half * 8 + j
                            TR(pbT[d][:, j * 128:(j + 1) * 128], Kd[d][:, tt * 128:(tt + 1) * 128],
                               [("Kd", d, tt // 4), "ident"], [PT[d]])
                        CP("act" if d == 0 else "dve", KdT[d][:, half * 8:(half + 1) * 8, :],
                           pbT[d][:].rearrange("p (j c) -> p j c", j=8), [PT[d]], [("KdT", d, half)])
                for d in range(2):
                    MEMSET("pool", S32[d][:], 0.0, [("S32", d)])
                    MEMSET("pool", Sbf[d][:], 0.0, [("Sbf", d)])
                for tt in range(16):
                    for d in range(2):
                        blk = tt // 4
                        tsl = slice(tt * 128, (tt + 1) * 128)
                        pa_i = (2 * tt + d) % 2
                        MM(pb[pa_i][:, 0:128], Kinv[d][:, tsl], Qb[d][:, tsl], True, True,
                           [("Kinv", d, blk), ("Qb", d, blk)], [PB[pa_i]])
                        TT("dve", Kinv[d][:, tsl], pb[pa_i][:, 0:128], hmask[:, d, :], ALU.mult,
                           [PB[pa_i], "hmask"], [("Kinv", d, blk)])
                for i in range(16):
                    tts = [i, 15 - i]
                    for d in range(2):
                        tt = tts[d]
                        MM(pb[2 + d][:, 0:128], Vh[:, tt, :], Kinv[d][:, tt * 128:(tt + 1) * 128], True, False,
                           [("Vh", tt // 4), ("Kinv", d, tt // 4)], [PB[2 + d]])
                    for ci in range(2):
                        for d in range(2):
                            tt = tts[d]
                            blk = tt // 4
                            ch = ci if d == 0 else 1 - ci
                            n = tt * 2 + ch
                            csl = slice(tt * 128 + ch * 64, tt * 128 + (ch + 1) * 64)
                            prow = slice(ch * 64, (ch + 1) * 64)
                            psO, psOk = pb[2 + d], PB[2 + d]
                            psU, psUk = pb[4 + d], PB[4 + d]
                            MM(psO[:, ch * 64:(ch + 1) * 64], Sbf[d][:], Qb[d][:, csl], False, ci == 1,
                               [("Sbf", d), ("Qb", d, blk)], [psOk])
                            MM(psU[:, 0:128], KdT[d][prow, tt, :], Vh[prow, tt, :], True, True,
                               [("KdT", d, tt // 8), ("Vh", blk)], [psUk])
                            STT("dve", S32[d][:], S32[d][:], Dall[:, d, n:n + 1], psU[:, 0:128],
                                ALU.mult, ALU.add, [("S32", d), ("Dall", d, blk), psUk], [("S32", d)])
                            CP("act", Sbf[d][:], S32[d][:], [("S32", d)], [("Sbf", d)])
                    for d in range(2):
                        tt = tts[d]
                        tsl = slice(tt * 128, (tt + 1) * 128)
                        if (d == 0) == (tt <= 7):
                            CP("act", oF[:, tsl], pb[2 + d][:, 0:128], [PB[2 + d]], [("oF", tt)])
                        else:
                            TT("dve", oF[:, tsl], oF[:, tsl], pb[2 + d][:, 0:128], ALU.add,
                               [("oF", tt), PB[2 + d]], [("oF", tt)])
                def post_blk(blk, Ta, Tak, Tb, Tbk, hcol):
                    bs = slice(blk * 512, (blk + 1) * 512)
                    ok = [("oF", blk * 4 + j) for j in range(4)]
                    ACT(Ta[:], oF[:, bs], AF.Square, ok, [Tak])
                    hi = Qb[0][:, hcol * 512:(hcol + 1) * 512]
                    lo = Qb[0][:, (hcol + 1) * 512:(hcol + 2) * 512]
                    hik, lok = ("Qb", 0, hcol), ("Qb", 0, hcol + 1)
                    CP("dve", hi, Ta[:], [Tak], [hik])
                    TT("dve", Tb[:], Ta[:], hi, ALU.subtract, [Tak, hik], [Tbk])
                    CP("dve", lo, Tb[:], [Tbk], [lok])
                    MM(pb[0][:, :], ones_bf[:], hi, True, False, ["ones_bf", hik], [PB[0]])
                    MM(pb[0][:, :], ones_bf[:], lo, False, True, ["ones_bf", lok], [PB[0]])
                    ACT(Tb[:], pb[0][:, :], AF.Ln, [PB[0]], [Tbk], scale=1.0 / 128.0, bias=RMS_EPS)
                    ACT(Tb[:], Tb[:], AF.Exp, [Tbk], [Tbk], scale=-0.5)
                    STT("dve", Ta[:], oF[:, bs], ng_t[:, l, hh:hh + 1], Tb[:], ALU.mult, ALU.mult,
                        ok + ["ng_t", Tbk], [Tak])
                    TT("dve", yT[:, hh, bs], Ta[:], sg[:, bs], ALU.mult, [Tak, ("sg", blk)],
                       [("yT", hh, blk)])

                class _Dummy:
                    grp = None

                for bp in range(2):
                    recs = []
                    for par in range(2):
                        rec = []
                        S.op = (lambda rec_: (lambda *a, **k: (rec_.append((a, k)), _Dummy())[1]))(rec)
                        if par == 0:
                            post_blk(2 * bp, T[5], TK[5], T[6], TK[6], 0)
                        else:
                            post_blk(2 * bp + 1, T[0], TK[0], T[1], TK[1], 2)
                        del S.op
                        recs.append(rec)
                    for i_ in range(len(recs[0]) + 3):
                        if i_ < len(recs[0]):
                            a_, k_ = recs[0][i_]
                            S.op(*a_, **k_)
                        j_ = i_ - 3
                        if 0 <= j_ < len(recs[1]):
                            a_, k_ = recs[1][j_]
                            S.op(*a_, **k_)

            wa = [A.alloc("wa%d" % i, [128, 8, 224], BF16) for i in range(2)]
            qT = A.alloc("qT", [128, S_], BF16)
            kT = A.alloc("kT", [128, S_], BF16)
            Va = A.alloc("Va", [128, 16, 128], BF16)
            pt = [A.alloc("pt%d" % i, [128, 512], BF16) for i in range(3)] + [T[6].bitcast(BF16)[:, 0:512]]
            PTK = ["pt0", "pt1", "pt2", TK[6]]
            pm = [A.alloc("pm%d" % i, [128, 512], BF16) for i in range(3)] + [q32.bitcast(BF16)[:, 0:512]]
            PMK = ["pm0", "pm1", "pm2", "q32"]
            rc = A.alloc("rc", [64, 512], F32)
            r1 = rc
            MEMSET("pool", Va[:], 1.0, [("Va", b) for b in range(4)])
            MEMSET("pool", qT[:], 0.0, [("qT", b) for b in range(4)])
            MEMSET("pool", kT[:], 0.0, [("kT", b) for b in range(4)])
            for h in range(8):
                S.fence()
                S.reorder_on = RE_ATT
                w = wa[h % 2]
                wk = "wa%d" % (h % 2)
                for gi in range(3):
                    c0 = 2560 + gi * 512 + h * 64
                    DMA("pool", w[:, :, gi * 80:gi * 80 + 64],
                        w_in[l, :, c0:c0 + 64].rearrange("(c p) n -> p c n", p=128), [], [wk])
                for gi in range(2):
                    CP("pool", w[:, :, gi * 80 + 64:gi * 80 + 72], w[:, :, gi * 80 + 8:gi * 80 + 16], [wk], [wk])
                    CP("pool", w[:, :, gi * 80 + 72:gi * 80 + 80], w[:, :, gi * 80 + 0:gi * 80 + 8], [wk], [wk])
                for blk in range(4):
                    bs = slice(blk * 512, (blk + 1) * 512)
                    hkeys = [("hT", blk * 4 + j) for j in range(4)]
                    for gi in range(2):
                        for c in range(8):
                            MM(pb[gi][0:80, :], w[:, c, gi * 80:(gi + 1) * 80], hT[:, c, bs],
                               c == 0, c == 7, [wk] + hkeys, [PB[gi]])
                    for j in range(4):
                        ts_ = slice(blk * 512 + j * 128, blk * 512 + (j + 1) * 128)
                        for c in range(8):
                            MM(pb[2][:, j * 64:(j + 1) * 64], hT[:, c, ts_], w[:, c, 160:224],
                               c == 0, c == 7, [wk] + hkeys, [PB[2]])
                    CP("act", Va[:, blk * 4:(blk + 1) * 4, 0:64],
                       pb[2][:, 0:256].rearrange("p (j v) -> p j v", j=4), [PB[2]], [("Va", blk)])
                    for gi, dst, dk in ((0, qT, "qT"), (1, kT, "kT")):
                        CP("act", dst[0:64, bs], pb[gi][0:64, :], [PB[gi]], [(dk, blk)])
                        ra, rak = T[2 * gi], TK[2 * gi]
                        rb, rbk = T[2 * gi + 1], TK[2 * gi + 1]
                        TT("dve", ra[0:16, :], pb[gi][0:16, :], rope[:, 0, bs], ALU.mult, [PB[gi], "rope"], [rak])
                        CP("act", rb[0:16, :], pb[gi][64:80, :], [PB[gi]], [rbk])
                        TT("dve", rb[0:16, :], rb[0:16, :], rope[:, 1, bs], ALU.mult, [rbk, "rope"], [rbk])
                        TT("dve", dst[0:16, bs], ra[0:16, :], rb[0:16, :], ALU.add, [rak, rbk], [(dk, blk)])
                pairs = []
                for qb in range(4):
                    q0 = qb * 512
                    kbs = [kb for kb in range(16)
                           if kb * 128 >= q0 - 1151 and kb * 128 <= q0 + 1535]
                    for ki, kb in enumerate(kbs):
                        pairs.append((qb, ki, kb, ki == len(kbs) - 1))

                def emit_S(i):
                    qb, ki, kb, lastk = pairs[i]
                    q0 = qb * 512
                    ms = q0 - kb * 128 - OFFMIN
                    assert 0 <= ms and ms + 512 <= AMW
                    b = i % 4
                    sb_ = (3, 4, 0, 1)[b]
                    psS, psSk = pb[sb_], PB[sb_]
                    MM(psS[:, :], kT[:, kb * 128:(kb + 1) * 128], qT[:, q0:q0 + 512], True, True,
                       [("kT", kb // 4), ("qT", qb)], [psSk])
                    ACT(pt[b][:], psS[:, :], AF.Exp, [psSk], [PTK[b]], scale=0.125)
                    TT("dve", pm[b][:], pt[b][:], amask[:, ms:ms + 512], ALU.mult,
                       [PTK[b], "amask"], [PMK[b]])

                def emit_PV(i):
                    qb, ki, kb, lastk = pairs[i]
                    q0 = qb * 512
                    b = i % 4
                    nb = 5 if qb % 2 == 0 else 2
                    psN, psNk = pb[nb], PB[nb]
                    MM(psN[:, :], Va[:, kb, :], pm[b][:], ki == 0, lastk,
                       [("Va", kb // 4), PMK[b]], [psNk])
                    if lastk:
                        ACT(rc[:], psN[64:128, :], AF.Ln, [psNk], ["rc"])
                        ACT(rc[:], rc[:], AF.Exp, ["rc"], ["rc"], scale=-1.0)
                        pr = slice((h % 2) * 64, (h % 2) * 64 + 64)
                        TT("dve", yT[pr, 4 + h // 2, q0:q0 + 512], psN[0:64, :], rc[:], ALU.mult,
                           [psNk, "rc"], [("yT", 4 + h // 2, qb)])

                emit_S(0)
                emit_S(1)
                emit_S(2)
                for i in range(len(pairs)):
                    if i + 3 < len(pairs):
                        emit_S(i + 3)
                    emit_PV(i)
            if dbg and l == n_layers - 1:
                DMA("sp", dbg_yT[s], yT[:], [("yT", c, q) for c in range(8) for q in range(4)], ["dbg_yT"])
            barrier()
            S.reorder_on = True
            A.release(b1mark)

            wo = A.alloc("wo", [128, 8, D], BF16)
            for c in range(8):
                DMA("pool", wo[:, c, :], w_out[l, c * 128:(c + 1) * 128, :], [], ["wo"])
            g1 = A.alloc("g1", [128, D], F32)
            b1 = A.alloc("b1", [128, D], F32)
            DMA("sp", g1[:], ln1_g[l:l + 1, :].broadcast_to([128, D]), [], ["g1"])
            DMA("sp", b1[:], ln1_b[l:l + 1, :].broadcast_to([128, D]), [], ["b1"])
            wr = A.alloc("wr", [128, 8, 36], F32)
            wr_hi = A.alloc("wr_hi", [128, 8, 36], BF16)
            wr_lo = A.alloc("wr_lo", [128, 8, 36], BF16)
            wr_t = A.alloc("wr_t", [128, 8, 36], F32)
            rb = A.alloc("rb", [128, 36], F32)
            DMA("sp", wr[:, :, 0:4], r_w1[l].rearrange("(c p) n -> p c n", p=128), [], ["wr"], slow=True)
            DMA("sp", wr[:, :, 4:36], r_w2[l].rearrange("(c p) n -> p c n", p=128), [], ["wr"], slow=True)
            DMA("sp", rb[:, 0:4], r_b1[l:l + 1, :].broadcast_to([128, 4]), [], ["rb"], slow=True)
            DMA("sp", rb[:, 4:36], r_b2[l:l + 1, :].broadcast_to([128, 32]), [], ["rb"], slow=True)
            CP("dve", wr_hi[:], wr[:], ["wr"], ["wr_hi"])
            TT("dve", wr_t[:], wr[:], wr_hi[:], ALU.subtract, ["wr", "wr_hi"], ["wr_t"])
            CP("dve", wr_lo[:], wr_t[:], ["wr_t"], ["wr_lo"])
            ht = [A.alloc("ht%d" % i, [128, D], F32) for i in range(2)]
            z = [A.alloc("z%d" % i, [128, D], F32) for i in range(2)]
            hb = [A.alloc("hb%d" % i, [128, D], BF16) for i in range(2)]
            hlo = [A.alloc("hlo%d" % i, [128, D], BF16) for i in range(2)]
            hTh = A.alloc("hTh", [128, 8, 128], BF16)
            hTl = A.alloc("hTl", [128, 8, 128], BF16)
            st = A.alloc("st", [128, 2, 6], F32)
            mv = A.alloc("mv", [128, 2], F32)
            rstd = A.alloc("rstd", [128, 1], F32)
            nmr = A.alloc("nmr", [128, 1], F32)
            L = A.alloc("L", [128, 36], F32)
            sm = A.alloc("sm", [128, 16], F32)
            e4 = A.alloc("e4", [128, 4], F32)
            oh1 = A.alloc("oh1", [128, 4], F32)
            L2 = A.alloc("L2", [128, 32], F32)
            L2b = A.alloc("L2b", [128, 32], F32)
            oha = A.alloc("oha", [128, 32], F32)
            ohb = A.alloc("ohb", [128, 32], F32)
            Mbf = A.alloc("Mbf", [128, 32], BF16)
            pos = A.alloc("pos", [128, 32], F32)
            tq = A.alloc("tq", [128, 32], F32)
            sl = A.alloc("sl", [128, 2], F32)
            for ti in range(16):
                tt = s * 16 + ti
                b = ti % 2
                tsl = slice(ti * 128, (ti + 1) * 128)
                rows = slice(t0 + ti * 128, t0 + (ti + 1) * 128)
                DMA("sp", ht[b][:], src[rows, :], ["hres_%d" % tt], ["ht%d" % b])
                for half in range(2):
                    for c in range(8):
                        MM(pb[half][:, :], yT[:, c, tsl], wo[:, c, half * 512:(half + 1) * 512],
                           c == 0, c == 7, ["wo"] + [("yT", c, ti // 4)], [PB[half]])
                    STT("dve", z[b][:, half * 512:(half + 1) * 512], ht[b][:, half * 512:(half + 1) * 512],
                        ALPHA, pb[half][:, :], ALU.mult, ALU.add, ["ht%d" % b, PB[half]], ["z%d" % b])
                ln_tail(z[b], "z%d" % b, st, mv, rstd, nmr, g1, "g1", b1, "b1")
                DMA("sp", hres[rows, :], z[b][:], ["z%d" % b], ["hres_%d" % tt], sem_key="st_z%d" % b)
                CP("act", hb[b][:], z[b][:], ["z%d" % b], ["hb%d" % b])
                TT("pool", ht[b][:], z[b][:], hb[b][:], ALU.subtract, ["z%d" % b, "hb%d" % b], ["ht%d" % b])
                CP("pool", hlo[b][:], ht[b][:], ["ht%d" % b], ["hlo%d" % b])
                for c in range(8):
                    TR(pbT[0][:, c * 128:(c + 1) * 128], hb[b][:, c * 128:(c + 1) * 128],
                       ["hb%d" % b, "ident"], [PT[0]])
                for c in range(8):
                    TR(pbT[1][:, c * 128:(c + 1) * 128], hlo[b][:, c * 128:(c + 1) * 128],
                       ["hlo%d" % b, "ident"], [PT[1]])
                CP("act", hTh[:], pbT[0][:].rearrange("p (c t) -> p c t", c=8), [PT[0]], ["hTh"])
                CP("dve", hTl[:], pbT[1][:].rearrange("p (c t) -> p c t", c=8), [PT[1]], ["hTl"])
                n = 0
                for (a_, ak, w_, wk_) in ((hTh, "hTh", wr_hi, "wr_hi"), (hTl, "hTl", wr_hi, "wr_hi"),
                                          (hTh, "hTh", wr_lo, "wr_lo")):
                    for c in range(8):
                        MM(pb[2][:, 0:36], a_[:, c, :], w_[:, c, :], n == 0, n == 23, [ak, wk_], [PB[2]])
                        n += 1
                route(tt, pb[2], PB[2], rb, L, sm, e4, oh1, L2, L2b, oha, ohb, Mbf, pos, tq, sl, cnt_bc)
                for k in range(2):
                    S.op("pool", (lambda idx_, src_: (lambda e: e.indirect_dma_start(
                        out=xs, out_offset=bass.IndirectOffsetOnAxis(ap=idx_, axis=0),
                        in_=src_, in_offset=None)))(slots_i[:, tt, k:k + 1], hb[b][:, :]),
                        reads=["hb%d" % b, ("slots", tt)], writes=["xs"], dma=True, sem_key="sc_hb%d" % b, cost=6.0)
            barrier()

        A.release(lmark)
        wg = [A.alloc("wg%d" % i, [128, 8, 512], BF16) for i in range(2)]
        wu = [A.alloc("wu%d" % i, [128, 8, 512], BF16) for i in range(2)]
        wd = [A.alloc("wd%d" % i, [128, 4, D], BF16) for i in range(2)]
        xr = [A.alloc("xr%d" % i, [128, 3, D], BF16) for i in range(2)]
        xT = [A.alloc("xT%d" % i, [128, 8, CAP], BF16) for i in range(2)]
        hid = A.alloc("hid", [128, 4, CAP], BF16)
        sil = [A.alloc("sil%d" % i, [128, CAP], F32) for i in range(2)]
        yo = [A.alloc("yo%d" % i, [128, D], F32) for i in range(2)]
        for e_ in range(NE):
            b = e_ % 2
            DMA("sp", xr[b][:], xs[e_ * CAP:(e_ + 1) * CAP, :].rearrange("(j p) d -> p j d", p=128),
                ["xs"], ["xr%d" % b])
            for c in range(8):
                DMA("pool", wg[b][:, c, :], w_gate[l, e_, c * 128:(c + 1) * 128, :], [], ["wg%d" % b])
                DMA("pool", wu[b][:, c, :], w_up[l, e_, c * 128:(c + 1) * 128, :], [], ["wu%d" % b])
            for f in range(4):
                DMA("pool", wd[b][:, f, :], w_down[l, e_, f * 128:(f + 1) * 128, :], [], ["wd%d" % b])
            for j in range(3):
                tb = j % 2
                for c in range(8):
                    TR(pbT[tb][:, c * 128:(c + 1) * 128], xr[b][:, j, c * 128:(c + 1) * 128],
                       ["xr%d" % b, "ident"], [PT[tb]])
                CP("act" if j % 2 == 0 else "dve", xT[b][:, :, j * 128:(j + 1) * 128],
                   pbT[tb][:].rearrange("p (c t) -> p c t", c=8), [PT[tb]], ["xT%d" % b])
            for f in range(4):
                fb = f % 2
                pg, pgk = pb[fb], PB[fb]
                pu, puk = pb[2 + fb], PB[2 + fb]
                for c in range(8):
                    MM(pg[:, 0:CAP], wg[b][:, c, f * 128:(f + 1) * 128], xT[b][:, c, :], c == 0, c == 7,
                       ["wg%d" % b, "xT%d" % b], [pgk])
                for c in range(8):
                    MM(pu[:, 0:CAP], wu[b][:, c, f * 128:(f + 1) * 128], xT[b][:, c, :], c == 0, c == 7,
                       ["wu%d" % b, "xT%d" % b], [puk])
                ACT(sil[fb][:], pg[:, 0:CAP], AF.Silu, [pgk], ["sil%d" % fb])
                TT("dve", hid[:, f, :], sil[fb][:], pu[:, 0:CAP], ALU.mult, ["sil%d" % fb, puk], [("hid", f)])
            for j in range(3):
                yb = j % 2
                for half in range(2):
                    py, pyk = pb[4 + half], PB[4 + half]
                    for f in range(4):
                        MM(py[:, :], hid[:, f, j * 128:(j + 1) * 128], wd[b][:, f, half * 512:(half + 1) * 512],
                           f == 0, f == 3, [("hid", f), "wd%d" % b], [pyk])
                    CP("act" if half == 0 else "dve", yo[yb][:, half * 512:(half + 1) * 512], py[:, :],
                       [pyk], ["yo%d" % yb])
                r0 = e_ * CAP + j * 128
                DMA("sp", ys[r0:r0 + 128, :], yo[yb][:], ["yo%d" % yb], ["ys"], sem_key="st_yo%d" % yb)
        barrier()

        A.release(lmark)
        g2 = A.alloc("g2", [128, D], F32)
        b2 = A.alloc("b2", [128, D], F32)
        DMA("sp", g2[:], ln2_g[l:l + 1, :].broadcast_to([128, D]), [], ["g2"])
        DMA("sp", b2[:], ln2_b[l:l + 1, :].broadcast_to([128, D]), [], ["b2"])
        ht = [A.alloc("ht%d" % i, [128, D], F32) for i in range(2)]
        yg = [A.alloc("yg%d" % i, [128, 2, D], F32) for i in range(2)]
        z = [A.alloc("z%d" % i, [128, D], F32) for i in range(2)]
        st = A.alloc("st", [128, 2, 6], F32)
        mv = A.alloc("mv", [128, 2], F32)
        rstd = A.alloc("rstd", [128, 1], F32)
        nmr = A.alloc("nmr", [128, 1], F32)
        dst = out if last else hres
        for tt in range(NTT):
            b = tt % 2
            rows = slice(tt * 128, (tt + 1) * 128)
            DMA("sp", ht[b][:], hres[rows, :], ["hres_%d" % tt], ["ht%d" % b])
            for k in range(2):
                S.op("pool", (lambda idx_, dst_: (lambda e: e.indirect_dma_start(
                    out=dst_, out_offset=None, in_=ys,
                    in_offset=bass.IndirectOffsetOnAxis(ap=idx_, axis=0))))(slots_i[:, tt, k:k + 1], yg[b][:, k, :]),
                    reads=["ys", ("slots", tt)], writes=["yg%d" % b], dma=True, cost=8.0)
            ACT(z[b][:], ht[b][:], AF.Identity, ["ht%d" % b], ["z%d" % b], scale=ALPHA)
            for k in range(2):
                STT("dve", z[b][:], yg[b][:, k, :], gates[:, tt, k:k + 1], z[b][:],
                    ALU.mult, ALU.add, ["yg%d" % b, ("gates", tt), "z%d" % b], ["z%d" % b])
            ln_tail(z[b], "z%d" % b, st, mv, rstd, nmr, g2, "g2", b2, "b2")
            DMA("sp", dst[rows, :], z[b][:], ["z%d" % b], ["out" if last else "hres_%d" % tt],
                sem_key="st_z%d" % b)
        barrier()

    def ln_tail(zt, zk, st, mv, rstd, nmr, g, gk, b_, bk):
        for half in range(2):
            S.op("dve", (lambda o_, i_: (lambda e: e.bn_stats(o_, i_)))(st[:, half, :], zt[:, half * 512:(half + 1) * 512]),
                 reads=[zk], writes=["st"], cost=0.7)
        S.op("dve", lambda e: e.bn_aggr(mv[:], st[:].rearrange("p a b -> p (a b)")), reads=["st"], writes=["mv"])
        ACT(rstd[:], mv[:, 1:2], AF.Ln, ["mv"], ["rstd"], bias=LN_EPS)
        ACT(rstd[:], rstd[:], AF.Exp, ["rstd"], ["rstd"], scale=-0.5)
        STT("dve", nmr[:], mv[:, 0:1], -1.0, rstd[:], ALU.mult, ALU.mult, ["mv", "rstd"], ["nmr"])
        ACT(zt[:], zt[:], AF.Identity, [zk, "rstd", "nmr"], [zk], scale=rstd[:, 0:1], bias=nmr[:, 0:1])
        TT("pool", zt[:], zt[:], g[:], ALU.mult, [zk, gk], [zk])
        TT("dve", zt[:], zt[:], b_[:], ALU.add, [zk, bk], [zk])

    def route(tt, pl, plk, rb, L, sm, e4, oh1, L2, L2b, oha, ohb, Mbf, pos, tq, sl, cnt_bc):
        TT("dve", L[:], pl[:, 0:36], rb[:], ALU.add, [plk, "rb"], ["L"])
        m1, nm1, s1, pg_, ma, mb, dd, ga = (sm[:, i:i + 1] for i in range(8))
        S.op("dve", lambda e: e.reduce_max(m1, L[:, 0:4], AX.X), reads=["L"], writes=["sm0"])
        TS("dve", oh1[:], L[:, 0:4], m1, None, ALU.is_equal, None, ["L", "sm0"], ["oh1"])
        TS("dve", nm1, m1, -1.0, None, ALU.mult, None, ["sm0"], ["sm1"])
        ACT(e4[:], L[:, 0:4], AF.Exp, ["L", "sm1"], ["e4"], bias=nm1)
        S.op("dve", lambda e: e.reduce_sum(s1, e4[:], AX.X), reads=["e4"], writes=["sm2"])
        RECIP(pg_, s1, ["sm2"], ["sm3"])
        TS("dve", e4[:], oh1[:], BIG, -BIG, ALU.mult, ALU.add, ["oh1", "e4"], ["e4"])
        TT("dve", L2[:].rearrange("p (g e) -> p g e", g=4), L[:, 4:36].rearrange("p (g e) -> p g e", g=4),
           e4[:].rearrange("p (g o) -> p g o", o=1).broadcast_to([128, 4, 8]), ALU.add, ["L", "e4"], ["L2"])
        S.op("dve", lambda e: e.reduce_max(ma, L2[:], AX.X), reads=["L2"], writes=["sm4"])
        TS("dve", oha[:], L2[:], ma, None, ALU.is_equal, None, ["L2", "sm4"], ["oha"])
        STT("dve", L2b[:], oha[:], -BIG, L2[:], ALU.mult, ALU.add, ["oha", "L2"], ["L2b"])
        S.op("dve", lambda e: e.reduce_max(mb, L2b[:], AX.X), reads=["L2b"], writes=["sm5"])
        TS("dve", ohb[:], L2b[:], mb, None, ALU.is_equal, None, ["L2b", "sm5"], ["ohb"])
        TT("dve", dd, mb, ma, ALU.subtract, ["sm4", "sm5"], ["sm6"])
        ACT(dd, dd, AF.Exp, ["sm6"], ["sm6"])
        TS("dve", dd, dd, 1.0, None, ALU.add, None, ["sm6"], ["sm6"])
        RECIP(ga, dd, ["sm6"], ["sm7"])
        TT("dve", gates[:, tt, 0:1], ga, pg_, ALU.mult, ["sm7", "sm3"], [("gates", tt)])
        TT("dve", gates[:, tt, 1:2], pg_, gates[:, tt, 0:1], ALU.subtract, ["sm3", ("gates", tt)], [("gates", tt)])
        TT("dve", Mbf[:], oha[:], ohb[:], ALU.add, ["oha", "ohb"], ["Mbf"])
        MM(pb[3][:, 0:32], ustrict[:], Mbf[:], True, True, ["ustrict", "Mbf"], [PB[3]])
        MM(pb[4][:, 0:32], ones_bf[:], Mbf[:], True, True, ["ones_bf", "Mbf"], [PB[4]])
        TT("dve", pos[:], pb[3][:, 0:32], cnt_bc[:], ALU.add, [PB[3], "cnt_bc"], ["pos"])
        TT("dve", cnt_bc[:], cnt_bc[:], pb[4][:, 0:32], ALU.add, ["cnt_bc", PB[4]], ["cnt_bc"])
        TT("dve", pos[:], pos[:], ecap[:], ALU.add, ["pos", "ecap"], ["pos"])
        TT("dve", tq[:], pos[:], oha[:], ALU.mult, ["pos", "oha"], ["tq"])
        S.op("dve", lambda e: e.reduce_sum(sl[:, 0:1], tq[:], AX.X), reads=["tq"], writes=["sl"])
        TT("dve", tq[:], pos[:], ohb[:], ALU.mult, ["pos", "ohb", "sl"], ["tq"])
        S.op("dve", lambda e: e.reduce_sum(sl[:, 1:2], tq[:], AX.X), reads=["tq"], writes=["sl"])
        TS("dve", sl[:], sl[:], 0.0, float(NE * CAP - 1), ALU.max, ALU.min, ["sl"], ["sl"])
        CP("dve", slots_i[:, tt, :], sl[:], ["sl"], [("slots", tt)])

    for l in range(n_layers):
        layer(l)
    import os
    S.emit(final_keys=["out"], reorder=os.environ.get("NOREORDER") is None)
    return nc


def _consts():
    bf = ml_dtypes.bfloat16
    ident = np.eye(128, dtype=np.float32).astype(bf)
    p = np.arange(128)[:, None]
    j = np.arange(AMW)[None, :]
    dl = j - p + OFFMIN
    ad = np.abs(dl)
    cnt = (ad <= 64).astype(np.float32) + ((dl % 4 == 0) & (ad <= 256)) + ((dl % 16 == 0) & (ad <= 1024))
    amask = cnt.astype(bf)
    s = np.arange(128)[:, None]
    t = np.arange(128)[None, :]
    same = (s // 64) == (t // 64)
    hm = np.stack([(same & (s <= t)), (same & (s >= t))], axis=1).astype(np.float32).astype(bf)
    ustrict = (s < t).astype(np.float32).astype(bf)
    rstart = np.ones((128, 512), np.float32)
    rstart[:, ::64] = 0.0
    half = 8
    inv_freq = (500000.0 ** (-np.arange(half, dtype=np.float32) / half)).astype(np.float32)
    pos = np.arange(S_, dtype=np.float32)
    ang = (pos[None, :] * inv_freq[:, None]).astype(np.float32)
    cos = np.cos(ang).astype(np.float32)
    sin = np.sin(ang).astype(np.float32)
    rope = np.zeros((16, 2, S_), np.float32)
    rope[0:8, 0] = cos
    rope[8:16, 0] = cos
    rope[0:8, 1] = -sin
    rope[8:16, 1] = sin
    ecap = np.tile((np.arange(NE, dtype=np.float32) * CAP)[None, :], (128, 1))
    return {"c_ident": ident, "c_amask": amask, "c_hmask": np.ascontiguousarray(hm), "c_ustrict": ustrict,
            "c_rstart": rstart, "c_rope": rope, "c_ecap": ecap}


_NC_CACHE = {}


def kernel(**inputs):
    if "nc" not in _NC_CACHE:
        _NC_CACHE["nc"] = build()
    nc = _NC_CACHE["nc"]
    consts = _consts()
    x = np.ascontiguousarray(inputs["x"], dtype=np.float32).reshape(NCORES, TOK, D)
    shared = {k: np.ascontiguousarray(v) for k, v in inputs.items() if k != "x"}
    in_maps = []
    for c in range(NCORES):
        m = {"x": x[c]}
        m.update(shared)
        m.update(consts)
        in_maps.append(m)
    res = run_bass_kernel_spmd(nc, in_maps, core_ids=list(range(NCORES)))
    o = np.stack([np.asarray(r["out"], dtype=np.float32) for r in res.results], axis=0)
    return o.reshape(16, S_, D)
```

```python
import contextlib
import numpy as np
import ml_dtypes
import concourse.bass as bass
import concourse.mybir as mybir
from concourse.bass_utils import run_bass_kernel_spmd

F32 = mybir.dt.float32
BF16 = mybir.dt.bfloat16
I32 = mybir.dt.int32
AF = mybir.ActivationFunctionType
ALU = mybir.AluOpType
AX = mybir.AxisListType

NCORES = 8
S_ = 2048
D = 1024
TOK = 2 * S_
NTT = TOK // 128
CAP = 384
NE = 32
ALPHA = 4.0 ** 0.25
LN_EPS = 1e-5
RMS_EPS = 1e-6
OFFMIN = -1408
AMW = 1024 - OFFMIN + 512
BIG = 1.0e4


class _Op:
    __slots__ = ("eng", "fn", "deps", "odeps", "is_dma", "dkey", "sig", "val", "idx", "cost", "tag", "grp")


class Sched:
    ENGS = ("pe", "act", "dve", "pool", "sp")
    LAT = 0.25

    def __init__(self, nc):
        self.nc = nc
        self.ops = []
        self.last_writer = {}
        self.readers = {}
        self.dma_count = {}
        self.last_dma = {}
        self.fixed = []
        self.pool_dmas = []
        self.reorder_on = True
        self.seg_flags = []

    def op(self, eng, fn, reads=(), writes=(), dma=False, sem_key=None, force=False, cost=0.3):
        o = _Op()
        o.eng = eng
        o.fn = fn
        o.is_dma = dma
        o.idx = len(self.ops)
        o.sig = False
        o.val = None
        o.dkey = None
        o.cost = cost
        o.grp = None
        o.tag = "%s r=%s w=%s" % ("DMA" if dma else "", list(reads)[:3], list(writes)[:2])
        odeps = set()
        if dma:
            o.dkey = sem_key if sem_key is not None else writes[0]
            self.dma_count[o.dkey] = self.dma_count.get(o.dkey, 0) + 1
            o.val = 16 * self.dma_count[o.dkey]
            if o.dkey in self.last_dma:
                odeps.add(self.last_dma[o.dkey])
            self.last_dma[o.dkey] = o.idx
        deps = set()
        for k in reads:
            for j in self.last_writer.get(k, ()):
                deps.add(j)
        for k in writes:
            ws = self.last_writer.get(k, [])
            rs = self.readers.get(k, [])
            if (dma and not force and not rs and ws
                    and all(self.ops[j].is_dma for j in ws)):
                self.last_writer[k] = ws + [o.idx]
            else:
                for j in ws:
                    deps.add(j)
                for j in rs:
                    p = self.ops[j]
                    deps.add(j)
                self.last_writer[k] = [o.idx]
                self.readers[k] = []
        fdeps = []
        for j in deps:
            p = self.ops[j]
            if p.fn is None:
                continue
            if p.eng == "pe" and eng == "pe" and not p.is_dma and not dma:
                odeps.add(j)
                continue
            fdeps.append(j)
        o.deps = fdeps
        o.odeps = list(odeps)
        for j in fdeps:
            self.ops[j].sig = True
        for k in reads:
            if k not in writes:
                self.readers.setdefault(k, []).append(o.idx)
        self.ops.append(o)
        return o

    def fence(self):
        n = len(self.ops)
        self.fixed.append((n, n))
        self.seg_flags.append(self.reorder_on)

    def barrier(self, fn_tiny):
        keys = list(set(list(self.last_writer.keys()) + list(self.readers.keys())))
        keys = [k for k in keys if k != "__bar"]
        a = len(self.ops)
        self.op("sp", fn_tiny, reads=[], writes=keys + ["__bar"], dma=True,
                sem_key="__bar", force=True, cost=2.0)
        bw = self.last_writer["__bar"]
        self.last_writer = {"__bar": bw}
        self.readers = {}
        for e in ("pe", "act", "dve", "pool"):
            self.op(e, None, reads=["__bar"], cost=0.05)
        self.fixed.append((a, len(self.ops)))
        self.seg_flags.append(self.reorder_on)

    def _schedule_segment(self, a, b, order):
        import heapq
        ops = self.ops
        n = b - a
        if n == 0:
            return
        succ = [[] for _ in range(n)]
        ndep = [0] * n
        import os
        chain = set(os.environ.get("CHAIN_ENGS", "").split(","))
        lastop = {}
        for i in range(a, b):
            o = ops[i]
            ds = set(j for j in list(o.deps) + list(o.odeps) if j >= a)
            if o.eng in chain:
                if o.eng in lastop:
                    ds.add(lastop[o.eng])
                    if lastop[o.eng] not in o.deps and lastop[o.eng] not in o.odeps:
                        o.odeps.append(lastop[o.eng])
                lastop[o.eng] = i
            ndep[i - a] = len(ds)
            for j in ds:
                succ[j - a].append(i)
        bl = [0.0] * n
        for i in range(b - 1, a - 1, -1):
            m = 0.0
            for sidx in succ[i - a]:
                if bl[sidx - a] > m:
                    m = bl[sidx - a]
            bl[i - a] = m + ops[i].cost
        ready_t = [0.0] * n
        fin = [0.0] * n
        efree = {e: 0.0 for e in self.ENGS}
        avail = {e: [] for e in self.ENGS}
        for i in range(a, b):
            if ndep[i - a] == 0:
                avail[ops[i].eng].append(i)
        left = n
        open_multi = None
        while left:
            best = None
            for e in self.ENGS:
                av = avail[e]
                if e == "pe" and open_multi is not None:
                    av = [i for i in av if ops[i].grp is None or ops[i].grp[0] == open_multi]
                if not av:
                    continue
                mn = min(ready_t[i - a] for i in av)
                t = max(efree[e], mn)
                c = None
                for i in av:
                    if ready_t[i - a] <= t + 1e-9:
                        k = (-bl[i - a], i)
                        if c is None or k < c[0]:
                            c = (k, i)
                if best is None or t < best[0]:
                    best = (t, e, c[1])
            if best is None:
                assert open_multi is not None
                open_multi = None
                continue
            t, e, i = best
            avail[e].remove(i)
            o = ops[i]
            if e == "pe" and o.grp is not None:
                open_multi = None if o.grp[1] else o.grp[0]
            if o.is_dma:
                efree[e] = t + (1.0 if e == "pool" else 0.06)
            else:
                efree[e] = t + o.cost
            fin[i - a] = t + o.cost
            order[e].append(i)
            left -= 1
            for sidx in succ[i - a]:
                so = ops[sidx]
                if i in so.deps:
                    r = fin[i - a] + self.LAT
                else:
                    r = t
                if r > ready_t[sidx - a]:
                    ready_t[sidx - a] = r
                ndep[sidx - a] -= 1
                if ndep[sidx - a] == 0:
                    avail[so.eng].append(sidx)

    def _check(self, order):
        ops = self.ops
        sem = {}
        ptr = {e: 0 for e in self.ENGS}
        total = sum(len(v) for v in order.values())
        done = 0
        while done < total:
            prog = False
            for e in self.ENGS:
                while ptr[e] < len(order[e]):
                    o = ops[order[e][ptr[e]]]
                    ok = True
                    for j in o.deps:
                        p = ops[j]
                        k = ("d", p.dkey) if p.is_dma else ("e", p.eng)
                        if sem.get(k, 0) < p.val:
                            ok = False
                            break
                    if not ok:
                        break
                    if o.fn is not None:
                        if o.is_dma:
                            k = ("d", o.dkey)
                            sem[k] = sem.get(k, 0) + 16
                            assert sem[k] == o.val, ("dma order", o.dkey, sem[k], o.val)
                        elif o.sig:
                            k = ("e", o.eng)
                            sem[k] = sem.get(k, 0) + 1
                            assert sem[k] == o.val
                    ptr[e] += 1
                    done += 1
                    prog = True
            if not prog:
                msg = []
                for e in self.ENGS:
                    if ptr[e] < len(order[e]):
                        o = ops[order[e][ptr[e]]]
                        msg.append((e, o.idx, [(j, ops[j].eng, ops[j].val, ops[j].dkey) for j in o.deps]))
                raise RuntimeError("DEADLOCK in emitted order: %r" % (msg,))

    def emit(self, final_keys=(), reorder=True):
        nc = self.nc
        self.op("sp", None, reads=list(final_keys))
        ops = self.ops
        order = {e: [] for e in self.ENGS}
        pos = 0
        import os
        segsel = os.environ.get("REORDER_SEGS")
        segsel = None if segsel is None else set(int(v) for v in segsel.split(",") if v != "")
        for si, (fa, fb) in enumerate(self.fixed + [(len(ops), len(ops))]):
            flag = self.seg_flags[si] if si < len(self.seg_flags) else True
            if reorder and flag and (segsel is None or si in segsel):
                self._schedule_segment(pos, fa, order)
            else:
                for i in range(pos, fa):
                    order[ops[i].eng].append(i)
            for i in range(fa, fb):
                order[ops[i].eng].append(i)
            pos = fb
        assert sum(len(v) for v in order.values()) == len(ops)
        for e in self.ENGS:
            c = 0
            for i in order[e]:
                o = ops[i]
                if not o.is_dma and o.sig:
                    c += 1
                    o.val = c
        dkeys = list(self.dma_count.keys())
        self._check(order)
        import os
        if os.environ.get("DUMP_ORDER"):
            with open(os.environ["DUMP_ORDER"], "w") as f:
                for e in self.ENGS:
                    f.write("=== %s\n" % e)
                    for i in order[e]:
                        o = ops[i]
                        f.write("%6d %s deps=%s\n" % (i, o.tag, sorted(o.deps)))
        with contextlib.ExitStack() as es:
            esem = {e: es.enter_context(nc.semaphore("s_" + e)) for e in self.ENGS}
            dsem = {k: es.enter_context(nc.semaphore("d%d" % i)) for i, k in enumerate(dkeys)}
            block = es.enter_context(nc.Block())

            def run(engname, eng):
                waited = {}
                for i in order[engname]:
                    o = ops[i]
                    need = {}
                    for j in o.deps:
                        p = ops[j]
                        s = dsem[p.dkey] if p.is_dma else esem[p.eng]
                        v = p.val
                        key = id(s)
                        if v > need.get(key, (None, 0))[1]:
                            need[key] = (s, v)
                    for key, (s, v) in need.items():
                        if waited.get(key, 0) >= v:
                            continue
                        eng.wait_ge(s, v)
                        waited[key] = v
                    if o.fn is None:
                        continue
                    ins = o.fn(eng)
                    if o.is_dma:
                        ins.then_inc(dsem[o.dkey], 16)
                    elif o.sig:
                        ins.then_inc(esem[o.eng], 1)

            @block.sync
            def _(e):
                run("sp", e)

            @block.scalar
            def _(e):
                run("act", e)

            @block.vector
            def _(e):
                run("dve", e)

            @block.tensor
            def _(e):
                run("pe", e)

            @block.gpsimd
            def _(e):
                run("pool", e)


class Arena:
    def __init__(self, nc, base=16512, limit=229344):
        self.nc = nc
        self.cur = base
        self.limit = limit
        self.n = 0

    def alloc(self, name, shape, dt):
        esz = 2 if dt == BF16 else 4
        nbytes = int(np.prod(shape[1:])) * esz
        nbytes = (nbytes + 63) // 64 * 64
        assert self.cur + nbytes <= self.limit, ("SBUF OOM", name, self.cur, nbytes)
        self.n += 1
        t = self.nc.alloc_sbuf_tensor_at("%s_%d" % (name, self.n), list(shape), dt, offset=self.cur)
        self.cur += nbytes
        return t

    def mark(self):
        return self.cur

    def release(self, m):
        self.cur = m


def build(n_layers=2, dbg=None):
    nc = bass.Bass("TRN2", target_bir_lowering=False)

    def din(name, shape, dt=F32):
        return nc.dram_tensor(name, list(shape), dt, kind="ExternalInput").ap()

    x = din("x", [TOK, D])
    w_in = din("w_in", [2, D, 4096])
    lb_logits = din("hg_lb_logits", [2, 2, 512])
    hg_norm_g = din("hg_norm_g", [2, 512])
    w_out = din("w_out", [2, D, D])
    ln1_g = din("ln1_g", [2, D])
    ln1_b = din("ln1_b", [2, D])
    r_w1 = din("router_w1", [2, D, 4])
    r_b1 = din("router_b1", [2, 4])
    r_w2 = din("router_w2", [2, D, 32])
    r_b2 = din("router_b2", [2, 32])
    w_gate = din("ex_w_gate", [2, NE, D, 512])
    w_up = din("ex_w_up", [2, NE, D, 512])
    w_down = din("ex_w_down", [2, NE, 512, D])
    ln2_g = din("ln2_g", [2, D])
    ln2_b = din("ln2_b", [2, D])
    c_ident = din("c_ident", [128, 128], BF16)
    c_amask = din("c_amask", [128, AMW], BF16)
    c_hmask = din("c_hmask", [128, 2, 128], BF16)
    c_ustrict = din("c_ustrict", [128, 128], BF16)
    c_rstart = din("c_rstart", [128, 512])
    c_rope = din("c_rope", [16, 2, S_])
    c_ecap = din("c_ecap", [128, NE])
    out = nc.dram_tensor("out", [TOK, D], F32, kind="ExternalOutput").ap()
    hres = nc.dram_tensor("hres", [TOK, D], F32, kind="ExternalOutput" if dbg else "Internal").ap()
    xs = nc.dram_tensor("xs", [NE * CAP, D], BF16, kind="Internal").ap()
    ys = nc.dram_tensor("ys", [NE * CAP, D], F32, kind="Internal").ap()
    bar_d = nc.dram_tensor("bar_d", [1, 16], F32, kind="Internal").ap()
    dbg_yT = nc.dram_tensor("dbg_yT", [2, 128, 8, S_], BF16, kind="ExternalOutput").ap() if dbg else None

    S = Sched(nc)
    A = Arena(nc)
    import os
    RE_HG = os.environ.get("RE_HG") is not None
    RE_ATT = os.environ.get("RE_ATT") is not None

    def nfree(ap):
        sh = list(ap.shape)
        n = 1
        for v in sh[1:]:
            n *= int(v)
        return n

    def vcost(eng, o):
        n = nfree(o)
        if eng == "act":
            return 0.2 + n / 1400.0
        if eng == "dve":
            return 0.1 + n / 1000.0
        return 0.3 + n / 600.0

    GRP = {"n": 0, "cur": {}}

    def MM(o, lhsT, rhs, start, stop, r, w):
        op_ = S.op("pe", lambda e: e.matmul(o, lhsT, rhs, start=start, stop=stop), reads=r, writes=w,
                   cost=0.04 + max(nfree(o), 64) / 1800.0)
        bank = w[0]
        if start and stop:
            return
        if start:
            GRP["n"] += 1
            GRP["cur"][bank] = GRP["n"]
        op_.grp = (GRP["cur"][bank], bool(stop))

    def TR(o, i, r, w):
        S.op("pe", lambda e: e.transpose(o, i, ident[:]), reads=r, writes=w, cost=0.1)

    def ACT(o, i, func, r, w, scale=1.0, bias=0.0):
        S.op("act", lambda e: e.activation(o, i, func, bias=bias, scale=scale), reads=r, writes=w,
             cost=vcost("act", o))

    def TT(eng, o, a, b, op, r, w):
        S.op(eng, lambda e: e.tensor_tensor(o, a, b, op), reads=r, writes=w, cost=vcost(eng, o))

    def TS(eng, o, a, s1, s2, op0, op1, r, w):
        if s2 is None:
            S.op(eng, lambda e: e.tensor_scalar(o, a, s1, None, op0), reads=r, writes=w, cost=vcost(eng, o))
        else:
            S.op(eng, lambda e: e.tensor_scalar(o, a, s1, s2, op0, op1), reads=r, writes=w, cost=vcost(eng, o))

    def STT(eng, o, a, sc, b, op0, op1, r, w):
        S.op(eng, lambda e: e.scalar_tensor_tensor(o, a, sc, b, op0, op1), reads=r, writes=w, cost=vcost(eng, o))

    def CP(eng, o, i, r, w):
        if eng == "act":
            S.op("act", lambda e: e.copy(o, i), reads=r, writes=w, cost=vcost(eng, o))
        else:
            S.op(eng, lambda e: e.tensor_copy(o, i), reads=r, writes=w, cost=vcost(eng, o))

    def RECIP(o, i, r, w):
        S.op("dve", lambda e: e.reciprocal(o, i), reads=r, writes=w, cost=0.15 + nfree(o) / 200.0)

    def MEMSET(eng, o, val, w):
        S.op(eng, lambda e: e.memset(o, val), reads=[], writes=w, cost=vcost(eng, o))

    def DMA(q, o, i, r, w, sem_key=None, slow=False):
        sh = list(o.shape)
        nb = 1
        for v in sh:
            nb *= int(v)
        nb *= 2 if o.dtype == BF16 else 4
        cost = 2.0 + nb / 100e3
        if slow:
            S.op(q, lambda e: e.dma_start(out=o, in_=i, allow_slow_non_contiguous=True),
                 reads=r, writes=w, dma=True, sem_key=sem_key, cost=cost + 3.0)
        else:
            S.op(q, lambda e: e.dma_start(out=o, in_=i), reads=r, writes=w, dma=True, sem_key=sem_key, cost=cost)

    ident = A.alloc("ident", [128, 128], BF16)
    hmask = A.alloc("hmask", [128, 2, 128], BF16)
    ustrict = A.alloc("ustrict", [128, 128], BF16)
    ones_bf = A.alloc("ones_bf", [128, 128], BF16)
    ecap = A.alloc("ecap", [128, NE], F32)
    lbl = A.alloc("lbl", [128, 2, 2, 4], F32)
    lb_t = A.alloc("lb_t", [128, 2, 4], F32)
    oml_t = A.alloc("oml_t", [128, 2, 4], F32)
    lnoml_t = A.alloc("lnoml_t", [128, 2, 4], F32)
    ng_t = A.alloc("ng_t", [128, 2, 4], F32)
    gates = A.alloc("gates", [128, NTT, 2], F32)
    slots_i = A.alloc("slots_i", [128, NTT, 2], I32)
    bar_s = A.alloc("bar_s", [1, 16], F32)

    DMA("sp", ident[:], c_ident, [], ["ident"])
    DMA("sp", hmask[:], c_hmask, [], ["hmask"])
    DMA("sp", ustrict[:], c_ustrict, [], ["ustrict"])
    DMA("sp", ecap[:], c_ecap, [], ["ecap"])
    MEMSET("dve", ones_bf[:], 1.0, ["ones_bf"])
    MEMSET("dve", bar_s[:], 0.0, ["bar_s"])
    for l in range(2):
        for d in range(2):
            DMA("sp", lbl[:, l, d, :], lb_logits[l, d, :].rearrange("(h c) -> c h", c=128),
                [], ["lbl"], slow=True)
        DMA("sp", ng_t[:, l, :], hg_norm_g[l, :].rearrange("(h c) -> c h", c=128), [], ["ng_t"], slow=True)

    def barrier():
        S.barrier(lambda e: e.dma_start(out=bar_d, in_=bar_s[:]))

    zt = A.alloc("zt", [128, D], BF16)
    MEMSET("pool", zt[:], 0.0, ["zt"])
    for j in range(NE * CAP // 128):
        DMA("pool", xs[j * 128:(j + 1) * 128, :], zt[:], ["zt"], ["xs"])

    PSUM_STATE = {}

    def psum_banks():
        if not PSUM_STATE:
            PSUM_STATE["f"] = [nc.alloc_psum_tensor("pb%d" % i, [128, 512], F32) for i in range(6)]
            PSUM_STATE["t"] = [nc.alloc_psum_tensor("pbT%d" % i, [128, 1024], BF16) for i in range(2)]
        return PSUM_STATE["f"], PSUM_STATE["t"]

    pb, pbT = psum_banks()
    PB = ["pb%d" % i for i in range(6)]
    PT = ["pbT0", "pbT1"]

    base_mark = A.mark()

    def layer(l):
        src = x if l == 0 else hres
        last = (l == n_layers - 1)
        A.release(base_mark)
        if l == 0:
            MEMSET("dve", lb_t[:], 0.0, ["lb_t"])
        else:
            tmpd = A.alloc("tmpd", [128, 2, 4], F32)
            TT("dve", tmpd[:], lbl[:, 0, :, :], lbl[:, 1, :, :], ALU.subtract, ["lbl"], ["tmpd"])
            ACT(tmpd[:], tmpd[:], AF.Exp, ["tmpd"], ["tmpd"])
            TS("dve", tmpd[:], tmpd[:], 1.0, None, ALU.add, None, ["tmpd"], ["tmpd"])
            RECIP(lb_t[:], tmpd[:], ["tmpd"], ["lb_t"])
        TS("dve", oml_t[:], lb_t[:], -1.0, 1.0, ALU.mult, ALU.add, ["lb_t"], ["oml_t"])
        ACT(lnoml_t[:], oml_t[:], AF.Ln, ["oml_t"], ["lnoml_t"])
        cnt_bc = A.alloc("cnt_bc", [128, NE], F32)
        MEMSET("dve", cnt_bc[:], 0.0, ["cnt_bc"])
        lmark = A.mark()

        for s in range(2):
            A.release(lmark)
            t0 = s * S_
            hT = A.alloc("hT", [128, 8, S_], BF16)
            yT = A.alloc("yT", [128, 8, S_], BF16)
            rope = A.alloc("rope", [16, 2, S_], F32)
            amask = A.alloc("amask", [128, AMW], BF16)
            b1mark = A.mark()
            DMA("sp", rope[:], c_rope, [], ["rope"])
            DMA("sp", amask[:], c_amask, [], ["amask"])
            xt = [A.alloc("xt%d" % i, [128, D], F32) for i in range(2)]
            xb = [A.alloc("xb%d" % i, [128, D], BF16) for i in range(2)]
            for tt in range(16):
                b = tt % 2
                DMA("sp", xt[b][:], src[t0 + tt * 128: t0 + (tt + 1) * 128, :], [], ["xt%d" % b])
                CP("act", xb[b][:], xt[b][:], ["xt%d" % b], ["xb%d" % b])
                for c in range(8):
                    TR(pbT[b][:, c * 128:(c + 1) * 128], xb[b][:, c * 128:(c + 1) * 128],
                       ["xb%d" % b, "ident"], [PT[b]])
                CP("dve", hT[:, :, tt * 128:(tt + 1) * 128],
                   pbT[b][:].rearrange("p (c t) -> p c t", c=8), [PT[b]], [("hT", tt)])
            barrier()
            A.release(b1mark)

            S.reorder_on = False
            wh = [A.alloc("wh%d" % i, [128, 8, 640], BF16) for i in range(2)]
            T = [A.alloc("T%d" % i, [128, 512], F32) for i in range(7)]
            TK = ["T%d" % i for i in range(7)]
            q32 = A.alloc("q32", [128, 512], F32)
            Qb = [A.alloc("Qb%d" % d, [128, S_], BF16) for d in range(2)]
            Kinv = [A.alloc("Kinv%d" % d, [128, S_], BF16) for d in range(2)]
            Kd = [A.alloc("Kd%d" % d, [128, S_], BF16) for d in range(2)]
            KdT = [A.alloc("KdT%d" % d, [128, 16, 128], BF16) for d in range(2)]
            Vh = A.alloc("Vh", [128, 16, 128], BF16)
            sg = A.alloc("sg", [128, S_], BF16)
            oF = A.alloc("oF", [128, S_], F32)
            Dall = A.alloc("Dall", [128, 2, 32], F32)
            S32 = [A.alloc("S32_%d" % d, [128, 128], F32) for d in range(2)]
            Sbf = [A.alloc("Sbf_%d" % d, [128, 128], BF16) for d in range(2)]
            T7 = A.alloc("T7", [128, 512], F32)
            ALIAS = {"oFa%d" % k: [("oF", 4 * k + j) for j in range(4)] for k in range(4)}

            def expand_keys(keys):
                out_ = []
                for k in keys:
                    out_ += ALIAS.get(k, [k])
                return out_

            TSET = [T[0:6], [oF[:, k * 512:(k + 1) * 512] for k in range(4)] + [T[4], T7]]
            TKSET = [TK[0:6], ["oFa0", "oFa1", "oFa2", "oFa3", TK[4], "T7"]]
            rstart = A.alloc("rstart", [128, 512], F32)
            DMA("sp", rstart[:], c_rstart, [], ["rstart"])

            for hh in range(4):
                S.fence()
                S.reorder_on = RE_HG
                w = wh[hh % 2]
                wk = "wh%d" % (hh % 2)
                for gi in range(5):
                    c0 = gi * 512 + hh * 128
                    DMA("pool", w[:, :, gi * 128:(gi + 1) * 128],
                        w_in[l, :, c0:c0 + 128].rearrange("(c p) n -> p c n", p=128), [], [wk])
                lbs = [lb_t[:, d, hh:hh + 1] for d in range(2)]
                omls = [oml_t[:, d, hh:hh + 1] for d in range(2)]
                lnomls = [lnoml_t[:, d, hh:hh + 1] for d in range(2)]
                for blk in range(4):
                    bs = slice(blk * 512, (blk + 1) * 512)
                    hkeys = [("hT", blk * 4 + j) for j in range(4)]
                    for gi, pbi in ((0, 0), (1, 1), (2, 2), (4, 3)):
                        for c in range(8):
                            MM(pb[pbi][:, :], w[:, c, gi * 128:(gi + 1) * 128], hT[:, c, bs],
                               c == 0, c == 7, [wk] + hkeys, [PB[pbi]])
                    for j in range(4):
                        ts_ = slice(blk * 512 + j * 128, blk * 512 + (j + 1) * 128)
                        for c in range(8):
                            MM(pb[4][:, j * 128:(j + 1) * 128], hT[:, c, ts_], w[:, c, 384:512],
                               c == 0, c == 7, [wk] + hkeys, [PB[4]])
                    CP("act", Vh[:, blk * 4:(blk + 1) * 4, :],
                       pb[4][:].rearrange("p (j v) -> p j v", j=4), [PB[4]], [("Vh", blk)])
                    ACT(q32[:], pb[0][:], AF.Identity, [PB[0]], ["q32"], scale=128.0 ** -0.5)
                    ACT(T[6][:], pb[3][:], AF.Exp, [PB[3]], [TK[6]], scale=-1.0)
                    ACT(T[6][:], T[6][:], AF.Ln, [TK[6]], [TK[6]], bias=1.0)
                    ACT(T[6][:], T[6][:], AF.Exp, [TK[6]], [TK[6]], scale=-1.0)
                    TT("dve", sg[:, bs], pb[3][:], T[6][:], ALU.mult, [PB[3], TK[6]], [("sg", blk)])
                    def gate_dir(d, T, TK):
                        pa = pb[1 + d]
                        pak = PB[1 + d]
                        ACT(T[0][:], pa[:], AF.Exp, [pak], [TK[0]], scale=-1.0)
                        ACT(T[1][:], T[0][:], AF.Ln, [TK[0]], [TK[1]], bias=1.0)
                        ACT(T[5][:], T[0][:], AF.Ln, [TK[0], "lb_t"], [TK[5]], scale=lbs[d], bias=1.0)
                        TT("pool", T[5][:], T[5][:], T[1][:], ALU.subtract, [TK[5], TK[1]], [TK[5]])
                        STT("dve", T[0][:], pa[:], -1.0, T[1][:], ALU.mult, ALU.subtract,
                            [pak, TK[1]], [TK[0]])
                        S.op("dve", (lambda o_, a_, b_: (lambda e: e.tensor_tensor_scan(
                            o_, a_, b_, 0.0, ALU.mult, ALU.add)))(T[2][:], rstart[:], T[5][:]),
                            reads=["rstart", TK[5]], writes=[TK[2]], cost=1.2)
                        B3 = T[2][:].rearrange("p (n t) -> p n t", t=64)
                        if d == 0:
                            Bx, Bxk = T[2], TK[2]
                            tot = B3[:, :, 63:64]
                        else:
                            TT("pool", T[3][:], T[5][:], T[2][:], ALU.subtract, [TK[5], TK[2]], [TK[3]])
                            TT("pool", T[4][:].rearrange("p (n t) -> p n t", t=64),
                               T[3][:].rearrange("p (n t) -> p n t", t=64),
                               B3[:, :, 63:64].broadcast_to([128, 8, 64]), ALU.add,
                               [TK[3], TK[2]], [TK[4]])
                            Bx, Bxk = T[4], TK[4]
                            tot = T[4][:].rearrange("p (n t) -> p n t", t=64)[:, :, 0:1]
                        ACT(Dall[:, d, blk * 8:(blk + 1) * 8].rearrange("p (n o) -> p n o", o=1), tot,
                            AF.Exp, [Bxk], [("Dall", d, blk)])
                        ACT(T[5][:], Bx[:], AF.Exp, [Bxk], [TK[5]])
                        TT("dve", Qb[d][:, bs], q32[:], T[5][:], ALU.mult, ["q32", TK[5]], [("Qb", d, blk)])
                        TT("pool", T[3][:], T[0][:], Bx[:], ALU.subtract, [TK[0], Bxk], [TK[3]])
                        ACT(Kinv[d][:, bs], T[3][:], AF.Exp, [TK[3], "lnoml_t"], [("Kinv", d, blk)],
                            bias=lnomls[d])
                        TT("pool", T[3][:].rearrange("p (n t) -> p n t", t=64),
                           T[3][:].rearrange("p (n t) -> p n t", t=64),
                           tot.broadcast_to([128, 8, 64]), ALU.add, [TK[3], Bxk], [TK[3]])
                        ACT(Kd[d][:, bs], T[3][:], AF.Exp, [TK[3], "lnoml_t"], [("Kd", d, blk)],
                            bias=lnomls[d])
                    recs = []
                    for d in range(2):
                        rec = []
                        S.op = (lambda rec_: (lambda *a, **k: rec_.append((a, k))))(rec)
                        gate_dir(d, TSET[d], TKSET[d])
                        del S.op
                        recs.append(rec)
                    for i_ in range(max(len(recs[0]), len(recs[1]))):
                        for rec in recs:
                            if i_ < len(rec):
                                a_, k_ = rec[i_]
                                k_ = dict(k_)
                                k_["reads"] = expand_keys(k_.get("reads", ()))
                                k_["writes"] = expand_keys(k_.get("writes", ()))
                                S.op(*a_, **k_)
                for d in range(2):
                    for half in range(2):
                        for j in range(8):
                            tt = half * 8 + j
                            TR(pbT[d][:, j * 128:(j + 1) * 128], Kd[d][:, tt * 128:(tt + 1) * 128],
                               [("Kd", d, tt // 4), "ident"], [PT[d]])
                        CP("act" if d == 0 else "dve", KdT[d][:, half * 8:(half + 1) * 8, :],
                           pbT[d][:].rearrange("p (j c) -> p j c", j=8), [PT[d]], [("KdT", d, half)])
                for d in range(2):
                    MEMSET("pool", S32[d][:], 0.0, [("S32", d)])
                    MEMSET("pool", Sbf[d][:], 0.0, [("Sbf", d)])
                for tt in range(16):
                    for d in range(2):
                        blk = tt // 4
                        tsl = slice(tt * 128, (tt + 1) * 128)
                        pa_i = (2 * tt + d) % 2
                        MM(pb[pa_i][:, 0:128], Kinv[d][:, tsl], Qb[d][:, tsl], True, True,
                           [("Kinv", d, blk), ("Qb", d, blk)], [PB[pa_i]])
                        TT("dve", Kinv[d][:, tsl], pb[pa_i][:, 0:128], hmask[:, d, :], ALU.mult,
                           [PB[pa_i], "hmask"], [("Kinv", d, blk)])
                for i in range(16):
                    tts = [i, 15 - i]
                    for d in range(2):
                        tt = tts[d]
                        MM(pb[2 + d][:, 0:128], Vh[:, tt, :], Kinv[d][:, tt * 128:(tt + 1) * 128], True, False,
                           [("Vh", tt // 4), ("Kinv", d, tt // 4)], [PB[2 + d]])
                    for ci in range(2):
                        for d in range(2):
                            tt = tts[d]
                            blk = tt // 4
                            ch = ci if d == 0 else 1 - ci
                            n = tt * 2 + ch
                            csl = slice(tt * 128 + ch * 64, tt * 128 + (ch + 1) * 64)
                            prow = slice(ch * 64, (ch + 1) * 64)
                            psO, psOk = pb[2 + d], PB[2 + d]
                            psU, psUk = pb[4 + d], PB[4 + d]
                            MM(psO[:, ch * 64:(ch + 1) * 64], Sbf[d][:], Qb[d][:, csl], False, ci == 1,
                               [("Sbf", d), ("Qb", d, blk)], [psOk])
                            MM(psU[:, 0:128], KdT[d][prow, tt, :], Vh[prow, tt, :], True, True,
                               [("KdT", d, tt // 8), ("Vh", blk)], [psUk])
                            STT("dve", S32[d][:], S32[d][:], Dall[:, d, n:n + 1], psU[:, 0:128],
                                ALU.mult, ALU.add, [("S32", d), ("Dall", d, blk), psUk], [("S32", d)])
                            CP("dve", Sbf[d][:], S32[d][:], [("S32", d)], [("Sbf", d)])
                    for d in range(2):
                        tt = tts[d]
                        tsl = slice(tt * 128, (tt + 1) * 128)
                        if (d == 0) == (tt <= 7):
                            CP("act", oF[:, tsl], pb[2 + d][:, 0:128], [PB[2 + d]], [("oF", tt)])
                        else:
                            TT("dve", oF[:, tsl], oF[:, tsl], pb[2 + d][:, 0:128], ALU.add,
                               [("oF", tt), PB[2 + d]], [("oF", tt)])
                def post_blk(blk, Ta, Tak, Tb, Tbk, hcol):
                    bs = slice(blk * 512, (blk + 1) * 512)
                    ok = [("oF", blk * 4 + j) for j in range(4)]
                    ACT(Ta[:], oF[:, bs], AF.Square, ok, [Tak])
                    hi = Qb[0][:, hcol * 512:(hcol + 1) * 512]
                    lo = Qb[0][:, (hcol + 1) * 512:(hcol + 2) * 512]
                    hik, lok = ("Qb", 0, hcol), ("Qb", 0, hcol + 1)
                    CP("dve", hi, Ta[:], [Tak], [hik])
                    TT("dve", Tb[:], Ta[:], hi, ALU.subtract, [Tak, hik], [Tbk])
                    CP("dve", lo, Tb[:], [Tbk], [lok])
                    MM(pb[0][:, :], ones_bf[:], hi, True, False, ["ones_bf", hik], [PB[0]])
                    MM(pb[0][:, :], ones_bf[:], lo, False, True, ["ones_bf", lok], [PB[0]])
                    ACT(Tb[:], pb[0][:, :], AF.Ln, [PB[0]], [Tbk], scale=1.0 / 128.0, bias=RMS_EPS)
                    ACT(Tb[:], Tb[:], AF.Exp, [Tbk], [Tbk], scale=-0.5)
                    STT("dve", Ta[:], oF[:, bs], ng_t[:, l, hh:hh + 1], Tb[:], ALU.mult, ALU.mult,
                        ok + ["ng_t", Tbk], [Tak])
                    TT("dve", yT[:, hh, bs], Ta[:], sg[:, bs], ALU.mult, [Tak, ("sg", blk)],
                       [("yT", hh, blk)])

                class _Dummy:
                    grp = None

                for bp in range(2):
                    recs = []
                    for par in range(2):
                        rec = []
                        S.op = (lambda rec_: (lambda *a, **k: (rec_.append((a, k)), _Dummy())[1]))(rec)
                        if par == 0:
                            post_blk(2 * bp, T[5], TK[5], T[6], TK[6], 0)
                        else:
                            post_blk(2 * bp + 1, T[0], TK[0], T[1], TK[1], 2)
                        del S.op
                        recs.append(rec)
                    for i_ in range(len(recs[0]) + 3):
                        if i_ < len(recs[0]):
                            a_, k_ = recs[0][i_]
                            S.op(*a_, **k_)
                        j_ = i_ - 3
                        if 0 <= j_ < len(recs[1]):
                            a_, k_ = recs[1][j_]
                            S.op(*a_, **k_)

            wa = [A.alloc("wa%d" % i, [128, 8, 224], BF16) for i in range(2)]
            qT = A.alloc("qT", [128, S_], BF16)
            kT = A.alloc("kT", [128, S_], BF16)
            Va = A.alloc("Va", [128, 16, 128], BF16)
            pt = [A.alloc("pt%d" % i, [128, 512], BF16) for i in range(3)] + [T[6].bitcast(BF16)[:, 0:512]]
            PTK = ["pt0", "pt1", "pt2", TK[6]]
            pm = [A.alloc("pm%d" % i, [128, 512], BF16) for i in range(3)] + [q32.bitcast(BF16)[:, 0:512]]
            PMK = ["pm0", "pm1", "pm2", "q32"]
            rc = A.alloc("rc", [64, 512], F32)
            r1 = rc
            MEMSET("pool", Va[:], 1.0, [("Va", b) for b in range(4)])
            MEMSET("pool", qT[:], 0.0, [("qT", b) for b in range(4)])
            MEMSET("pool", kT[:], 0.0, [("kT", b) for b in range(4)])
            for h in range(8):
                S.fence()
                S.reorder_on = RE_ATT
                w = wa[h % 2]
                wk = "wa%d" % (h % 2)
                for gi in range(3):
                    c0 = 2560 + gi * 512 + h * 64
                    DMA("pool", w[:, :, gi * 80:gi * 80 + 64],
                        w_in[l, :, c0:c0 + 64].rearrange("(c p) n -> p c n", p=128), [], [wk])
                for gi in range(2):
                    CP("pool", w[:, :, gi * 80 + 64:gi * 80 + 72], w[:, :, gi * 80 + 8:gi * 80 + 16], [wk], [wk])
                    CP("pool", w[:, :, gi * 80 + 72:gi * 80 + 80], w[:, :, gi * 80 + 0:gi * 80 + 8], [wk], [wk])
                for blk in range(4):
                    bs = slice(blk * 512, (blk + 1) * 512)
                    hkeys = [("hT", blk * 4 + j) for j in range(4)]
                    for gi in range(2):
                        for c in range(8):
                            MM(pb[gi][0:80, :], w[:, c, gi * 80:(gi + 1) * 80], hT[:, c, bs],
                               c == 0, c == 7, [wk] + hkeys, [PB[gi]])
                    for j in range(4):
                        ts_ = slice(blk * 512 + j * 128, blk * 512 + (j + 1) * 128)
                        for c in range(8):
                            MM(pb[2][:, j * 64:(j + 1) * 64], hT[:, c, ts_], w[:, c, 160:224],
                               c == 0, c == 7, [wk] + hkeys, [PB[2]])
                    CP("act", Va[:, blk * 4:(blk + 1) * 4, 0:64],
                       pb[2][:, 0:256].rearrange("p (j v) -> p j v", j=4), [PB[2]], [("Va", blk)])
                    for gi, dst, dk in ((0, qT, "qT"), (1, kT, "kT")):
                        CP("act", dst[0:64, bs], pb[gi][0:64, :], [PB[gi]], [(dk, blk)])
                        ra, rak = T[2 * gi], TK[2 * gi]
                        rb, rbk = T[2 * gi + 1], TK[2 * gi + 1]
                        TT("dve", ra[0:16, :], pb[gi][0:16, :], rope[:, 0, bs], ALU.mult, [PB[gi], "rope"], [rak])
                        CP("act", rb[0:16, :], pb[gi][64:80, :], [PB[gi]], [rbk])
                        TT("dve", rb[0:16, :], rb[0:16, :], rope[:, 1, bs], ALU.mult, [rbk, "rope"], [rbk])
                        TT("dve", dst[0:16, bs], ra[0:16, :], rb[0:16, :], ALU.add, [rak, rbk], [(dk, blk)])
                pairs = []
                for qb in range(4):
                    q0 = qb * 512
                    kbs = [kb for kb in range(16)
                           if kb * 128 >= q0 - 1151 and kb * 128 <= q0 + 1535]
                    for ki, kb in enumerate(kbs):
                        pairs.append((qb, ki, kb, ki == len(kbs) - 1))

                def emit_S(i):
                    qb, ki, kb, lastk = pairs[i]
                    q0 = qb * 512
                    ms = q0 - kb * 128 - OFFMIN
                    assert 0 <= ms and ms + 512 <= AMW
                    b = i % 4
                    sb_ = (3, 4, 0, 1)[b]
                    psS, psSk = pb[sb_], PB[sb_]
                    MM(psS[:, :], kT[:, kb * 128:(kb + 1) * 128], qT[:, q0:q0 + 512], True, True,
                       [("kT", kb // 4), ("qT", qb)], [psSk])
                    ACT(pt[b][:], psS[:, :], AF.Exp, [psSk], [PTK[b]], scale=0.125)
                    TT("dve", pm[b][:], pt[b][:], amask[:, ms:ms + 512], ALU.mult,
                       [PTK[b], "amask"], [PMK[b]])

                def emit_PV(i):
                    qb, ki, kb, lastk = pairs[i]
                    q0 = qb * 512
                    b = i % 4
                    nb = 5 if qb % 2 == 0 else 2
                    psN, psNk = pb[nb], PB[nb]
                    MM(psN[:, :], Va[:, kb, :], pm[b][:], ki == 0, lastk,
                       [("Va", kb // 4), PMK[b]], [psNk])
                    if lastk:
                        ACT(rc[:], psN[64:128, :], AF.Ln, [psNk], ["rc"])
                        ACT(rc[:], rc[:], AF.Exp, ["rc"], ["rc"], scale=-1.0)
                        pr = slice((h % 2) * 64, (h % 2) * 64 + 64)
                        TT("dve", yT[pr, 4 + h // 2, q0:q0 + 512], psN[0:64, :], rc[:], ALU.mult,
                           [psNk, "rc"], [("yT", 4 + h // 2, qb)])

                emit_S(0)
                emit_S(1)
                emit_S(2)
                for i in range(len(pairs)):
                    if i + 3 < len(pairs):
                        emit_S(i + 3)
                    emit_PV(i)
            if dbg and l == n_layers - 1:
                DMA("sp", dbg_yT[s], yT[:], [("yT", c, q) for c in range(8) for q in range(4)], ["dbg_yT"])
            barrier()
            S.reorder_on = True
            A.release(b1mark)

            wo = A.alloc("wo", [128, 8, D], BF16)
            for c in range(8):
                DMA("pool", wo[:, c, :], w_out[l, c * 128:(c + 1) * 128, :], [], ["wo"])
            g1 = A.alloc("g1", [128, D], F32)
            b1 = A.alloc("b1", [128, D], F32)
            DMA("sp", g1[:], ln1_g[l:l + 1, :].broadcast_to([128, D]), [], ["g1"])
            DMA("sp", b1[:], ln1_b[l:l + 1, :].broadcast_to([128, D]), [], ["b1"])
            wr = A.alloc("wr", [128, 8, 36], F32)
            wr_hi = A.alloc("wr_hi", [128, 8, 36], BF16)
            wr_lo = A.alloc("wr_lo", [128, 8, 36], BF16)
            wr_t = A.alloc("wr_t", [128, 8, 36], F32)
            rb = A.alloc("rb", [128, 36], F32)
            DMA("sp", wr[:, :, 0:4], r_w1[l].rearrange("(c p) n -> p c n", p=128), [], ["wr"], slow=True)
            DMA("sp", wr[:, :, 4:36], r_w2[l].rearrange("(c p) n -> p c n", p=128), [], ["wr"], slow=True)
            DMA("sp", rb[:, 0:4], r_b1[l:l + 1, :].broadcast_to([128, 4]), [], ["rb"], slow=True)
            DMA("sp", rb[:, 4:36], r_b2[l:l + 1, :].broadcast_to([128, 32]), [], ["rb"], slow=True)
            CP("dve", wr_hi[:], wr[:], ["wr"], ["wr_hi"])
            TT("dve", wr_t[:], wr[:], wr_hi[:], ALU.subtract, ["wr", "wr_hi"], ["wr_t"])
            CP("dve", wr_lo[:], wr_t[:], ["wr_t"], ["wr_lo"])
            ht = [A.alloc("ht%d" % i, [128, D], F32) for i in range(2)]
            z = [A.alloc("z%d" % i, [128, D], F32) for i in range(2)]
            hb = [A.alloc("hb%d" % i, [128, D], BF16) for i in range(2)]
            hlo = [A.alloc("hlo%d" % i, [128, D], BF16) for i in range(2)]
            hTh = A.alloc("hTh", [128, 8, 128], BF16)
            hTl = A.alloc("hTl", [128, 8, 128], BF16)
            st = A.alloc("st", [128, 2, 6], F32)
            mv = A.alloc("mv", [128, 2], F32)
            rstd = A.alloc("rstd", [128, 1], F32)
            nmr = A.alloc("nmr", [128, 1], F32)
            L = A.alloc("L", [128, 36], F32)
            sm = A.alloc("sm", [128, 16], F32)
            e4 = A.alloc("e4", [128, 4], F32)
            oh1 = A.alloc("oh1", [128, 4], F32)
            L2 = A.alloc("L2", [128, 32], F32)
            L2b = A.alloc("L2b", [128, 32], F32)
            oha = A.alloc("oha", [128, 32], F32)
            ohb = A.alloc("ohb", [128, 32], F32)
            Mbf = A.alloc("Mbf", [128, 32], BF16)
            pos = A.alloc("pos", [128, 32], F32)
            tq = A.alloc("tq", [128, 32], F32)
            sl = A.alloc("sl", [128, 2], F32)
            for ti in range(16):
                tt = s * 16 + ti
                b = ti % 2
                tsl = slice(ti * 128, (ti + 1) * 128)
                rows = slice(t0 + ti * 128, t0 + (ti + 1) * 128)
                DMA("sp", ht[b][:], src[rows, :], ["hres_%d" % tt], ["ht%d" % b])
                for half in range(2):
                    for c in range(8):
                        MM(pb[half][:, :], yT[:, c, tsl], wo[:, c, half * 512:(half + 1) * 512],
                           c == 0, c == 7, ["wo"] + [("yT", c, ti // 4)], [PB[half]])
                    STT("dve", z[b][:, half * 512:(half + 1) * 512], ht[b][:, half * 512:(half + 1) * 512],
                        ALPHA, pb[half][:, :], ALU.mult, ALU.add, ["ht%d" % b, PB[half]], ["z%d" % b])
                ln_tail(z[b], "z%d" % b, st, mv, rstd, nmr, g1, "g1", b1, "b1")
                DMA("sp", hres[rows, :], z[b][:], ["z%d" % b], ["hres_%d" % tt], sem_key="st_z%d" % b)
                CP("act", hb[b][:], z[b][:], ["z%d" % b], ["hb%d" % b])
                TT("pool", ht[b][:], z[b][:], hb[b][:], ALU.subtract, ["z%d" % b, "hb%d" % b], ["ht%d" % b])
                CP("pool", hlo[b][:], ht[b][:], ["ht%d" % b], ["hlo%d" % b])
                for c in range(8):
                    TR(pbT[0][:, c * 128:(c + 1) * 128], hb[b][:, c * 128:(c + 1) * 128],
                       ["hb%d" % b, "ident"], [PT[0]])
                for c in range(8):
                    TR(pbT[1][:, c * 128:(c + 1) * 128], hlo[b][:, c * 128:(c + 1) * 128],
                       ["hlo%d" % b, "ident"], [PT[1]])
                CP("act", hTh[:], pbT[0][:].rearrange("p (c t) -> p c t", c=8), [PT[0]], ["hTh"])
                CP("dve", hTl[:], pbT[1][:].rearrange("p (c t) -> p c t", c=8), [PT[1]], ["hTl"])
                n = 0
                for (a_, ak, w_, wk_) in ((hTh, "hTh", wr_hi, "wr_hi"), (hTl, "hTl", wr_hi, "wr_hi"),
                                          (hTh, "hTh", wr_lo, "wr_lo")):
                    for c in range(8):
                        MM(pb[2][:, 0:36], a_[:, c, :], w_[:, c, :], n == 0, n == 23, [ak, wk_], [PB[2]])
                        n += 1
                route(tt, pb[2], PB[2], rb, L, sm, e4, oh1, L2, L2b, oha, ohb, Mbf, pos, tq, sl, cnt_bc)
                for k in range(2):
                    S.op("pool", (lambda idx_, src_: (lambda e: e.indirect_dma_start(
                        out=xs, out_offset=bass.IndirectOffsetOnAxis(ap=idx_, axis=0),
                        in_=src_, in_offset=None)))(slots_i[:, tt, k:k + 1], hb[b][:, :]),
                        reads=["hb%d" % b, ("slots", tt)], writes=["xs"], dma=True, sem_key="sc_hb%d" % b, cost=6.0)
            barrier()

        A.release(lmark)
        wg = [A.alloc("wg%d" % i, [128, 8, 512], BF16) for i in range(2)]
        wu = [A.alloc("wu%d" % i, [128, 8, 512], BF16) for i in range(2)]
        wd = [A.alloc("wd%d" % i, [128, 4, D], BF16) for i in range(2)]
        xr = [A.alloc("xr%d" % i, [128, 3, D], BF16) for i in range(2)]
        xT = [A.alloc("xT%d" % i, [128, 8, CAP], BF16) for i in range(2)]
        hid = A.alloc("hid", [128, 4, CAP], BF16)
        sil = [A.alloc("sil%d" % i, [128, CAP], F32) for i in range(2)]
        yo = [A.alloc("yo%d" % i, [128, D], F32) for i in range(2)]
        for e_ in range(NE):
            b = e_ % 2
            DMA("sp", xr[b][:], xs[e_ * CAP:(e_ + 1) * CAP, :].rearrange("(j p) d -> p j d", p=128),
                ["xs"], ["xr%d" % b])
            for c in range(8):
                DMA("pool", wg[b][:, c, :], w_gate[l, e_, c * 128:(c + 1) * 128, :], [], ["wg%d" % b])
                DMA("pool", wu[b][:, c, :], w_up[l, e_, c * 128:(c + 1) * 128, :], [], ["wu%d" % b])
            for f in range(4):
                DMA("pool", wd[b][:, f, :], w_down[l, e_, f * 128:(f + 1) * 128, :], [], ["wd%d" % b])
            for j in range(3):
                tb = j % 2
                for c in range(8):
                    TR(pbT[tb][:, c * 128:(c + 1) * 128], xr[b][:, j, c * 128:(c + 1) * 128],
                       ["xr%d" % b, "ident"], [PT[tb]])
                CP("act" if j % 2 == 0 else "dve", xT[b][:, :, j * 128:(j + 1) * 128],
                   pbT[tb][:].rearrange("p (c t) -> p c t", c=8), [PT[tb]], ["xT%d" % b])
            for f in range(4):
                fb = f % 2
                pg, pgk = pb[fb], PB[fb]
                pu, puk = pb[2 + fb], PB[2 + fb]
                for c in range(8):
                    MM(pg[:, 0:CAP], wg[b][:, c, f * 128:(f + 1) * 128], xT[b][:, c, :], c == 0, c == 7,
                       ["wg%d" % b, "xT%d" % b], [pgk])
                for c in range(8):
                    MM(pu[:, 0:CAP], wu[b][:, c, f * 128:(f + 1) * 128], xT[b][:, c, :], c == 0, c == 7,
                       ["wu%d" % b, "xT%d" % b], [puk])
                ACT(sil[fb][:], pg[:, 0:CAP], AF.Silu, [pgk], ["sil%d" % fb])
                TT("dve", hid[:, f, :], sil[fb][:], pu[:, 0:CAP], ALU.mult, ["sil%d" % fb, puk], [("hid", f)])
            for j in range(3):
                yb = j % 2
                for half in range(2):
                    py, pyk = pb[4 + half], PB[4 + half]
                    for f in range(4):
                        MM(py[:, :], hid[:, f, j * 128:(j + 1) * 128], wd[b][:, f, half * 512:(half + 1) * 512],
                           f == 0, f == 3, [("hid", f), "wd%d" % b], [pyk])
                    CP("act" if half == 0 else "dve", yo[yb][:, half * 512:(half + 1) * 512], py[:, :],
                       [pyk], ["yo%d" % yb])
                r0 = e_ * CAP + j * 128
                DMA("sp", ys[r0:r0 + 128, :], yo[yb][:], ["yo%d" % yb], ["ys"], sem_key="st_yo%d" % yb)
        barrier()

        A.release(lmark)
        g2 = A.alloc("g2", [128, D], F32)
        b2 = A.alloc("b2", [128, D], F32)
        DMA("sp", g2[:], ln2_g[l:l + 1, :].broadcast_to([128, D]), [], ["g2"])
        DMA("sp", b2[:], ln2_b[l:l + 1, :].broadcast_to([128, D]), [], ["b2"])
        ht = [A.alloc("ht%d" % i, [128, D], F32) for i in range(2)]
        yg = [A.alloc("yg%d" % i, [128, 2, D], F32) for i in range(2)]
        z = [A.alloc("z%d" % i, [128, D], F32) for i in range(2)]
        st = A.alloc("st", [128, 2, 6], F32)
        mv = A.alloc("mv", [128, 2], F32)
        rstd = A.alloc("rstd", [128, 1], F32)
        nmr = A.alloc("nmr", [128, 1], F32)
        dst = out if last else hres
        for tt in range(NTT):
            b = tt % 2
            rows = slice(tt * 128, (tt + 1) * 128)
            DMA("sp", ht[b][:], hres[rows, :], ["hres_%d" % tt], ["ht%d" % b])
            for k in range(2):
                S.op("pool", (lambda idx_, dst_: (lambda e: e.indirect_dma_start(
                    out=dst_, out_offset=None, in_=ys,
                    in_offset=bass.IndirectOffsetOnAxis(ap=idx_, axis=0))))(slots_i[:, tt, k:k + 1], yg[b][:, k, :]),
                    reads=["ys", ("slots", tt)], writes=["yg%d" % b], dma=True, cost=8.0)
            ACT(z[b][:], ht[b][:], AF.Identity, ["ht%d" % b], ["z%d" % b], scale=ALPHA)
            for k in range(2):
                STT("dve", z[b][:], yg[b][:, k, :], gates[:, tt, k:k + 1], z[b][:],
                    ALU.mult, ALU.add, ["yg%d" % b, ("gates", tt), "z%d" % b], ["z%d" % b])
            ln_tail(z[b], "z%d" % b, st, mv, rstd, nmr, g2, "g2", b2, "b2")
            DMA("sp", dst[rows, :], z[b][:], ["z%d" % b], ["out" if last else "hres_%d" % tt],
                sem_key="st_z%d" % b)
        barrier()

    def ln_tail(zt, zk, st, mv, rstd, nmr, g, gk, b_, bk):
        for half in range(2):
            S.op("dve", (lambda o_, i_: (lambda e: e.bn_stats(o_, i_)))(st[:, half, :], zt[:, half * 512:(half + 1) * 512]),
                 reads=[zk], writes=["st"], cost=0.7)
        S.op("dve", lambda e: e.bn_aggr(mv[:], st[:].rearrange("p a b -> p (a b)")), reads=["st"], writes=["mv"])
        ACT(rstd[:], mv[:, 1:2], AF.Ln, ["mv"], ["rstd"], bias=LN_EPS)
        ACT(rstd[:], rstd[:], AF.Exp, ["rstd"], ["rstd"], scale=-0.5)
        STT("dve", nmr[:], mv[:, 0:1], -1.0, rstd[:], ALU.mult, ALU.mult, ["mv", "rstd"], ["nmr"])
        ACT(zt[:], zt[:], AF.Identity, [zk, "rstd", "nmr"], [zk], scale=rstd[:, 0:1], bias=nmr[:, 0:1])
        TT("pool", zt[:], zt[:], g[:], ALU.mult, [zk, gk], [zk])
        TT("dve", zt[:], zt[:], b_[:], ALU.add, [zk, bk], [zk])

    def route(tt, pl, plk, rb, L, sm, e4, oh1, L2, L2b, oha, ohb, Mbf, pos, tq, sl, cnt_bc):
        TT("dve", L[:], pl[:, 0:36], rb[:], ALU.add, [plk, "rb"], ["L"])
        m1, nm1, s1, pg_, ma, mb, dd, ga = (sm[:, i:i + 1] for i in range(8))
        S.op("dve", lambda e: e.reduce_max(m1, L[:, 0:4], AX.X), reads=["L"], writes=["sm0"])
        TS("dve", oh1[:], L[:, 0:4], m1, None, ALU.is_equal, None, ["L", "sm0"], ["oh1"])
        TS("dve", nm1, m1, -1.0, None, ALU.mult, None, ["sm0"], ["sm1"])
        ACT(e4[:], L[:, 0:4], AF.Exp, ["L", "sm1"], ["e4"], bias=nm1)
        S.op("dve", lambda e: e.reduce_sum(s1, e4[:], AX.X), reads=["e4"], writes=["sm2"])
        RECIP(pg_, s1, ["sm2"], ["sm3"])
        TS("dve", e4[:], oh1[:], BIG, -BIG, ALU.mult, ALU.add, ["oh1", "e4"], ["e4"])
        TT("dve", L2[:].rearrange("p (g e) -> p g e", g=4), L[:, 4:36].rearrange("p (g e) -> p g e", g=4),
           e4[:].rearrange("p (g o) -> p g o", o=1).broadcast_to([128, 4, 8]), ALU.add, ["L", "e4"], ["L2"])
        S.op("dve", lambda e: e.reduce_max(ma, L2[:], AX.X), reads=["L2"], writes=["sm4"])
        TS("dve", oha[:], L2[:], ma, None, ALU.is_equal, None, ["L2", "sm4"], ["oha"])
        STT("dve", L2b[:], oha[:], -BIG, L2[:], ALU.mult, ALU.add, ["oha", "L2"], ["L2b"])
        S.op("dve", lambda e: e.reduce_max(mb, L2b[:], AX.X), reads=["L2b"], writes=["sm5"])
        TS("dve", ohb[:], L2b[:], mb, None, ALU.is_equal, None, ["L2b", "sm5"], ["ohb"])
        TT("dve", dd, mb, ma, ALU.subtract, ["sm4", "sm5"], ["sm6"])
        ACT(dd, dd, AF.Exp, ["sm6"], ["sm6"])
        TS("dve", dd, dd, 1.0, None, ALU.add, None, ["sm6"], ["sm6"])
        RECIP(ga, dd, ["sm6"], ["sm7"])
        TT("dve", gates[:, tt, 0:1], ga, pg_, ALU.mult, ["sm7", "sm3"], [("gates", tt)])
        TT("dve", gates[:, tt, 1:2], pg_, gates[:, tt, 0:1], ALU.subtract, ["sm3", ("gates", tt)], [("gates", tt)])
        TT("dve", Mbf[:], oha[:], ohb[:], ALU.add, ["oha", "ohb"], ["Mbf"])
        MM(pb[3][:, 0:32], ustrict[:], Mbf[:], True, True, ["ustrict", "Mbf"], [PB[3]])
        MM(pb[4][:, 0:32], ones_bf[:], Mbf[:], True, True, ["ones_bf", "Mbf"], [PB[4]])
        TT("dve", pos[:], pb[3][:, 0:32], cnt_bc[:], ALU.add, [PB[3], "cnt_bc"], ["pos"])
        TT("dve", cnt_bc[:], cnt_bc[:], pb[4][:, 0:32], ALU.add, ["cnt_bc", PB[4]], ["cnt_bc"])
        TT("dve", pos[:], pos[:], ecap[:], ALU.add, ["pos", "ecap"], ["pos"])
        TT("dve", tq[:], pos[:], oha[:], ALU.mult, ["pos", "oha"], ["tq"])
        S.op("dve", lambda e: e.reduce_sum(sl[:, 0:1], tq[:], AX.X), reads=["tq"], writes=["sl"])
        TT("dve", tq[:], pos[:], ohb[:], ALU.mult, ["pos", "ohb", "sl"], ["tq"])
        S.op("dve", lambda e: e.reduce_sum(sl[:, 1:2], tq[:], AX.X), reads=["tq"], writes=["sl"])
        TS("dve", sl[:], sl[:], 0.0, float(NE * CAP - 1), ALU.max, ALU.min, ["sl"], ["sl"])
        CP("dve", slots_i[:, tt, :], sl[:], ["sl"], [("slots", tt)])

    for l in range(n_layers):
        layer(l)
    import os
    S.emit(final_keys=["out"], reorder=os.environ.get("NOREORDER") is None)
    return nc


def _consts():
    bf = ml_dtypes.bfloat16
    ident = np.eye(128, dtype=np.float32).astype(bf)
    p = np.arange(128)[:, None]
    j = np.arange(AMW)[None, :]
    dl = j - p + OFFMIN
    ad = np.abs(dl)
    cnt = (ad <= 64).astype(np.float32) + ((dl % 4 == 0) & (ad <= 256)) + ((dl % 16 == 0) & (ad <= 1024))
    amask = cnt.astype(bf)
    s = np.arange(128)[:, None]
    t = np.arange(128)[None, :]
    same = (s // 64) == (t // 64)
    hm = np.stack([(same & (s <= t)), (same & (s >= t))], axis=1).astype(np.float32).astype(bf)
    ustrict = (s < t).astype(np.float32).astype(bf)
    rstart = np.ones((128, 512), np.float32)
    rstart[:, ::64] = 0.0
    half = 8
    inv_freq = (500000.0 ** (-np.arange(half, dtype=np.float32) / half)).astype(np.float32)
    pos = np.arange(S_, dtype=np.float32)
    ang = (pos[None, :] * inv_freq[:, None]).astype(np.float32)
    cos = np.cos(ang).astype(np.float32)
    sin = np.sin(ang).astype(np.float32)
    rope = np.zeros((16, 2, S_), np.float32)
    rope[0:8, 0] = cos
    rope[8:16, 0] = cos
    rope[0:8, 1] = -sin
    rope[8:16, 1] = sin
    ecap = np.tile((np.arange(NE, dtype=np.float32) * CAP)[None, :], (128, 1))
    return {"c_ident": ident, "c_amask": amask, "c_hmask": np.ascontiguousarray(hm), "c_ustrict": ustrict,
            "c_rstart": rstart, "c_rope": rope, "c_ecap": ecap}


_NC_CACHE = {}


def kernel(**inputs):
    if "nc" not in _NC_CACHE:
        _NC_CACHE["nc"] = build()
    nc = _NC_CACHE["nc"]
    consts = _consts()
    x = np.ascontiguousarray(inputs["x"], dtype=np.float32).reshape(NCORES, TOK, D)
    shared = {k: np.ascontiguousarray(v) for k, v in inputs.items() if k != "x"}
    in_maps = []
    for c in range(NCORES):
        m = {"x": x[c]}
        m.update(shared)
        m.update(consts)
        in_maps.append(m)
    res = run_bass_kernel_spmd(nc, in_maps, core_ids=list(range(NCORES)))
    o = np.stack([np.asarray(r["out"], dtype=np.float32) for r in res.results], axis=0)
    return o.reshape(16, S_, D)
```
